# Optimizing a Trainium2 kernel written in Bass

```python
import jax, jax.numpy as jnp
from jax import lax
import numpy as np

D_MODEL = 1024
BATCH = 1
SEQ = 16384
DEPTH = 2

CHUNK = 64
HEAD_DIM = 64
N_HEADS = D_MODEL // HEAD_DIM
DECAY_LORA = 64
AAA_LORA = 64
GATE_LORA = 128
GN_EPS = 64e-5
RMS_EPS = 1e-6
Q_BLOCK = 128
D_FF = 3584
N_EXPERTS = 8
TOP_K = 2
MOE_ROW_BLOCK = 256
N_A = (DEPTH + 1) // 2
N_B = DEPTH // 2

kernel_name = 'hybrid_rwkv7_fox_moe_trunk'


def rmsnorm(x, g):
    xf = x.astype(jnp.float32)
    y = xf * lax.rsqrt(jnp.mean(xf * xf, axis=-1, keepdims=True) + RMS_EPS)
    return (y * g).astype(x.dtype)


def swiglu(x, w_gu, w_down):
    gate, up = jnp.split(x @ w_gu, 2, axis=-1)
    return (jax.nn.silu(gate) * up) @ w_down


def rwkv7_time_mix(x, mu, w_rkv, w0, w1, w2, a0, a1, a2, g1, g2, k_k, k_a, r_k, gn_g, gn_b, w_o):
    B, T, D = x.shape
    H, N = N_HEADS, HEAD_DIM
    f32 = jnp.float32
    x_prev = jnp.pad(x, ((0, 0), (1, 0), (0, 0)))[:, :T]
    xs = x[None] + (x_prev - x)[None] * mu[:, None, None, :]
    r, k, v = jnp.einsum('nbtd,nde->nbte', xs[:3], w_rkv)
    xw, xa, xg = xs[3], xs[4], xs[5]
    w = -jax.nn.softplus(-(w0 + jnp.tanh(xw @ w1) @ w2).astype(f32)) - 0.5
    decay = jnp.exp(-jnp.exp(w))
    a = jax.nn.sigmoid((a0 + (xa @ a1) @ a2).astype(f32))
    g = jax.nn.sigmoid(xg @ g1) @ g2
    heads = lambda z: z.astype(f32).reshape(B, T, H, N)
    kk = heads(k * k_k)
    kk = kk / jnp.maximum(jnp.sqrt(jnp.sum(kk * kk, axis=-1, keepdims=True)), 1e-12)
    k_mod = k.astype(f32) * (1.0 + (a - 1.0) * k_a)
    r_h, k_h, v_h, w_h, a_h = heads(r), heads(k_mod), heads(v), heads(decay), heads(a)
    tm = lambda z: z.transpose(1, 0, 2, 3)

    def step(S, inp):
        r_t, w_t, k_t, v_t, kk_t, a_t = inp
        sa = jnp.einsum('bhij,bhj->bhi', S, -kk_t)
        S = (S * w_t[:, :, None, :] + sa[..., None] * (kk_t * a_t)[:, :, None, :]
             + v_t[..., None] * k_t[:, :, None, :])
        return S, jnp.einsum('bhij,bhj->bhi', S, r_t)

    S0 = jnp.zeros((B, H, N, N), f32)
    _, y = lax.scan(step, S0, (tm(r_h), tm(w_h), tm(k_h), tm(v_h), tm(kk), tm(a_h)))
    y = y.transpose(1, 0, 2, 3)
    mean = jnp.mean(y, axis=-1, keepdims=True)
    var = jnp.mean(jnp.square(y - mean), axis=-1, keepdims=True)
    y = ((y - mean) * lax.rsqrt(var + GN_EPS)).reshape(B, T, D) * gn_g + gn_b
    bonus = jnp.sum(r_h * k_h * r_k, axis=-1, keepdims=True) * v_h
    y = (y + bonus.reshape(B, T, D)) * g
    return y.astype(x.dtype) @ w_o


def forgetting_attention(x, w_in, b_f, q_gain, k_gain, w_o):
    B, T, D = x.shape
    H, N, QB = N_HEADS, HEAD_DIM, Q_BLOCK
    f32 = jnp.float32
    proj = x @ w_in
    q, k, v, og = jnp.split(proj[..., :4 * D], 4, axis=-1)
    log_f = jax.nn.log_sigmoid((proj[..., 4 * D:] + b_f).astype(f32))
    c = jnp.cumsum(log_f, axis=1).transpose(0, 2, 1)
    q = rmsnorm(q.reshape(B, T, H, N), q_gain).transpose(0, 2, 1, 3)
    k = rmsnorm(k.reshape(B, T, H, N), k_gain).transpose(0, 2, 1, 3)
    v = v.reshape(B, T, H, N).transpose(0, 2, 1, 3)
    nblk = T // QB
    q_blocks = q.reshape(B, H, nblk, QB, N).transpose(2, 0, 1, 3, 4)
    c_blocks = c.reshape(B, H, nblk, QB).transpose(2, 0, 1, 3)
    kpos = jnp.arange(T)
    scale = N ** -0.5

    def attend(args):
        i, q_i, c_i = args
        qpos = i * QB + jnp.arange(QB)
        s = (jnp.einsum('bhqd,bhkd->bhqk', q_i, k).astype(f32) * scale
             + (c_i[..., :, None] - c[..., None, :]))
        s = jnp.where(kpos[None, :] <= qpos[:, None], s, -jnp.inf)
        p = jax.nn.softmax(s, axis=-1)
        return jnp.einsum('bhqk,bhkd->bhqd', p.astype(v.dtype), v)

    o = lax.map(attend, (jnp.arange(nblk), q_blocks, c_blocks))
    o = o.transpose(1, 0, 3, 2, 4).reshape(B, T, D)
    return (o * jax.nn.sigmoid(og)) @ w_o


def moe_swiglu(x, w_router, w_gu, w_down):
    B, T, D = x.shape
    E, K, RB = N_EXPERTS, TOP_K, MOE_ROW_BLOCK
    xf = x.reshape(B * T, D)
    logits = (xf @ w_router).astype(jnp.float32)
    top_v, top_i = lax.top_k(logits, K)
    gates = jax.nn.softmax(top_v, axis=-1)
    A = B * T * K
    NB = -(-A // RB) + E
    flat_e = top_i.reshape(A)
    flat_tok = jnp.repeat(jnp.arange(B * T, dtype=jnp.int32), K)
    flat_w = gates.reshape(A)
    order = jnp.argsort(flat_e, stable=True)
    sorted_e = flat_e[order]
    counts = jnp.bincount(flat_e, length=E)
    padded = (counts + RB - 1) // RB * RB
    ends_pad = jnp.cumsum(padded)
    start_pad = ends_pad - padded
    start = jnp.cumsum(counts) - counts
    dest = start_pad[sorted_e] + jnp.arange(A, dtype=jnp.int32) - start[sorted_e]
    buf_tok = jnp.zeros((NB * RB,), jnp.int32).at[dest].set(flat_tok[order])
    buf_w = jnp.zeros((NB * RB,), jnp.float32).at[dest].set(flat_w[order])
    block_e = jnp.minimum(jnp.sum(jnp.arange(NB)[:, None] * RB >= ends_pad[None, :], axis=1), E - 1)

    def expert_block(args):
        e, tok, wt = args
        return swiglu(xf[tok], w_gu[e], w_down[e]).astype(jnp.float32) * wt[:, None]

    ys = lax.map(expert_block, (block_e, buf_tok.reshape(NB, RB), buf_w.reshape(NB, RB)))
    out = jnp.zeros((B * T, D), jnp.float32).at[buf_tok].add(ys.reshape(NB * RB, D))
    return out.astype(x.dtype).reshape(B, T, D)


def setup_inputs(seed: int = 0) -> dict:
    key = jax.random.key(seed)
    ks = iter(jax.random.split(key, 40))
    f32 = jnp.float32
    D, H, N, F, E = D_MODEL, N_HEADS, HEAD_DIM, D_FF, N_EXPERTS
    nrm = lambda shape, s: jax.random.normal(next(ks), shape, f32) * s
    uni = lambda shape, lo, hi: jax.random.uniform(next(ks), shape, f32, lo, hi)
    return {
        'x': nrm((BATCH, SEQ, D), 1.0),
        'norm_g': 1.0 + nrm((DEPTH, 2, D), 0.02),
        'final_g': 1.0 + nrm((D,), 0.02),
        'rwkv_mu': uni((N_A, 6, D), 0.0, 1.0),
        'rwkv_w_rkv': nrm((N_A, 3, D, D), D ** -0.5),
        'rwkv_w0': uni((N_A, D), -5.0, 1.0),
        'rwkv_w1': nrm((N_A, D, DECAY_LORA), D ** -0.5),
        'rwkv_w2': nrm((N_A, DECAY_LORA, D), 0.5 * DECAY_LORA ** -0.5),
        'rwkv_a0': nrm((N_A, D), 0.1),
        'rwkv_a1': nrm((N_A, D, AAA_LORA), D ** -0.5),
        'rwkv_a2': nrm((N_A, AAA_LORA, D), 0.5 * AAA_LORA ** -0.5),
        'rwkv_g1': nrm((N_A, D, GATE_LORA), D ** -0.5),
        'rwkv_g2': nrm((N_A, GATE_LORA, D), GATE_LORA ** -0.5),
        'rwkv_k_k': 0.85 + nrm((N_A, D), 0.02),
        'rwkv_k_a': 1.0 + nrm((N_A, D), 0.02),
        'rwkv_r_k': nrm((N_A, H, N), 0.1),
        'rwkv_gn_g': 1.0 + nrm((N_A, D), 0.02),
        'rwkv_gn_b': nrm((N_A, D), 0.02),
        'rwkv_w_o': nrm((N_A, D, D), D ** -0.5),
        'fox_w_in': nrm((N_B, D, 4 * D + H), D ** -0.5),
        'fox_b_f': uni((N_B, H), 1.0, 4.0),
        'fox_q_gain': 1.0 + nrm((N_B, N), 0.02),
        'fox_k_gain': 1.0 + nrm((N_B, N), 0.02),
        'fox_w_o': nrm((N_B, D, D), D ** -0.5),
        'ffn_w_gu': nrm((N_A, D, 2 * F), D ** -0.5),
        'ffn_w_down': nrm((N_A, F, D), F ** -0.5),
        'moe_w_router': nrm((N_B, D, E), D ** -0.5),
        'moe_w_gu': nrm((N_B, E, D, 2 * F), D ** -0.5),
        'moe_w_down': nrm((N_B, E, F, D), F ** -0.5),
    }


def reference(x, norm_g, final_g, rwkv_mu, rwkv_w_rkv, rwkv_w0, rwkv_w1, rwkv_w2, rwkv_a0, rwkv_a1,
              rwkv_a2, rwkv_g1, rwkv_g2, rwkv_k_k, rwkv_k_a, rwkv_r_k, rwkv_gn_g, rwkv_gn_b, rwkv_w_o,
              fox_w_in, fox_b_f, fox_q_gain, fox_k_gain, fox_w_o, ffn_w_gu, ffn_w_down,
              moe_w_router, moe_w_gu, moe_w_down):
    h = x
    for i in range(DEPTH):
        j = i // 2
        u = rmsnorm(h, norm_g[i, 0])
        if i % 2 == 0:
            h = h + rwkv7_time_mix(u, rwkv_mu[j], rwkv_w_rkv[j], rwkv_w0[j], rwkv_w1[j], rwkv_w2[j],
                                   rwkv_a0[j], rwkv_a1[j], rwkv_a2[j], rwkv_g1[j], rwkv_g2[j],
                                   rwkv_k_k[j], rwkv_k_a[j], rwkv_r_k[j], rwkv_gn_g[j], rwkv_gn_b[j],
                                   rwkv_w_o[j])
            u = rmsnorm(h, norm_g[i, 1])
            h = h + swiglu(u, ffn_w_gu[j], ffn_w_down[j])
        else:
            h = h + forgetting_attention(u, fox_w_in[j], fox_b_f[j], fox_q_gain[j], fox_k_gain[j],
                                         fox_w_o[j])
            u = rmsnorm(h, norm_g[i, 1])
            h = h + moe_swiglu(u, moe_w_router[j], moe_w_gu[j], moe_w_down[j])
    return rmsnorm(h, final_g)
```

```python
import ml_dtypes
import numpy as np
import concourse.bass as bass
import concourse.mybir as mybir
from concourse.bass_utils import run_bass_kernel_spmd
from contextlib import ExitStack

F32 = mybir.dt.float32
BF16 = mybir.dt.bfloat16
I32 = mybir.dt.int32
ALU = mybir.AluOpType
AF = mybir.ActivationFunctionType
AX = mybir.AxisListType

SAME_ENGINE_SYNC = True


class _Rec:
    def __init__(self):
        self.call = None

    def __getattr__(self, name):
        def cap(*a, **k):
            self.call = (name, a, k)
            return self
        return cap


def _replay(call):
    name, a, k = call
    return lambda e: getattr(e, name)(*a, **k)


class _Trk:
    __slots__ = ("w", "r")

    def __init__(self):
        self.w = {}
        self.r = {}


class Buf:
    def __init__(self, fw, name, t, kind):
        self.fw = fw
        self.name = name
        self.t = t
        self.kind = kind
        self.whole = _Trk()
        self.subs = {}
        self.dsem = None

    def __getitem__(self, idx):
        return self.t[idx]

    def k(self, key):
        return (self, key)


def _split(b):
    if isinstance(b, tuple):
        return b
    return (b, None)


class FW:
    CE = ("tensor", "vector", "scalar", "gpsimd")

    def __init__(self, nc):
        self.nc = nc
        self.es = ExitStack()
        self.q = {e: [] for e in ("tensor", "vector", "scalar", "gpsimd", "sync")}
        self.cnt = {e: 0 for e in self.CE}
        self.waited = {}
        self.sems = {}
        self.dcnt = {}
        self.nbuf = 0

    def sb(self, shape, dt, name=None):
        self.nbuf += 1
        name = "S_" + (name or f"sb{self.nbuf}")
        t = self.es.enter_context(self.nc.sbuf_tensor(name, list(shape), dt))
        return Buf(self, name, t, "sb")

    def ps(self, shape, dt=F32, name=None):
        self.nbuf += 1
        name = "P_" + (name or f"ps{self.nbuf}")
        t = self.es.enter_context(self.nc.psum_tensor(name, list(shape), dt))
        return Buf(self, name, t, "ps")

    def dram(self, name, shape, dt, kind):
        t = self.nc.dram_tensor(name, list(shape), dt, kind=kind).ap()
        return Buf(self, name, t, "dram")

    def _sem(self, key):
        if key not in self.sems:
            self.sems[key] = self.es.enter_context(self.nc.semaphore("s_" + str(key)))
        return self.sems[key]

    def _collect(self, eng, reads, writes):
        waits = {}

        def need_w(wd):
            for kv in wd.items():
                need(kv)

        def need(tok):
            if tok is None:
                return
            k, v = tok
            if k == eng and (eng == "tensor" or not SAME_ENGINE_SYNC):
                return
            if waits.get(k, 0) < v:
                waits[k] = v

        reads = list(reads)
        writes = list(writes)
        for b in list(reads):
            bb, _k = _split(b)
            if bb.kind == "ps":
                writes.append(bb)
        writes = [(_split(b)[0] if _split(b)[0].kind == "ps" else b) for b in writes]
        for b in reads:
            b, key = _split(b)
            if b.kind == "ps":
                continue
            trks = [b.whole] + ([b.subs[key]] if (key is not None and key in b.subs) else
                                (list(b.subs.values()) if key is None else []))
            for t in trks:
                need_w(t.w)
        for b in writes:
            b, key = _split(b)
            trks = [b.whole] + ([b.subs[key]] if (key is not None and key in b.subs) else
                                (list(b.subs.values()) if key is None else []))
            for t in trks:
                need_w(t.w)
                for k, v in t.r.items():
                    need((k, v))
        out = []
        for k, v in waits.items():
            if self.waited.get((eng, k), 0) >= v:
                continue
            self.waited[(eng, k)] = v
            out.append((k, v))
        return out

    def _update(self, reads, writes, tok):
        reads = list(reads)
        writes = list(writes)
        for b in list(reads):
            bb, _k = _split(b)
            if bb.kind == "ps":
                writes.append(bb)
        reads = [b for b in reads if _split(b)[0].kind != "ps"]
        writes = [(_split(b)[0] if _split(b)[0].kind == "ps" else b) for b in writes]
        for b in reads:
            b, key = _split(b)
            t = b.whole if key is None else b.subs.setdefault(key, _Trk())
            k, v = tok
            if t.r.get(k, 0) < v:
                t.r[k] = v
        for b in writes:
            b, key = _split(b)
            t = b.whole if key is None else b.subs.setdefault(key, _Trk())
            if b.kind == "dram":
                if t.w.get(tok[0], 0) < tok[1]:
                    t.w[tok[0]] = tok[1]
            else:
                t.w = {tok[0]: tok[1]}
                t.r = {}
                if key is None:
                    b.subs = {}

    def op(self, eng, fn, reads=(), writes=()):
        waits = self._collect(eng, reads, writes)
        self.cnt[eng] += 1
        tok = (eng, self.cnt[eng])
        self._update(reads, writes, tok)
        rec = _Rec()
        fn(rec)
        self.q[eng].append((waits, _replay(rec.call), (eng, 1)))

    def dma(self, out, in_, outb, inb, q="sync", **kw):
        ob, ok_ = _split(outb)
        ib, ik_ = _split(inb)
        sb, sk = (ob, ok_) if ob.kind != "dram" else (ib, ik_)
        key = "d_" + sb.name + ("" if sk is None else "_" + "_".join(str(z) for z in (sk if isinstance(sk, tuple) else (sk,))))
        waits = self._collect(q, [inb], [outb])
        self.dcnt[key] = self.dcnt.get(key, 0) + 16
        tok = (key, self.dcnt[key])
        self._update([inb], [outb], tok)
        self.q[q].append((waits, lambda e: e.dma_start(out=out, in_=in_, **kw), (key, 16)))

    def final_wait(self, bufs, q="sync"):
        waits = self._collect(q, bufs, [])
        self.q[q].append((waits, None, None))

    def build(self):
        nc = self.nc
        for e in self.CE:
            self._sem(e)
        for q in self.q.values():
            for waits, fn, inc in q:
                for k, v in waits:
                    self._sem(k)
                if inc is not None:
                    self._sem(inc[0])
        fwself = self
        self.nsem = len(self.sems)
        with nc.Block() as block:
            def mk(ename):
                def body(eng):
                    for waits, fn, inc in fwself.q[ename]:
                        for k, v in waits:
                            eng.wait_ge(fwself.sems[k], v)
                        if fn is not None:
                            ins = fn(eng)
                            ins.then_inc(fwself.sems[inc[0]], inc[1])
                return body
            block.tensor(mk("tensor"))
            block.vector(mk("vector"))
            block.scalar(mk("scalar"))
            block.gpsimd(mk("gpsimd"))
            block.sync(mk("sync"))
        self.es.close()
        return nc


C0 = 0.6065306597126334
RMS_EPS = 1e-6
DEBUG = False

PVA = {"g0": 0, "mu": 8, "w0": 56, "a0": 64, "k_k": 72, "k_a": 80, "r_k": 88}
NPA = 96


def rmsnorm_to_fm(f, xsrc_ap, xbuf, uT, col0, ncols, src_col0, ident, gcol, pv, tmp, NTOK=128):
    pass


def build_A():
    nc = bass.Bass("TRN2", target_bir_lowering=False)
    f = FW(nc)
    xa = f.dram("xa", [17 * 128, 1024], F32, "ExternalInput")
    pvd = f.dram("pv", [128, NPA], F32, "ExternalInput")
    cst = f.dram("cst", [128, 256], F32, "ExternalInput")
    w_rkv = f.dram("w_rkv", [3, 1024, 1024], F32, "ExternalInput")
    w1d = f.dram("w1", [1024, 64], F32, "ExternalInput")
    a1d = f.dram("a1", [1024, 64], F32, "ExternalInput")
    g1d = f.dram("g1", [1024, 128], F32, "ExternalInput")
    w2d = f.dram("w2", [64, 1024], F32, "ExternalInput")
    a2d = f.dram("a2", [64, 1024], F32, "ExternalInput")
    g2d = f.dram("g2", [128, 1024], F32, "ExternalInput")
    o_bf = f.dram("o_bf", [8, 1024, 2048], BF16, "ExternalOutput")
    o_rt = f.dram("o_rt", [1024, 2048], F32, "ExternalOutput")
    o_wc = f.dram("o_wc", [1024, 32], F32, "ExternalOutput")

    pv = f.sb([128, NPA], F32, "pv")
    f.dma(pv[:], pvd[:], pv, pvd)
    cs = f.sb([128, 256], F32, "cs")
    f.dma(cs[:], cst[:], cs, cst)
    ident = f.sb([128, 128], BF16, "ident")
    bones = f.sb([128, 128], BF16, "bones")
    f.op("vector", lambda e: e.tensor_copy(out=ident[:], in_=cs[:, 0:128]), [cs], [ident])
    f.op("vector", lambda e: e.tensor_copy(out=bones[:], in_=cs[:, 128:256]), [cs], [bones])
    ones = f.sb([128, 64], F32, "ones")
    f.op("gpsimd", lambda e: e.memset(ones[:], 1.0), [], [ones])

    wr = f.sb([128, 3, 8, 1024], BF16, "wr")
    for n in range(3):
        for kc in range(0, 8, 4):
            f.dma(wr[:, n, kc:kc + 4, :], w_rkv[n, kc * 128:(kc + 4) * 128, :].rearrange("(k p) e -> p k e", p=128), wr.k((n, kc)), w_rkv, q="gpsimd")
    w1 = f.sb([128, 8, 64], BF16, "w1s"); a1 = f.sb([128, 8, 64], BF16, "a1s"); g1 = f.sb([128, 8, 128], BF16, "g1s")
    f.dma(w1[:], w1d[:].rearrange("(k p) e -> p k e", p=128), w1, w1d, q="gpsimd")
    f.dma(a1[:], a1d[:].rearrange("(k p) e -> p k e", p=128), a1, a1d, q="gpsimd")
    f.dma(g1[:], g1d[:].rearrange("(k p) e -> p k e", p=128), g1, g1d, q="gpsimd")
    w2 = f.sb([64, 1024], BF16, "w2s"); a2 = f.sb([64, 1024], BF16, "a2s"); g2 = f.sb([128, 1024], BF16, "g2s")
    f.dma(w2[:], w2d[:], w2, w2d, q="gpsimd")
    f.dma(a2[:], a2d[:], a2, a2d, q="gpsimd")
    f.dma(g2[:], g2d[:], g2, g2d, q="gpsimd")

    uT = f.sb([128, 8, 2049], BF16, "uT")
    xb = [f.sb([128, 1024], F32, f"xb{i}") for i in range(2)]
    xn = [f.sb([128, 1024], BF16, f"xn{i}") for i in range(2)]
    junk = f.sb([128, 1024], BF16, "junk")
    ssq = [f.sb([128, 1], F32, f"ssq{i}") for i in range(2)]
    ptr = [f.ps([128, 8, 128], BF16, "ptr0")] * 2
    for t in range(17):
        b = t % 2
        X, XN, SS, PT = xb[b], xn[b], ssq[b], ptr[b]
        f.dma(X[:], xa[t * 128:(t + 1) * 128, :], X, xa)
        f.op("scalar", lambda e, X=X, SS=SS: e.activation(out=junk[:], in_=X[:], func=AF.Square, accum_out=SS[:]), [X], [junk, SS])
        f.op("scalar", lambda e, SS=SS: e.activation(out=SS[:], in_=SS[:], func=AF.Sqrt, scale=1.0 / 1024, bias=RMS_EPS), [SS], [SS])
        f.op("vector", lambda e, SS=SS: e.reciprocal(out=SS[:], in_=SS[:]), [SS], [SS])
        f.op("vector", lambda e, X=X, XN=XN, SS=SS: e.tensor_scalar(out=XN[:], in0=X[:], scalar1=SS[:, 0:1], scalar2=None, op0=ALU.mult), [X, SS], [XN])
        for c in range(8):
            f.op("tensor", lambda e, c=c, XN=XN, PT=PT: e.transpose(out=PT[:, c, :], in_=XN[:, c * 128:(c + 1) * 128], identity=ident[:]), [XN, ident], [PT])
        for c in range(8):
            eng = "vector" if c % 2 == 0 else "gpsimd"
            eng = "vector"
            if t == 0:
                f.op(eng, lambda e, c=c, PT=PT: e.tensor_scalar(out=uT[:, c, 0:1], in0=PT[:, c, 127:128], scalar1=pv[:, PVA["g0"] + c:PVA["g0"] + c + 1], scalar2=None, op0=ALU.mult), [PT, pv], [uT.k(("t", t))])
            else:
                c0 = 1 + (t - 1) * 128
                f.op(eng, lambda e, c=c, PT=PT, c0=c0: e.tensor_scalar(out=uT[:, c, c0:c0 + 128], in0=PT[:, c, :], scalar1=pv[:, PVA["g0"] + c:PVA["g0"] + c + 1], scalar2=None, op0=ALU.mult), [PT, pv], [uT.k(("t", t))])

    xs = f.sb([128, 6, 8, 512], BF16, "xs")
    dd = [f.sb([128, 512], BF16, f"dd{i}") for i in range(2)]
    pr = f.ps([128, 512], F32, "pr"); pk = f.ps([128, 512], F32, "pk"); pvv = f.ps([128, 512], F32, "pvv")
    pw = f.ps([128, 512], F32, "pw"); pa = f.ps([128, 512], F32, "pa"); pg = f.ps([128, 512], F32, "pg")
    px1 = f.ps([128, 512], F32, "px1"); px2 = px1
    h1 = f.sb([64, 512], BF16, "h1"); ha = f.sb([64, 512], BF16, "ha"); hg = f.sb([128, 512], BF16, "hg")
    T = lambda n, dt=F32: f.sb([128, 512], dt, n)
    r_s, k_s, v_s, sg, a_s, cum, cprev = T("r_s"), T("k_s"), T("v_s"), T("sg"), T("a_s"), T("cum"), T("cprev")
    e_pos, e_neg, e_prev = T("e_pos"), T("e_neg"), T("e_prev")
    kkr, sq, ssm, kk, t1, kmod, bb, btf, ktf, rk = T("kkr"), T("sq", BF16), T("ssm"), T("kk"), T("t1"), T("kmod"), T("bb"), T("btf"), T("ktf"), T("rk", BF16)
    rt = T("rt")
    wc = f.sb([128, 8], F32, "wc")
    ob = f.sb([128, 8, 512], BF16, "ob")
    for s in range(4):
        cur = lambda c: uT[:, c, 1 + 512 * s:1 + 512 * s + 512]
        prv = lambda c: uT[:, c, 512 * s:512 * s + 512]
        ureads = [uT.k(("t", t)) for t in range(max(0, 4 * s), 4 * s + 5)]
        for c in range(8):
            D = dd[c % 2]
            f.op("gpsimd", lambda e, c=c, D=D: e.tensor_tensor(out=D[:], in0=prv(c), in1=cur(c), op=ALU.subtract), ureads, [D])
            for n in range(6):
                col = PVA["mu"] + n * 8 + c
                f.op("vector", lambda e, c=c, n=n, D=D, col=col: e.scalar_tensor_tensor(out=xs[:, n, c, :], in0=D[:], scalar=pv[:, col:col + 1], in1=cur(c), op0=ALU.mult, op1=ALU.add), [D, pv] + ureads, [xs.k(n)])
        for c in range(8):
            f.op("tensor", lambda e, c=c: e.matmul(pw[0:64, :], lhsT=w1[:, c, :], rhs=xs[:, 3, c, :], start=(c == 0), stop=(c == 7)), [w1, xs.k(3)], [pw])
        f.op("scalar", lambda e: e.activation(out=h1[:], in_=pw[0:64, :], func=AF.Tanh), [pw], [h1])
        for c in range(8):
            f.op("tensor", lambda e, c=c: e.matmul(pa[0:64, :], lhsT=a1[:, c, :], rhs=xs[:, 4, c, :], start=(c == 0), stop=(c == 7)), [a1, xs.k(4)], [pa])
        f.op("vector", lambda e: e.tensor_copy(out=ha[:], in_=pa[0:64, :]), [pa], [ha])
        for c in range(8):
            f.op("tensor", lambda e, c=c: e.matmul(pg[:], lhsT=g1[:, c, :], rhs=xs[:, 5, c, :], start=(c == 0), stop=(c == 7)), [g1, xs.k(5)], [pg])
        f.op("scalar", lambda e: e.activation(out=hg[:], in_=pg[:], func=AF.Sigmoid), [pg], [hg])
        for ec in range(8):
            es = slice(ec * 128, (ec + 1) * 128)
            for n, P in enumerate((pr, pk, pvv)):
                for c in range(8):
                    f.op("tensor", lambda e, c=c, n=n, P=P: e.matmul(P[:], lhsT=wr[:, n, c, es], rhs=xs[:, n, c, :], start=(c == 0), stop=(c == 7)), [wr.k((n, (c // 4) * 4)), xs.k(n)], [P])
            f.op("tensor", lambda e: e.matmul(pw[:], lhsT=w2[:, es], rhs=h1[:], start=True, stop=True), [w2, h1], [pw])
            f.op("tensor", lambda e: e.matmul(pa[:], lhsT=a2[:, es], rhs=ha[:], start=True, stop=True), [a2, ha], [pa])
            f.op("tensor", lambda e: e.matmul(pg[:], lhsT=g2[:, es], rhs=hg[:], start=True, stop=True), [g2, hg], [pg])
            pcol = lambda nm: pv[:, PVA[nm] + ec:PVA[nm] + ec + 1]
            f.op("scalar", lambda e: e.activation(out=r_s[:], in_=pr[:], func=AF.Copy), [pr], [r_s])
            f.op("scalar", lambda e: e.activation(out=k_s[:], in_=pk[:], func=AF.Copy), [pk], [k_s])
            f.op("vector", lambda e: e.tensor_copy(out=v_s[:], in_=pvv[:]), [pvv], [v_s])
            f.op("scalar", lambda e: e.activation(out=sg[:], in_=pw[:], func=AF.Sigmoid, bias=pcol("w0")), [pw, pv], [sg])
            f.op("scalar", lambda e: e.activation(out=a_s[:], in_=pa[:], func=AF.Sigmoid, bias=pcol("a0")), [pa, pv], [a_s])
            f.op("scalar", lambda e: e.activation(out=ob[:, 6, :], in_=pg[:], func=AF.Copy), [pg], [ob.k(6)])
            for q in range(8):
                f.op("vector", lambda e, q=q: e.tensor_tensor_scan(out=cum[:, q * 64:(q + 1) * 64], data0=ones[:], data1=sg[:, q * 64:(q + 1) * 64], initial=0.0, op0=ALU.mult, op1=ALU.add), [ones, sg], [cum])
            f.op("gpsimd", lambda e: e.tensor_tensor(out=cprev[:], in0=cum[:], in1=sg[:], op=ALU.subtract), [cum, sg], [cprev])
            f.op("scalar", lambda e: e.activation(out=e_pos[:], in_=cum[:], func=AF.Exp, scale=-C0), [cum], [e_pos])
            f.op("scalar", lambda e: e.activation(out=e_neg[:], in_=cum[:], func=AF.Exp, scale=C0), [cum], [e_neg])
            f.op("scalar", lambda e: e.activation(out=e_prev[:], in_=cprev[:], func=AF.Exp, scale=-C0), [cprev], [e_prev])
            f.op("scalar", lambda e: e.activation(out=wc[:], in_=cum[:].rearrange("p (q t) -> p q t", t=64)[:, :, 63], func=AF.Exp, scale=-C0), [cum], [wc])
            f.op("gpsimd", lambda e: e.tensor_scalar(out=kkr[:], in0=k_s[:], scalar1=pcol("k_k"), scalar2=None, op0=ALU.mult), [k_s, pv], [kkr])
            f.op("gpsimd", lambda e: e.tensor_tensor(out=sq[:], in0=kkr[:], in1=kkr[:], op=ALU.mult), [kkr], [sq])
            f.op("tensor", lambda e: e.matmul(px1[:], lhsT=bones[:], rhs=sq[:], start=True, stop=True), [bones, sq], [px1])
            f.op("vector", lambda e: e.tensor_scalar(out=ssm[:], in0=px1[:], scalar1=1e-24, scalar2=None, op0=ALU.max), [px1], [ssm])
            f.op("scalar", lambda e: e.activation(out=ssm[:], in_=ssm[:], func=AF.Sqrt), [ssm], [ssm])
            f.op("vector", lambda e: e.reciprocal(out=ssm[:], in_=ssm[:]), [ssm], [ssm])
            f.op("gpsimd", lambda e: e.tensor_tensor(out=kk[:], in0=kkr[:], in1=ssm[:], op=ALU.mult), [kkr, ssm], [kk])
            f.op("vector", lambda e: e.tensor_scalar(out=t1[:], in0=a_s[:], scalar1=-1.0, scalar2=pcol("k_a"), op0=ALU.add, op1=ALU.mult), [a_s, pv], [t1])
            f.op("vector", lambda e: e.scalar_tensor_tensor(out=kmod[:], in0=t1[:], scalar=1.0, in1=k_s[:], op0=ALU.add, op1=ALU.mult), [t1, k_s], [kmod])
            f.op("gpsimd", lambda e: e.tensor_tensor(out=bb[:], in0=kk[:], in1=a_s[:], op=ALU.mult), [kk, a_s], [bb])
            f.op("vector", lambda e: e.scalar_tensor_tensor(out=ob[:, 0, :], in0=kk[:], scalar=-1.0, in1=e_prev[:], op0=ALU.mult, op1=ALU.mult), [kk, e_prev], [ob.k(0)])
            f.op("gpsimd", lambda e: e.tensor_tensor(out=rt[:], in0=r_s[:], in1=e_pos[:], op=ALU.mult), [r_s, e_pos], [rt])
            f.op("gpsimd", lambda e: e.tensor_tensor(out=btf[:], in0=bb[:], in1=e_neg[:], op=ALU.mult), [bb, e_neg], [btf])
            f.op("gpsimd", lambda e: e.tensor_tensor(out=ktf[:], in0=kmod[:], in1=e_neg[:], op=ALU.mult), [kmod, e_neg], [ktf])
            f.op("scalar", lambda e: e.activation(out=ob[:, 1, :], in_=btf[:], func=AF.Copy), [btf], [ob.k(1)])
            f.op("scalar", lambda e: e.activation(out=ob[:, 2, :], in_=ktf[:], func=AF.Copy), [ktf], [ob.k(2)])
            for q in range(8):
                qs = slice(q * 64, (q + 1) * 64)
                f.op("vector", lambda e, q=q, qs=qs: e.tensor_scalar(out=ob[:, 3, qs], in0=btf[:, qs], scalar1=wc[:, q:q + 1], scalar2=None, op0=ALU.mult), [btf, wc], [ob.k(3)])
                f.op("gpsimd", lambda e, q=q, qs=qs: e.tensor_scalar(out=ob[:, 4, qs], in0=ktf[:, qs], scalar1=wc[:, q:q + 1], scalar2=None, op0=ALU.mult), [ktf, wc], [ob.k(4)])
            f.op("vector", lambda e: e.scalar_tensor_tensor(out=rk[:], in0=r_s[:], scalar=pcol("r_k"), in1=kmod[:], op0=ALU.mult, op1=ALU.mult), [r_s, pv, kmod], [rk])
            f.op("tensor", lambda e: e.matmul(px2[:], lhsT=bones[:], rhs=rk[:], start=True, stop=True), [bones, rk], [px2])
            f.op("vector", lambda e: e.tensor_tensor(out=ob[:, 7, :], in0=px2[:], in1=v_s[:], op=ALU.mult), [px2, v_s], [ob.k(7)])
            f.op("scalar", lambda e: e.activation(out=ob[:, 5, :], in_=v_s[:], func=AF.Copy), [v_s], [ob.k(5)])
            f.dma(o_bf[:, es, 512 * s:512 * s + 512].rearrange("q p t -> p q t"), ob[:], o_bf, ob)
            f.dma(o_rt[es, 512 * s:512 * s + 512], rt[:], o_rt, rt)
            f.dma(o_wc[es, 8 * s:8 * s + 8], wc[:], o_wc, wc)
    if DEBUG:
        o_dbg = f.dram("o_dbg", [128, 8, 2049], BF16, "ExternalOutput")
        f.dma(o_dbg[:], uT[:], o_dbg, uT)
        o_dbg2 = f.dram("o_dbg2", [128, 8, 1024], BF16, "ExternalOutput")
        f.dma(o_dbg2[:], wr[:, 2, :, :], o_dbg2, wr)
        f.final_wait([o_dbg, o_dbg2])
    f.final_wait([o_bf, o_rt, o_wc])
    return f.build()


def consts_A():
    c = np.zeros((128, 256), np.float32)
    c[:, 0:128] = np.eye(128)
    blk = np.arange(128) // 64
    c[:, 128:256] = (blk[:, None] == blk[None, :]).astype(np.float32)
    return c


def fmcols(v):
    return np.ascontiguousarray(np.asarray(v, np.float32).reshape(8, 128).T)


def inputs_A(inp):
    x = inp["x"][0]
    pvv = np.zeros((128, NPA), np.float32)
    pvv[:, 0:8] = fmcols(inp["norm_g"][0, 0])
    for n in range(6):
        pvv[:, 8 + 8 * n:16 + 8 * n] = fmcols(inp["rwkv_mu"][0, n])
    pvv[:, 56:64] = fmcols(inp["rwkv_w0"][0]); pvv[:, 64:72] = fmcols(inp["rwkv_a0"][0])
    pvv[:, 72:80] = fmcols(inp["rwkv_k_k"][0]); pvv[:, 80:88] = fmcols(inp["rwkv_k_a"][0])
    pvv[:, 88:96] = fmcols(inp["rwkv_r_k"][0].reshape(-1))
    cst = consts_A()
    xpad = np.concatenate([np.zeros((128, 1024), np.float32), x], 0)
    maps = []
    for c in range(8):
        maps.append({"xa": np.ascontiguousarray(xpad[2048 * c:2048 * c + 2048 + 128]), "pv": pvv, "cst": cst,
                     "w_rkv": inp["rwkv_w_rkv"][0], "w1": inp["rwkv_w1"][0], "a1": inp["rwkv_a1"][0], "g1": inp["rwkv_g1"][0],
                     "w2": inp["rwkv_w2"][0], "a2": inp["rwkv_a2"][0], "g2": inp["rwkv_g2"][0]})
    return maps


GC = 8
NCH = 256
NG = NCH // GC
DEBUG = False


def build_B(nch=NCH):
    ng = nch // GC
    nc = bass.Bass("TRN2", target_bir_lowering=False)
    f = FW(nc)
    d_bk = f.dram("d_bk", [ng, 64, GC * 2 * 2 * 64], BF16, "ExternalInput")
    d_at = f.dram("d_at", [ng, 64, GC * 2 * 64], BF16, "ExternalInput")
    d_rf = f.dram("d_rf", [ng, 64, GC * 2 * 64], F32, "ExternalInput")
    d_tm = f.dram("d_tm", [ng, 64, GC * 2 * 4 * 64], BF16, "ExternalInput")
    d_wc = f.dram("d_wc", [64, nch * 2], F32, "ExternalInput")
    d_mask = f.dram("d_mask", [64, 2 * 320], F32, "ExternalInput")
    d_id = f.dram("d_id", [64, 128], F32, "ExternalInput")
    o_y = f.dram("o_y", [ng, 64, GC * 2 * 64], F32, "ExternalOutput")

    wcs = f.sb([64, nch, 2], F32, "wcs")
    f.dma(wcs[:].rearrange("p c h -> p (c h)"), d_wc[:], wcs, d_wc)
    mask = f.sb([64, 2, 320], F32, "mask")
    f.dma(mask[:].rearrange("p h c -> p (h c)"), d_mask[:], mask, d_mask)
    idf2 = f.sb([64, 2, 64], F32, "idf2")
    f.dma(idf2[:].rearrange("p h c -> p (h c)"), d_id[:], idf2, d_id)
    idb2 = f.sb([64, 2, 64], BF16, "idb2")
    f.op("vector", lambda e: e.tensor_copy(out=idb2[:], in_=idf2[:]), [idf2], [idb2])
    identb = idb2[:, 0, :]
    identf = idf2[:, 0, :]

    NB = 2
    BK = [f.sb([64, GC, 2, 2, 64], BF16, f"BK{i}") for i in range(NB)]
    AR = [f.sb([64, GC, 2, 128], BF16, f"AR{i}") for i in range(NB)]
    RF = [f.sb([64, GC, 2, 64], F32, f"RF{i}") for i in range(NB)]
    TM = [f.sb([64, GC, 2, 4, 64], BF16, f"TM{i}") for i in range(NB)]
    YB = [f.sb([64, GC, 2, 64], F32, f"YB{i}") for i in range(NB)]

    def load_group(g):
        b = g % NB
        f.dma(BK[b][:].rearrange("p c h q t -> p (c h q t)"), d_bk[g], BK[b], d_bk)
        f.dma(AR[b][:, :, :, 0:64], d_at[g].rearrange("p (c h t) -> p c h t", c=GC, h=2), AR[b].k("a"), d_at)
        f.dma(AR[b][:, :, :, 64:128], d_rf[g].rearrange("p (c h t) -> p c h t", c=GC, h=2), AR[b].k("r"), d_rf, q="gpsimd")
        f.dma(RF[b][:].rearrange("p c h t -> p (c h t)"), d_rf[g], RF[b], d_rf)
        f.dma(TM[b][:].rearrange("p c h q t -> p (c h q t)"), d_tm[g], TM[b], d_tm)

    class _V:
        def __init__(self, buf, ap):
            self.buf, self.ap = buf, ap

        def __getitem__(self, idx):
            return self.ap[idx]

        def k(self, key):
            return self.buf

    def bank(name):
        return f.ps([64, 512], F32, name)
    PM, PL, PXG = [], [], []
    for x in range(2):
        bm, bl, bx = bank(f"PM{x}"), bank(f"PL{x}"), bank(f"PXG{x}")
        PM.append(_V(bm, bm[:, 0:512].rearrange("p (h c) -> p h c", h=2)))
        PL.append(_V(bl, bl[:, 0:384].rearrange("p (h c) -> p h c", h=2)))
        PXG.append(_V(bx, bx[:, 0:512].rearrange("p (h c) -> p h c", h=2)))
    bus, by = bank("PUS"), bank("PYb")
    PU_ap = bus[:, 0:128].rearrange("p (h c) -> p h c", h=2)
    PS_ap = bus[:, 128:256].rearrange("p (h c) -> p h c", h=2)
    PY_ap = by[:, 0:128].rearrange("p (h c) -> p h c", h=2)
    PUk, PSk, PYk = bus, bus, by
    NP = 4
    Ms = [f.sb([64, 2, 320], BF16, f"Ms{i}") for i in range(NP)]
    XT0 = [f.sb([64, 2, 64], BF16, f"XT0{i}") for i in range(NP)]
    LV = [[f.sb([64, 2, 192], BF16, f"LV{i}_{k}") for k in range(7)] for i in range(NP)]
    GX = [f.sb([64, 2, 128], F32, f"GX{i}") for i in range(NP)]
    UT = [f.sb([64, 2, 64], BF16, f"UT{i}") for i in range(2)]
    S = [f.sb([64, 2, 64], F32, f"S{i}") for i in range(2)]
    f.op("vector", lambda e: e.memset(S[0][:], 0.0), [], [S[0]])

    def pre_stages(c):
        p = c % NP
        x = c % 2
        PMx, PLx, PXGx = PM[x], PL[x], PXG[x]
        g, cg = divmod(c, GC)
        b = g % NB
        bk, ar, tm = BK[b], AR[b], TM[b]
        st = []

        def p1():
            for h in range(2):
                f.op("tensor", lambda e: e.matmul(PMx[:, h, 0:128], lhsT=bk[:, cg, h, 0, :], rhs=ar[:, cg, h, :], start=True, stop=True), [bk, ar], [PMx.k(h)])
                f.op("tensor", lambda e: e.matmul(PMx[:, h, 128:256], lhsT=bk[:, cg, h, 1, :], rhs=ar[:, cg, h, :], start=True, stop=True), [bk, ar], [PMx.k(h)])
                f.op("tensor", lambda e: e.matmul(PXGx[:, h, 192:256], lhsT=ar[:, cg, h, 0:64], rhs=bk[:, cg, h, 0, :], start=True, stop=True), [bk, ar], [PXGx.k("l")])
        st.append(p1)

        def p2():
            f.op("vector", lambda e: e.tensor_tensor(out=Ms[p][:, :, 0:256], in0=PMx[:, :, 0:256], in1=mask[:, :, 0:256], op=ALU.mult), [PMx.buf, mask], [Ms[p].k("m")])
            f.op("vector", lambda e: e.tensor_tensor(out=Ms[p][:, :, 256:320], in0=PXGx[:, :, 192:256], in1=mask[:, :, 256:320], op=ALU.mult), [PXGx.k("l"), mask], [Ms[p].k("l")])
            f.op("gpsimd", lambda e: e.tensor_tensor(out=LV[p][1][:, :, 128:192], in0=Ms[p][:, :, 0:64], in1=idb2[:], op=ALU.add), [Ms[p].k("m"), idb2], [LV[p][1].k("T")])
        st.append(p2)

        def p3():
            for h in range(2):
                f.op("tensor", lambda e: e.matmul(PXGx[:, h, 0:64], lhsT=Ms[p][:, h, 128:192], rhs=tm[:, cg, h, 3, :], start=True, stop=True), [Ms[p].k("m"), tm], [PXGx.k("x")])
        st.append(p3)

        def p4():
            f.op("scalar", lambda e: e.activation(out=XT0[p][:], in_=PXGx[:, :, 0:64], func=AF.Copy), [PXGx.k("x")], [XT0[p]])
        st.append(p4)

        def level(k):
            def mm():
                for h in range(2):
                    if k == 1:
                        Np, Lp = Ms[p][:, h, 0:64], Ms[p][:, h, 256:320]
                        rd = [Ms[p].k("m"), Ms[p].k("l")]
                    else:
                        Np, Lp = LV[p][k - 1][:, h, 0:64], LV[p][k - 1][:, h, 64:128]
                        rd = [LV[p][k - 1].k("NL")]
                    if k <= 5:
                        f.op("tensor", lambda e: e.matmul(PLx[:, h, 0:64], lhsT=Lp, rhs=Np, start=True, stop=True), rd, [PLx.k(h)])
                        f.op("tensor", lambda e: e.matmul(PLx[:, h, 64:128], lhsT=Np, rhs=Lp, start=True, stop=True), rd, [PLx.k(h)])
                    if k >= 2:
                        Tp = LV[p][k - 1][:, h, 128:192]
                        rdt = rd + [LV[p][k - 1].k("T"), idb2]
                        f.op("tensor", lambda e: e.matmul(PLx[:, h, 128:192], lhsT=identb, rhs=Tp, start=True, stop=False), rdt, [PLx.k(h)])
                        f.op("tensor", lambda e: e.matmul(PLx[:, h, 128:192], lhsT=Lp, rhs=Tp, start=False, stop=True), rdt, [PLx.k(h)])

            def ev():
                eng = "scalar" if k % 2 == 0 else "vector"
                if k == 1:
                    sl, wk = slice(0, 128), [LV[p][k].k("NL")]
                elif k <= 5:
                    sl, wk = slice(0, 192), [LV[p][k].k("NL"), LV[p][k].k("T")]
                else:
                    sl, wk = slice(128, 192), [LV[p][k].k("T")]
                if eng == "scalar":
                    f.op("scalar", lambda e: e.activation(out=LV[p][k][:, :, sl], in_=PLx[:, :, sl], func=AF.Copy), [PLx.buf], wk)
                else:
                    f.op("vector", lambda e: e.tensor_copy(out=LV[p][k][:, :, sl], in_=PLx[:, :, sl]), [PLx.buf], wk)
            return [mm, ev]
        for k in range(1, 7):
            st.extend(level(k))

        def pf():
            for h in range(2):
                Tf = LV[p][6][:, h, 128:192]
                f.op("tensor", lambda e: e.matmul(PXGx[:, h, 64:128], lhsT=tm[:, cg, h, 0, :], rhs=Tf, start=True, stop=True), [tm, LV[p][6].k("T")], [PXGx.k("g")])
                f.op("tensor", lambda e: e.matmul(PXGx[:, h, 128:192], lhsT=Tf, rhs=XT0[p][:, h, :], start=True, stop=True), [XT0[p], LV[p][6].k("T")], [PXGx.k("g")])
        st.append(pf)

        def pe():
            f.op("scalar", lambda e: e.activation(out=GX[p][:], in_=PXGx[:, :, 64:192], func=AF.Copy), [PXGx.k("g")], [GX[p]])
        st.append(pe)
        return st

    def seq_stages(c):
        p = c % NP
        q2 = c % 2
        g, cg = divmod(c, GC)
        b = g % NB
        bk, ar, tm, rf, yb = BK[b], AR[b], TM[b], RF[b], YB[b]
        S0, S1 = S[q2], S[1 - q2]
        UTq = UT[q2]
        PU, PS, PY = PU_ap, PS_ap, PY_ap
        st = []

        def s1():
            for h in range(2):
                f.op("tensor", lambda e: e.matmul(PU[:, h, :], lhsT=GX[p][:, h, 0:64], rhs=S0[:, h, :], start=True, stop=False), [GX[p], S0], [PUk])
                f.op("tensor", lambda e: e.matmul(PU[:, h, :], lhsT=identf, rhs=GX[p][:, h, 64:128], start=False, stop=True), [GX[p], idf2], [PUk])
        st.append(s1)

        def s2():
            f.op("scalar", lambda e: e.activation(out=UTq[:], in_=PU, func=AF.Copy), [PUk], [UTq])
        st.append(s2)

        def s3():
            for h in range(2):
                f.op("tensor", lambda e: e.matmul(PS[:, h, :], lhsT=tm[:, cg, h, 1, :], rhs=UTq[:, h, :], start=True, stop=False), [tm, UTq], [PSk])
                f.op("tensor", lambda e: e.matmul(PS[:, h, :], lhsT=tm[:, cg, h, 2, :], rhs=tm[:, cg, h, 3, :], start=False, stop=True), [tm], [PSk])
            for h in range(2):
                f.op("tensor", lambda e: e.matmul(PY[:, h, :], lhsT=rf[:, cg, h, :], rhs=S0[:, h, :], start=True, stop=False), [rf, S0], [PYk])
                f.op("tensor", lambda e: e.matmul(PY[:, h, :], lhsT=Ms[p][:, h, 192:256], rhs=tm[:, cg, h, 3, :], start=False, stop=False), [Ms[p].k("m"), tm], [PYk])
                f.op("tensor", lambda e: e.matmul(PY[:, h, :], lhsT=Ms[p][:, h, 64:128], rhs=UTq[:, h, :], start=False, stop=True), [Ms[p].k("m"), UTq], [PYk])
        st.append(s3)

        def s4():
            for h in range(2):
                f.op("vector", lambda e: e.scalar_tensor_tensor(out=S1[:, h, :], in0=S0[:, h, :], scalar=wcs[:, c, h:h + 1], in1=PS[:, h, :], op0=ALU.mult, op1=ALU.add), [S0, wcs, PSk], [S1])
            f.op("scalar", lambda e: e.activation(out=yb[:, cg, :, :], in_=PY, func=AF.Copy), [PYk], [yb.k(cg)])
            if cg == GC - 1 or DEBUG:
                f.dma(o_y[g], yb[:].rearrange("p c h i -> p (c h i)"), o_y, yb)
        st.append(s4)
        return st

    load_group(0)
    if ng > 1:
        load_group(1)

    def lockstep(a, b):
        out = []
        for i in range(max(len(a), len(b))):
            if i < len(a):
                out.append(a[i])
            if i < len(b):
                out.append(b[i])
        return out

    for s_ in lockstep(pre_stages(0), pre_stages(1) if nch > 1 else []):
        s_()
    for c in range(0, nch, 2):
        g, cg = divmod(c, GC)
        if DEBUG and c >= 1:
            break
        a = []
        if not DEBUG:
            a = lockstep(pre_stages(c + 2) if c + 2 < nch else [], pre_stages(c + 3) if c + 3 < nch else [])
        bq = seq_stages(c) + (seq_stages(c + 1) if c + 1 < nch and not DEBUG else [])
        n = max(len(a), len(bq))
        ia = ib = 0
        for i in range(n):
            want_a = (i + 1) * len(a) // n
            while ia < want_a:
                a[ia](); ia += 1
            want_b = (i + 1) * len(bq) // n
            while ib < want_b:
                bq[ib](); ib += 1
        if (c + 1) % GC == GC - 1 and g + 2 < ng:
            load_group(g + 2)
    if DEBUG:
        dbg = f.dram("o_dbg", [64, 2 * (320 + 192 * 7 + 128 + 64)], F32, "ExternalOutput")
        db = f.sb([64, 2, 320 + 192 * 7 + 128 + 64], F32, "dbgs")
        f.op("vector", lambda e: e.tensor_copy(out=db[:, :, 0:320], in_=Ms[0][:]), [Ms[0]], [db])
        for k in range(7):
            f.op("vector", lambda e: e.tensor_copy(out=db[:, :, 320 + 192 * k:320 + 192 * (k + 1)], in_=LV[0][k][:]), [LV[0][k]], [db])
        o = 320 + 192 * 7
        f.op("vector", lambda e: e.tensor_copy(out=db[:, :, o:o + 128], in_=GX[0][:]), [GX[0]], [db])
        f.op("vector", lambda e: e.tensor_copy(out=db[:, :, o + 128:o + 192], in_=UT[0][:]), [UT[0]], [db])
        f.dma(dbg[:], db[:].rearrange("p h c -> p (h c)"), dbg, db)
        f.final_wait([dbg])
    f.final_wait([o_y])
    return f.build()


def consts_B():
    m = np.zeros((64, 2, 320), np.float32)
    s = np.arange(64)[:, None]; t = np.arange(64)[None, :]
    su = (s < t).astype(np.float32); iu = (s <= t).astype(np.float32)
    for h in range(2):
        m[:, h, 0:64] = su; m[:, h, 64:128] = iu; m[:, h, 128:192] = su; m[:, h, 192:256] = iu
        m[:, h, 256:320] = (t < s).astype(np.float32)
    idm = np.concatenate([np.eye(64, dtype=np.float32)] * 2, axis=1)
    return m.reshape(64, 640), idm


def inputs_B(obf, ort, owc, nch=NCH):
    ng = nch // GC
    T = nch * 64
    mask, idm = consts_B()
    maps = []
    for c in range(8):
        cs = slice(128 * c, 128 * c + 128)
        fm = lambda a: a[cs, :T].reshape(2, 64, ng, GC, 64)
        At, Bt, Kt, Bh, Kh, V = [fm(obf[q]) for q in range(6)]
        Rt = fm(ort)
        d_bk = np.stack([Bt, Kt], axis=0).transpose(3, 2, 4, 1, 0, 5)
        d_at = At.transpose(2, 1, 3, 0, 4)
        d_rf = Rt.transpose(2, 1, 3, 0, 4)
        tmq = np.stack([At, Bh, Kh, V], axis=0)
        d_tm = tmq.transpose(3, 5, 4, 1, 0, 2)
        d_wc = owc[cs, :nch].reshape(2, 64, nch).transpose(1, 2, 0)
        maps.append({"d_bk": np.ascontiguousarray(d_bk).reshape(ng, 64, -1), "d_at": np.ascontiguousarray(d_at).reshape(ng, 64, -1),
                     "d_rf": np.ascontiguousarray(d_rf).reshape(ng, 64, -1), "d_tm": np.ascontiguousarray(d_tm).reshape(ng, 64, -1),
                     "d_wc": np.ascontiguousarray(d_wc).reshape(64, -1), "d_mask": mask, "d_id": idm})
    return maps


def gather_B(results, nch=NCH):
    ng = nch // GC
    ys = []
    for r in results:
        y = np.asarray(r["o_y"]).reshape(ng, 64, GC, 2, 64)
        ys.append(y.transpose(0, 2, 1, 3, 4).reshape(nch * 64, 128))
    return np.concatenate(ys, axis=1)


RMS_EPS = 1e-6
GN_EPS = 64e-5
ST = 512


class Ctx:
    pass


def setup_common(f, c, cst_dram):
    cs = f.sb([128, 384], F32, "cs")
    f.dma(cs[:], cst_dram[:], cs, cst_dram)
    c.identf = cs
    c.bones = f.sb([128, 128], BF16, "bones")
    c.ones = f.sb([128, 128], BF16, "onesb")
    f.op("vector", lambda e: e.tensor_copy(out=c.bones[:], in_=cs[:, 128:256]), [cs], [c.bones])
    f.op("vector", lambda e: e.tensor_copy(out=c.ones[:], in_=cs[:, 256:384]), [cs], [c.ones])
    c.cs = cs
    c.PA = [f.ps([128, 512], F32, f"PA{i}") for i in range(4)]
    c.PB = [f.ps([128, 512], F32, f"PB{i}") for i in range(2)]
    c.PC = [f.ps([128, 512], F32, f"PC{i}") for i in range(2)]
    c.ia = c.ib = c.ic = 0
    c.tmpi = 0
    c.tmps = {}


def nxt(c, pool):
    if pool == "A":
        c.ia += 1; return c.PA[c.ia % 4]
    if pool == "B":
        c.ib += 1; return c.PB[c.ib % 2]
    c.ic += 1; return c.PC[c.ic % 2]


def tmp(f, c, name, dt=F32, n=2, shape=(128, 512)):
    key = (name, dt)
    if key not in c.tmps:
        c.tmps[key] = [[f.sb(list(shape), dt, f"t_{name}_{i}") for i in range(n)], 0]
    lst = c.tmps[key]
    lst[1] += 1
    return lst[0][lst[1] % n]


def rmsnorm_fm(f, c, hT, pv, gcol0, uT, uF=None, eps=RMS_EPS):
    P = nxt(c, "C")
    for k in range(8):
        sq = tmp(f, c, "sq", BF16, 3)
        f.op("scalar", lambda e: e.activation(out=sq[:], in_=hT[:, k, :], func=AF.Square), [hT.k(k)], [sq])
        f.op("tensor", lambda e: e.matmul(P[:], lhsT=c.ones[:], rhs=sq[:], start=(k == 0), stop=(k == 7)), [c.ones, sq], [P])
    rs = tmp(f, c, "rs")
    f.op("scalar", lambda e: e.activation(out=rs[:], in_=P[:], func=AF.Sqrt, scale=1.0 / 1024, bias=eps), [P], [rs])
    f.op("vector", lambda e: e.reciprocal(out=rs[:], in_=rs[:]), [rs], [rs])
    for k in range(8):
        f.op("vector", lambda e: e.scalar_tensor_tensor(out=uT[:, k, :], in0=hT[:, k, :], scalar=pv[:, gcol0 + k:gcol0 + k + 1], in1=rs[:], op0=ALU.mult, op1=ALU.mult), [hT.k(k), pv, rs], [uT.k(k)])
        if uF is not None:
            f.op("gpsimd", lambda e: e.tensor_scalar(out=uF[:, k, :], in0=hT[:, k, :], scalar1=pv[:, gcol0 + k:gcol0 + k + 1], scalar2=None, op0=ALU.mult), [hT.k(k), pv], [uF.k(k)])
            f.op("gpsimd", lambda e: e.tensor_tensor(out=uF[:, k, :], in0=uF[:, k, :], in1=rs[:], op=ALU.mult), [uF.k(k), rs], [uF.k(k)])


def linear_res(f, c, w_sb, zT, hT, nk=8):
    for ec in range(8):
        P = nxt(c, "B")
        for k in range(nk):
            f.op("tensor", lambda e: e.matmul(P[:], lhsT=w_sb[:, k, ec * 128:(ec + 1) * 128], rhs=zT[:, k, :], start=(k == 0), stop=(k == nk - 1)), [w_sb, zT.k(k)], [P])
        f.op("vector", lambda e: e.tensor_tensor(out=hT[:, ec, :], in0=hT[:, ec, :], in1=P[:], op=ALU.add), [hT.k(ec), P], [hT.k(ec)])


def ffn_pass(f, c, uT, aT, wgu_dram, wd_dram, wgu_bufs, wd_bufs, out_fn, gate_b=None, FG=2, EG=2):
    NF = 28
    for g in range(NF // FG):
        c.wgi = getattr(c, "wgi", 0) + 1
        W = wgu_bufs[c.wgi % len(wgu_bufs)]
        for half in range(2):
            col0 = half * 3584 + g * FG * 128
            f.dma(W[:, :, half, :], wgu_dram[:, col0:col0 + FG * 128].rearrange("(k p) e -> p k e", p=128), W.k(half), wgu_dram_buf(c, wgu_dram), q="gpsimd")
        for j in range(FG):
            fc = g * FG + j
            Pg = nxt(c, "A"); Pu = nxt(c, "A")
            for k in range(8):
                f.op("tensor", lambda e: e.matmul(Pg[:], lhsT=W[:, k, 0, j * 128:(j + 1) * 128], rhs=uT[:, k, :], start=(k == 0), stop=(k == 7)), [W.k(0), uT.k(k)], [Pg])
            for k in range(8):
                f.op("tensor", lambda e: e.matmul(Pu[:], lhsT=W[:, k, 1, j * 128:(j + 1) * 128], rhs=uT[:, k, :], start=(k == 0), stop=(k == 7)), [W.k(1), uT.k(k)], [Pu])
            sg = tmp(f, c, "sg", F32, 3)
            f.op("scalar", lambda e: e.activation(out=sg[:], in_=Pg[:], func=AF.Silu), [Pg], [sg])
            if gate_b is None:
                f.op("vector", lambda e: e.tensor_tensor(out=aT[:, fc, :], in0=sg[:], in1=Pu[:], op=ALU.mult), [sg, Pu], [aT.k(fc)])
            else:
                sg2 = tmp(f, c, "sg2", F32, 3)
                f.op("vector", lambda e: e.tensor_tensor(out=sg2[:], in0=sg[:], in1=Pu[:], op=ALU.mult), [sg, Pu], [sg2])
                f.op("gpsimd", lambda e: e.tensor_tensor(out=aT[:, fc, :], in0=sg2[:], in1=gate_b[:], op=ALU.mult), [sg2, gate_b], [aT.k(fc)])
    for g in range(8 // EG):
        c.wdi = getattr(c, "wdi", 0) + 1
        W = wd_bufs[c.wdi % len(wd_bufs)]
        for q4 in range(4):
            f.dma(W[:, q4 * 7:(q4 + 1) * 7, :], wd_dram[q4 * 896:(q4 + 1) * 896, g * EG * 128:(g + 1) * EG * 128].rearrange("(k p) e -> p k e", p=128), W.k(q4), wgu_dram_buf(c, wd_dram), q="gpsimd")
        for j in range(EG):
            ec = g * EG + j
            P = nxt(c, "B")
            for fc in range(NF):
                f.op("tensor", lambda e: e.matmul(P[:], lhsT=W[:, fc, j * 128:(j + 1) * 128], rhs=aT[:, fc, :], start=(fc == 0), stop=(fc == NF - 1)), [W.k(fc // 7), aT.k(fc)], [P])
            out_fn(ec, P)


_dram_bufs = {}


def wgu_dram_buf(c, ap):
    return c.wdram


def consts_CE():
    cst = np.zeros((128, 384), np.float32)
    cst[:, 0:128] = np.eye(128)
    blk = np.arange(128) // 64
    cst[:, 128:256] = (blk[:, None] == blk[None, :]).astype(np.float32)
    cst[:, 256:384] = 1.0
    return cst


def fmcols(v):
    return np.ascontiguousarray(np.asarray(v, np.float32).reshape(-1, 128).T)


def ffn_ws(f, c, uTs, hTs, w_gu_ap, w_dn_ap, wgu_bufs, wd_bufs, aT_bufs, gbs=None, FG=4):
    NTT = len(uTs)
    for g in range(28 // FG):
        c.wgi = getattr(c, "wgi", 0) + 1
        W = wgu_bufs[c.wgi % 2]; WD = wd_bufs[c.wgi % 2]
        for hf in range(2):
            col0 = hf * 3584 + g * FG * 128
            f.dma(W[:, :, hf, :], w_gu_ap[:, col0:col0 + FG * 128].rearrange("(k p) e -> p k e", p=128), W.k(hf), c.wdram, q="gpsimd")
        f.dma(WD[:], w_dn_ap[g * FG * 128:(g + 1) * FG * 128, :].rearrange("(k p) e -> p k e", p=128), WD, c.wdram, q="gpsimd")
        for tt in range(NTT):
            c.ai = getattr(c, "ai", 0) + 1
            A = aT_bufs[c.ai % 2]
            for j in range(FG):
                Pg = nxt(c, "A"); Pu = nxt(c, "A")
                for k in range(8):
                    f.op("tensor", lambda e: e.matmul(Pg[:], lhsT=W[:, k, 0, j * 128:(j + 1) * 128], rhs=uTs[tt][:, k, :], start=(k == 0), stop=(k == 7)), [W.k(0), uTs[tt].k(k)], [Pg])
                for k in range(8):
                    f.op("tensor", lambda e: e.matmul(Pu[:], lhsT=W[:, k, 1, j * 128:(j + 1) * 128], rhs=uTs[tt][:, k, :], start=(k == 0), stop=(k == 7)), [W.k(1), uTs[tt].k(k)], [Pu])
                sg = tmp(f, c, "sg", F32, 3)
                f.op("scalar", lambda e: e.activation(out=sg[:], in_=Pg[:], func=AF.Silu), [Pg], [sg])
                if gbs is None:
                    f.op("vector", lambda e: e.tensor_tensor(out=A[:, j, :], in0=sg[:], in1=Pu[:], op=ALU.mult), [sg, Pu], [A.k(j)])
                else:
                    sg2 = tmp(f, c, "sg2", F32, 3)
                    f.op("vector", lambda e: e.tensor_tensor(out=sg2[:], in0=sg[:], in1=Pu[:], op=ALU.mult), [sg, Pu], [sg2])
                    f.op("vector", lambda e: e.tensor_tensor(out=A[:, j, :], in0=sg2[:], in1=gbs[tt][:], op=ALU.mult), [sg2, gbs[tt]], [A.k(j)])
            for ec in range(8):
                P = nxt(c, "B")
                for j in range(FG):
                    f.op("tensor", lambda e: e.matmul(P[:], lhsT=WD[:, j, ec * 128:(ec + 1) * 128], rhs=A[:, j, :], start=(j == 0), stop=(j == FG - 1)), [WD, A.k(j)], [P])
                f.op("vector", lambda e: e.tensor_tensor(out=hTs[tt][:, ec, :], in0=hTs[tt][:, ec, :], in1=P[:], op=ALU.add), [hTs[tt].k(ec), P], [hTs[tt].k(ec)])


PVC = {"gn_g": 0, "gn_b": 8, "g1": 16, "g2": 24, "qg": 32, "kg": 33, "bf": 34}
NPC = 35


def build_C():
    nc = bass.Bass("TRN2", target_bir_lowering=False)
    f = FW(nc)
    c = Ctx()
    xT = f.dram("xT", [1024, 2048], F32, "ExternalInput")
    yT = f.dram("yT", [1024, 2048], F32, "ExternalInput")
    gb = f.dram("gb", [2, 1024, 2048], BF16, "ExternalInput")
    pvd = f.dram("pv", [128, NPC], F32, "ExternalInput")
    cst = f.dram("cst", [128, 384], F32, "ExternalInput")
    w_o = f.dram("w_o", [1024, 1024], F32, "ExternalInput")
    w_gu = f.dram("w_gu", [1024, 7168], F32, "ExternalInput")
    w_dn = f.dram("w_dn", [3584, 1024], F32, "ExternalInput")
    w_in = f.dram("w_in", [1024, 4112], F32, "ExternalInput")
    o_h = f.dram("o_h", [1024, 2048], F32, "ExternalOutput")
    o_c = f.dram("o_c", [4, 1024, 2048], BF16, "ExternalOutput")
    o_lf = f.dram("o_lf", [16, 2048], F32, "ExternalOutput")
    c.wdram = f.dram("wdummy", [1, 1], F32, "Internal")
    setup_common(f, c, cst)
    pv = f.sb([128, NPC], F32, "pvs")
    f.dma(pv[:], pvd[:], pv, pvd)
    wo = f.sb([128, 8, 1024], BF16, "wo")
    for k4 in range(0, 8, 4):
        f.dma(wo[:, k4:k4 + 4, :], w_o[k4 * 128:(k4 + 4) * 128, :].rearrange("(k p) e -> p k e", p=128), wo.k(k4), w_o, q="gpsimd")
    wfl = f.sb([128, 8, 16], BF16, "wfl")
    f.dma(wfl[:], w_in[:, 4096:4112].rearrange("(k p) e -> p k e", p=128), wfl, w_in, q="gpsimd")
    NTT = 2
    hTs = [f.sb([128, 8, 512], F32, f"hT{i}") for i in range(NTT)]
    uTs = [f.sb([128, 8, 512], BF16, f"uT{i}") for i in range(NTT)]
    zT = f.sb([128, 8, 512], BF16, "zT")
    aT = [f.sb([128, 4, 512], BF16, f"aTg{i}") for i in range(2)]
    wgu_bufs = [f.sb([128, 8, 2, 512], BF16, f"wgu{i}") for i in range(2)]
    wd_bufs = [f.sb([128, 4, 1024], BF16, f"wd{i}") for i in range(2)]
    oc = [f.sb([128, 512], BF16, f"oc{i}") for i in range(3)]
    oci = [0]
    lf = f.sb([16, 512], F32, "lf")
    for half in range(2):
        for tt in range(NTT):
            hT = hTs[tt]
            ts = slice(1024 * half + 512 * tt, 1024 * half + 512 * tt + 512)
            f.dma(hT[:], xT[:, ts].rearrange("(k p) t -> p k t", p=128), hT, xT)
            for ec in range(8):
                es = slice(ec * 128, (ec + 1) * 128)
                y = tmp(f, c, "y"); g2 = tmp(f, c, "gbt", BF16, 2, (128, 2, 512))
                f.dma(y[:], yT[es, ts], y, yT)
                f.dma(g2[:], gb[:, es, ts].rearrange("q p t -> p q t"), g2, gb)
                yb = tmp(f, c, "yb", BF16)
                f.op("scalar", lambda e: e.activation(out=yb[:], in_=y[:], func=AF.Copy), [y], [yb])
                P = nxt(c, "C")
                f.op("tensor", lambda e: e.matmul(P[:], lhsT=c.bones[:], rhs=yb[:], start=True, stop=True), [c.bones, yb], [P])
                yc = tmp(f, c, "yc")
                f.op("vector", lambda e: e.scalar_tensor_tensor(out=yc[:], in0=P[:], scalar=-1.0 / 64, in1=y[:], op0=ALU.mult, op1=ALU.add), [P, y], [yc])
                sq = tmp(f, c, "sq", BF16, 3)
                f.op("scalar", lambda e: e.activation(out=sq[:], in_=yc[:], func=AF.Square), [yc], [sq])
                P2 = nxt(c, "C")
                f.op("tensor", lambda e: e.matmul(P2[:], lhsT=c.bones[:], rhs=sq[:], start=True, stop=True), [c.bones, sq], [P2])
                rs = tmp(f, c, "rs")
                f.op("scalar", lambda e: e.activation(out=rs[:], in_=P2[:], func=AF.Sqrt, scale=1.0 / 64, bias=GN_EPS), [P2], [rs])
                f.op("vector", lambda e: e.reciprocal(out=rs[:], in_=rs[:]), [rs], [rs])
                f.op("vector", lambda e: e.tensor_tensor(out=yc[:], in0=yc[:], in1=rs[:], op=ALU.mult), [yc, rs], [yc])
                f.op("vector", lambda e: e.tensor_scalar(out=yc[:], in0=yc[:], scalar1=pv[:, PVC["gn_g"] + ec:PVC["gn_g"] + ec + 1], scalar2=pv[:, PVC["gn_b"] + ec:PVC["gn_b"] + ec + 1], op0=ALU.mult, op1=ALU.add), [yc, pv], [yc])
                f.op("vector", lambda e: e.tensor_tensor(out=yc[:], in0=yc[:], in1=g2[:, 1, :], op=ALU.add), [yc, g2], [yc])
                f.op("vector", lambda e: e.tensor_tensor(out=zT[:, ec, :], in0=yc[:], in1=g2[:, 0, :], op=ALU.mult), [yc, g2], [zT.k(ec)])
            linear_res(f, c, wo, zT, hT)
            rmsnorm_fm(f, c, hT, pv, PVC["g1"], uTs[tt])
        ffn_ws(f, c, uTs, hTs, w_gu[:], w_dn[:], wgu_bufs, wd_bufs, aT)
        for tt in range(NTT):
            ts = slice(1024 * half + 512 * tt, 1024 * half + 512 * tt + 512)
            f.dma(o_h[:, ts].rearrange("(k p) t -> p k t", p=128), hTs[tt][:], o_h, hTs[tt])
            rmsnorm_fm(f, c, hTs[tt], pv, PVC["g2"], uTs[tt])
        for g in range(8):
            c.wgi += 1
            W = wgu_bufs[c.wgi % 2]
            Wv = W[:].rearrange("p k h e -> p k (h e)")
            f.dma(Wv[:, :, 0:512], w_in[:, g * 512:(g + 1) * 512].rearrange("(k p) e -> p k e", p=128), W, c.wdram, q="gpsimd")
            for tt in range(NTT):
                ts = slice(1024 * half + 512 * tt, 1024 * half + 512 * tt + 512)
                uT = uTs[tt]
                for j in range(4):
                    e32 = g * 4 + j
                    kind, ec = divmod(e32, 8)
                    P = nxt(c, "A")
                    for k in range(8):
                        f.op("tensor", lambda e: e.matmul(P[:], lhsT=Wv[:, k, j * 128:(j + 1) * 128], rhs=uT[:, k, :], start=(k == 0), stop=(k == 7)), [W, uT.k(k)], [P])
                    oci[0] += 1
                    O = oc[oci[0] % 3]
                    if kind in (0, 1):
                        qs = tmp(f, c, "qs")
                        f.op("scalar", lambda e: e.activation(out=qs[:], in_=P[:], func=AF.Copy), [P], [qs])
                        sq = tmp(f, c, "sq", BF16, 3)
                        f.op("scalar", lambda e: e.activation(out=sq[:], in_=qs[:], func=AF.Square), [qs], [sq])
                        P2 = nxt(c, "C")
                        f.op("tensor", lambda e: e.matmul(P2[:], lhsT=c.bones[:], rhs=sq[:], start=True, stop=True), [c.bones, sq], [P2])
                        rs = tmp(f, c, "rs")
                        if kind == 0:
                            f.op("scalar", lambda e: e.activation(out=rs[:], in_=P2[:], func=AF.Sqrt, scale=1.0, bias=64 * RMS_EPS), [P2], [rs])
                        else:
                            f.op("scalar", lambda e: e.activation(out=rs[:], in_=P2[:], func=AF.Sqrt, scale=1.0 / 64, bias=RMS_EPS), [P2], [rs])
                        f.op("vector", lambda e: e.reciprocal(out=rs[:], in_=rs[:]), [rs], [rs])
                        gc = PVC["qg"] if kind == 0 else PVC["kg"]
                        f.op("vector", lambda e: e.scalar_tensor_tensor(out=O[:], in0=qs[:], scalar=pv[:, gc:gc + 1], in1=rs[:], op0=ALU.mult, op1=ALU.mult), [qs, pv, rs], [O])
                    elif kind == 2:
                        f.op("scalar", lambda e: e.activation(out=O[:], in_=P[:], func=AF.Copy), [P], [O])
                    else:
                        f.op("scalar", lambda e: e.activation(out=O[:], in_=P[:], func=AF.Sigmoid), [P], [O])
                    f.dma(o_c[kind, ec * 128:(ec + 1) * 128, ts], O[:], o_c, O)
        for tt in range(NTT):
            ts = slice(1024 * half + 512 * tt, 1024 * half + 512 * tt + 512)
            uT = uTs[tt]
            P = nxt(c, "C")
            for k in range(8):
                f.op("tensor", lambda e: e.matmul(P[0:16, :], lhsT=wfl[:, k, :], rhs=uT[:, k, :], start=(k == 0), stop=(k == 7)), [wfl, uT.k(k)], [P])
            f.op("vector", lambda e: e.tensor_scalar(out=lf[:], in0=P[0:16, :], scalar1=pv[0:16, PVC["bf"]:PVC["bf"] + 1], scalar2=None, op0=ALU.add), [P, pv], [lf])
            f.op("scalar", lambda e: e.activation(out=lf[:], in_=lf[:], func=AF.Exp, scale=-1.0), [lf], [lf])
            f.op("scalar", lambda e: e.activation(out=lf[:], in_=lf[:], func=AF.Ln, bias=1.0), [lf], [lf])
            f.op("vector", lambda e: e.tensor_scalar(out=lf[:], in0=lf[:], scalar1=-1.0, scalar2=None, op0=ALU.mult), [lf], [lf])
            f.dma(o_lf[:, ts], lf[:], o_lf, lf)
    f.final_wait([o_h, o_c, o_lf])
    return f.build()


def inputs_C(inp, yfull, obfA):
    x = inp["x"][0]
    pvv = np.zeros((128, NPC), np.float32)
    pvv[:, 0:8] = fmcols(inp["rwkv_gn_g"][0]); pvv[:, 8:16] = fmcols(inp["rwkv_gn_b"][0])
    pvv[:, 16:24] = fmcols(inp["norm_g"][0, 1]); pvv[:, 24:32] = fmcols(inp["norm_g"][1, 0])
    pvv[:, 32] = np.tile(inp["fox_q_gain"][0], 2); pvv[:, 33] = np.tile(inp["fox_k_gain"][0], 2)
    pvv[0:16, 34] = inp["fox_b_f"][0]
    cst = consts_CE()
    maps = []
    for c in range(8):
        ts = slice(2048 * c, 2048 * c + 2048)
        maps.append({"xT": np.ascontiguousarray(x[ts].T), "yT": np.ascontiguousarray(yfull[ts].T),
                     "gb": np.ascontiguousarray(obfA[c][6:8]), "pv": pvv, "cst": cst,
                     "w_o": inp["rwkv_w_o"][0], "w_gu": inp["ffn_w_gu"][0], "w_dn": inp["ffn_w_down"][0], "w_in": inp["fox_w_in"][0]})
    return maps


PVE = {"g3": 0, "gf": 8}
NPE = 16
NEXP = 8
NTE = 1024
FGE = 4


def build_E():
    nc = bass.Bass("TRN2", target_bir_lowering=False)
    f = FW(nc)
    c = Ctx()
    hTd = f.dram("hT", [1024, 2048], F32, "ExternalInput")
    oTd = f.dram("oT", [1024, 2048], BF16, "ExternalInput")
    pvd = f.dram("pv", [128, NPE], F32, "ExternalInput")
    cst = f.dram("cst", [128, 384], F32, "ExternalInput")
    seld = f.dram("sele", [8, 8 * 128], F32, "ExternalInput")
    w_o = f.dram("w_o", [1024, 1024], F32, "ExternalInput")
    w_r = f.dram("w_r", [1024, 8], F32, "ExternalInput")
    w_gu = f.dram("w_gu", [8, 1024, 7168], F32, "ExternalInput")
    w_dn = f.dram("w_dn", [8, 3584, 1024], F32, "ExternalInput")
    o_out = f.dram("o_out", [1024, 2048], F32, "ExternalOutput")
    c.wdram = f.dram("wdummy", [1, 1], F32, "Internal")
    setup_common(f, c, cst)
    pv = f.sb([128, NPE], F32, "pvs")
    f.dma(pv[:], pvd[:], pv, pvd)
    sele = f.sb([8, 8, 128], F32, "sele")
    f.dma(sele[:].rearrange("k e m -> k (e m)"), seld[:], sele, seld)
    wo = f.sb([128, 8, 1024], BF16, "wo")
    for k4 in range(0, 8, 4):
        f.dma(wo[:, k4:k4 + 4, :], w_o[k4 * 128:(k4 + 4) * 128, :].rearrange("(k p) e -> p k e", p=128), wo.k(k4), w_o, q="gpsimd")
    wr = f.sb([128, 8, 8], F32, "wr")
    f.dma(wr[:], w_r[:].rearrange("(k p) e -> p k e", p=128), wr, w_r)
    NTT = NTE // 512
    hT = [f.sb([128, 8, 512], F32, f"hT{i}") for i in range(NTT)]
    uT = [f.sb([128, 8, 512], BF16, f"uT{i}") for i in range(NTT)]
    gTs = [f.sb([8, 512], F32, f"gTs{i}") for i in range(NTT)]
    zT = f.sb([128, 8, 512], BF16, "zT")
    uF = f.sb([128, 8, 512], F32, "uF")
    wgu_bufs = [f.sb([128, 8, 2, FGE * 128], BF16, f"wgu{i}") for i in range(2)]
    wd_bufs = [f.sb([128, FGE, 1024], BF16, f"wd{i}") for i in range(2)]
    aT = [f.sb([128, FGE, 512], BF16, f"aTg{i}") for i in range(2)]
    gb = [f.sb([128, 512], F32, f"gb{i}") for i in range(NTT)]
    lg = f.sb([8, 512], F32, "lg")
    lt = f.sb([128, 4, 8], F32, "lt")
    gts = f.sb([128, 4, 8], F32, "gts")
    sm = {n: f.sb([128, 8], F32, "sm_" + n) for n in ("eq", "l2", "sel", "ex")}
    sc = {n: f.sb([128, 4], F32, "sc_" + n) for n in ("m1", "nm1", "m2", "sum")}
    wgi = 0
    ai = 0
    for half in range(2048 // NTE):
        for tt in range(NTT):
            ts = slice(half * NTE + 512 * tt, half * NTE + 512 * tt + 512)
            H, U = hT[tt], uT[tt]
            f.dma(H[:], hTd[:, ts].rearrange("(k p) t -> p k t", p=128), H, hTd)
            f.dma(zT[:], oTd[:, ts].rearrange("(k p) t -> p k t", p=128), zT, oTd)
            linear_res(f, c, wo, zT, H)
            rmsnorm_fm(f, c, H, pv, PVE["g3"], U, uF)
            P = nxt(c, "C")
            for k in range(8):
                f.op("tensor", lambda e: e.matmul(P[0:8, :], lhsT=wr[:, k, :], rhs=uF[:, k, :], start=(k == 0), stop=(k == 7)), [wr, uF.k(k)], [P])
            f.op("vector", lambda e: e.tensor_copy(out=lg[:], in_=P[0:8, :]), [P], [lg])
            P2 = nxt(c, "C")
            for j in range(4):
                f.op("tensor", lambda e: e.transpose(out=P2[:, j * 8:(j + 1) * 8], in_=lg[:, j * 128:(j + 1) * 128], identity=c.cs[0:8, 0:8]), [lg, c.cs], [P2])
            f.op("vector", lambda e: e.tensor_copy(out=lt[:].rearrange("p j e -> p (j e)"), in_=P2[:, 0:32]), [P2], [lt])
            f.op("vector", lambda e: e.tensor_reduce(out=sc["m1"][:], in_=lt[:], axis=AX.X, op=ALU.max), [lt], [sc["m1"]])
            f.op("vector", lambda e: e.tensor_scalar(out=sc["nm1"][:], in0=sc["m1"][:], scalar1=-1.0, scalar2=None, op0=ALU.mult), [sc["m1"]], [sc["nm1"]])
            for j in range(4):
                L = lt[:, j, :]
                f.op("vector", lambda e: e.tensor_scalar(out=sm["eq"][:], in0=L, scalar1=sc["m1"][:, j:j + 1], scalar2=None, op0=ALU.is_ge), [lt, sc["m1"]], [sm["eq"]])
                f.op("vector", lambda e: e.scalar_tensor_tensor(out=sm["l2"][:], in0=sm["eq"][:], scalar=-1e30, in1=L, op0=ALU.mult, op1=ALU.add), [sm["eq"], lt], [sm["l2"]])
                f.op("vector", lambda e: e.tensor_reduce(out=sc["m2"][:, j:j + 1], in_=sm["l2"][:], axis=AX.X, op=ALU.max), [sm["l2"]], [sc["m2"]])
                f.op("vector", lambda e: e.tensor_scalar(out=sm["sel"][:], in0=L, scalar1=sc["m2"][:, j:j + 1], scalar2=None, op0=ALU.is_ge), [lt, sc["m2"]], [sm["sel"]])
                f.op("scalar", lambda e: e.activation(out=sm["ex"][:], in_=L, func=AF.Exp, bias=sc["nm1"][:, j:j + 1]), [lt, sc["nm1"]], [sm["ex"]])
                f.op("vector", lambda e: e.tensor_tensor(out=sm["ex"][:], in0=sm["ex"][:], in1=sm["sel"][:], op=ALU.mult), [sm["ex"], sm["sel"]], [sm["ex"]])
                f.op("vector", lambda e: e.tensor_reduce(out=sc["sum"][:, j:j + 1], in_=sm["ex"][:], axis=AX.X, op=ALU.add), [sm["ex"]], [sc["sum"]])
                f.op("vector", lambda e: e.reciprocal(out=sc["sum"][:, j:j + 1], in_=sc["sum"][:, j:j + 1]), [sc["sum"]], [sc["sum"]])
                f.op("vector", lambda e: e.tensor_scalar(out=gts[:, j, :], in0=sm["ex"][:], scalar1=sc["sum"][:, j:j + 1], scalar2=None, op0=ALU.mult), [sm["ex"], sc["sum"]], [gts])
            P3 = nxt(c, "C")
            for j in range(4):
                f.op("tensor", lambda e: e.transpose(out=P3[0:8, j * 128:(j + 1) * 128], in_=gts[:, j, :], identity=c.cs[:, 0:128]), [gts, c.cs], [P3])
            f.op("vector", lambda e: e.tensor_copy(out=gTs[tt][:], in_=P3[0:8, :]), [P3], [gTs[tt]])
        for ex in range(NEXP):
            for tt in range(NTT):
                P4 = nxt(c, "C")
                f.op("tensor", lambda e: e.matmul(P4[:], lhsT=sele[:, ex, :], rhs=gTs[tt][:], start=True, stop=True), [sele, gTs[tt]], [P4])
                f.op("scalar", lambda e: e.activation(out=gb[tt][:], in_=P4[:], func=AF.Copy), [P4], [gb[tt]])
            ffn_ws(f, c, uT, hT, w_gu[ex], w_dn[ex], wgu_bufs, wd_bufs, aT, gbs=gb, FG=FGE)
        for tt in range(NTT):
            ts = slice(half * NTE + 512 * tt, half * NTE + 512 * tt + 512)
            rmsnorm_fm(f, c, hT[tt], pv, PVE["gf"], uT[tt], uF)
            f.dma(o_out[:, ts].rearrange("(k p) t -> p k t", p=128), uF[:], o_out, uF)
    f.final_wait([o_out])
    return f.build()


def inputs_E(inp, hT_list, oT):
    pvv = np.zeros((128, NPE), np.float32)
    pvv[:, 0:8] = fmcols(inp["norm_g"][1, 1]); pvv[:, 8:16] = fmcols(inp["final_g"])
    cst = consts_CE()
    sele = np.zeros((8, 8, 128), np.float32)
    for e in range(8):
        sele[e, e, :] = 1.0
    maps = []
    for c in range(8):
        maps.append({"hT": hT_list[c], "oT": np.ascontiguousarray(oT[:, 2048 * c:2048 * c + 2048]), "pv": pvv, "cst": cst,
                     "sele": sele.reshape(8, 1024), "w_o": inp["fox_w_o"][0], "w_r": inp["moe_w_router"][0],
                     "w_gu": inp["moe_w_gu"][0], "w_dn": inp["moe_w_down"][0]})
    return maps


T_ALL = 16384


def build_D(T=T_ALL):
    nc = bass.Bass("TRN2", target_bir_lowering=False)
    f = FW(nc)
    NQ = T // 512
    NKB = T // 128
    NSEG = T // 2048
    qT = f.dram("qT", [2, 64, T], BF16, "ExternalInput")
    kT = f.dram("kT", [2, 64, T], BF16, "ExternalInput")
    vt = f.dram("vt", [2, 128, NKB * 65], BF16, "ExternalInput")
    og = f.dram("og", [2, 64, T], BF16, "ExternalInput")
    lfd = f.dram("lf", [2, T], F32, "ExternalInput")
    mkd = f.dram("mk", [128, 4 * 512], F32, "ExternalInput")
    sld = f.dram("sel", [65, 64], F32, "ExternalInput")
    o_o = f.dram("o_o", [2, 64, T], BF16, "ExternalOutput")

    mkf = f.sb([128, 4, 512], F32, "mkf")
    f.dma(mkf[:].rearrange("p m t -> p (m t)"), mkd[:], mkf, mkd)
    mk = f.sb([128, 4, 512], BF16, "mk")
    f.op("vector", lambda e: e.tensor_copy(out=mk[:], in_=mkf[:]), [mkf], [mk])
    sel = f.sb([65, 64], F32, "sels")
    f.dma(sel[:], sld[:], sel, sld)
    ones = f.sb([1, 2048], F32, "ones1")
    f.op("gpsimd", lambda e: e.memset(ones[:], 1.0), [], [ones])

    Qa = f.sb([70, T], BF16, "Qa")
    Ka = f.sb([70, T], BF16, "Ka")
    Vt = f.sb([128, NKB, 65], BF16, "Vt")
    lfs = [f.sb([1, 2048], F32, f"lfs{i}") for i in range(2)]
    cseg = [f.sb([1, 2048], F32, f"cseg{i}") for i in range(2)]
    parts = [f.sb([1, 3, 2048], BF16, f"parts{i}") for i in range(2)]
    r1 = f.sb([1, 2048], F32, "r1")
    PSs = [f.ps([128, 512], F32, f"PSs{i}") for i in range(4)]
    PO = [f.ps([65, 512], F32, f"PO{i}") for i in range(2)]
    PD = f.ps([64, 512], F32, "PD")
    PT = [f.sb([128, 512], BF16, f"PT{i}") for i in range(5)]
    Osb = [f.sb([65, 512], F32, f"Osb{i}") for i in range(2)]
    rden = f.sb([64, 512], F32, "rden")
    o1 = f.sb([64, 512], F32, "o1")
    ogt = [f.sb([64, 512], BF16, f"ogt{i}") for i in range(2)]
    o2 = [f.sb([64, 512], BF16, f"o2{i}") for i in range(2)]
    scl = [f.sb([128, 512], F32, f"scl{i}") for i in range(2)]
    ti = 0
    for h in range(2):
        f.dma(Qa[0:64, :], qT[h], Qa.k("top"), qT)
        f.dma(Ka[0:64, :], kT[h], Ka.k("top"), kT)
        f.dma(Vt[:].rearrange("p k d -> p (k d)"), vt[h], Vt, vt)
        f.op("gpsimd", lambda e: e.memset(Vt[:, :, 64:65], 1.0), [], [Vt])
        f.op("gpsimd", lambda e: e.memset(Qa[64:70, :], -1.0), [], [Qa.k("aug")])
        f.op("gpsimd", lambda e: e.memset(Ka[64:70, :], 1.0), [], [Ka.k("aug")])
        for sg in range(NSEG):
            b = sg % 2
            ss = slice(sg * 2048, (sg + 1) * 2048)
            f.dma(lfs[b][:], lfd[h:h + 1, ss], lfs[b], lfd)
            init = 0.0 if sg == 0 else cseg[1 - b][:, 2047:2048]
            rd = [ones, lfs[b]] + ([] if sg == 0 else [cseg[1 - b]])
            f.op("vector", lambda e: e.tensor_tensor_scan(out=cseg[b][:], data0=ones[:], data1=lfs[b][:], initial=init, op0=ALU.mult, op1=ALU.add), rd, [cseg[b]])
            P3 = parts[b]
            f.op("vector", lambda e: e.tensor_copy(out=P3[:, 0, :], in_=cseg[b][:]), [cseg[b]], [P3])
            f.op("vector", lambda e: e.tensor_tensor(out=r1[:], in0=cseg[b][:], in1=P3[:, 0, :], op=ALU.subtract), [cseg[b], P3], [r1])
            f.op("vector", lambda e: e.tensor_copy(out=P3[:, 1, :], in_=r1[:]), [r1], [P3])
            f.op("vector", lambda e: e.tensor_tensor(out=r1[:], in0=r1[:], in1=P3[:, 1, :], op=ALU.subtract), [r1, P3], [r1])
            f.op("vector", lambda e: e.tensor_copy(out=P3[:, 2, :], in_=r1[:]), [r1], [P3])
            for r in range(3):
                f.dma(Qa[64 + r:65 + r, ss], P3[:, r, :], Qa.k("aug"), P3)
                f.dma(Ka[67 + r:68 + r, ss], P3[:, r, :], Ka.k("aug"), P3)
        tiles = [(qi, kb) for qi in range(NQ) for kb in range(4 * qi + 4)]
        LA = 3
        base = ti

        def emit_qk(i):
            qi, kb = tiles[i]
            qs = slice(qi * 512, (qi + 1) * 512)
            ps = PSs[(base + i) % 4]
            f.op("tensor", lambda e: e.matmul(ps[:], lhsT=Ka[0:70, kb * 128:(kb + 1) * 128], rhs=Qa[0:70, qs], start=True, stop=True), [Ka, Qa], [ps])

        def emit_rest(i):
            qi, kb = tiles[i]
            qs = slice(qi * 512, (qi + 1) * 512)
            nkb = 4 * qi + 4
            ps = PSs[(base + i) % 4]; pt = PT[(base + i) % 5]
            po = PO[qi % 2]
            if kb == 0:
                f.dma(ogt[qi % 2][:], og[h, :, qs], ogt[qi % 2], og)
            if kb >= 4 * qi:
                sc = scl[kb % 2]
                f.op("vector", lambda e: e.tensor_scalar(out=sc[:], in0=ps[:], scalar1=30.0, scalar2=None, op0=ALU.min), [ps], [sc])
                f.op("scalar", lambda e: e.activation(out=pt[:], in_=sc[:], func=AF.Exp), [sc], [pt])
                m = kb - 4 * qi
                eng = "vector" if m % 2 == 0 else "gpsimd"
                f.op(eng, lambda e: e.tensor_tensor(out=pt[:], in0=pt[:], in1=mk[:, m, :], op=ALU.mult), [pt, mk], [pt])
            else:
                f.op("scalar", lambda e: e.activation(out=pt[:], in_=ps[:], func=AF.Exp), [ps], [pt])
            f.op("tensor", lambda e: e.matmul(po[:], lhsT=Vt[:, kb, :], rhs=pt[:], start=(kb == 0), stop=(kb == nkb - 1)), [Vt, pt], [po])
            if kb == nkb - 1:
                osb = Osb[qi % 2]
                f.op("vector", lambda e: e.tensor_copy(out=osb[:], in_=po[:]), [po], [osb])
                f.op("tensor", lambda e: e.matmul(PD[:], lhsT=sel[:], rhs=osb[:], start=True, stop=True), [sel, osb], [PD])
                f.op("vector", lambda e: e.reciprocal(out=rden[:], in_=PD[:]), [PD], [rden])
                f.op("gpsimd", lambda e: e.tensor_tensor(out=o1[:], in0=osb[0:64, :], in1=rden[:], op=ALU.mult), [osb, rden], [o1])
                f.op("gpsimd", lambda e: e.tensor_tensor(out=o2[qi % 2][:], in0=o1[:], in1=ogt[qi % 2][:], op=ALU.mult), [o1, ogt[qi % 2]], [o2[qi % 2]])
                f.dma(o_o[h, :, qs], o2[qi % 2][:], o_o, o2[qi % 2])
        n = len(tiles)
        for i in range(n + LA):
            if i < n:
                emit_qk(i)
            if i >= LA:
                emit_rest(i - LA)
        ti += n
    f.final_wait([o_o])
    return f.build()


def consts_D():
    m = np.zeros((128, 4, 512), np.float32)
    p = np.arange(128)[:, None]; j = np.arange(512)[None, :]
    for i in range(4):
        m[:, i, :] = ((i * 128 + p) <= j).astype(np.float32)
    sel = np.zeros((65, 64), np.float32); sel[64, :] = 1.0
    return m.reshape(128, 2048), sel


def inputs_D(oc_list, lf_list, T=T_ALL):
    oc = np.concatenate(oc_list, axis=2)[:, :, :T]
    lf = np.concatenate(lf_list, axis=1)[:, :T]
    mk, sel = consts_D()
    NKB = T // 128
    maps = []
    for c in range(8):
        cs = slice(128 * c, 128 * c + 128)
        q = oc[0][cs].reshape(2, 64, T); k = oc[1][cs].reshape(2, 64, T); og = oc[3][cs].reshape(2, 64, T)
        v = oc[2][cs].reshape(2, 64, NKB, 128)
        vp = np.zeros((2, 128, NKB, 65), dtype=oc.dtype)
        vp[:, :, :, 0:64] = v.transpose(0, 3, 2, 1)
        maps.append({"qT": np.ascontiguousarray(q), "kT": np.ascontiguousarray(k), "vt": vp.reshape(2, 128, NKB * 65),
                     "og": np.ascontiguousarray(og), "lf": np.ascontiguousarray(lf[2 * c:2 * c + 2]), "mk": mk, "sel": sel})
    return maps


def gather_D(results):
    return np.concatenate([np.asarray(r["o_o"]).reshape(128, -1) for r in results], axis=0)


def _run(nc, maps):
    return run_bass_kernel_spmd(nc, maps, core_ids=list(range(8)))


def kernel(**inputs):
    inp = {k: np.asarray(v) for k, v in inputs.items()}
    resA = _run(build_A(), inputs_A(inp))
    obfA = [np.asarray(r["o_bf"]) for r in resA.results]
    obf = np.concatenate(obfA, axis=2)
    ort = np.concatenate([np.asarray(r["o_rt"]) for r in resA.results], axis=1)
    owc = np.concatenate([np.asarray(r["o_wc"]) for r in resA.results], axis=1)
    resB = _run(build_B(), inputs_B(obf, ort, owc))
    y = gather_B(resB.results)
    del obf, ort, owc
    resC = _run(build_C(), inputs_C(inp, y, obfA))
    oc_list = [np.asarray(r["o_c"]) for r in resC.results]
    lf_list = [np.asarray(r["o_lf"]) for r in resC.results]
    hT_list = [np.asarray(r["o_h"]) for r in resC.results]
    resD = _run(build_D(), inputs_D(oc_list, lf_list))
    oT = gather_D(resD.results)
    resE = _run(build_E(), inputs_E(inp, hT_list, oT))
    out = np.concatenate([np.asarray(r["o_out"]) for r in resE.results], axis=1).T
    return np.ascontiguousarray(out, dtype=np.float32).reshape(1, 16384, 1024)
```

```python
import ml_dtypes
import numpy as np
import concourse.bass as bass
import concourse.mybir as mybir
from concourse.bass_utils import run_bass_kernel_spmd
from contextlib import ExitStack

F32 = mybir.dt.float32
BF16 = mybir.dt.bfloat16
I32 = mybir.dt.int32
ALU = mybir.AluOpType
AF = mybir.ActivationFunctionType
AX = mybir.AxisListType

SAME_ENGINE_SYNC = True


class _Rec:
    def __init__(self):
        self.call = None

    def __getattr__(self, name):
        def cap(*a, **k):
            self.call = (name, a, k)
            return self
        return cap


def _replay(call):
    name, a, k = call
    return lambda e: getattr(e, name)(*a, **k)


class _Trk:
    __slots__ = ("w", "r")

    def __init__(self):
        self.w = {}
        self.r = {}


class Buf:
    def __init__(self, fw, name, t, kind):
        self.fw = fw
        self.name = name
        self.t = t
        self.kind = kind
        self.whole = _Trk()
        self.subs = {}
        self.dsem = None

    def __getitem__(self, idx):
        return self.t[idx]

    def k(self, key):
        return (self, key)


def _split(b):
    if isinstance(b, tuple):
        return b
    return (b, None)


class FW:
    CE = ("tensor", "vector", "scalar", "gpsimd")

    def __init__(self, nc):
        self.nc = nc
        self.es = ExitStack()
        self.q = {e: [] for e in ("tensor", "vector", "scalar", "gpsimd", "sync")}
        self.cnt = {e: 0 for e in self.CE}
        self.waited = {}
        self.sems = {}
        self.dcnt = {}
        self.nbuf = 0

    def sb(self, shape, dt, name=None):
        self.nbuf += 1
        name = "S_" + (name or f"sb{self.nbuf}")
        t = self.es.enter_context(self.nc.sbuf_tensor(name, list(shape), dt))
        return Buf(self, name, t, "sb")

    def ps(self, shape, dt=F32, name=None):
        self.nbuf += 1
        name = "P_" + (name or f"ps{self.nbuf}")
        t = self.es.enter_context(self.nc.psum_tensor(name, list(shape), dt))
        return Buf(self, name, t, "ps")

    def dram(self, name, shape, dt, kind):
        t = self.nc.dram_tensor(name, list(shape), dt, kind=kind).ap()
        return Buf(self, name, t, "dram")

    def _sem(self, key):
        if key not in self.sems:
            self.sems[key] = self.es.enter_context(self.nc.semaphore("s_" + str(key)))
        return self.sems[key]

    def _collect(self, eng, reads, writes):
        waits = {}

        def need_w(wd):
            for kv in wd.items():
                need(kv)

        def need(tok):
            if tok is None:
                return
            k, v = tok
            if k == eng and (eng == "tensor" or not SAME_ENGINE_SYNC):
                return
            if waits.get(k, 0) < v:
                waits[k] = v

        reads = list(reads)
        writes = list(writes)
        for b in list(reads):
            bb, _k = _split(b)
            if bb.kind == "ps":
                writes.append(bb)
        writes = [(_split(b)[0] if _split(b)[0].kind == "ps" else b) for b in writes]
        for b in reads:
            b, key = _split(b)
            if b.kind == "ps":
                continue
            trks = [b.whole] + ([b.subs[key]] if (key is not None and key in b.subs) else
                                (list(b.subs.values()) if key is None else []))
            for t in trks:
                need_w(t.w)
        for b in writes:
            b, key = _split(b)
            trks = [b.whole] + ([b.subs[key]] if (key is not None and key in b.subs) else
                                (list(b.subs.values()) if key is None else []))
            for t in trks:
                need_w(t.w)
                for k, v in t.r.items():
                    need((k, v))
        out = []
        for k, v in waits.items():
            if self.waited.get((eng, k), 0) >= v:
                continue
            self.waited[(eng, k)] = v
            out.append((k, v))
        return out

    def _update(self, reads, writes, tok):
        reads = list(reads)
        writes = list(writes)
        for b in list(reads):
            bb, _k = _split(b)
            if bb.kind == "ps":
                writes.append(bb)
        reads = [b for b in reads if _split(b)[0].kind != "ps"]
        writes = [(_split(b)[0] if _split(b)[0].kind == "ps" else b) for b in writes]
        for b in reads:
            b, key = _split(b)
            t = b.whole if key is None else b.subs.setdefault(key, _Trk())
            k, v = tok
            if t.r.get(k, 0) < v:
                t.r[k] = v
        for b in writes:
            b, key = _split(b)
            t = b.whole if key is None else b.subs.setdefault(key, _Trk())
            if b.kind == "dram":
                if t.w.get(tok[0], 0) < tok[1]:
                    t.w[tok[0]] = tok[1]
            else:
                t.w = {tok[0]: tok[1]}
                t.r = {}
                if key is None:
                    b.subs = {}

    def op(self, eng, fn, reads=(), writes=()):
        waits = self._collect(eng, reads, writes)
        self.cnt[eng] += 1
        tok = (eng, self.cnt[eng])
        self._update(reads, writes, tok)
        rec = _Rec()
        fn(rec)
        self.q[eng].append((waits, _replay(rec.call), (eng, 1)))

    def dma(self, out, in_, outb, inb, q="sync", **kw):
        ob, ok_ = _split(outb)
        ib, ik_ = _split(inb)
        sb, sk = (ob, ok_) if ob.kind != "dram" else (ib, ik_)
        key = "d_" + sb.name + ("" if sk is None else "_" + "_".join(str(z) for z in (sk if isinstance(sk, tuple) else (sk,))))
        waits = self._collect(q, [inb], [outb])
        self.dcnt[key] = self.dcnt.get(key, 0) + 16
        tok = (key, self.dcnt[key])
        self._update([inb], [outb], tok)
        self.q[q].append((waits, lambda e: e.dma_start(out=out, in_=in_, **kw), (key, 16)))

    def final_wait(self, bufs, q="sync"):
        waits = self._collect(q, bufs, [])
        self.q[q].append((waits, None, None))

    def build(self):
        nc = self.nc
        for e in self.CE:
            self._sem(e)
        for q in self.q.values():
            for waits, fn, inc in q:
                for k, v in waits:
                    self._sem(k)
                if inc is not None:
                    self._sem(inc[0])
        fwself = self
        self.nsem = len(self.sems)
        with nc.Block() as block:
            def mk(ename):
                def body(eng):
                    for waits, fn, inc in fwself.q[ename]:
                        for k, v in waits:
                            eng.wait_ge(fwself.sems[k], v)
                        if fn is not None:
                            ins = fn(eng)
                            ins.then_inc(fwself.sems[inc[0]], inc[1])
                return body
            block.tensor(mk("tensor"))
            block.vector(mk("vector"))
            block.scalar(mk("scalar"))
            block.gpsimd(mk("gpsimd"))
            block.sync(mk("sync"))
        self.es.close()
        return nc


C0 = 0.6065306597126334
RMS_EPS = 1e-6
DEBUG = False

PVA = {"g0": 0, "mu": 8, "w0": 56, "a0": 64, "k_k": 72, "k_a": 80, "r_k": 88}
NPA = 96


def rmsnorm_to_fm(f, xsrc_ap, xbuf, uT, col0, ncols, src_col0, ident, gcol, pv, tmp, NTOK=128):
    pass


def build_A():
    nc = bass.Bass("TRN2", target_bir_lowering=False)
    f = FW(nc)
    xa = f.dram("xa", [17 * 128, 1024], F32, "ExternalInput")
    pvd = f.dram("pv", [128, NPA], F32, "ExternalInput")
    cst = f.dram("cst", [128, 256], F32, "ExternalInput")
    w_rkv = f.dram("w_rkv", [3, 1024, 1024], F32, "ExternalInput")
    w1d = f.dram("w1", [1024, 64], F32, "ExternalInput")
    a1d = f.dram("a1", [1024, 64], F32, "ExternalInput")
    g1d = f.dram("g1", [1024, 128], F32, "ExternalInput")
    w2d = f.dram("w2", [64, 1024], F32, "ExternalInput")
    a2d = f.dram("a2", [64, 1024], F32, "ExternalInput")
    g2d = f.dram("g2", [128, 1024], F32, "ExternalInput")
    o_bf = f.dram("o_bf", [8, 1024, 2048], BF16, "ExternalOutput")
    o_rt = f.dram("o_rt", [1024, 2048], F32, "ExternalOutput")
    o_wc = f.dram("o_wc", [1024, 32], F32, "ExternalOutput")

    pv = f.sb([128, NPA], F32, "pv")
    f.dma(pv[:], pvd[:], pv, pvd)
    cs = f.sb([128, 256], F32, "cs")
    f.dma(cs[:], cst[:], cs, cst)
    ident = f.sb([128, 128], BF16, "ident")
    bones = f.sb([128, 128], BF16, "bones")
    f.op("vector", lambda e: e.tensor_copy(out=ident[:], in_=cs[:, 0:128]), [cs], [ident])
    f.op("vector", lambda e: e.tensor_copy(out=bones[:], in_=cs[:, 128:256]), [cs], [bones])
    ones = f.sb([128, 64], F32, "ones")
    f.op("gpsimd", lambda e: e.memset(ones[:], 1.0), [], [ones])

    wr = f.sb([128, 3, 8, 1024], BF16, "wr")
    for n in range(3):
        for kc in range(0, 8, 4):
            f.dma(wr[:, n, kc:kc + 4, :], w_rkv[n, kc * 128:(kc + 4) * 128, :].rearrange("(k p) e -> p k e", p=128), wr.k((n, kc)), w_rkv, q="gpsimd")
    w1 = f.sb([128, 8, 64], BF16, "w1s"); a1 = f.sb([128, 8, 64], BF16, "a1s"); g1 = f.sb([128, 8, 128], BF16, "g1s")
    f.dma(w1[:], w1d[:].rearrange("(k p) e -> p k e", p=128), w1, w1d, q="gpsimd")
    f.dma(a1[:], a1d[:].rearrange("(k p) e -> p k e", p=128), a1, a1d, q="gpsimd")
    f.dma(g1[:], g1d[:].rearrange("(k p) e -> p k e", p=128), g1, g1d, q="gpsimd")
    w2 = f.sb([64, 1024], BF16, "w2s"); a2 = f.sb([64, 1024], BF16, "a2s"); g2 = f.sb([128, 1024], BF16, "g2s")
    f.dma(w2[:], w2d[:], w2, w2d, q="gpsimd")
    f.dma(a2[:], a2d[:], a2, a2d, q="gpsimd")
    f.dma(g2[:], g2d[:], g2, g2d, q="gpsimd")

    uT = f.sb([128, 8, 2049], BF16, "uT")
    xb = [f.sb([128, 1024], F32, f"xb{i}") for i in range(2)]
    xn = [f.sb([128, 1024], BF16, f"xn{i}") for i in range(2)]
    junk = f.sb([128, 1024], BF16, "junk")
    ssq = [f.sb([128, 1], F32, f"ssq{i}") for i in range(2)]
    ptr = [f.ps([128, 8, 128], BF16, "ptr0")] * 2
    for t in range(17):
        b = t % 2
        X, XN, SS, PT = xb[b], xn[b], ssq[b], ptr[b]
        f.dma(X[:], xa[t * 128:(t + 1) * 128, :], X, xa)
        f.op("scalar", lambda e, X=X, SS=SS: e.activation(out=junk[:], in_=X[:], func=AF.Square, accum_out=SS[:]), [X], [junk, SS])
        f.op("scalar", lambda e, SS=SS: e.activation(out=SS[:], in_=SS[:], func=AF.Sqrt, scale=1.0 / 1024, bias=RMS_EPS), [SS], [SS])
        f.op("vector", lambda e, SS=SS: e.reciprocal(out=SS[:], in_=SS[:]), [SS], [SS])
        f.op("vector", lambda e, X=X, XN=XN, SS=SS: e.tensor_scalar(out=XN[:], in0=X[:], scalar1=SS[:, 0:1], scalar2=None, op0=ALU.mult), [X, SS], [XN])
        for c in range(8):
            f.op("tensor", lambda e, c=c, XN=XN, PT=PT: e.transpose(out=PT[:, c, :], in_=XN[:, c * 128:(c + 1) * 128], identity=ident[:]), [XN, ident], [PT])
        for c in range(8):
            eng = "vector" if c % 2 == 0 else "gpsimd"
            eng = "vector"
            if t == 0:
                f.op(eng, lambda e, c=c, PT=PT: e.tensor_scalar(out=uT[:, c, 0:1], in0=PT[:, c, 127:128], scalar1=pv[:, PVA["g0"] + c:PVA["g0"] + c + 1], scalar2=None, op0=ALU.mult), [PT, pv], [uT.k(("t", t))])
            else:
                c0 = 1 + (t - 1) * 128
                f.op(eng, lambda e, c=c, PT=PT, c0=c0: e.tensor_scalar(out=uT[:, c, c0:c0 + 128], in0=PT[:, c, :], scalar1=pv[:, PVA["g0"] + c:PVA["g0"] + c + 1], scalar2=None, op0=ALU.mult), [PT, pv], [uT.k(("t", t))])

    xs = f.sb([128, 6, 8, 512], BF16, "xs")
    dd = [f.sb([128, 512], BF16, f"dd{i}") for i in range(2)]
    pr = f.ps([128, 512], F32, "pr"); pk = f.ps([128, 512], F32, "pk"); pvv = f.ps([128, 512], F32, "pvv")
    pw = f.ps([128, 512], F32, "pw"); pa = f.ps([128, 512], F32, "pa"); pg = f.ps([128, 512], F32, "pg")
    px1 = f.ps([128, 512], F32, "px1"); px2 = px1
    h1 = f.sb([64, 512], BF16, "h1"); ha = f.sb([64, 512], BF16, "ha"); hg = f.sb([128, 512], BF16, "hg")
    T = lambda n, dt=F32: f.sb([128, 512], dt, n)
    r_s, k_s, v_s, sg, a_s, cum, cprev = T("r_s"), T("k_s"), T("v_s"), T("sg"), T("a_s"), T("cum"), T("cprev")
    e_pos, e_neg, e_prev = T("e_pos"), T("e_neg"), T("e_prev")
    kkr, sq, ssm, kk, t1, kmod, bb, btf, ktf, rk = T("kkr"), T("sq", BF16), T("ssm"), T("kk"), T("t1"), T("kmod"), T("bb"), T("btf"), T("ktf"), T("rk", BF16)
    rt = T("rt")
    wc = f.sb([128, 8], F32, "wc")
    ob = f.sb([128, 8, 512], BF16, "ob")
    for s in range(4):
        cur = lambda c: uT[:, c, 1 + 512 * s:1 + 512 * s + 512]
        prv = lambda c: uT[:, c, 512 * s:512 * s + 512]
        ureads = [uT.k(("t", t)) for t in range(max(0, 4 * s), 4 * s + 5)]
        for c in range(8):
            D = dd[c % 2]
            f.op("gpsimd", lambda e, c=c, D=D: e.tensor_tensor(out=D[:], in0=prv(c), in1=cur(c), op=ALU.subtract), ureads, [D])
            for n in range(6):
                col = PVA["mu"] + n * 8 + c
                f.op("vector", lambda e, c=c, n=n, D=D, col=col: e.scalar_tensor_tensor(out=xs[:, n, c, :], in0=D[:], scalar=pv[:, col:col + 1], in1=cur(c), op0=ALU.mult, op1=ALU.add), [D, pv] + ureads, [xs.k(n)])
        for c in range(8):
            f.op("tensor", lambda e, c=c: e.matmul(pw[0:64, :], lhsT=w1[:, c, :], rhs=xs[:, 3, c, :], start=(c == 0), stop=(c == 7)), [w1, xs.k(3)], [pw])
        f.op("scalar", lambda e: e.activation(out=h1[:], in_=pw[0:64, :], func=AF.Tanh), [pw], [h1])
        for c in range(8):
            f.op("tensor", lambda e, c=c: e.matmul(pa[0:64, :], lhsT=a1[:, c, :], rhs=xs[:, 4, c, :], start=(c == 0), stop=(c == 7)), [a1, xs.k(4)], [pa])
        f.op("vector", lambda e: e.tensor_copy(out=ha[:], in_=pa[0:64, :]), [pa], [ha])
        for c in range(8):
            f.op("tensor", lambda e, c=c: e.matmul(pg[:], lhsT=g1[:, c, :], rhs=xs[:, 5, c, :], start=(c == 0), stop=(c == 7)), [g1, xs.k(5)], [pg])
        f.op("scalar", lambda e: e.activation(out=hg[:], in_=pg[:], func=AF.Sigmoid), [pg], [hg])
        for ec in range(8):
            es = slice(ec * 128, (ec + 1) * 128)
            for n, P in enumerate((pr, pk, pvv)):
                for c in range(8):
                    f.op("tensor", lambda e, c=c, n=n, P=P: e.matmul(P[:], lhsT=wr[:, n, c, es], rhs=xs[:, n, c, :], start=(c == 0), stop=(c == 7)), [wr.k((n, (c // 4) * 4)), xs.k(n)], [P])
            f.op("tensor", lambda e: e.matmul(pw[:], lhsT=w2[:, es], rhs=h1[:], start=True, stop=True), [w2, h1], [pw])
            f.op("tensor", lambda e: e.matmul(pa[:], lhsT=a2[:, es], rhs=ha[:], start=True, stop=True), [a2, ha], [pa])
            f.op("tensor", lambda e: e.matmul(pg[:], lhsT=g2[:, es], rhs=hg[:], start=True, stop=True), [g2, hg], [pg])
            pcol = lambda nm: pv[:, PVA[nm] + ec:PVA[nm] + ec + 1]
            f.op("scalar", lambda e: e.activation(out=r_s[:], in_=pr[:], func=AF.Copy), [pr], [r_s])
            f.op("scalar", lambda e: e.activation(out=k_s[:], in_=pk[:], func=AF.Copy), [pk], [k_s])
            f.op("vector", lambda e: e.tensor_copy(out=v_s[:], in_=pvv[:]), [pvv], [v_s])
            f.op("scalar", lambda e: e.activation(out=sg[:], in_=pw[:], func=AF.Sigmoid, bias=pcol("w0")), [pw, pv], [sg])
            f.op("scalar", lambda e: e.activation(out=a_s[:], in_=pa[:], func=AF.Sigmoid, bias=pcol("a0")), [pa, pv], [a_s])
            f.op("scalar", lambda e: e.activation(out=ob[:, 6, :], in_=pg[:], func=AF.Copy), [pg], [ob.k(6)])
            for q in range(8):
                f.op("vector", lambda e, q=q: e.tensor_tensor_scan(out=cum[:, q * 64:(q + 1) * 64], data0=ones[:], data1=sg[:, q * 64:(q + 1) * 64], initial=0.0, op0=ALU.mult, op1=ALU.add), [ones, sg], [cum])
            f.op("gpsimd", lambda e: e.tensor_tensor(out=cprev[:], in0=cum[:], in1=sg[:], op=ALU.subtract), [cum, sg], [cprev])
            f.op("scalar", lambda e: e.activation(out=e_pos[:], in_=cum[:], func=AF.Exp, scale=-C0), [cum], [e_pos])
            f.op("scalar", lambda e: e.activation(out=e_neg[:], in_=cum[:], func=AF.Exp, scale=C0), [cum], [e_neg])
            f.op("scalar", lambda e: e.activation(out=e_prev[:], in_=cprev[:], func=AF.Exp, scale=-C0), [cprev], [e_prev])
            f.op("scalar", lambda e: e.activation(out=wc[:], in_=cum[:].rearrange("p (q t) -> p q t", t=64)[:, :, 63], func=AF.Exp, scale=-C0), [cum], [wc])
            f.op("gpsimd", lambda e: e.tensor_scalar(out=kkr[:], in0=k_s[:], scalar1=pcol("k_k"), scalar2=None, op0=ALU.mult), [k_s, pv], [kkr])
            f.op("gpsimd", lambda e: e.tensor_tensor(out=sq[:], in0=kkr[:], in1=kkr[:], op=ALU.mult), [kkr], [sq])
            f.op("tensor", lambda e: e.matmul(px1[:], lhsT=bones[:], rhs=sq[:], start=True, stop=True), [bones, sq], [px1])
            f.op("vector", lambda e: e.tensor_scalar(out=ssm[:], in0=px1[:], scalar1=1e-24, scalar2=None, op0=ALU.max), [px1], [ssm])
            f.op("scalar", lambda e: e.activation(out=ssm[:], in_=ssm[:], func=AF.Sqrt), [ssm], [ssm])
            f.op("vector", lambda e: e.reciprocal(out=ssm[:], in_=ssm[:]), [ssm], [ssm])
            f.op("gpsimd", lambda e: e.tensor_tensor(out=kk[:], in0=kkr[:], in1=ssm[:], op=ALU.mult), [kkr, ssm], [kk])
            f.op("vector", lambda e: e.tensor_scalar(out=t1[:], in0=a_s[:], scalar1=-1.0, scalar2=pcol("k_a"), op0=ALU.add, op1=ALU.mult), [a_s, pv], [t1])
            f.op("vector", lambda e: e.scalar_tensor_tensor(out=kmod[:], in0=t1[:], scalar=1.0, in1=k_s[:], op0=ALU.add, op1=ALU.mult), [t1, k_s], [kmod])
            f.op("gpsimd", lambda e: e.tensor_tensor(out=bb[:], in0=kk[:], in1=a_s[:], op=ALU.mult), [kk, a_s], [bb])
            f.op("vector", lambda e: e.scalar_tensor_tensor(out=ob[:, 0, :], in0=kk[:], scalar=-1.0, in1=e_prev[:], op0=ALU.mult, op1=ALU.mult), [kk, e_prev], [ob.k(0)])
            f.op("gpsimd", lambda e: e.tensor_tensor(out=rt[:], in0=r_s[:], in1=e_pos[:], op=ALU.mult), [r_s, e_pos], [rt])
            f.op("gpsimd", lambda e: e.tensor_tensor(out=btf[:], in0=bb[:], in1=e_neg[:], op=ALU.mult), [bb, e_neg], [btf])
            f.op("gpsimd", lambda e: e.tensor_tensor(out=ktf[:], in0=kmod[:], in1=e_neg[:], op=ALU.mult), [kmod, e_neg], [ktf])
            f.op("scalar", lambda e: e.activation(out=ob[:, 1, :], in_=btf[:], func=AF.Copy), [btf], [ob.k(1)])
            f.op("scalar", lambda e: e.activation(out=ob[:, 2, :], in_=ktf[:], func=AF.Copy), [ktf], [ob.k(2)])
            for q in range(8):
                qs = slice(q * 64, (q + 1) * 64)
                f.op("vector", lambda e, q=q, qs=qs: e.tensor_scalar(out=ob[:, 3, qs], in0=btf[:, qs], scalar1=wc[:, q:q + 1], scalar2=None, op0=ALU.mult), [btf, wc], [ob.k(3)])
                f.op("gpsimd", lambda e, q=q, qs=qs: e.tensor_scalar(out=ob[:, 4, qs], in0=ktf[:, qs], scalar1=wc[:, q:q + 1], scalar2=None, op0=ALU.mult), [ktf, wc], [ob.k(4)])
            f.op("vector", lambda e: e.scalar_tensor_tensor(out=rk[:], in0=r_s[:], scalar=pcol("r_k"), in1=kmod[:], op0=ALU.mult, op1=ALU.mult), [r_s, pv, kmod], [rk])
            f.op("tensor", lambda e: e.matmul(px2[:], lhsT=bones[:], rhs=rk[:], start=True, stop=True), [bones, rk], [px2])
            f.op("vector", lambda e: e.tensor_tensor(out=ob[:, 7, :], in0=px2[:], in1=v_s[:], op=ALU.mult), [px2, v_s], [ob.k(7)])
            f.op("scalar", lambda e: e.activation(out=ob[:, 5, :], in_=v_s[:], func=AF.Copy), [v_s], [ob.k(5)])
            f.dma(o_bf[:, es, 512 * s:512 * s + 512].rearrange("q p t -> p q t"), ob[:], o_bf, ob)
            f.dma(o_rt[es, 512 * s:512 * s + 512], rt[:], o_rt, rt)
            f.dma(o_wc[es, 8 * s:8 * s + 8], wc[:], o_wc, wc)
    if DEBUG:
        o_dbg = f.dram("o_dbg", [128, 8, 2049], BF16, "ExternalOutput")
        f.dma(o_dbg[:], uT[:], o_dbg, uT)
        o_dbg2 = f.dram("o_dbg2", [128, 8, 1024], BF16, "ExternalOutput")
        f.dma(o_dbg2[:], wr[:, 2, :, :], o_dbg2, wr)
        f.final_wait([o_dbg, o_dbg2])
    f.final_wait([o_bf, o_rt, o_wc])
    return f.build()


def consts_A():
    c = np.zeros((128, 256), np.float32)
    c[:, 0:128] = np.eye(128)
    blk = np.arange(128) // 64
    c[:, 128:256] = (blk[:, None] == blk[None, :]).astype(np.float32)
    return c


def fmcols(v):
    return np.ascontiguousarray(np.asarray(v, np.float32).reshape(8, 128).T)


def inputs_A(inp):
    x = inp["x"][0]
    pvv = np.zeros((128, NPA), np.float32)
    pvv[:, 0:8] = fmcols(inp["norm_g"][0, 0])
    for n in range(6):
        pvv[:, 8 + 8 * n:16 + 8 * n] = fmcols(inp["rwkv_mu"][0, n])
    pvv[:, 56:64] = fmcols(inp["rwkv_w0"][0]); pvv[:, 64:72] = fmcols(inp["rwkv_a0"][0])
    pvv[:, 72:80] = fmcols(inp["rwkv_k_k"][0]); pvv[:, 80:88] = fmcols(inp["rwkv_k_a"][0])
    pvv[:, 88:96] = fmcols(inp["rwkv_r_k"][0].reshape(-1))
    cst = consts_A()
    xpad = np.concatenate([np.zeros((128, 1024), np.float32), x], 0)
    maps = []
    for c in range(8):
        maps.append({"xa": np.ascontiguousarray(xpad[2048 * c:2048 * c + 2048 + 128]), "pv": pvv, "cst": cst,
                     "w_rkv": inp["rwkv_w_rkv"][0], "w1": inp["rwkv_w1"][0], "a1": inp["rwkv_a1"][0], "g1": inp["rwkv_g1"][0],
                     "w2": inp["rwkv_w2"][0], "a2": inp["rwkv_a2"][0], "g2": inp["rwkv_g2"][0]})
    return maps


GC = 8
NCH = 256
NG = NCH // GC
DEBUG = False


def build_B(nch=NCH):
    ng = nch // GC
    nc = bass.Bass("TRN2", target_bir_lowering=False)
    f = FW(nc)
    d_bk = f.dram("d_bk", [ng, 64, GC * 2 * 2 * 64], BF16, "ExternalInput")
    d_at = f.dram("d_at", [ng, 64, GC * 2 * 64], BF16, "ExternalInput")
    d_rf = f.dram("d_rf", [ng, 64, GC * 2 * 64], F32, "ExternalInput")
    d_tm = f.dram("d_tm", [ng, 64, GC * 2 * 4 * 64], BF16, "ExternalInput")
    d_wc = f.dram("d_wc", [64, nch * 2], F32, "ExternalInput")
    d_mask = f.dram("d_mask", [64, 2 * 320], F32, "ExternalInput")
    d_id = f.dram("d_id", [64, 128], F32, "ExternalInput")
    o_y = f.dram("o_y", [ng, 64, GC * 2 * 64], F32, "ExternalOutput")

    wcs = f.sb([64, nch, 2], F32, "wcs")
    f.dma(wcs[:].rearrange("p c h -> p (c h)"), d_wc[:], wcs, d_wc)
    mask = f.sb([64, 2, 320], F32, "mask")
    f.dma(mask[:].rearrange("p h c -> p (h c)"), d_mask[:], mask, d_mask)
    idf2 = f.sb([64, 2, 64], F32, "idf2")
    f.dma(idf2[:].rearrange("p h c -> p (h c)"), d_id[:], idf2, d_id)
    idb2 = f.sb([64, 2, 64], BF16, "idb2")
    f.op("vector", lambda e: e.tensor_copy(out=idb2[:], in_=idf2[:]), [idf2], [idb2])
    identb = idb2[:, 0, :]
    identf = idf2[:, 0, :]

    NB = 2
    BK = [f.sb([64, GC, 2, 2, 64], BF16, f"BK{i}") for i in range(NB)]
    AR = [f.sb([64, GC, 2, 128], BF16, f"AR{i}") for i in range(NB)]
    RF = [f.sb([64, GC, 2, 64], F32, f"RF{i}") for i in range(NB)]
    TM = [f.sb([64, GC, 2, 4, 64], BF16, f"TM{i}") for i in range(NB)]
    YB = [f.sb([64, GC, 2, 64], F32, f"YB{i}") for i in range(NB)]

    def load_group(g):
        b = g % NB
        f.dma(BK[b][:].rearrange("p c h q t -> p (c h q t)"), d_bk[g], BK[b], d_bk)
        f.dma(AR[b][:, :, :, 0:64], d_at[g].rearrange("p (c h t) -> p c h t", c=GC, h=2), AR[b].k("a"), d_at)
        f.dma(AR[b][:, :, :, 64:128], d_rf[g].rearrange("p (c h t) -> p c h t", c=GC, h=2), AR[b].k("r"), d_rf, q="gpsimd")
        f.dma(RF[b][:].rearrange("p c h t -> p (c h t)"), d_rf[g], RF[b], d_rf)
        f.dma(TM[b][:].rearrange("p c h q t -> p (c h q t)"), d_tm[g], TM[b], d_tm)

    class _V:
        def __init__(self, buf, ap):
            self.buf, self.ap = buf, ap

        def __getitem__(self, idx):
            return self.ap[idx]

        def k(self, key):
            return self.buf

    def bank(name):
        return f.ps([64, 512], F32, name)
    PM, PL, PXG = [], [], []
    for x in range(2):
        bm, bl, bx = bank(f"PM{x}"), bank(f"PL{x}"), bank(f"PXG{x}")
        PM.append(_V(bm, bm[:, 0:512].rearrange("p (h c) -> p h c", h=2)))
        PL.append(_V(bl, bl[:, 0:384].rearrange("p (h c) -> p h c", h=2)))
        PXG.append(_V(bx, bx[:, 0:512].rearrange("p (h c) -> p h c", h=2)))
    bus, by = bank("PUS"), bank("PYb")
    PU_ap = bus[:, 0:128].rearrange("p (h c) -> p h c", h=2)
    PS_ap = bus[:, 128:256].rearrange("p (h c) -> p h c", h=2)
    PY_ap = by[:, 0:128].rearrange("p (h c) -> p h c", h=2)
    PUk, PSk, PYk = bus, bus, by
    NP = 4
    Ms = [f.sb([64, 2, 320], BF16, f"Ms{i}") for i in range(NP)]
    XT0 = [f.sb([64, 2, 64], BF16, f"XT0{i}") for i in range(NP)]
    LV = [[f.sb([64, 2, 192], BF16, f"LV{i}_{k}") for k in range(7)] for i in range(NP)]
    GX = [f.sb([64, 2, 128], F32, f"GX{i}") for i in range(NP)]
    UT = [f.sb([64, 2, 64], BF16, f"UT{i}") for i in range(2)]
    S = [f.sb([64, 2, 64], F32, f"S{i}") for i in range(2)]
    f.op("vector", lambda e: e.memset(S[0][:], 0.0), [], [S[0]])

    def pre_stages(c):
        p = c % NP
        x = c % 2
        PMx, PLx, PXGx = PM[x], PL[x], PXG[x]
        g, cg = divmod(c, GC)
        b = g % NB
        bk, ar, tm = BK[b], AR[b], TM[b]
        st = []

        def p1():
            for h in range(2):
                f.op("tensor", lambda e: e.matmul(PMx[:, h, 0:128], lhsT=bk[:, cg, h, 0, :], rhs=ar[:, cg, h, :], start=True, stop=True), [bk, ar], [PMx.k(h)])
                f.op("tensor", lambda e: e.matmul(PMx[:, h, 128:256], lhsT=bk[:, cg, h, 1, :], rhs=ar[:, cg, h, :], start=True, stop=True), [bk, ar], [PMx.k(h)])
                f.op("tensor", lambda e: e.matmul(PXGx[:, h, 192:256], lhsT=ar[:, cg, h, 0:64], rhs=bk[:, cg, h, 0, :], start=True, stop=True), [bk, ar], [PXGx.k("l")])
        st.append(p1)

        def p2():
            f.op("vector", lambda e: e.tensor_tensor(out=Ms[p][:, :, 0:256], in0=PMx[:, :, 0:256], in1=mask[:, :, 0:256], op=ALU.mult), [PMx.buf, mask], [Ms[p].k("m")])
            f.op("vector", lambda e: e.tensor_tensor(out=Ms[p][:, :, 256:320], in0=PXGx[:, :, 192:256], in1=mask[:, :, 256:320], op=ALU.mult), [PXGx.k("l"), mask], [Ms[p].k("l")])
            f.op("gpsimd", lambda e: e.tensor_tensor(out=LV[p][1][:, :, 128:192], in0=Ms[p][:, :, 0:64], in1=idb2[:], op=ALU.add), [Ms[p].k("m"), idb2], [LV[p][1].k("T")])
        st.append(p2)

        def p3():
            for h in range(2):
                f.op("tensor", lambda e: e.matmul(PXGx[:, h, 0:64], lhsT=Ms[p][:, h, 128:192], rhs=tm[:, cg, h, 3, :], start=True, stop=True), [Ms[p].k("m"), tm], [PXGx.k("x")])
        st.append(p3)

        def p4():
            f.op("scalar", lambda e: e.activation(out=XT0[p][:], in_=PXGx[:, :, 0:64], func=AF.Copy), [PXGx.k("x")], [XT0[p]])
        st.append(p4)

        def level(k):
            def mm():
                for h in range(2):
                    if k == 1:
                        Np, Lp = Ms[p][:, h, 0:64], Ms[p][:, h, 256:320]
                        rd = [Ms[p].k("m"), Ms[p].k("l")]
                    else:
                        Np, Lp = LV[p][k - 1][:, h, 0:64], LV[p][k - 1][:, h, 64:128]
                        rd = [LV[p][k - 1].k("NL")]
                    if k <= 5:
                        f.op("tensor", lambda e: e.matmul(PLx[:, h, 0:64], lhsT=Lp, rhs=Np, start=True, stop=True), rd, [PLx.k(h)])
                        f.op("tensor", lambda e: e.matmul(PLx[:, h, 64:128], lhsT=Np, rhs=Lp, start=True, stop=True), rd, [PLx.k(h)])
                    if k >= 2:
                        Tp = LV[p][k - 1][:, h, 128:192]
                        rdt = rd + [LV[p][k - 1].k("T"), idb2]
                        f.op("tensor", lambda e: e.matmul(PLx[:, h, 128:192], lhsT=identb, rhs=Tp, start=True, stop=False), rdt, [PLx.k(h)])
                        f.op("tensor", lambda e: e.matmul(PLx[:, h, 128:192], lhsT=Lp, rhs=Tp, start=False, stop=True), rdt, [PLx.k(h)])

            def ev():
                eng = "scalar" if k % 2 == 0 else "vector"
                if k == 1:
                    sl, wk = slice(0, 128), [LV[p][k].k("NL")]
                elif k <= 5:
                    sl, wk = slice(0, 192), [LV[p][k].k("NL"), LV[p][k].k("T")]
                else:
                    sl, wk = slice(128, 192), [LV[p][k].k("T")]
                if eng == "scalar":
                    f.op("scalar", lambda e: e.activation(out=LV[p][k][:, :, sl], in_=PLx[:, :, sl], func=AF.Copy), [PLx.buf], wk)
                else:
                    f.op("vector", lambda e: e.tensor_copy(out=LV[p][k][:, :, sl], in_=PLx[:, :, sl]), [PLx.buf], wk)
            return [mm, ev]
        for k in range(1, 7):
            st.extend(level(k))

        def pf():
            for h in range(2):
                Tf = LV[p][6][:, h, 128:192]
                f.op("tensor", lambda e: e.matmul(PXGx[:, h, 64:128], lhsT=tm[:, cg, h, 0, :], rhs=Tf, start=True, stop=True), [tm, LV[p][6].k("T")], [PXGx.k("g")])
                f.op("tensor", lambda e: e.matmul(PXGx[:, h, 128:192], lhsT=Tf, rhs=XT0[p][:, h, :], start=True, stop=True), [XT0[p], LV[p][6].k("T")], [PXGx.k("g")])
        st.append(pf)

        def pe():
            f.op("scalar", lambda e: e.activation(out=GX[p][:], in_=PXGx[:, :, 64:192], func=AF.Copy), [PXGx.k("g")], [GX[p]])
        st.append(pe)
        return st

    def seq_stages(c):
        p = c % NP
        q2 = c % 2
        g, cg = divmod(c, GC)
        b = g % NB
        bk, ar, tm, rf, yb = BK[b], AR[b], TM[b], RF[b], YB[b]
        S0, S1 = S[q2], S[1 - q2]
        UTq = UT[q2]
        PU, PS, PY = PU_ap, PS_ap, PY_ap
        st = []

        def s1():
            for h in range(2):
                f.op("tensor", lambda e: e.matmul(PU[:, h, :], lhsT=GX[p][:, h, 0:64], rhs=S0[:, h, :], start=True, stop=False), [GX[p], S0], [PUk])
                f.op("tensor", lambda e: e.matmul(PU[:, h, :], lhsT=identf, rhs=GX[p][:, h, 64:128], start=False, stop=True), [GX[p], idf2], [PUk])
        st.append(s1)

        def s2():
            f.op("scalar", lambda e: e.activation(out=UTq[:], in_=PU, func=AF.Copy), [PUk], [UTq])
        st.append(s2)

        def s3():
            for h in range(2):
                f.op("tensor", lambda e: e.matmul(PS[:, h, :], lhsT=tm[:, cg, h, 1, :], rhs=UTq[:, h, :], start=True, stop=False), [tm, UTq], [PSk])
                f.op("tensor", lambda e: e.matmul(PS[:, h, :], lhsT=tm[:, cg, h, 2, :], rhs=tm[:, cg, h, 3, :], start=False, stop=True), [tm], [PSk])
            for h in range(2):
                f.op("tensor", lambda e: e.matmul(PY[:, h, :], lhsT=rf[:, cg, h, :], rhs=S0[:, h, :], start=True, stop=False), [rf, S0], [PYk])
                f.op("tensor", lambda e: e.matmul(PY[:, h, :], lhsT=Ms[p][:, h, 192:256], rhs=tm[:, cg, h, 3, :], start=False, stop=False), [Ms[p].k("m"), tm], [PYk])
                f.op("tensor", lambda e: e.matmul(PY[:, h, :], lhsT=Ms[p][:, h, 64:128], rhs=UTq[:, h, :], start=False, stop=True), [Ms[p].k("m"), UTq], [PYk])
        st.append(s3)

        def s4():
            for h in range(2):
                f.op("vector", lambda e: e.scalar_tensor_tensor(out=S1[:, h, :], in0=S0[:, h, :], scalar=wcs[:, c, h:h + 1], in1=PS[:, h, :], op0=ALU.mult, op1=ALU.add), [S0, wcs, PSk], [S1])
            f.op("scalar", lambda e: e.activation(out=yb[:, cg, :, :], in_=PY, func=AF.Copy), [PYk], [yb.k(cg)])
            if cg == GC - 1 or DEBUG:
                f.dma(o_y[g], yb[:].rearrange("p c h i -> p (c h i)"), o_y, yb)
        st.append(s4)
        return st

    load_group(0)
    if ng > 1:
        load_group(1)

    def lockstep(a, b):
        out = []
        for i in range(max(len(a), len(b))):
            if i < len(a):
                out.append(a[i])
            if i < len(b):
                out.append(b[i])
        return out

    for s_ in lockstep(pre_stages(0), pre_stages(1) if nch > 1 else []):
        s_()
    for c in range(0, nch, 2):
        g, cg = divmod(c, GC)
        if DEBUG and c >= 1:
            break
        a = []
        if not DEBUG:
            a = lockstep(pre_stages(c + 2) if c + 2 < nch else [], pre_stages(c + 3) if c + 3 < nch else [])
        bq = seq_stages(c) + (seq_stages(c + 1) if c + 1 < nch and not DEBUG else [])
        n = max(len(a), len(bq))
        ia = ib = 0
        for i in range(n):
            want_a = (i + 1) * len(a) // n
            while ia < want_a:
                a[ia](); ia += 1
            want_b = (i + 1) * len(bq) // n
            while ib < want_b:
                bq[ib](); ib += 1
        if (c + 1) % GC == GC - 1 and g + 2 < ng:
            load_group(g + 2)
    if DEBUG:
        dbg = f.dram("o_dbg", [64, 2 * (320 + 192 * 7 + 128 + 64)], F32, "ExternalOutput")
        db = f.sb([64, 2, 320 + 192 * 7 + 128 + 64], F32, "dbgs")
        f.op("vector", lambda e: e.tensor_copy(out=db[:, :, 0:320], in_=Ms[0][:]), [Ms[0]], [db])
        for k in range(7):
            f.op("vector", lambda e: e.tensor_copy(out=db[:, :, 320 + 192 * k:320 + 192 * (k + 1)], in_=LV[0][k][:]), [LV[0][k]], [db])
        o = 320 + 192 * 7
        f.op("vector", lambda e: e.tensor_copy(out=db[:, :, o:o + 128], in_=GX[0][:]), [GX[0]], [db])
        f.op("vector", lambda e: e.tensor_copy(out=db[:, :, o + 128:o + 192], in_=UT[0][:]), [UT[0]], [db])
        f.dma(dbg[:], db[:].rearrange("p h c -> p (h c)"), dbg, db)
        f.final_wait([dbg])
    f.final_wait([o_y])
    return f.build()


def consts_B():
    m = np.zeros((64, 2, 320), np.float32)
    s = np.arange(64)[:, None]; t = np.arange(64)[None, :]
    su = (s < t).astype(np.float32); iu = (s <= t).astype(np.float32)
    for h in range(2):
        m[:, h, 0:64] = su; m[:, h, 64:128] = iu; m[:, h, 128:192] = su; m[:, h, 192:256] = iu
        m[:, h, 256:320] = (t < s).astype(np.float32)
    idm = np.concatenate([np.eye(64, dtype=np.float32)] * 2, axis=1)
    return m.reshape(64, 640), idm


def inputs_B(obf, ort, owc, nch=NCH):
    ng = nch // GC
    T = nch * 64
    mask, idm = consts_B()
    maps = []
    for c in range(8):
        cs = slice(128 * c, 128 * c + 128)
        fm = lambda a: a[cs, :T].reshape(2, 64, ng, GC, 64)
        At, Bt, Kt, Bh, Kh, V = [fm(obf[q]) for q in range(6)]
        Rt = fm(ort)
        d_bk = np.stack([Bt, Kt], axis=0).transpose(3, 2, 4, 1, 0, 5)
        d_at = At.transpose(2, 1, 3, 0, 4)
        d_rf = Rt.transpose(2, 1, 3, 0, 4)
        tmq = np.stack([At, Bh, Kh, V], axis=0)
        d_tm = tmq.transpose(3, 5, 4, 1, 0, 2)
        d_wc = owc[cs, :nch].reshape(2, 64, nch).transpose(1, 2, 0)
        maps.append({"d_bk": np.ascontiguousarray(d_bk).reshape(ng, 64, -1), "d_at": np.ascontiguousarray(d_at).reshape(ng, 64, -1),
                     "d_rf": np.ascontiguousarray(d_rf).reshape(ng, 64, -1), "d_tm": np.ascontiguousarray(d_tm).reshape(ng, 64, -1),
                     "d_wc": np.ascontiguousarray(d_wc).reshape(64, -1), "d_mask": mask, "d_id": idm})
    return maps


def gather_B(results, nch=NCH):
    ng = nch // GC
    ys = []
    for r in results:
        y = np.asarray(r["o_y"]).reshape(ng, 64, GC, 2, 64)
        ys.append(y.transpose(0, 2, 1, 3, 4).reshape(nch * 64, 128))
    return np.concatenate(ys, axis=1)


RMS_EPS = 1e-6
GN_EPS = 64e-5
ST = 512


class Ctx:
    pass


def setup_common(f, c, cst_dram):
    cs = f.sb([128, 384], F32, "cs")
    f.dma(cs[:], cst_dram[:], cs, cst_dram)
    c.identf = cs
    c.bones = f.sb([128, 128], BF16, "bones")
    c.ones = f.sb([128, 128], BF16, "onesb")
    f.op("vector", lambda e: e.tensor_copy(out=c.bones[:], in_=cs[:, 128:256]), [cs], [c.bones])
    f.op("vector", lambda e: e.tensor_copy(out=c.ones[:], in_=cs[:, 256:384]), [cs], [c.ones])
    c.cs = cs
    c.PA = [f.ps([128, 512], F32, f"PA{i}") for i in range(4)]
    c.PB = [f.ps([128, 512], F32, f"PB{i}") for i in range(2)]
    c.PC = [f.ps([128, 512], F32, f"PC{i}") for i in range(2)]
    c.ia = c.ib = c.ic = 0
    c.tmpi = 0
    c.tmps = {}


def nxt(c, pool):
    if pool == "A":
        c.ia += 1; return c.PA[c.ia % 4]
    if pool == "B":
        c.ib += 1; return c.PB[c.ib % 2]
    c.ic += 1; return c.PC[c.ic % 2]


def tmp(f, c, name, dt=F32, n=2, shape=(128, 512)):
    key = (name, dt)
    if key not in c.tmps:
        c.tmps[key] = [[f.sb(list(shape), dt, f"t_{name}_{i}") for i in range(n)], 0]
    lst = c.tmps[key]
    lst[1] += 1
    return lst[0][lst[1] % n]


def rmsnorm_fm(f, c, hT, pv, gcol0, uT, uF=None, eps=RMS_EPS):
    P = nxt(c, "C")
    for k in range(8):
        sq = tmp(f, c, "sq", BF16, 3)
        f.op("scalar", lambda e: e.activation(out=sq[:], in_=hT[:, k, :], func=AF.Square), [hT.k(k)], [sq])
        f.op("tensor", lambda e: e.matmul(P[:], lhsT=c.ones[:], rhs=sq[:], start=(k == 0), stop=(k == 7)), [c.ones, sq], [P])
    rs = tmp(f, c, "rs")
    f.op("scalar", lambda e: e.activation(out=rs[:], in_=P[:], func=AF.Sqrt, scale=1.0 / 1024, bias=eps), [P], [rs])
    f.op("vector", lambda e: e.reciprocal(out=rs[:], in_=rs[:]), [rs], [rs])
    for k in range(8):
        f.op("vector", lambda e: e.scalar_tensor_tensor(out=uT[:, k, :], in0=hT[:, k, :], scalar=pv[:, gcol0 + k:gcol0 + k + 1], in1=rs[:], op0=ALU.mult, op1=ALU.mult), [hT.k(k), pv, rs], [uT.k(k)])
        if uF is not None:
            f.op("gpsimd", lambda e: e.tensor_scalar(out=uF[:, k, :], in0=hT[:, k, :], scalar1=pv[:, gcol0 + k:gcol0 + k + 1], scalar2=None, op0=ALU.mult), [hT.k(k), pv], [uF.k(k)])
            f.op("gpsimd", lambda e: e.tensor_tensor(out=uF[:, k, :], in0=uF[:, k, :], in1=rs[:], op=ALU.mult), [uF.k(k), rs], [uF.k(k)])


def linear_res(f, c, w_sb, zT, hT, nk=8):
    for ec in range(8):
        P = nxt(c, "B")
        for k in range(nk):
            f.op("tensor", lambda e: e.matmul(P[:], lhsT=w_sb[:, k, ec * 128:(ec + 1) * 128], rhs=zT[:, k, :], start=(k == 0), stop=(k == nk - 1)), [w_sb, zT.k(k)], [P])
        f.op("vector", lambda e: e.tensor_tensor(out=hT[:, ec, :], in0=hT[:, ec, :], in1=P[:], op=ALU.add), [hT.k(ec), P], [hT.k(ec)])


def ffn_pass(f, c, uT, aT, wgu_dram, wd_dram, wgu_bufs, wd_bufs, out_fn, gate_b=None, FG=2, EG=2):
    NF = 28
    for g in range(NF // FG):
        c.wgi = getattr(c, "wgi", 0) + 1
        W = wgu_bufs[c.wgi % len(wgu_bufs)]
        for half in range(2):
            col0 = half * 3584 + g * FG * 128
            f.dma(W[:, :, half, :], wgu_dram[:, col0:col0 + FG * 128].rearrange("(k p) e -> p k e", p=128), W.k(half), wgu_dram_buf(c, wgu_dram), q="gpsimd")
        for j in range(FG):
            fc = g * FG + j
            Pg = nxt(c, "A"); Pu = nxt(c, "A")
            for k in range(8):
                f.op("tensor", lambda e: e.matmul(Pg[:], lhsT=W[:, k, 0, j * 128:(j + 1) * 128], rhs=uT[:, k, :], start=(k == 0), stop=(k == 7)), [W.k(0), uT.k(k)], [Pg])
            for k in range(8):
                f.op("tensor", lambda e: e.matmul(Pu[:], lhsT=W[:, k, 1, j * 128:(j + 1) * 128], rhs=uT[:, k, :], start=(k == 0), stop=(k == 7)), [W.k(1), uT.k(k)], [Pu])
            sg = tmp(f, c, "sg", F32, 3)
            f.op("scalar", lambda e: e.activation(out=sg[:], in_=Pg[:], func=AF.Silu), [Pg], [sg])
            if gate_b is None:
                f.op("vector", lambda e: e.tensor_tensor(out=aT[:, fc, :], in0=sg[:], in1=Pu[:], op=ALU.mult), [sg, Pu], [aT.k(fc)])
            else:
                sg2 = tmp(f, c, "sg2", F32, 3)
                f.op("vector", lambda e: e.tensor_tensor(out=sg2[:], in0=sg[:], in1=Pu[:], op=ALU.mult), [sg, Pu], [sg2])
                f.op("gpsimd", lambda e: e.tensor_tensor(out=aT[:, fc, :], in0=sg2[:], in1=gate_b[:], op=ALU.mult), [sg2, gate_b], [aT.k(fc)])
    for g in range(8 // EG):
        c.wdi = getattr(c, "wdi", 0) + 1
        W = wd_bufs[c.wdi % len(wd_bufs)]
        for q4 in range(4):
            f.dma(W[:, q4 * 7:(q4 + 1) * 7, :], wd_dram[q4 * 896:(q4 + 1) * 896, g * EG * 128:(g + 1) * EG * 128].rearrange("(k p) e -> p k e", p=128), W.k(q4), wgu_dram_buf(c, wd_dram), q="gpsimd")
        for j in range(EG):
            ec = g * EG + j
            P = nxt(c, "B")
            for fc in range(NF):
                f.op("tensor", lambda e: e.matmul(P[:], lhsT=W[:, fc, j * 128:(j + 1) * 128], rhs=aT[:, fc, :], start=(fc == 0), stop=(fc == NF - 1)), [W.k(fc // 7), aT.k(fc)], [P])
            out_fn(ec, P)


_dram_bufs = {}


def wgu_dram_buf(c, ap):
    return c.wdram


def consts_CE():
    cst = np.zeros((128, 384), np.float32)
    cst[:, 0:128] = np.eye(128)
    blk = np.arange(128) // 64
    cst[:, 128:256] = (blk[:, None] == blk[None, :]).astype(np.float32)
    cst[:, 256:384] = 1.0
    return cst


def fmcols(v):
    return np.ascontiguousarray(np.asarray(v, np.float32).reshape(-1, 128).T)


def ffn_ws(f, c, uTs, hTs, w_gu_ap, w_dn_ap, wgu_bufs, wd_bufs, aT_bufs, gbs=None, FG=4):
    NTT = len(uTs)
    for g in range(28 // FG):
        c.wgi = getattr(c, "wgi", 0) + 1
        W = wgu_bufs[c.wgi % 2]; WD = wd_bufs[c.wgi % 2]
        for hf in range(2):
            col0 = hf * 3584 + g * FG * 128
            f.dma(W[:, :, hf, :], w_gu_ap[:, col0:col0 + FG * 128].rearrange("(k p) e -> p k e", p=128), W.k(hf), c.wdram, q="gpsimd")
        f.dma(WD[:], w_dn_ap[g * FG * 128:(g + 1) * FG * 128, :].rearrange("(k p) e -> p k e", p=128), WD, c.wdram, q="gpsimd")
        for tt in range(NTT):
            c.ai = getattr(c, "ai", 0) + 1
            A = aT_bufs[c.ai % 2]
            for j in range(FG):
                Pg = nxt(c, "A"); Pu = nxt(c, "A")
                for k in range(8):
                    f.op("tensor", lambda e: e.matmul(Pg[:], lhsT=W[:, k, 0, j * 128:(j + 1) * 128], rhs=uTs[tt][:, k, :], start=(k == 0), stop=(k == 7)), [W.k(0), uTs[tt].k(k)], [Pg])
                for k in range(8):
                    f.op("tensor", lambda e: e.matmul(Pu[:], lhsT=W[:, k, 1, j * 128:(j + 1) * 128], rhs=uTs[tt][:, k, :], start=(k == 0), stop=(k == 7)), [W.k(1), uTs[tt].k(k)], [Pu])
                sg = tmp(f, c, "sg", F32, 3)
                f.op("scalar", lambda e: e.activation(out=sg[:], in_=Pg[:], func=AF.Silu), [Pg], [sg])
                if gbs is None:
                    f.op("vector", lambda e: e.tensor_tensor(out=A[:, j, :], in0=sg[:], in1=Pu[:], op=ALU.mult), [sg, Pu], [A.k(j)])
                else:
                    sg2 = tmp(f, c, "sg2", F32, 3)
                    f.op("vector", lambda e: e.tensor_tensor(out=sg2[:], in0=sg[:], in1=Pu[:], op=ALU.mult), [sg, Pu], [sg2])
                    f.op("vector", lambda e: e.tensor_tensor(out=A[:, j, :], in0=sg2[:], in1=gbs[tt][:], op=ALU.mult), [sg2, gbs[tt]], [A.k(j)])
            for ec in range(8):
                P = nxt(c, "B")
                for j in range(FG):
                    f.op("tensor", lambda e: e.matmul(P[:], lhsT=WD[:, j, ec * 128:(ec + 1) * 128], rhs=A[:, j, :], start=(j == 0), stop=(j == FG - 1)), [WD, A.k(j)], [P])
                f.op("vector", lambda e: e.tensor_tensor(out=hTs[tt][:, ec, :], in0=hTs[tt][:, ec, :], in1=P[:], op=ALU.add), [hTs[tt].k(ec), P], [hTs[tt].k(ec)])


PVC = {"gn_g": 0, "gn_b": 8, "g1": 16, "g2": 24, "qg": 32, "kg": 33, "bf": 34}
NPC = 35


def build_C():
    nc = bass.Bass("TRN2", target_bir_lowering=False)
    f = FW(nc)
    c = Ctx()
    xT = f.dram("xT", [1024, 2048], F32, "ExternalInput")
    yT = f.dram("yT", [1024, 2048], F32, "ExternalInput")
    gb = f.dram("gb", [2, 1024, 2048], BF16, "ExternalInput")
    pvd = f.dram("pv", [128, NPC], F32, "ExternalInput")
    cst = f.dram("cst", [128, 384], F32, "ExternalInput")
    w_o = f.dram("w_o", [1024, 1024], F32, "ExternalInput")
    w_gu = f.dram("w_gu", [1024, 7168], F32, "ExternalInput")
    w_dn = f.dram("w_dn", [3584, 1024], F32, "ExternalInput")
    w_in = f.dram("w_in", [1024, 4112], F32, "ExternalInput")
    o_h = f.dram("o_h", [1024, 2048], F32, "ExternalOutput")
    o_c = f.dram("o_c", [4, 1024, 2048], BF16, "ExternalOutput")
    o_lf = f.dram("o_lf", [16, 2048], F32, "ExternalOutput")
    c.wdram = f.dram("wdummy", [1, 1], F32, "Internal")
    setup_common(f, c, cst)
    pv = f.sb([128, NPC], F32, "pvs")
    f.dma(pv[:], pvd[:], pv, pvd)
    wo = f.sb([128, 8, 1024], BF16, "wo")
    for k4 in range(0, 8, 4):
        f.dma(wo[:, k4:k4 + 4, :], w_o[k4 * 128:(k4 + 4) * 128, :].rearrange("(k p) e -> p k e", p=128), wo.k(k4), w_o, q="gpsimd")
    wfl = f.sb([128, 8, 16], BF16, "wfl")
    f.dma(wfl[:], w_in[:, 4096:4112].rearrange("(k p) e -> p k e", p=128), wfl, w_in, q="gpsimd")
    NTT = 2
    hTs = [f.sb([128, 8, 512], F32, f"hT{i}") for i in range(NTT)]
    uTs = [f.sb([128, 8, 512], BF16, f"uT{i}") for i in range(NTT)]
    zT = f.sb([128, 8, 512], BF16, "zT")
    aT = [f.sb([128, 4, 512], BF16, f"aTg{i}") for i in range(2)]
    wgu_bufs = [f.sb([128, 8, 2, 512], BF16, f"wgu{i}") for i in range(2)]
    wd_bufs = [f.sb([128, 4, 1024], BF16, f"wd{i}") for i in range(2)]
    oc = [f.sb([128, 512], BF16, f"oc{i}") for i in range(3)]
    oci = [0]
    lf = f.sb([16, 512], F32, "lf")
    for half in range(2):
        for tt in range(NTT):
            hT = hTs[tt]
            ts = slice(1024 * half + 512 * tt, 1024 * half + 512 * tt + 512)
            f.dma(hT[:], xT[:, ts].rearrange("(k p) t -> p k t", p=128), hT, xT)
            for ec in range(8):
                es = slice(ec * 128, (ec + 1) * 128)
                y = tmp(f, c, "y"); g2 = tmp(f, c, "gbt", BF16, 2, (128, 2, 512))
                f.dma(y[:], yT[es, ts], y, yT)
                f.dma(g2[:], gb[:, es, ts].rearrange("q p t -> p q t"), g2, gb)
                yb = tmp(f, c, "yb", BF16)
                f.op("scalar", lambda e: e.activation(out=yb[:], in_=y[:], func=AF.Copy), [y], [yb])
                P = nxt(c, "C")
                f.op("tensor", lambda e: e.matmul(P[:], lhsT=c.bones[:], rhs=yb[:], start=True, stop=True), [c.bones, yb], [P])
                yc = tmp(f, c, "yc")
                f.op("vector", lambda e: e.scalar_tensor_tensor(out=yc[:], in0=P[:], scalar=-1.0 / 64, in1=y[:], op0=ALU.mult, op1=ALU.add), [P, y], [yc])
                sq = tmp(f, c, "sq", BF16, 3)
                f.op("scalar", lambda e: e.activation(out=sq[:], in_=yc[:], func=AF.Square), [yc], [sq])
                P2 = nxt(c, "C")
                f.op("tensor", lambda e: e.matmul(P2[:], lhsT=c.bones[:], rhs=sq[:], start=True, stop=True), [c.bones, sq], [P2])
                rs = tmp(f, c, "rs")
                f.op("scalar", lambda e: e.activation(out=rs[:], in_=P2[:], func=AF.Sqrt, scale=1.0 / 64, bias=GN_EPS), [P2], [rs])
                f.op("vector", lambda e: e.reciprocal(out=rs[:], in_=rs[:]), [rs], [rs])
                f.op("vector", lambda e: e.tensor_tensor(out=yc[:], in0=yc[:], in1=rs[:], op=ALU.mult), [yc, rs], [yc])
                f.op("vector", lambda e: e.tensor_scalar(out=yc[:], in0=yc[:], scalar1=pv[:, PVC["gn_g"] + ec:PVC["gn_g"] + ec + 1], scalar2=pv[:, PVC["gn_b"] + ec:PVC["gn_b"] + ec + 1], op0=ALU.mult, op1=ALU.add), [yc, pv], [yc])
                f.op("vector", lambda e: e.tensor_tensor(out=yc[:], in0=yc[:], in1=g2[:, 1, :], op=ALU.add), [yc, g2], [yc])
                f.op("vector", lambda e: e.tensor_tensor(out=zT[:, ec, :], in0=yc[:], in1=g2[:, 0, :], op=ALU.mult), [yc, g2], [zT.k(ec)])
            linear_res(f, c, wo, zT, hT)
            rmsnorm_fm(f, c, hT, pv, PVC["g1"], uTs[tt])
        ffn_ws(f, c, uTs, hTs, w_gu[:], w_dn[:], wgu_bufs, wd_bufs, aT)
        for tt in range(NTT):
            ts = slice(1024 * half + 512 * tt, 1024 * half + 512 * tt + 512)
            f.dma(o_h[:, ts].rearrange("(k p) t -> p k t", p=128), hTs[tt][:], o_h, hTs[tt])
            rmsnorm_fm(f, c, hTs[tt], pv, PVC["g2"], uTs[tt])
        for g in range(8):
            c.wgi += 1
            W = wgu_bufs[c.wgi % 2]
            Wv = W[:].rearrange("p k h e -> p k (h e)")
            f.dma(Wv[:, :, 0:512], w_in[:, g * 512:(g + 1) * 512].rearrange("(k p) e -> p k e", p=128), W, c.wdram, q="gpsimd")
            for tt in range(NTT):
                ts = slice(1024 * half + 512 * tt, 1024 * half + 512 * tt + 512)
                uT = uTs[tt]
                for j in range(4):
                    e32 = g * 4 + j
                    kind, ec = divmod(e32, 8)
                    P = nxt(c, "A")
                    for k in range(8):
                        f.op("tensor", lambda e: e.matmul(P[:], lhsT=Wv[:, k, j * 128:(j + 1) * 128], rhs=uT[:, k, :], start=(k == 0), stop=(k == 7)), [W, uT.k(k)], [P])
                    oci[0] += 1
                    O = oc[oci[0] % 3]
                    if kind in (0, 1):
                        qs = tmp(f, c, "qs")
                        f.op("scalar", lambda e: e.activation(out=qs[:], in_=P[:], func=AF.Copy), [P], [qs])
                        sq = tmp(f, c, "sq", BF16, 3)
                        f.op("scalar", lambda e: e.activation(out=sq[:], in_=qs[:], func=AF.Square), [qs], [sq])
                        P2 = nxt(c, "C")
                        f.op("tensor", lambda e: e.matmul(P2[:], lhsT=c.bones[:], rhs=sq[:], start=True, stop=True), [c.bones, sq], [P2])
                        rs = tmp(f, c, "rs")
                        if kind == 0:
                            f.op("scalar", lambda e: e.activation(out=rs[:], in_=P2[:], func=AF.Sqrt, scale=1.0, bias=64 * RMS_EPS), [P2], [rs])
                        else:
                            f.op("scalar", lambda e: e.activation(out=rs[:], in_=P2[:], func=AF.Sqrt, scale=1.0 / 64, bias=RMS_EPS), [P2], [rs])
                        f.op("vector", lambda e: e.reciprocal(out=rs[:], in_=rs[:]), [rs], [rs])
                        gc = PVC["qg"] if kind == 0 else PVC["kg"]
                        f.op("vector", lambda e: e.scalar_tensor_tensor(out=O[:], in0=qs[:], scalar=pv[:, gc:gc + 1], in1=rs[:], op0=ALU.mult, op1=ALU.mult), [qs, pv, rs], [O])
                    elif kind == 2:
                        f.op("scalar", lambda e: e.activation(out=O[:], in_=P[:], func=AF.Copy), [P], [O])
                    else:
                        f.op("scalar", lambda e: e.activation(out=O[:], in_=P[:], func=AF.Sigmoid), [P], [O])
                    f.dma(o_c[kind, ec * 128:(ec + 1) * 128, ts], O[:], o_c, O)
        for tt in range(NTT):
            ts = slice(1024 * half + 512 * tt, 1024 * half + 512 * tt + 512)
            uT = uTs[tt]
            P = nxt(c, "C")
            for k in range(8):
                f.op("tensor", lambda e: e.matmul(P[0:16, :], lhsT=wfl[:, k, :], rhs=uT[:, k, :], start=(k == 0), stop=(k == 7)), [wfl, uT.k(k)], [P])
            f.op("vector", lambda e: e.tensor_scalar(out=lf[:], in0=P[0:16, :], scalar1=pv[0:16, PVC["bf"]:PVC["bf"] + 1], scalar2=None, op0=ALU.add), [P, pv], [lf])
            f.op("scalar", lambda e: e.activation(out=lf[:], in_=lf[:], func=AF.Exp, scale=-1.0), [lf], [lf])
            f.op("scalar", lambda e: e.activation(out=lf[:], in_=lf[:], func=AF.Ln, bias=1.0), [lf], [lf])
            f.op("vector", lambda e: e.tensor_scalar(out=lf[:], in0=lf[:], scalar1=-1.0, scalar2=None, op0=ALU.mult), [lf], [lf])
            f.dma(o_lf[:, ts], lf[:], o_lf, lf)
    f.final_wait([o_h, o_c, o_lf])
    return f.build()


def inputs_C(inp, yfull, obfA):
    x = inp["x"][0]
    pvv = np.zeros((128, NPC), np.float32)
    pvv[:, 0:8] = fmcols(inp["rwkv_gn_g"][0]); pvv[:, 8:16] = fmcols(inp["rwkv_gn_b"][0])
    pvv[:, 16:24] = fmcols(inp["norm_g"][0, 1]); pvv[:, 24:32] = fmcols(inp["norm_g"][1, 0])
    pvv[:, 32] = np.tile(inp["fox_q_gain"][0], 2); pvv[:, 33] = np.tile(inp["fox_k_gain"][0], 2)
    pvv[0:16, 34] = inp["fox_b_f"][0]
    cst = consts_CE()
    maps = []
    for c in range(8):
        ts = slice(2048 * c, 2048 * c + 2048)
        maps.append({"xT": np.ascontiguousarray(x[ts].T), "yT": np.ascontiguousarray(yfull[ts].T),
                     "gb": np.ascontiguousarray(obfA[c][6:8]), "pv": pvv, "cst": cst,
                     "w_o": inp["rwkv_w_o"][0], "w_gu": inp["ffn_w_gu"][0], "w_dn": inp["ffn_w_down"][0], "w_in": inp["fox_w_in"][0]})
    return maps


PVE = {"g3": 0, "gf": 8}
NPE = 16
NEXP = 8
NTE = 1024
FGE = 4


def build_E():
    nc = bass.Bass("TRN2", target_bir_lowering=False)
    f = FW(nc)
    c = Ctx()
    hTd = f.dram("hT", [1024, 2048], F32, "ExternalInput")
    oTd = f.dram("oT", [1024, 2048], BF16, "ExternalInput")
    pvd = f.dram("pv", [128, NPE], F32, "ExternalInput")
    cst = f.dram("cst", [128, 384], F32, "ExternalInput")
    seld = f.dram("sele", [8, 8 * 128], F32, "ExternalInput")
    w_o = f.dram("w_o", [1024, 1024], F32, "ExternalInput")
    w_r = f.dram("w_r", [1024, 8], F32, "ExternalInput")
    w_gu = f.dram("w_gu", [8, 1024, 7168], F32, "ExternalInput")
    w_dn = f.dram("w_dn", [8, 3584, 1024], F32, "ExternalInput")
    o_out = f.dram("o_out", [1024, 2048], F32, "ExternalOutput")
    c.wdram = f.dram("wdummy", [1, 1], F32, "Internal")
    setup_common(f, c, cst)
    pv = f.sb([128, NPE], F32, "pvs")
    f.dma(pv[:], pvd[:], pv, pvd)
    sele = f.sb([8, 8, 128], F32, "sele")
    f.dma(sele[:].rearrange("k e m -> k (e m)"), seld[:], sele, seld)
    wo = f.sb([128, 8, 1024], BF16, "wo")
    for k4 in range(0, 8, 4):
        f.dma(wo[:, k4:k4 + 4, :], w_o[k4 * 128:(k4 + 4) * 128, :].rearrange("(k p) e -> p k e", p=128), wo.k(k4), w_o, q="gpsimd")
    wr = f.sb([128, 8, 8], F32, "wr")
    f.dma(wr[:], w_r[:].rearrange("(k p) e -> p k e", p=128), wr, w_r)
    NTT = NTE // 512
    hT = [f.sb([128, 8, 512], F32, f"hT{i}") for i in range(NTT)]
    uT = [f.sb([128, 8, 512], BF16, f"uT{i}") for i in range(NTT)]
    gTs = [f.sb([8, 512], F32, f"gTs{i}") for i in range(NTT)]
    zT = f.sb([128, 8, 512], BF16, "zT")
    uF = f.sb([128, 8, 512], F32, "uF")
    wgu_bufs = [f.sb([128, 8, 2, FGE * 128], BF16, f"wgu{i}") for i in range(2)]
    wd_bufs = [f.sb([128, FGE, 1024], BF16, f"wd{i}") for i in range(2)]
    aT = [f.sb([128, FGE, 512], BF16, f"aTg{i}") for i in range(2)]
    gb = [f.sb([128, 512], F32, f"gb{i}") for i in range(NTT)]
    lg = f.sb([8, 512], F32, "lg")
    lt = f.sb([128, 4, 8], F32, "lt")
    gts = f.sb([128, 4, 8], F32, "gts")
    sm = {n: f.sb([128, 8], F32, "sm_" + n) for n in ("eq", "l2", "sel", "ex")}
    sc = {n: f.sb([128, 4], F32, "sc_" + n) for n in ("m1", "nm1", "m2", "sum")}
    wgi = 0
    ai = 0
    for half in range(2048 // NTE):
        for tt in range(NTT):
            ts = slice(half * NTE + 512 * tt, half * NTE + 512 * tt + 512)
            H, U = hT[tt], uT[tt]
            f.dma(H[:], hTd[:, ts].rearrange("(k p) t -> p k t", p=128), H, hTd)
            f.dma(zT[:], oTd[:, ts].rearrange("(k p) t -> p k t", p=128), zT, oTd)
            linear_res(f, c, wo, zT, H)
            rmsnorm_fm(f, c, H, pv, PVE["g3"], U, uF)
            P = nxt(c, "C")
            for k in range(8):
                f.op("tensor", lambda e: e.matmul(P[0:8, :], lhsT=wr[:, k, :], rhs=uF[:, k, :], start=(k == 0), stop=(k == 7)), [wr, uF.k(k)], [P])
            f.op("vector", lambda e: e.tensor_copy(out=lg[:], in_=P[0:8, :]), [P], [lg])
            P2 = nxt(c, "C")
            for j in range(4):
                f.op("tensor", lambda e: e.transpose(out=P2[:, j * 8:(j + 1) * 8], in_=lg[:, j * 128:(j + 1) * 128], identity=c.cs[0:8, 0:8]), [lg, c.cs], [P2])
            f.op("vector", lambda e: e.tensor_copy(out=lt[:].rearrange("p j e -> p (j e)"), in_=P2[:, 0:32]), [P2], [lt])
            f.op("vector", lambda e: e.tensor_reduce(out=sc["m1"][:], in_=lt[:], axis=AX.X, op=ALU.max), [lt], [sc["m1"]])
            f.op("vector", lambda e: e.tensor_scalar(out=sc["nm1"][:], in0=sc["m1"][:], scalar1=-1.0, scalar2=None, op0=ALU.mult), [sc["m1"]], [sc["nm1"]])
            for j in range(4):
                L = lt[:, j, :]
                f.op("vector", lambda e: e.tensor_scalar(out=sm["eq"][:], in0=L, scalar1=sc["m1"][:, j:j + 1], scalar2=None, op0=ALU.is_ge), [lt, sc["m1"]], [sm["eq"]])
                f.op("vector", lambda e: e.scalar_tensor_tensor(out=sm["l2"][:], in0=sm["eq"][:], scalar=-1e30, in1=L, op0=ALU.mult, op1=ALU.add), [sm["eq"], lt], [sm["l2"]])
                f.op("vector", lambda e: e.tensor_reduce(out=sc["m2"][:, j:j + 1], in_=sm["l2"][:], axis=AX.X, op=ALU.max), [sm["l2"]], [sc["m2"]])
                f.op("vector", lambda e: e.tensor_scalar(out=sm["sel"][:], in0=L, scalar1=sc["m2"][:, j:j + 1], scalar2=None, op0=ALU.is_ge), [lt, sc["m2"]], [sm["sel"]])
                f.op("scalar", lambda e: e.activation(out=sm["ex"][:], in_=L, func=AF.Exp, bias=sc["nm1"][:, j:j + 1]), [lt, sc["nm1"]], [sm["ex"]])
                f.op("vector", lambda e: e.tensor_tensor(out=sm["ex"][:], in0=sm["ex"][:], in1=sm["sel"][:], op=ALU.mult), [sm["ex"], sm["sel"]], [sm["ex"]])
                f.op("vector", lambda e: e.tensor_reduce(out=sc["sum"][:, j:j + 1], in_=sm["ex"][:], axis=AX.X, op=ALU.add), [sm["ex"]], [sc["sum"]])
                f.op("vector", lambda e: e.reciprocal(out=sc["sum"][:, j:j + 1], in_=sc["sum"][:, j:j + 1]), [sc["sum"]], [sc["sum"]])
                f.op("vector", lambda e: e.tensor_scalar(out=gts[:, j, :], in0=sm["ex"][:], scalar1=sc["sum"][:, j:j + 1], scalar2=None, op0=ALU.mult), [sm["ex"], sc["sum"]], [gts])
            P3 = nxt(c, "C")
            for j in range(4):
                f.op("tensor", lambda e: e.transpose(out=P3[0:8, j * 128:(j + 1) * 128], in_=gts[:, j, :], identity=c.cs[:, 0:128]), [gts, c.cs], [P3])
            f.op("vector", lambda e: e.tensor_copy(out=gTs[tt][:], in_=P3[0:8, :]), [P3], [gTs[tt]])
        for ex in range(NEXP):
            for tt in range(NTT):
                P4 = nxt(c, "C")
                f.op("tensor", lambda e: e.matmul(P4[:], lhsT=sele[:, ex, :], rhs=gTs[tt][:], start=True, stop=True), [sele, gTs[tt]], [P4])
                f.op("scalar", lambda e: e.activation(out=gb[tt][:], in_=P4[:], func=AF.Copy), [P4], [gb[tt]])
            ffn_ws(f, c, uT, hT, w_gu[ex], w_dn[ex], wgu_bufs, wd_bufs, aT, gbs=gb, FG=FGE)
        for tt in range(NTT):
            ts = slice(half * NTE + 512 * tt, half * NTE + 512 * tt + 512)
            rmsnorm_fm(f, c, hT[tt], pv, PVE["gf"], uT[tt], uF)
            f.dma(o_out[:, ts].rearrange("(k p) t -> p k t", p=128), uF[:], o_out, uF)
    f.final_wait([o_out])
    return f.build()


def inputs_E(inp, hT_list, oT):
    pvv = np.zeros((128, NPE), np.float32)
    pvv[:, 0:8] = fmcols(inp["norm_g"][1, 1]); pvv[:, 8:16] = fmcols(inp["final_g"])
    cst = consts_CE()
    sele = np.zeros((8, 8, 128), np.float32)
    for e in range(8):
        sele[e, e, :] = 1.0
    maps = []
    for c in range(8):
        maps.append({"hT": hT_list[c], "oT": np.ascontiguousarray(oT[:, 2048 * c:2048 * c + 2048]), "pv": pvv, "cst": cst,
                     "sele": sele.reshape(8, 1024), "w_o": inp["fox_w_o"][0], "w_r": inp["moe_w_router"][0],
                     "w_gu": inp["moe_w_gu"][0], "w_dn": inp["moe_w_down"][0]})
    return maps


T_ALL = 16384


def build_D(T=T_ALL):
    nc = bass.Bass("TRN2", target_bir_lowering=False)
    f = FW(nc)
    NQ = T // 512
    NKB = T // 128
    NSEG = T // 2048
    qT = f.dram("qT", [2, 64, T], BF16, "ExternalInput")
    kT = f.dram("kT", [2, 64, T], BF16, "ExternalInput")
    vt = f.dram("vt", [2, 128, NKB * 65], BF16, "ExternalInput")
    og = f.dram("og", [2, 64, T], BF16, "ExternalInput")
    lfd = f.dram("lf", [2, T], F32, "ExternalInput")
    mkd = f.dram("mk", [128, 4 * 512], F32, "ExternalInput")
    sld = f.dram("sel", [65, 64], F32, "ExternalInput")
    o_o = f.dram("o_o", [2, 64, T], BF16, "ExternalOutput")

    mkf = f.sb([128, 4, 512], F32, "mkf")
    f.dma(mkf[:].rearrange("p m t -> p (m t)"), mkd[:], mkf, mkd)
    mk = f.sb([128, 4, 512], BF16, "mk")
    f.op("vector", lambda e: e.tensor_copy(out=mk[:], in_=mkf[:]), [mkf], [mk])
    sel = f.sb([65, 64], F32, "sels")
    f.dma(sel[:], sld[:], sel, sld)
    ones = f.sb([1, 2048], F32, "ones1")
    f.op("gpsimd", lambda e: e.memset(ones[:], 1.0), [], [ones])

    Qa = f.sb([70, T], BF16, "Qa")
    Ka = f.sb([70, T], BF16, "Ka")
    Vt = f.sb([128, NKB, 65], BF16, "Vt")
    lfs = [f.sb([1, 2048], F32, f"lfs{i}") for i in range(2)]
    cseg = [f.sb([1, 2048], F32, f"cseg{i}") for i in range(2)]
    c_d = f.dram("c_scr", [2, T], F32, "Internal")
    p_d = f.dram("p_scr", [2, 3, T], BF16, "Internal")
    c2d = [f.sb([128, T // 128], F32, f"c2d{i}") for i in range(2)]
    r2d = [f.sb([128, T // 128], F32, f"r2d{i}") for i in range(2)]
    parts2 = [f.sb([128, 3, T // 128], BF16, f"parts2_{i}") for i in range(2)]
    for h in range(2):
        for sg in range(NSEG):
            b = (h * NSEG + sg) % 2
            ss = slice(sg * 2048, (sg + 1) * 2048)
            f.dma(lfs[b][:], lfd[h:h + 1, ss], lfs[b], lfd)
            init = 0.0 if sg == 0 else cseg[1 - b][:, 2047:2048]
            rd = [ones, lfs[b]] + ([] if sg == 0 else [cseg[1 - b]])
            f.op("vector", lambda e: e.tensor_tensor_scan(out=cseg[b][:], data0=ones[:], data1=lfs[b][:], initial=init, op0=ALU.mult, op1=ALU.add), rd, [cseg[b]])
            f.dma(c_d[h:h + 1, ss], cseg[b][:], c_d, cseg[b])
        C2, R2, P2 = c2d[h], r2d[h], parts2[h]
        f.dma(C2[:], c_d[h].rearrange("(p c) -> p c", p=128), C2, c_d)
        f.op("vector", lambda e: e.tensor_copy(out=P2[:, 0, :], in_=C2[:]), [C2], [P2])
        f.op("vector", lambda e: e.tensor_tensor(out=R2[:], in0=C2[:], in1=P2[:, 0, :], op=ALU.subtract), [C2, P2], [R2])
        f.op("vector", lambda e: e.tensor_copy(out=P2[:, 1, :], in_=R2[:]), [R2], [P2])
        f.op("vector", lambda e: e.tensor_tensor(out=R2[:], in0=R2[:], in1=P2[:, 1, :], op=ALU.subtract), [R2, P2], [R2])
        f.op("vector", lambda e: e.tensor_copy(out=P2[:, 2, :], in_=R2[:]), [R2], [P2])
        for r in range(3):
            f.dma(p_d[h, r].rearrange("(p c) -> p c", p=128), P2[:, r, :], p_d, P2)
    PSs = [f.ps([128, 512], F32, f"PSs{i}") for i in range(4)]
    PO = [f.ps([65, 512], F32, f"PO{i}") for i in range(2)]
    PD = f.ps([64, 512], F32, "PD")
    PT = [f.sb([128, 512], BF16, f"PT{i}") for i in range(5)]
    Osb = [f.sb([65, 512], F32, f"Osb{i}") for i in range(2)]
    rden = f.sb([64, 512], F32, "rden")
    o1 = f.sb([64, 512], F32, "o1")
    ogt = [f.sb([64, 512], BF16, f"ogt{i}") for i in range(2)]
    o2 = [f.sb([64, 512], BF16, f"o2{i}") for i in range(2)]
    scl = [f.sb([128, 512], F32, f"scl{i}") for i in range(2)]
    ti = 0
    for h in range(2):
        f.dma(Qa[0:64, :], qT[h], Qa.k("top"), qT)
        f.dma(Ka[0:64, :], kT[h], Ka.k("top"), kT)
        f.dma(Vt[:].rearrange("p k d -> p (k d)"), vt[h], Vt, vt)
        f.op("gpsimd", lambda e: e.memset(Vt[:, :, 64:65], 1.0), [], [Vt])
        f.op("gpsimd", lambda e: e.memset(Qa[64:70, :], -1.0), [], [Qa.k("aug")])
        f.op("gpsimd", lambda e: e.memset(Ka[64:70, :], 1.0), [], [Ka.k("aug")])
        for r in range(3):
            f.dma(Qa[64 + r:65 + r, :], p_d[h, r:r + 1, :], Qa.k("aug"), p_d)
            f.dma(Ka[67 + r:68 + r, :], p_d[h, r:r + 1, :], Ka.k("aug"), p_d)
        tiles = [(qi, kb) for qi in range(NQ) for kb in range(4 * qi + 4)]
        LA = 3
        base = ti

        def emit_qk(i):
            qi, kb = tiles[i]
            qs = slice(qi * 512, (qi + 1) * 512)
            ps = PSs[(base + i) % 4]
            f.op("tensor", lambda e: e.matmul(ps[:], lhsT=Ka[0:70, kb * 128:(kb + 1) * 128], rhs=Qa[0:70, qs], start=True, stop=True), [Ka, Qa], [ps])

        def emit_rest(i):
            qi, kb = tiles[i]
            qs = slice(qi * 512, (qi + 1) * 512)
            nkb = 4 * qi + 4
            ps = PSs[(base + i) % 4]; pt = PT[(base + i) % 5]
            po = PO[qi % 2]
            if kb == 0:
                f.dma(ogt[qi % 2][:], og[h, :, qs], ogt[qi % 2], og)
            if kb >= 4 * qi:
                sc = scl[kb % 2]
                f.op("vector", lambda e: e.tensor_scalar(out=sc[:], in0=ps[:], scalar1=30.0, scalar2=None, op0=ALU.min), [ps], [sc])
                f.op("scalar", lambda e: e.activation(out=pt[:], in_=sc[:], func=AF.Exp), [sc], [pt])
                m = kb - 4 * qi
                eng = "vector" if m % 2 == 0 else "gpsimd"
                f.op(eng, lambda e: e.tensor_tensor(out=pt[:], in0=pt[:], in1=mk[:, m, :], op=ALU.mult), [pt, mk], [pt])
            else:
                f.op("scalar", lambda e: e.activation(out=pt[:], in_=ps[:], func=AF.Exp), [ps], [pt])
            f.op("tensor", lambda e: e.matmul(po[:], lhsT=Vt[:, kb, :], rhs=pt[:], start=(kb == 0), stop=(kb == nkb - 1)), [Vt, pt], [po])
            if kb == nkb - 1:
                osb = Osb[qi % 2]
                f.op("vector", lambda e: e.tensor_copy(out=osb[:], in_=po[:]), [po], [osb])
                f.op("tensor", lambda e: e.matmul(PD[:], lhsT=sel[:], rhs=osb[:], start=True, stop=True), [sel, osb], [PD])
                f.op("vector", lambda e: e.reciprocal(out=rden[:], in_=PD[:]), [PD], [rden])
                f.op("gpsimd", lambda e: e.tensor_tensor(out=o1[:], in0=osb[0:64, :], in1=rden[:], op=ALU.mult), [osb, rden], [o1])
                f.op("gpsimd", lambda e: e.tensor_tensor(out=o2[qi % 2][:], in0=o1[:], in1=ogt[qi % 2][:], op=ALU.mult), [o1, ogt[qi % 2]], [o2[qi % 2]])
                f.dma(o_o[h, :, qs], o2[qi % 2][:], o_o, o2[qi % 2])
        n = len(tiles)
        for i in range(n + LA):
            if i < n:
                emit_qk(i)
            if i >= LA:
                emit_rest(i - LA)
        ti += n
    f.final_wait([o_o])
    return f.build()


def consts_D():
    m = np.zeros((128, 4, 512), np.float32)
    p = np.arange(128)[:, None]; j = np.arange(512)[None, :]
    for i in range(4):
        m[:, i, :] = ((i * 128 + p) <= j).astype(np.float32)
    sel = np.zeros((65, 64), np.float32); sel[64, :] = 1.0
    return m.reshape(128, 2048), sel


def inputs_D(oc_list, lf_list, T=T_ALL):
    oc = np.concatenate(oc_list, axis=2)[:, :, :T]
    lf = np.concatenate(lf_list, axis=1)[:, :T]
    mk, sel = consts_D()
    NKB = T // 128
    maps = []
    for c in range(8):
        cs = slice(128 * c, 128 * c + 128)
        q = oc[0][cs].reshape(2, 64, T); k = oc[1][cs].reshape(2, 64, T); og = oc[3][cs].reshape(2, 64, T)
        v = oc[2][cs].reshape(2, 64, NKB, 128)
        vp = np.zeros((2, 128, NKB, 65), dtype=oc.dtype)
        vp[:, :, :, 0:64] = v.transpose(0, 3, 2, 1)
        maps.append({"qT": np.ascontiguousarray(q), "kT": np.ascontiguousarray(k), "vt": vp.reshape(2, 128, NKB * 65),
                     "og": np.ascontiguousarray(og), "lf": np.ascontiguousarray(lf[2 * c:2 * c + 2]), "mk": mk, "sel": sel})
    return maps


def gather_D(results):
    return np.concatenate([np.asarray(r["o_o"]).reshape(128, -1) for r in results], axis=0)


def _run(nc, maps):
    return run_bass_kernel_spmd(nc, maps, core_ids=list(range(8)))


def kernel(**inputs):
    inp = {k: np.asarray(v) for k, v in inputs.items()}
    resA = _run(build_A(), inputs_A(inp))
    obfA = [np.asarray(r["o_bf"]) for r in resA.results]
    obf = np.concatenate(obfA, axis=2)
    ort = np.concatenate([np.asarray(r["o_rt"]) for r in resA.results], axis=1)
    owc = np.concatenate([np.asarray(r["o_wc"]) for r in resA.results], axis=1)
    resB = _run(build_B(), inputs_B(obf, ort, owc))
    y = gather_B(resB.results)
    del obf, ort, owc
    resC = _run(build_C(), inputs_C(inp, y, obfA))
    oc_list = [np.asarray(r["o_c"]) for r in resC.results]
    lf_list = [np.asarray(r["o_lf"]) for r in resC.results]
    hT_list = [np.asarray(r["o_h"]) for r in resC.results]
    resD = _run(build_D(), inputs_D(oc_list, lf_list))
    oT = gather_D(resD.results)
    resE = _run(build_E(), inputs_E(inp, hT_list, oT))
    out = np.concatenate([np.asarray(r["o_out"]) for r in resE.results], axis=1).T
    return np.ascontiguousarray(out, dtype=np.float32).reshape(1, 16384, 1024)
```

```python
import ml_dtypes
import numpy as np
import concourse.bass as bass
import concourse.mybir as mybir
from concourse.bass_utils import run_bass_kernel_spmd
from contextlib import ExitStack

F32 = mybir.dt.float32
BF16 = mybir.dt.bfloat16
I32 = mybir.dt.int32
ALU = mybir.AluOpType
AF = mybir.ActivationFunctionType
AX = mybir.AxisListType

SAME_ENGINE_SYNC = True


class _Rec:
    def __init__(self):
        self.call = None

    def __getattr__(self, name):
        def cap(*a, **k):
            self.call = (name, a, k)
            return self
        return cap


def _replay(call):
    name, a, k = call
    return lambda e: getattr(e, name)(*a, **k)


class _Trk:
    __slots__ = ("w", "r")

    def __init__(self):
        self.w = {}
        self.r = {}


class Buf:
    def __init__(self, fw, name, t, kind):
        self.fw = fw
        self.name = name
        self.t = t
        self.kind = kind
        self.whole = _Trk()
        self.subs = {}
        self.dsem = None

    def __getitem__(self, idx):
        return self.t[idx]

    def k(self, key):
        return (self, key)


def _split(b):
    if isinstance(b, tuple):
        return b
    return (b, None)


class FW:
    CE = ("tensor", "vector", "scalar", "gpsimd")

    def __init__(self, nc):
        self.nc = nc
        self.es = ExitStack()
        self.q = {e: [] for e in ("tensor", "vector", "scalar", "gpsimd", "sync")}
        self.cnt = {e: 0 for e in self.CE}
        self.waited = {}
        self.sems = {}
        self.dcnt = {}
        self.nbuf = 0

    def sb(self, shape, dt, name=None):
        self.nbuf += 1
        name = "S_" + (name or f"sb{self.nbuf}")
        t = self.es.enter_context(self.nc.sbuf_tensor(name, list(shape), dt))
        return Buf(self, name, t, "sb")

    def ps(self, shape, dt=F32, name=None):
        self.nbuf += 1
        name = "P_" + (name or f"ps{self.nbuf}")
        t = self.es.enter_context(self.nc.psum_tensor(name, list(shape), dt))
        return Buf(self, name, t, "ps")

    def dram(self, name, shape, dt, kind):
        t = self.nc.dram_tensor(name, list(shape), dt, kind=kind).ap()
        return Buf(self, name, t, "dram")

    def _sem(self, key):
        if key not in self.sems:
            self.sems[key] = self.es.enter_context(self.nc.semaphore("s_" + str(key)))
        return self.sems[key]

    def _collect(self, eng, reads, writes):
        waits = {}

        def need_w(wd):
            for kv in wd.items():
                need(kv)

        def need(tok):
            if tok is None:
                return
            k, v = tok
            if k == eng and (eng == "tensor" or not SAME_ENGINE_SYNC):
                return
            if waits.get(k, 0) < v:
                waits[k] = v

        reads = list(reads)
        writes = list(writes)
        for b in list(reads):
            bb, _k = _split(b)
            if bb.kind == "ps":
                writes.append(bb)
        writes = [(_split(b)[0] if _split(b)[0].kind == "ps" else b) for b in writes]
        for b in reads:
            b, key = _split(b)
            if b.kind == "ps":
                continue
            trks = [b.whole] + ([b.subs[key]] if (key is not None and key in b.subs) else
                                (list(b.subs.values()) if key is None else []))
            for t in trks:
                need_w(t.w)
        for b in writes:
            b, key = _split(b)
            trks = [b.whole] + ([b.subs[key]] if (key is not None and key in b.subs) else
                                (list(b.subs.values()) if key is None else []))
            for t in trks:
                need_w(t.w)
                for k, v in t.r.items():
                    need((k, v))
        out = []
        for k, v in waits.items():
            if self.waited.get((eng, k), 0) >= v:
                continue
            self.waited[(eng, k)] = v
            out.append((k, v))
        return out

    def _update(self, reads, writes, tok):
        reads = list(reads)
        writes = list(writes)
        for b in list(reads):
            bb, _k = _split(b)
            if bb.kind == "ps":
                writes.append(bb)
        reads = [b for b in reads if _split(b)[0].kind != "ps"]
        writes = [(_split(b)[0] if _split(b)[0].kind == "ps" else b) for b in writes]
        for b in reads:
            b, key = _split(b)
            t = b.whole if key is None else b.subs.setdefault(key, _Trk())
            k, v = tok
            if t.r.get(k, 0) < v:
                t.r[k] = v
        for b in writes:
            b, key = _split(b)
            t = b.whole if key is None else b.subs.setdefault(key, _Trk())
            if b.kind == "dram":
                if t.w.get(tok[0], 0) < tok[1]:
                    t.w[tok[0]] = tok[1]
            else:
                t.w = {tok[0]: tok[1]}
                t.r = {}
                if key is None:
                    b.subs = {}

    def op(self, eng, fn, reads=(), writes=()):
        waits = self._collect(eng, reads, writes)
        self.cnt[eng] += 1
        tok = (eng, self.cnt[eng])
        self._update(reads, writes, tok)
        rec = _Rec()
        fn(rec)
        self.q[eng].append((waits, _replay(rec.call), (eng, 1)))

    def dma(self, out, in_, outb, inb, q="sync", **kw):
        ob, ok_ = _split(outb)
        ib, ik_ = _split(inb)
        sb, sk = (ob, ok_) if ob.kind != "dram" else (ib, ik_)
        key = "d_" + sb.name + ("" if sk is None else "_" + "_".join(str(z) for z in (sk if isinstance(sk, tuple) else (sk,))))
        waits = self._collect(q, [inb], [outb])
        self.dcnt[key] = self.dcnt.get(key, 0) + 16
        tok = (key, self.dcnt[key])
        self._update([inb], [outb], tok)
        self.q[q].append((waits, lambda e: e.dma_start(out=out, in_=in_, **kw), (key, 16)))

    def final_wait(self, bufs, q="sync"):
        waits = self._collect(q, bufs, [])
        self.q[q].append((waits, None, None))

    def build(self):
        nc = self.nc
        for e in self.CE:
            self._sem(e)
        for q in self.q.values():
            for waits, fn, inc in q:
                for k, v in waits:
                    self._sem(k)
                if inc is not None:
                    self._sem(inc[0])
        fwself = self
        self.nsem = len(self.sems)
        with nc.Block() as block:
            def mk(ename):
                def body(eng):
                    for waits, fn, inc in fwself.q[ename]:
                        for k, v in waits:
                            eng.wait_ge(fwself.sems[k], v)
                        if fn is not None:
                            ins = fn(eng)
                            ins.then_inc(fwself.sems[inc[0]], inc[1])
                return body
            block.tensor(mk("tensor"))
            block.vector(mk("vector"))
            block.scalar(mk("scalar"))
            block.gpsimd(mk("gpsimd"))
            block.sync(mk("sync"))
        self.es.close()
        return nc


C0 = 0.6065306597126334
RMS_EPS = 1e-6
DEBUG = False

PVA = {"g0": 0, "mu": 8, "w0": 56, "a0": 64, "k_k": 72, "k_a": 80, "r_k": 88}
NPA = 96


def rmsnorm_to_fm(f, xsrc_ap, xbuf, uT, col0, ncols, src_col0, ident, gcol, pv, tmp, NTOK=128):
    pass


def build_A():
    nc = bass.Bass("TRN2", target_bir_lowering=False)
    f = FW(nc)
    xa = f.dram("xa", [17 * 128, 1024], F32, "ExternalInput")
    pvd = f.dram("pv", [128, NPA], F32, "ExternalInput")
    cst = f.dram("cst", [128, 256], F32, "ExternalInput")
    w_rkv = f.dram("w_rkv", [3, 1024, 1024], F32, "ExternalInput")
    w1d = f.dram("w1", [1024, 64], F32, "ExternalInput")
    a1d = f.dram("a1", [1024, 64], F32, "ExternalInput")
    g1d = f.dram("g1", [1024, 128], F32, "ExternalInput")
    w2d = f.dram("w2", [64, 1024], F32, "ExternalInput")
    a2d = f.dram("a2", [64, 1024], F32, "ExternalInput")
    g2d = f.dram("g2", [128, 1024], F32, "ExternalInput")
    o_bf = f.dram("o_bf", [8, 1024, 2048], BF16, "ExternalOutput")
    o_rt = f.dram("o_rt", [1024, 2048], F32, "ExternalOutput")
    o_wc = f.dram("o_wc", [1024, 32], F32, "ExternalOutput")

    pv = f.sb([128, NPA], F32, "pv")
    f.dma(pv[:], pvd[:], pv, pvd)
    cs = f.sb([128, 256], F32, "cs")
    f.dma(cs[:], cst[:], cs, cst)
    ident = f.sb([128, 128], BF16, "ident")
    bones = f.sb([128, 128], BF16, "bones")
    f.op("vector", lambda e: e.tensor_copy(out=ident[:], in_=cs[:, 0:128]), [cs], [ident])
    f.op("vector", lambda e: e.tensor_copy(out=bones[:], in_=cs[:, 128:256]), [cs], [bones])
    ones = f.sb([128, 64], F32, "ones")
    f.op("gpsimd", lambda e: e.memset(ones[:], 1.0), [], [ones])

    wr = f.sb([128, 3, 8, 1024], BF16, "wr")
    for n in range(3):
        for kc in range(0, 8, 4):
            f.dma(wr[:, n, kc:kc + 4, :], w_rkv[n, kc * 128:(kc + 4) * 128, :].rearrange("(k p) e -> p k e", p=128), wr.k((n, kc)), w_rkv, q="gpsimd")
    w1 = f.sb([128, 8, 64], BF16, "w1s"); a1 = f.sb([128, 8, 64], BF16, "a1s"); g1 = f.sb([128, 8, 128], BF16, "g1s")
    f.dma(w1[:], w1d[:].rearrange("(k p) e -> p k e", p=128), w1, w1d, q="gpsimd")
    f.dma(a1[:], a1d[:].rearrange("(k p) e -> p k e", p=128), a1, a1d, q="gpsimd")
    f.dma(g1[:], g1d[:].rearrange("(k p) e -> p k e", p=128), g1, g1d, q="gpsimd")
    w2 = f.sb([64, 1024], BF16, "w2s"); a2 = f.sb([64, 1024], BF16, "a2s"); g2 = f.sb([128, 1024], BF16, "g2s")
    f.dma(w2[:], w2d[:], w2, w2d, q="gpsimd")
    f.dma(a2[:], a2d[:], a2, a2d, q="gpsimd")
    f.dma(g2[:], g2d[:], g2, g2d, q="gpsimd")

    uT = f.sb([128, 8, 2049], BF16, "uT")
    xb = [f.sb([128, 1024], F32, f"xb{i}") for i in range(2)]
    xn = [f.sb([128, 1024], BF16, f"xn{i}") for i in range(2)]
    junk = f.sb([128, 1024], BF16, "junk")
    ssq = [f.sb([128, 1], F32, f"ssq{i}") for i in range(2)]
    ptr = [f.ps([128, 8, 128], BF16, "ptr0")] * 2
    for t in range(17):
        b = t % 2
        X, XN, SS, PT = xb[b], xn[b], ssq[b], ptr[b]
        f.dma(X[:], xa[t * 128:(t + 1) * 128, :], X, xa)
        f.op("scalar", lambda e, X=X, SS=SS: e.activation(out=junk[:], in_=X[:], func=AF.Square, accum_out=SS[:]), [X], [junk, SS])
        f.op("scalar", lambda e, SS=SS: e.activation(out=SS[:], in_=SS[:], func=AF.Sqrt, scale=1.0 / 1024, bias=RMS_EPS), [SS], [SS])
        f.op("vector", lambda e, SS=SS: e.reciprocal(out=SS[:], in_=SS[:]), [SS], [SS])
        f.op("vector", lambda e, X=X, XN=XN, SS=SS: e.tensor_scalar(out=XN[:], in0=X[:], scalar1=SS[:, 0:1], scalar2=None, op0=ALU.mult), [X, SS], [XN])
        for c in range(8):
            f.op("tensor", lambda e, c=c, XN=XN, PT=PT: e.transpose(out=PT[:, c, :], in_=XN[:, c * 128:(c + 1) * 128], identity=ident[:]), [XN, ident], [PT])
        for c in range(8):
            eng = "vector" if c % 2 == 0 else "gpsimd"
            eng = "vector"
            if t == 0:
                f.op(eng, lambda e, c=c, PT=PT: e.tensor_scalar(out=uT[:, c, 0:1], in0=PT[:, c, 127:128], scalar1=pv[:, PVA["g0"] + c:PVA["g0"] + c + 1], scalar2=None, op0=ALU.mult), [PT, pv], [uT.k(("t", t))])
            else:
                c0 = 1 + (t - 1) * 128
                f.op(eng, lambda e, c=c, PT=PT, c0=c0: e.tensor_scalar(out=uT[:, c, c0:c0 + 128], in0=PT[:, c, :], scalar1=pv[:, PVA["g0"] + c:PVA["g0"] + c + 1], scalar2=None, op0=ALU.mult), [PT, pv], [uT.k(("t", t))])

    xs = f.sb([128, 6, 8, 512], BF16, "xs")
    dd = [f.sb([128, 512], BF16, f"dd{i}") for i in range(2)]
    pr = f.ps([128, 512], F32, "pr"); pk = f.ps([128, 512], F32, "pk"); pvv = f.ps([128, 512], F32, "pvv")
    pw = f.ps([128, 512], F32, "pw"); pa = f.ps([128, 512], F32, "pa"); pg = f.ps([128, 512], F32, "pg")
    px1 = f.ps([128, 512], F32, "px1"); px2 = px1
    h1 = f.sb([64, 512], BF16, "h1"); ha = f.sb([64, 512], BF16, "ha"); hg = f.sb([128, 512], BF16, "hg")
    T = lambda n, dt=F32: f.sb([128, 512], dt, n)
    r_s, k_s, v_s, sg, a_s, cum, cprev = T("r_s"), T("k_s"), T("v_s"), T("sg"), T("a_s"), T("cum"), T("cprev")
    e_pos, e_neg, e_prev = T("e_pos"), T("e_neg"), T("e_prev")
    kkr, sq, ssm, kk, t1, kmod, bb, btf, ktf, rk = T("kkr"), T("sq", BF16), T("ssm"), T("kk"), T("t1"), T("kmod"), T("bb"), T("btf"), T("ktf"), T("rk", BF16)
    rt = T("rt")
    wc = f.sb([128, 8], F32, "wc")
    ob = f.sb([128, 8, 512], BF16, "ob")
    for s in range(4):
        cur = lambda c: uT[:, c, 1 + 512 * s:1 + 512 * s + 512]
        prv = lambda c: uT[:, c, 512 * s:512 * s + 512]
        ureads = [uT.k(("t", t)) for t in range(max(0, 4 * s), 4 * s + 5)]
        for c in range(8):
            D = dd[c % 2]
            f.op("gpsimd", lambda e, c=c, D=D: e.tensor_tensor(out=D[:], in0=prv(c), in1=cur(c), op=ALU.subtract), ureads, [D])
            for n in range(6):
                col = PVA["mu"] + n * 8 + c
                f.op("vector", lambda e, c=c, n=n, D=D, col=col: e.scalar_tensor_tensor(out=xs[:, n, c, :], in0=D[:], scalar=pv[:, col:col + 1], in1=cur(c), op0=ALU.mult, op1=ALU.add), [D, pv] + ureads, [xs.k(n)])
        for c in range(8):
            f.op("tensor", lambda e, c=c: e.matmul(pw[0:64, :], lhsT=w1[:, c, :], rhs=xs[:, 3, c, :], start=(c == 0), stop=(c == 7)), [w1, xs.k(3)], [pw])
        f.op("scalar", lambda e: e.activation(out=h1[:], in_=pw[0:64, :], func=AF.Tanh), [pw], [h1])
        for c in range(8):
            f.op("tensor", lambda e, c=c: e.matmul(pa[0:64, :], lhsT=a1[:, c, :], rhs=xs[:, 4, c, :], start=(c == 0), stop=(c == 7)), [a1, xs.k(4)], [pa])
        f.op("vector", lambda e: e.tensor_copy(out=ha[:], in_=pa[0:64, :]), [pa], [ha])
        for c in range(8):
            f.op("tensor", lambda e, c=c: e.matmul(pg[:], lhsT=g1[:, c, :], rhs=xs[:, 5, c, :], start=(c == 0), stop=(c == 7)), [g1, xs.k(5)], [pg])
        f.op("scalar", lambda e: e.activation(out=hg[:], in_=pg[:], func=AF.Sigmoid), [pg], [hg])
        for ec in range(8):
            es = slice(ec * 128, (ec + 1) * 128)
            for n, P in enumerate((pr, pk, pvv)):
                for c in range(8):
                    f.op("tensor", lambda e, c=c, n=n, P=P: e.matmul(P[:], lhsT=wr[:, n, c, es], rhs=xs[:, n, c, :], start=(c == 0), stop=(c == 7)), [wr.k((n, (c // 4) * 4)), xs.k(n)], [P])
            f.op("tensor", lambda e: e.matmul(pw[:], lhsT=w2[:, es], rhs=h1[:], start=True, stop=True), [w2, h1], [pw])
            f.op("tensor", lambda e: e.matmul(pa[:], lhsT=a2[:, es], rhs=ha[:], start=True, stop=True), [a2, ha], [pa])
            f.op("tensor", lambda e: e.matmul(pg[:], lhsT=g2[:, es], rhs=hg[:], start=True, stop=True), [g2, hg], [pg])
            pcol = lambda nm: pv[:, PVA[nm] + ec:PVA[nm] + ec + 1]
            f.op("scalar", lambda e: e.activation(out=r_s[:], in_=pr[:], func=AF.Copy), [pr], [r_s])
            f.op("scalar", lambda e: e.activation(out=k_s[:], in_=pk[:], func=AF.Copy), [pk], [k_s])
            f.op("vector", lambda e: e.tensor_copy(out=v_s[:], in_=pvv[:]), [pvv], [v_s])
            f.op("scalar", lambda e: e.activation(out=sg[:], in_=pw[:], func=AF.Sigmoid, bias=pcol("w0")), [pw, pv], [sg])
            f.op("scalar", lambda e: e.activation(out=a_s[:], in_=pa[:], func=AF.Sigmoid, bias=pcol("a0")), [pa, pv], [a_s])
            f.op("scalar", lambda e: e.activation(out=ob[:, 6, :], in_=pg[:], func=AF.Copy), [pg], [ob.k(6)])
            for q in range(8):
                f.op("vector", lambda e, q=q: e.tensor_tensor_scan(out=cum[:, q * 64:(q + 1) * 64], data0=ones[:], data1=sg[:, q * 64:(q + 1) * 64], initial=0.0, op0=ALU.mult, op1=ALU.add), [ones, sg], [cum])
            f.op("gpsimd", lambda e: e.tensor_tensor(out=cprev[:], in0=cum[:], in1=sg[:], op=ALU.subtract), [cum, sg], [cprev])
            f.op("scalar", lambda e: e.activation(out=e_pos[:], in_=cum[:], func=AF.Exp, scale=-C0), [cum], [e_pos])
            f.op("scalar", lambda e: e.activation(out=e_neg[:], in_=cum[:], func=AF.Exp, scale=C0), [cum], [e_neg])
            f.op("scalar", lambda e: e.activation(out=e_prev[:], in_=cprev[:], func=AF.Exp, scale=-C0), [cprev], [e_prev])
            f.op("scalar", lambda e: e.activation(out=wc[:], in_=cum[:].rearrange("p (q t) -> p q t", t=64)[:, :, 63], func=AF.Exp, scale=-C0), [cum], [wc])
            f.op("gpsimd", lambda e: e.tensor_scalar(out=kkr[:], in0=k_s[:], scalar1=pcol("k_k"), scalar2=None, op0=ALU.mult), [k_s, pv], [kkr])
            f.op("gpsimd", lambda e: e.tensor_tensor(out=sq[:], in0=kkr[:], in1=kkr[:], op=ALU.mult), [kkr], [sq])
            f.op("tensor", lambda e: e.matmul(px1[:], lhsT=bones[:], rhs=sq[:], start=True, stop=True), [bones, sq], [px1])
            f.op("vector", lambda e: e.tensor_scalar(out=ssm[:], in0=px1[:], scalar1=1e-24, scalar2=None, op0=ALU.max), [px1], [ssm])
            f.op("scalar", lambda e: e.activation(out=ssm[:], in_=ssm[:], func=AF.Sqrt), [ssm], [ssm])
            f.op("vector", lambda e: e.reciprocal(out=ssm[:], in_=ssm[:]), [ssm], [ssm])
            f.op("gpsimd", lambda e: e.tensor_tensor(out=kk[:], in0=kkr[:], in1=ssm[:], op=ALU.mult), [kkr, ssm], [kk])
            f.op("vector", lambda e: e.tensor_scalar(out=t1[:], in0=a_s[:], scalar1=-1.0, scalar2=pcol("k_a"), op0=ALU.add, op1=ALU.mult), [a_s, pv], [t1])
            f.op("vector", lambda e: e.scalar_tensor_tensor(out=kmod[:], in0=t1[:], scalar=1.0, in1=k_s[:], op0=ALU.add, op1=ALU.mult), [t1, k_s], [kmod])
            f.op("gpsimd", lambda e: e.tensor_tensor(out=bb[:], in0=kk[:], in1=a_s[:], op=ALU.mult), [kk, a_s], [bb])
            f.op("vector", lambda e: e.scalar_tensor_tensor(out=ob[:, 0, :], in0=kk[:], scalar=-1.0, in1=e_prev[:], op0=ALU.mult, op1=ALU.mult), [kk, e_prev], [ob.k(0)])
            f.op("gpsimd", lambda e: e.tensor_tensor(out=rt[:], in0=r_s[:], in1=e_pos[:], op=ALU.mult), [r_s, e_pos], [rt])
            f.op("gpsimd", lambda e: e.tensor_tensor(out=btf[:], in0=bb[:], in1=e_neg[:], op=ALU.mult), [bb, e_neg], [btf])
            f.op("gpsimd", lambda e: e.tensor_tensor(out=ktf[:], in0=kmod[:], in1=e_neg[:], op=ALU.mult), [kmod, e_neg], [ktf])
            f.op("scalar", lambda e: e.activation(out=ob[:, 1, :], in_=btf[:], func=AF.Copy), [btf], [ob.k(1)])
            f.op("scalar", lambda e: e.activation(out=ob[:, 2, :], in_=ktf[:], func=AF.Copy), [ktf], [ob.k(2)])
            for q in range(8):
                qs = slice(q * 64, (q + 1) * 64)
                f.op("vector", lambda e, q=q, qs=qs: e.tensor_scalar(out=ob[:, 3, qs], in0=btf[:, qs], scalar1=wc[:, q:q + 1], scalar2=None, op0=ALU.mult), [btf, wc], [ob.k(3)])
                f.op("gpsimd", lambda e, q=q, qs=qs: e.tensor_scalar(out=ob[:, 4, qs], in0=ktf[:, qs], scalar1=wc[:, q:q + 1], scalar2=None, op0=ALU.mult), [ktf, wc], [ob.k(4)])
            f.op("vector", lambda e: e.scalar_tensor_tensor(out=rk[:], in0=r_s[:], scalar=pcol("r_k"), in1=kmod[:], op0=ALU.mult, op1=ALU.mult), [r_s, pv, kmod], [rk])
            f.op("tensor", lambda e: e.matmul(px2[:], lhsT=bones[:], rhs=rk[:], start=True, stop=True), [bones, rk], [px2])
            f.op("vector", lambda e: e.tensor_tensor(out=ob[:, 7, :], in0=px2[:], in1=v_s[:], op=ALU.mult), [px2, v_s], [ob.k(7)])
            f.op("scalar", lambda e: e.activation(out=ob[:, 5, :], in_=v_s[:], func=AF.Copy), [v_s], [ob.k(5)])
            f.dma(o_bf[:, es, 512 * s:512 * s + 512].rearrange("q p t -> p q t"), ob[:], o_bf, ob)
            f.dma(o_rt[es, 512 * s:512 * s + 512], rt[:], o_rt, rt)
            f.dma(o_wc[es, 8 * s:8 * s + 8], wc[:], o_wc, wc)
    if DEBUG:
        o_dbg = f.dram("o_dbg", [128, 8, 2049], BF16, "ExternalOutput")
        f.dma(o_dbg[:], uT[:], o_dbg, uT)
        o_dbg2 = f.dram("o_dbg2", [128, 8, 1024], BF16, "ExternalOutput")
        f.dma(o_dbg2[:], wr[:, 2, :, :], o_dbg2, wr)
        f.final_wait([o_dbg, o_dbg2])
    f.final_wait([o_bf, o_rt, o_wc])
    return f.build()


def consts_A():
    c = np.zeros((128, 256), np.float32)
    c[:, 0:128] = np.eye(128)
    blk = np.arange(128) // 64
    c[:, 128:256] = (blk[:, None] == blk[None, :]).astype(np.float32)
    return c


def fmcols(v):
    return np.ascontiguousarray(np.asarray(v, np.float32).reshape(8, 128).T)


def inputs_A(inp):
    x = inp["x"][0]
    pvv = np.zeros((128, NPA), np.float32)
    pvv[:, 0:8] = fmcols(inp["norm_g"][0, 0])
    for n in range(6):
        pvv[:, 8 + 8 * n:16 + 8 * n] = fmcols(inp["rwkv_mu"][0, n])
    pvv[:, 56:64] = fmcols(inp["rwkv_w0"][0]); pvv[:, 64:72] = fmcols(inp["rwkv_a0"][0])
    pvv[:, 72:80] = fmcols(inp["rwkv_k_k"][0]); pvv[:, 80:88] = fmcols(inp["rwkv_k_a"][0])
    pvv[:, 88:96] = fmcols(inp["rwkv_r_k"][0].reshape(-1))
    cst = consts_A()
    xpad = np.concatenate([np.zeros((128, 1024), np.float32), x], 0)
    maps = []
    for c in range(8):
        maps.append({"xa": np.ascontiguousarray(xpad[2048 * c:2048 * c + 2048 + 128]), "pv": pvv, "cst": cst,
                     "w_rkv": inp["rwkv_w_rkv"][0], "w1": inp["rwkv_w1"][0], "a1": inp["rwkv_a1"][0], "g1": inp["rwkv_g1"][0],
                     "w2": inp["rwkv_w2"][0], "a2": inp["rwkv_a2"][0], "g2": inp["rwkv_g2"][0]})
    return maps


GC = 8
NCH = 256
NG = NCH // GC
DEBUG = False


def build_B(nch=NCH):
    ng = nch // GC
    nc = bass.Bass("TRN2", target_bir_lowering=False)
    f = FW(nc)
    d_bk = f.dram("d_bk", [ng, 64, GC * 2 * 2 * 64], BF16, "ExternalInput")
    d_at = f.dram("d_at", [ng, 64, GC * 2 * 64], BF16, "ExternalInput")
    d_rf = f.dram("d_rf", [ng, 64, GC * 2 * 64], F32, "ExternalInput")
    d_tm = f.dram("d_tm", [ng, 64, GC * 2 * 4 * 64], BF16, "ExternalInput")
    d_wc = f.dram("d_wc", [64, nch * 2], F32, "ExternalInput")
    d_mask = f.dram("d_mask", [64, 2 * 320], F32, "ExternalInput")
    d_id = f.dram("d_id", [64, 128], F32, "ExternalInput")
    o_y = f.dram("o_y", [ng, 64, GC * 2 * 64], F32, "ExternalOutput")

    wcs = f.sb([64, nch, 2], F32, "wcs")
    f.dma(wcs[:].rearrange("p c h -> p (c h)"), d_wc[:], wcs, d_wc)
    mask = f.sb([64, 2, 320], F32, "mask")
    f.dma(mask[:].rearrange("p h c -> p (h c)"), d_mask[:], mask, d_mask)
    idf2 = f.sb([64, 2, 64], F32, "idf2")
    f.dma(idf2[:].rearrange("p h c -> p (h c)"), d_id[:], idf2, d_id)
    idb2 = f.sb([64, 2, 64], BF16, "idb2")
    f.op("vector", lambda e: e.tensor_copy(out=idb2[:], in_=idf2[:]), [idf2], [idb2])
    identb = idb2[:, 0, :]
    identf = idf2[:, 0, :]

    NB = 2
    BK = [f.sb([64, GC, 2, 2, 64], BF16, f"BK{i}") for i in range(NB)]
    AR = [f.sb([64, GC, 2, 128], BF16, f"AR{i}") for i in range(NB)]
    RF = [f.sb([64, GC, 2, 64], F32, f"RF{i}") for i in range(NB)]
    TM = [f.sb([64, GC, 2, 4, 64], BF16, f"TM{i}") for i in range(NB)]
    YB = [f.sb([64, GC, 2, 64], F32, f"YB{i}") for i in range(NB)]

    def load_group(g):
        b = g % NB
        f.dma(BK[b][:].rearrange("p c h q t -> p (c h q t)"), d_bk[g], BK[b], d_bk)
        f.dma(AR[b][:, :, :, 0:64], d_at[g].rearrange("p (c h t) -> p c h t", c=GC, h=2), AR[b].k("a"), d_at)
        f.dma(AR[b][:, :, :, 64:128], d_rf[g].rearrange("p (c h t) -> p c h t", c=GC, h=2), AR[b].k("r"), d_rf, q="gpsimd")
        f.dma(RF[b][:].rearrange("p c h t -> p (c h t)"), d_rf[g], RF[b], d_rf)
        f.dma(TM[b][:].rearrange("p c h q t -> p (c h q t)"), d_tm[g], TM[b], d_tm)

    class _V:
        def __init__(self, buf, ap):
            self.buf, self.ap = buf, ap

        def __getitem__(self, idx):
            return self.ap[idx]

        def k(self, key):
            return self.buf

    def bank(name):
        return f.ps([64, 512], F32, name)
    PM, PL, PXG = [], [], []
    for x in range(2):
        bm, bl, bx = bank(f"PM{x}"), bank(f"PL{x}"), bank(f"PXG{x}")
        PM.append(_V(bm, bm[:, 0:512].rearrange("p (h c) -> p h c", h=2)))
        PL.append(_V(bl, bl[:, 0:384].rearrange("p (h c) -> p h c", h=2)))
        PXG.append(_V(bx, bx[:, 0:512].rearrange("p (h c) -> p h c", h=2)))
    bus, by = bank("PUS"), bank("PYb")
    PU_ap = bus[:, 0:128].rearrange("p (h c) -> p h c", h=2)
    PS_ap = bus[:, 128:256].rearrange("p (h c) -> p h c", h=2)
    PY_ap = by[:, 0:128].rearrange("p (h c) -> p h c", h=2)
    PUk, PSk, PYk = bus, bus, by
    NP = 4
    Ms = [f.sb([64, 2, 320], BF16, f"Ms{i}") for i in range(NP)]
    XT0 = [f.sb([64, 2, 64], BF16, f"XT0{i}") for i in range(NP)]
    LV = [[f.sb([64, 2, 192], BF16, f"LV{i}_{k}") for k in range(7)] for i in range(NP)]
    GX = [f.sb([64, 2, 128], F32, f"GX{i}") for i in range(NP)]
    UT = [f.sb([64, 2, 64], BF16, f"UT{i}") for i in range(2)]
    S = [f.sb([64, 2, 64], F32, f"S{i}") for i in range(2)]
    f.op("vector", lambda e: e.memset(S[0][:], 0.0), [], [S[0]])

    def pre_stages(c):
        p = c % NP
        x = c % 2
        PMx, PLx, PXGx = PM[x], PL[x], PXG[x]
        g, cg = divmod(c, GC)
        b = g % NB
        bk, ar, tm = BK[b], AR[b], TM[b]
        st = []

        def p1():
            for h in range(2):
                f.op("tensor", lambda e: e.matmul(PMx[:, h, 0:128], lhsT=bk[:, cg, h, 0, :], rhs=ar[:, cg, h, :], start=True, stop=True), [bk, ar], [PMx.k(h)])
                f.op("tensor", lambda e: e.matmul(PMx[:, h, 128:256], lhsT=bk[:, cg, h, 1, :], rhs=ar[:, cg, h, :], start=True, stop=True), [bk, ar], [PMx.k(h)])
                f.op("tensor", lambda e: e.matmul(PXGx[:, h, 192:256], lhsT=ar[:, cg, h, 0:64], rhs=bk[:, cg, h, 0, :], start=True, stop=True), [bk, ar], [PXGx.k("l")])
        st.append(p1)

        def p2():
            f.op("vector", lambda e: e.tensor_tensor(out=Ms[p][:, :, 0:256], in0=PMx[:, :, 0:256], in1=mask[:, :, 0:256], op=ALU.mult), [PMx.buf, mask], [Ms[p].k("m")])
            f.op("vector", lambda e: e.tensor_tensor(out=Ms[p][:, :, 256:320], in0=PXGx[:, :, 192:256], in1=mask[:, :, 256:320], op=ALU.mult), [PXGx.k("l"), mask], [Ms[p].k("l")])
            f.op("gpsimd", lambda e: e.tensor_tensor(out=LV[p][1][:, :, 128:192], in0=Ms[p][:, :, 0:64], in1=idb2[:], op=ALU.add), [Ms[p].k("m"), idb2], [LV[p][1].k("T")])
        st.append(p2)

        def p3():
            for h in range(2):
                f.op("tensor", lambda e: e.matmul(PXGx[:, h, 0:64], lhsT=Ms[p][:, h, 128:192], rhs=tm[:, cg, h, 3, :], start=True, stop=True), [Ms[p].k("m"), tm], [PXGx.k("x")])
        st.append(p3)

        def p4():
            f.op("scalar", lambda e: e.activation(out=XT0[p][:], in_=PXGx[:, :, 0:64], func=AF.Copy), [PXGx.k("x")], [XT0[p]])
        st.append(p4)

        def level(k):
            def mm():
                for h in range(2):
                    if k == 1:
                        Np, Lp = Ms[p][:, h, 0:64], Ms[p][:, h, 256:320]
                        rd = [Ms[p].k("m"), Ms[p].k("l")]
                    else:
                        Np, Lp = LV[p][k - 1][:, h, 0:64], LV[p][k - 1][:, h, 64:128]
                        rd = [LV[p][k - 1].k("NL")]
                    if k <= 5:
                        f.op("tensor", lambda e: e.matmul(PLx[:, h, 0:64], lhsT=Lp, rhs=Np, start=True, stop=True), rd, [PLx.k(h)])
                        f.op("tensor", lambda e: e.matmul(PLx[:, h, 64:128], lhsT=Np, rhs=Lp, start=True, stop=True), rd, [PLx.k(h)])
                    if k >= 2:
                        Tp = LV[p][k - 1][:, h, 128:192]
                        rdt = rd + [LV[p][k - 1].k("T"), idb2]
                        f.op("tensor", lambda e: e.matmul(PLx[:, h, 128:192], lhsT=identb, rhs=Tp, start=True, stop=False), rdt, [PLx.k(h)])
                        f.op("tensor", lambda e: e.matmul(PLx[:, h, 128:192], lhsT=Lp, rhs=Tp, start=False, stop=True), rdt, [PLx.k(h)])

            def ev():
                eng = "scalar" if k % 2 == 0 else "vector"
                if k == 1:
                    sl, wk = slice(0, 128), [LV[p][k].k("NL")]
                elif k <= 5:
                    sl, wk = slice(0, 192), [LV[p][k].k("NL"), LV[p][k].k("T")]
                else:
                    sl, wk = slice(128, 192), [LV[p][k].k("T")]
                if eng == "scalar":
                    f.op("scalar", lambda e: e.activation(out=LV[p][k][:, :, sl], in_=PLx[:, :, sl], func=AF.Copy), [PLx.buf], wk)
                else:
                    f.op("vector", lambda e: e.tensor_copy(out=LV[p][k][:, :, sl], in_=PLx[:, :, sl]), [PLx.buf], wk)
            return [mm, ev]
        for k in range(1, 7):
            st.extend(level(k))

        def pf():
            for h in range(2):
                Tf = LV[p][6][:, h, 128:192]
                f.op("tensor", lambda e: e.matmul(PXGx[:, h, 64:128], lhsT=tm[:, cg, h, 0, :], rhs=Tf, start=True, stop=True), [tm, LV[p][6].k("T")], [PXGx.k("g")])
                f.op("tensor", lambda e: e.matmul(PXGx[:, h, 128:192], lhsT=Tf, rhs=XT0[p][:, h, :], start=True, stop=True), [XT0[p], LV[p][6].k("T")], [PXGx.k("g")])
        st.append(pf)

        def pe():
            f.op("scalar", lambda e: e.activation(out=GX[p][:], in_=PXGx[:, :, 64:192], func=AF.Copy), [PXGx.k("g")], [GX[p]])
        st.append(pe)
        return st

    def seq_stages(c):
        p = c % NP
        q2 = c % 2
        g, cg = divmod(c, GC)
        b = g % NB
        bk, ar, tm, rf, yb = BK[b], AR[b], TM[b], RF[b], YB[b]
        S0, S1 = S[q2], S[1 - q2]
        UTq = UT[q2]
        PU, PS, PY = PU_ap, PS_ap, PY_ap
        st = []

        def s1():
            for h in range(2):
                f.op("tensor", lambda e: e.matmul(PU[:, h, :], lhsT=GX[p][:, h, 0:64], rhs=S0[:, h, :], start=True, stop=False), [GX[p], S0], [PUk])
                f.op("tensor", lambda e: e.matmul(PU[:, h, :], lhsT=identf, rhs=GX[p][:, h, 64:128], start=False, stop=True), [GX[p], idf2], [PUk])
        st.append(s1)

        def s2():
            f.op("scalar", lambda e: e.activation(out=UTq[:], in_=PU, func=AF.Copy), [PUk], [UTq])
        st.append(s2)

        def s3():
            for h in range(2):
                f.op("tensor", lambda e: e.matmul(PS[:, h, :], lhsT=tm[:, cg, h, 1, :], rhs=UTq[:, h, :], start=True, stop=False), [tm, UTq], [PSk])
                f.op("tensor", lambda e: e.matmul(PS[:, h, :], lhsT=tm[:, cg, h, 2, :], rhs=tm[:, cg, h, 3, :], start=False, stop=True), [tm], [PSk])
            for h in range(2):
                f.op("tensor", lambda e: e.matmul(PY[:, h, :], lhsT=rf[:, cg, h, :], rhs=S0[:, h, :], start=True, stop=False), [rf, S0], [PYk])
                f.op("tensor", lambda e: e.matmul(PY[:, h, :], lhsT=Ms[p][:, h, 192:256], rhs=tm[:, cg, h, 3, :], start=False, stop=False), [Ms[p].k("m"), tm], [PYk])
                f.op("tensor", lambda e: e.matmul(PY[:, h, :], lhsT=Ms[p][:, h, 64:128], rhs=UTq[:, h, :], start=False, stop=True), [Ms[p].k("m"), UTq], [PYk])
        st.append(s3)

        def s4():
            for h in range(2):
                f.op("vector", lambda e: e.scalar_tensor_tensor(out=S1[:, h, :], in0=S0[:, h, :], scalar=wcs[:, c, h:h + 1], in1=PS[:, h, :], op0=ALU.mult, op1=ALU.add), [S0, wcs, PSk], [S1])
            f.op("scalar", lambda e: e.activation(out=yb[:, cg, :, :], in_=PY, func=AF.Copy), [PYk], [yb.k(cg)])
            if cg == GC - 1 or DEBUG:
                f.dma(o_y[g], yb[:].rearrange("p c h i -> p (c h i)"), o_y, yb)
        st.append(s4)
        return st

    load_group(0)
    if ng > 1:
        load_group(1)

    def lockstep(a, b):
        out = []
        for i in range(max(len(a), len(b))):
            if i < len(a):
                out.append(a[i])
            if i < len(b):
                out.append(b[i])
        return out

    for s_ in lockstep(pre_stages(0), pre_stages(1) if nch > 1 else []):
        s_()
    for c in range(0, nch, 2):
        g, cg = divmod(c, GC)
        if DEBUG and c >= 1:
            break
        a = []
        if not DEBUG:
            a = lockstep(pre_stages(c + 2) if c + 2 < nch else [], pre_stages(c + 3) if c + 3 < nch else [])
        bq = seq_stages(c) + (seq_stages(c + 1) if c + 1 < nch and not DEBUG else [])
        n = max(len(a), len(bq))
        ia = ib = 0
        for i in range(n):
            want_a = (i + 1) * len(a) // n
            while ia < want_a:
                a[ia](); ia += 1
            want_b = (i + 1) * len(bq) // n
            while ib < want_b:
                bq[ib](); ib += 1
        if (c + 1) % GC == GC - 1 and g + 2 < ng:
            load_group(g + 2)
    if DEBUG:
        dbg = f.dram("o_dbg", [64, 2 * (320 + 192 * 7 + 128 + 64)], F32, "ExternalOutput")
        db = f.sb([64, 2, 320 + 192 * 7 + 128 + 64], F32, "dbgs")
        f.op("vector", lambda e: e.tensor_copy(out=db[:, :, 0:320], in_=Ms[0][:]), [Ms[0]], [db])
        for k in range(7):
            f.op("vector", lambda e: e.tensor_copy(out=db[:, :, 320 + 192 * k:320 + 192 * (k + 1)], in_=LV[0][k][:]), [LV[0][k]], [db])
        o = 320 + 192 * 7
        f.op("vector", lambda e: e.tensor_copy(out=db[:, :, o:o + 128], in_=GX[0][:]), [GX[0]], [db])
        f.op("vector", lambda e: e.tensor_copy(out=db[:, :, o + 128:o + 192], in_=UT[0][:]), [UT[0]], [db])
        f.dma(dbg[:], db[:].rearrange("p h c -> p (h c)"), dbg, db)
        f.final_wait([dbg])
    f.final_wait([o_y])
    return f.build()


def consts_B():
    m = np.zeros((64, 2, 320), np.float32)
    s = np.arange(64)[:, None]; t = np.arange(64)[None, :]
    su = (s < t).astype(np.float32); iu = (s <= t).astype(np.float32)
    for h in range(2):
        m[:, h, 0:64] = su; m[:, h, 64:128] = iu; m[:, h, 128:192] = su; m[:, h, 192:256] = iu
        m[:, h, 256:320] = (t < s).astype(np.float32)
    idm = np.concatenate([np.eye(64, dtype=np.float32)] * 2, axis=1)
    return m.reshape(64, 640), idm


def inputs_B(obf, ort, owc, nch=NCH):
    ng = nch // GC
    T = nch * 64
    mask, idm = consts_B()
    maps = []
    for c in range(8):
        cs = slice(128 * c, 128 * c + 128)
        fm = lambda a: a[cs, :T].reshape(2, 64, ng, GC, 64)
        At, Bt, Kt, Bh, Kh, V = [fm(obf[q]) for q in range(6)]
        Rt = fm(ort)
        d_bk = np.stack([Bt, Kt], axis=0).transpose(3, 2, 4, 1, 0, 5)
        d_at = At.transpose(2, 1, 3, 0, 4)
        d_rf = Rt.transpose(2, 1, 3, 0, 4)
        tmq = np.stack([At, Bh, Kh, V], axis=0)
        d_tm = tmq.transpose(3, 5, 4, 1, 0, 2)
        d_wc = owc[cs, :nch].reshape(2, 64, nch).transpose(1, 2, 0)
        maps.append({"d_bk": np.ascontiguousarray(d_bk).reshape(ng, 64, -1), "d_at": np.ascontiguousarray(d_at).reshape(ng, 64, -1),
                     "d_rf": np.ascontiguousarray(d_rf).reshape(ng, 64, -1), "d_tm": np.ascontiguousarray(d_tm).reshape(ng, 64, -1),
                     "d_wc": np.ascontiguousarray(d_wc).reshape(64, -1), "d_mask": mask, "d_id": idm})
    return maps


def gather_B(results, nch=NCH):
    ng = nch // GC
    ys = []
    for r in results:
        y = np.asarray(r["o_y"]).reshape(ng, 64, GC, 2, 64)
        ys.append(y.transpose(0, 2, 1, 3, 4).reshape(nch * 64, 128))
    return np.concatenate(ys, axis=1)


RMS_EPS = 1e-6
GN_EPS = 64e-5
ST = 512


class Ctx:
    pass


def setup_common(f, c, cst_dram):
    cs = f.sb([128, 384], F32, "cs")
    f.dma(cs[:], cst_dram[:], cs, cst_dram)
    c.identf = cs
    c.bones = f.sb([128, 128], BF16, "bones")
    c.ones = f.sb([128, 128], BF16, "onesb")
    f.op("vector", lambda e: e.tensor_copy(out=c.bones[:], in_=cs[:, 128:256]), [cs], [c.bones])
    f.op("vector", lambda e: e.tensor_copy(out=c.ones[:], in_=cs[:, 256:384]), [cs], [c.ones])
    c.cs = cs
    c.PA = [f.ps([128, 512], F32, f"PA{i}") for i in range(4)]
    c.PB = [f.ps([128, 512], F32, f"PB{i}") for i in range(2)]
    c.PC = [f.ps([128, 512], F32, f"PC{i}") for i in range(2)]
    c.ia = c.ib = c.ic = 0
    c.tmpi = 0
    c.tmps = {}


def nxt(c, pool):
    if pool == "A":
        c.ia += 1; return c.PA[c.ia % 4]
    if pool == "B":
        c.ib += 1; return c.PB[c.ib % 2]
    c.ic += 1; return c.PC[c.ic % 2]


def tmp(f, c, name, dt=F32, n=2, shape=(128, 512)):
    key = (name, dt)
    if key not in c.tmps:
        c.tmps[key] = [[f.sb(list(shape), dt, f"t_{name}_{i}") for i in range(n)], 0]
    lst = c.tmps[key]
    lst[1] += 1
    return lst[0][lst[1] % n]


def rmsnorm_fm(f, c, hT, pv, gcol0, uT, uF=None, eps=RMS_EPS):
    P = nxt(c, "C")
    for k in range(8):
        sq = tmp(f, c, "sq", BF16, 3)
        f.op("scalar", lambda e: e.activation(out=sq[:], in_=hT[:, k, :], func=AF.Square), [hT.k(k)], [sq])
        f.op("tensor", lambda e: e.matmul(P[:], lhsT=c.ones[:], rhs=sq[:], start=(k == 0), stop=(k == 7)), [c.ones, sq], [P])
    rs = tmp(f, c, "rs")
    f.op("scalar", lambda e: e.activation(out=rs[:], in_=P[:], func=AF.Sqrt, scale=1.0 / 1024, bias=eps), [P], [rs])
    f.op("vector", lambda e: e.reciprocal(out=rs[:], in_=rs[:]), [rs], [rs])
    for k in range(8):
        f.op("vector", lambda e: e.scalar_tensor_tensor(out=uT[:, k, :], in0=hT[:, k, :], scalar=pv[:, gcol0 + k:gcol0 + k + 1], in1=rs[:], op0=ALU.mult, op1=ALU.mult), [hT.k(k), pv, rs], [uT.k(k)])
        if uF is not None:
            f.op("gpsimd", lambda e: e.tensor_scalar(out=uF[:, k, :], in0=hT[:, k, :], scalar1=pv[:, gcol0 + k:gcol0 + k + 1], scalar2=None, op0=ALU.mult), [hT.k(k), pv], [uF.k(k)])
            f.op("gpsimd", lambda e: e.tensor_tensor(out=uF[:, k, :], in0=uF[:, k, :], in1=rs[:], op=ALU.mult), [uF.k(k), rs], [uF.k(k)])


def linear_res(f, c, w_sb, zT, hT, nk=8):
    for ec in range(8):
        P = nxt(c, "B")
        for k in range(nk):
            f.op("tensor", lambda e: e.matmul(P[:], lhsT=w_sb[:, k, ec * 128:(ec + 1) * 128], rhs=zT[:, k, :], start=(k == 0), stop=(k == nk - 1)), [w_sb, zT.k(k)], [P])
        f.op("vector", lambda e: e.tensor_tensor(out=hT[:, ec, :], in0=hT[:, ec, :], in1=P[:], op=ALU.add), [hT.k(ec), P], [hT.k(ec)])


def ffn_pass(f, c, uT, aT, wgu_dram, wd_dram, wgu_bufs, wd_bufs, out_fn, gate_b=None, FG=2, EG=2):
    NF = 28
    for g in range(NF // FG):
        c.wgi = getattr(c, "wgi", 0) + 1
        W = wgu_bufs[c.wgi % len(wgu_bufs)]
        for half in range(2):
            col0 = half * 3584 + g * FG * 128
            f.dma(W[:, :, half, :], wgu_dram[:, col0:col0 + FG * 128].rearrange("(k p) e -> p k e", p=128), W.k(half), wgu_dram_buf(c, wgu_dram), q="gpsimd")
        for j in range(FG):
            fc = g * FG + j
            Pg = nxt(c, "A"); Pu = nxt(c, "A")
            for k in range(8):
                f.op("tensor", lambda e: e.matmul(Pg[:], lhsT=W[:, k, 0, j * 128:(j + 1) * 128], rhs=uT[:, k, :], start=(k == 0), stop=(k == 7)), [W.k(0), uT.k(k)], [Pg])
            for k in range(8):
                f.op("tensor", lambda e: e.matmul(Pu[:], lhsT=W[:, k, 1, j * 128:(j + 1) * 128], rhs=uT[:, k, :], start=(k == 0), stop=(k == 7)), [W.k(1), uT.k(k)], [Pu])
            sg = tmp(f, c, "sg", F32, 3)
            f.op("scalar", lambda e: e.activation(out=sg[:], in_=Pg[:], func=AF.Silu), [Pg], [sg])
            if gate_b is None:
                f.op("vector", lambda e: e.tensor_tensor(out=aT[:, fc, :], in0=sg[:], in1=Pu[:], op=ALU.mult), [sg, Pu], [aT.k(fc)])
            else:
                sg2 = tmp(f, c, "sg2", F32, 3)
                f.op("vector", lambda e: e.tensor_tensor(out=sg2[:], in0=sg[:], in1=Pu[:], op=ALU.mult), [sg, Pu], [sg2])
                f.op("gpsimd", lambda e: e.tensor_tensor(out=aT[:, fc, :], in0=sg2[:], in1=gate_b[:], op=ALU.mult), [sg2, gate_b], [aT.k(fc)])
    for g in range(8 // EG):
        c.wdi = getattr(c, "wdi", 0) + 1
        W = wd_bufs[c.wdi % len(wd_bufs)]
        for q4 in range(4):
            f.dma(W[:, q4 * 7:(q4 + 1) * 7, :], wd_dram[q4 * 896:(q4 + 1) * 896, g * EG * 128:(g + 1) * EG * 128].rearrange("(k p) e -> p k e", p=128), W.k(q4), wgu_dram_buf(c, wd_dram), q="gpsimd")
        for j in range(EG):
            ec = g * EG + j
            P = nxt(c, "B")
            for fc in range(NF):
                f.op("tensor", lambda e: e.matmul(P[:], lhsT=W[:, fc, j * 128:(j + 1) * 128], rhs=aT[:, fc, :], start=(fc == 0), stop=(fc == NF - 1)), [W.k(fc // 7), aT.k(fc)], [P])
            out_fn(ec, P)


_dram_bufs = {}


def wgu_dram_buf(c, ap):
    return c.wdram


def consts_CE():
    cst = np.zeros((128, 384), np.float32)
    cst[:, 0:128] = np.eye(128)
    blk = np.arange(128) // 64
    cst[:, 128:256] = (blk[:, None] == blk[None, :]).astype(np.float32)
    cst[:, 256:384] = 1.0
    return cst


def fmcols(v):
    return np.ascontiguousarray(np.asarray(v, np.float32).reshape(-1, 128).T)


def ffn_ws(f, c, uTs, hTs, w_gu_ap, w_dn_ap, wgu_bufs, wd_bufs, aT_bufs, gbs=None, FG=4):
    NTT = len(uTs)
    for g in range(28 // FG):
        c.wgi = getattr(c, "wgi", 0) + 1
        W = wgu_bufs[c.wgi % 2]; WD = wd_bufs[c.wgi % 2]
        for hf in range(2):
            col0 = hf * 3584 + g * FG * 128
            f.dma(W[:, :, hf, :], w_gu_ap[:, col0:col0 + FG * 128].rearrange("(k p) e -> p k e", p=128), W.k(hf), c.wdram, q="gpsimd")
        f.dma(WD[:], w_dn_ap[g * FG * 128:(g + 1) * FG * 128, :].rearrange("(k p) e -> p k e", p=128), WD, c.wdram, q="gpsimd")
        for tt in range(NTT):
            c.ai = getattr(c, "ai", 0) + 1
            A = aT_bufs[c.ai % 2]
            for j in range(FG):
                Pg = nxt(c, "A"); Pu = nxt(c, "A")
                for k in range(8):
                    f.op("tensor", lambda e: e.matmul(Pg[:], lhsT=W[:, k, 0, j * 128:(j + 1) * 128], rhs=uTs[tt][:, k, :], start=(k == 0), stop=(k == 7)), [W.k(0), uTs[tt].k(k)], [Pg])
                for k in range(8):
                    f.op("tensor", lambda e: e.matmul(Pu[:], lhsT=W[:, k, 1, j * 128:(j + 1) * 128], rhs=uTs[tt][:, k, :], start=(k == 0), stop=(k == 7)), [W.k(1), uTs[tt].k(k)], [Pu])
                sg = tmp(f, c, "sg", F32, 3)
                f.op("scalar", lambda e: e.activation(out=sg[:], in_=Pg[:], func=AF.Silu), [Pg], [sg])
                if gbs is None:
                    f.op("vector", lambda e: e.tensor_tensor(out=A[:, j, :], in0=sg[:], in1=Pu[:], op=ALU.mult), [sg, Pu], [A.k(j)])
                else:
                    sg2 = tmp(f, c, "sg2", F32, 3)
                    f.op("vector", lambda e: e.tensor_tensor(out=sg2[:], in0=sg[:], in1=Pu[:], op=ALU.mult), [sg, Pu], [sg2])
                    f.op("vector", lambda e: e.tensor_tensor(out=A[:, j, :], in0=sg2[:], in1=gbs[tt][:], op=ALU.mult), [sg2, gbs[tt]], [A.k(j)])
            for ec in range(8):
                P = nxt(c, "B")
                for j in range(FG):
                    f.op("tensor", lambda e: e.matmul(P[:], lhsT=WD[:, j, ec * 128:(ec + 1) * 128], rhs=A[:, j, :], start=(j == 0), stop=(j == FG - 1)), [WD, A.k(j)], [P])
                f.op("vector", lambda e: e.tensor_tensor(out=hTs[tt][:, ec, :], in0=hTs[tt][:, ec, :], in1=P[:], op=ALU.add), [hTs[tt].k(ec), P], [hTs[tt].k(ec)])


PVC = {"gn_g": 0, "gn_b": 8, "g1": 16, "g2": 24, "qg": 32, "kg": 33, "bf": 34}
NPC = 35


def build_C():
    nc = bass.Bass("TRN2", target_bir_lowering=False)
    f = FW(nc)
    c = Ctx()
    xT = f.dram("xT", [1024, 2048], F32, "ExternalInput")
    yT = f.dram("yT", [1024, 2048], F32, "ExternalInput")
    gb = f.dram("gb", [2, 1024, 2048], BF16, "ExternalInput")
    pvd = f.dram("pv", [128, NPC], F32, "ExternalInput")
    cst = f.dram("cst", [128, 384], F32, "ExternalInput")
    w_o = f.dram("w_o", [1024, 1024], F32, "ExternalInput")
    w_gu = f.dram("w_gu", [1024, 7168], F32, "ExternalInput")
    w_dn = f.dram("w_dn", [3584, 1024], F32, "ExternalInput")
    w_in = f.dram("w_in", [1024, 4112], F32, "ExternalInput")
    o_h = f.dram("o_h", [1024, 2048], F32, "ExternalOutput")
    o_c = f.dram("o_c", [4, 1024, 2048], BF16, "ExternalOutput")
    o_lf = f.dram("o_lf", [16, 2048], F32, "ExternalOutput")
    c.wdram = f.dram("wdummy", [1, 1], F32, "Internal")
    setup_common(f, c, cst)
    pv = f.sb([128, NPC], F32, "pvs")
    f.dma(pv[:], pvd[:], pv, pvd)
    wo = f.sb([128, 8, 1024], BF16, "wo")
    for k4 in range(0, 8, 4):
        f.dma(wo[:, k4:k4 + 4, :], w_o[k4 * 128:(k4 + 4) * 128, :].rearrange("(k p) e -> p k e", p=128), wo.k(k4), w_o, q="gpsimd")
    wfl = f.sb([128, 8, 16], BF16, "wfl")
    f.dma(wfl[:], w_in[:, 4096:4112].rearrange("(k p) e -> p k e", p=128), wfl, w_in, q="gpsimd")
    NTT = 2
    hTs = [f.sb([128, 8, 512], F32, f"hT{i}") for i in range(NTT)]
    uTs = [f.sb([128, 8, 512], BF16, f"uT{i}") for i in range(NTT)]
    zT = f.sb([128, 8, 512], BF16, "zT")
    aT = [f.sb([128, 4, 512], BF16, f"aTg{i}") for i in range(2)]
    wgu_bufs = [f.sb([128, 8, 2, 512], BF16, f"wgu{i}") for i in range(2)]
    wd_bufs = [f.sb([128, 4, 1024], BF16, f"wd{i}") for i in range(2)]
    oc = [f.sb([128, 512], BF16, f"oc{i}") for i in range(3)]
    oci = [0]
    lf = f.sb([16, 512], F32, "lf")
    for half in range(2):
        for tt in range(NTT):
            hT = hTs[tt]
            ts = slice(1024 * half + 512 * tt, 1024 * half + 512 * tt + 512)
            f.dma(hT[:], xT[:, ts].rearrange("(k p) t -> p k t", p=128), hT, xT)
            for ec in range(8):
                es = slice(ec * 128, (ec + 1) * 128)
                y = tmp(f, c, "y"); g2 = tmp(f, c, "gbt", BF16, 2, (128, 2, 512))
                f.dma(y[:], yT[es, ts], y, yT)
                f.dma(g2[:], gb[:, es, ts].rearrange("q p t -> p q t"), g2, gb)
                yb = tmp(f, c, "yb", BF16)
                f.op("scalar", lambda e: e.activation(out=yb[:], in_=y[:], func=AF.Copy), [y], [yb])
                P = nxt(c, "C")
                f.op("tensor", lambda e: e.matmul(P[:], lhsT=c.bones[:], rhs=yb[:], start=True, stop=True), [c.bones, yb], [P])
                yc = tmp(f, c, "yc")
                f.op("vector", lambda e: e.scalar_tensor_tensor(out=yc[:], in0=P[:], scalar=-1.0 / 64, in1=y[:], op0=ALU.mult, op1=ALU.add), [P, y], [yc])
                sq = tmp(f, c, "sq", BF16, 3)
                f.op("scalar", lambda e: e.activation(out=sq[:], in_=yc[:], func=AF.Square), [yc], [sq])
                P2 = nxt(c, "C")
                f.op("tensor", lambda e: e.matmul(P2[:], lhsT=c.bones[:], rhs=sq[:], start=True, stop=True), [c.bones, sq], [P2])
                rs = tmp(f, c, "rs")
                f.op("scalar", lambda e: e.activation(out=rs[:], in_=P2[:], func=AF.Sqrt, scale=1.0 / 64, bias=GN_EPS), [P2], [rs])
                f.op("vector", lambda e: e.reciprocal(out=rs[:], in_=rs[:]), [rs], [rs])
                f.op("vector", lambda e: e.tensor_tensor(out=yc[:], in0=yc[:], in1=rs[:], op=ALU.mult), [yc, rs], [yc])
                f.op("vector", lambda e: e.tensor_scalar(out=yc[:], in0=yc[:], scalar1=pv[:, PVC["gn_g"] + ec:PVC["gn_g"] + ec + 1], scalar2=pv[:, PVC["gn_b"] + ec:PVC["gn_b"] + ec + 1], op0=ALU.mult, op1=ALU.add), [yc, pv], [yc])
                f.op("vector", lambda e: e.tensor_tensor(out=yc[:], in0=yc[:], in1=g2[:, 1, :], op=ALU.add), [yc, g2], [yc])
                f.op("vector", lambda e: e.tensor_tensor(out=zT[:, ec, :], in0=yc[:], in1=g2[:, 0, :], op=ALU.mult), [yc, g2], [zT.k(ec)])
            linear_res(f, c, wo, zT, hT)
            rmsnorm_fm(f, c, hT, pv, PVC["g1"], uTs[tt])
        ffn_ws(f, c, uTs, hTs, w_gu[:], w_dn[:], wgu_bufs, wd_bufs, aT)
        for tt in range(NTT):
            ts = slice(1024 * half + 512 * tt, 1024 * half + 512 * tt + 512)
            f.dma(o_h[:, ts].rearrange("(k p) t -> p k t", p=128), hTs[tt][:], o_h, hTs[tt])
            rmsnorm_fm(f, c, hTs[tt], pv, PVC["g2"], uTs[tt])
        for g in range(8):
            c.wgi += 1
            W = wgu_bufs[c.wgi % 2]
            Wv = W[:].rearrange("p k h e -> p k (h e)")
            f.dma(Wv[:, :, 0:512], w_in[:, g * 512:(g + 1) * 512].rearrange("(k p) e -> p k e", p=128), W, c.wdram, q="gpsimd")
            for tt in range(NTT):
                ts = slice(1024 * half + 512 * tt, 1024 * half + 512 * tt + 512)
                uT = uTs[tt]
                for j in range(4):
                    e32 = g * 4 + j
                    kind, ec = divmod(e32, 8)
                    P = nxt(c, "A")
                    for k in range(8):
                        f.op("tensor", lambda e: e.matmul(P[:], lhsT=Wv[:, k, j * 128:(j + 1) * 128], rhs=uT[:, k, :], start=(k == 0), stop=(k == 7)), [W, uT.k(k)], [P])
                    oci[0] += 1
                    O = oc[oci[0] % 3]
                    if kind in (0, 1):
                        qs = tmp(f, c, "qs")
                        f.op("scalar", lambda e: e.activation(out=qs[:], in_=P[:], func=AF.Copy), [P], [qs])
                        sq = tmp(f, c, "sq", BF16, 3)
                        f.op("scalar", lambda e: e.activation(out=sq[:], in_=qs[:], func=AF.Square), [qs], [sq])
                        P2 = nxt(c, "C")
                        f.op("tensor", lambda e: e.matmul(P2[:], lhsT=c.bones[:], rhs=sq[:], start=True, stop=True), [c.bones, sq], [P2])
                        rs = tmp(f, c, "rs")
                        if kind == 0:
                            f.op("scalar", lambda e: e.activation(out=rs[:], in_=P2[:], func=AF.Sqrt, scale=1.0, bias=64 * RMS_EPS), [P2], [rs])
                        else:
                            f.op("scalar", lambda e: e.activation(out=rs[:], in_=P2[:], func=AF.Sqrt, scale=1.0 / 64, bias=RMS_EPS), [P2], [rs])
                        f.op("vector", lambda e: e.reciprocal(out=rs[:], in_=rs[:]), [rs], [rs])
                        gc = PVC["qg"] if kind == 0 else PVC["kg"]
                        f.op("vector", lambda e: e.scalar_tensor_tensor(out=O[:], in0=qs[:], scalar=pv[:, gc:gc + 1], in1=rs[:], op0=ALU.mult, op1=ALU.mult), [qs, pv, rs], [O])
                    elif kind == 2:
                        f.op("scalar", lambda e: e.activation(out=O[:], in_=P[:], func=AF.Copy), [P], [O])
                    else:
                        f.op("scalar", lambda e: e.activation(out=O[:], in_=P[:], func=AF.Sigmoid), [P], [O])
                    f.dma(o_c[kind, ec * 128:(ec + 1) * 128, ts], O[:], o_c, O)
        for tt in range(NTT):
            ts = slice(1024 * half + 512 * tt, 1024 * half + 512 * tt + 512)
            uT = uTs[tt]
            P = nxt(c, "C")
            for k in range(8):
                f.op("tensor", lambda e: e.matmul(P[0:16, :], lhsT=wfl[:, k, :], rhs=uT[:, k, :], start=(k == 0), stop=(k == 7)), [wfl, uT.k(k)], [P])
            f.op("vector", lambda e: e.tensor_scalar(out=lf[:], in0=P[0:16, :], scalar1=pv[0:16, PVC["bf"]:PVC["bf"] + 1], scalar2=None, op0=ALU.add), [P, pv], [lf])
            f.op("scalar", lambda e: e.activation(out=lf[:], in_=lf[:], func=AF.Exp, scale=-1.0), [lf], [lf])
            f.op("scalar", lambda e: e.activation(out=lf[:], in_=lf[:], func=AF.Ln, bias=1.0), [lf], [lf])
            f.op("vector", lambda e: e.tensor_scalar(out=lf[:], in0=lf[:], scalar1=-1.0, scalar2=None, op0=ALU.mult), [lf], [lf])
            f.dma(o_lf[:, ts], lf[:], o_lf, lf)
    f.final_wait([o_h, o_c, o_lf])
    return f.build()


def inputs_C(inp, yfull, obfA):
    x = inp["x"][0]
    pvv = np.zeros((128, NPC), np.float32)
    pvv[:, 0:8] = fmcols(inp["rwkv_gn_g"][0]); pvv[:, 8:16] = fmcols(inp["rwkv_gn_b"][0])
    pvv[:, 16:24] = fmcols(inp["norm_g"][0, 1]); pvv[:, 24:32] = fmcols(inp["norm_g"][1, 0])
    pvv[:, 32] = np.tile(inp["fox_q_gain"][0], 2); pvv[:, 33] = np.tile(inp["fox_k_gain"][0], 2)
    pvv[0:16, 34] = inp["fox_b_f"][0]
    cst = consts_CE()
    maps = []
    for c in range(8):
        ts = slice(2048 * c, 2048 * c + 2048)
        maps.append({"xT": np.ascontiguousarray(x[ts].T), "yT": np.ascontiguousarray(yfull[ts].T),
                     "gb": np.ascontiguousarray(obfA[c][6:8]), "pv": pvv, "cst": cst,
                     "w_o": inp["rwkv_w_o"][0], "w_gu": inp["ffn_w_gu"][0], "w_dn": inp["ffn_w_down"][0], "w_in": inp["fox_w_in"][0]})
    return maps


PVE = {"g3": 0, "gf": 8}
NPE = 16
NEXP = 8
NTE = 1024
FGE = 4


def build_E():
    nc = bass.Bass("TRN2", target_bir_lowering=False)
    f = FW(nc)
    c = Ctx()
    hTd = f.dram("hT", [1024, 2048], F32, "ExternalInput")
    oTd = f.dram("oT", [1024, 2048], BF16, "ExternalInput")
    pvd = f.dram("pv", [128, NPE], F32, "ExternalInput")
    cst = f.dram("cst", [128, 384], F32, "ExternalInput")
    seld = f.dram("sele", [8, 8 * 128], F32, "ExternalInput")
    w_o = f.dram("w_o", [1024, 1024], F32, "ExternalInput")
    w_r = f.dram("w_r", [1024, 8], F32, "ExternalInput")
    w_gu = f.dram("w_gu", [8, 1024, 7168], F32, "ExternalInput")
    w_dn = f.dram("w_dn", [8, 3584, 1024], F32, "ExternalInput")
    o_out = f.dram("o_out", [1024, 2048], F32, "ExternalOutput")
    c.wdram = f.dram("wdummy", [1, 1], F32, "Internal")
    setup_common(f, c, cst)
    pv = f.sb([128, NPE], F32, "pvs")
    f.dma(pv[:], pvd[:], pv, pvd)
    sele = f.sb([8, 8, 128], F32, "sele")
    f.dma(sele[:].rearrange("k e m -> k (e m)"), seld[:], sele, seld)
    wo = f.sb([128, 8, 1024], BF16, "wo")
    for k4 in range(0, 8, 4):
        f.dma(wo[:, k4:k4 + 4, :], w_o[k4 * 128:(k4 + 4) * 128, :].rearrange("(k p) e -> p k e", p=128), wo.k(k4), w_o, q="gpsimd")
    wr = f.sb([128, 8, 8], F32, "wr")
    f.dma(wr[:], w_r[:].rearrange("(k p) e -> p k e", p=128), wr, w_r)
    NTT = NTE // 512
    hT = [f.sb([128, 8, 512], F32, f"hT{i}") for i in range(NTT)]
    uT = [f.sb([128, 8, 512], BF16, f"uT{i}") for i in range(NTT)]
    gTs = [f.sb([8, 512], F32, f"gTs{i}") for i in range(NTT)]
    zT = f.sb([128, 8, 512], BF16, "zT")
    uF = f.sb([128, 8, 512], F32, "uF")
    wgu_bufs = [f.sb([128, 8, 2, FGE * 128], BF16, f"wgu{i}") for i in range(2)]
    wd_bufs = [f.sb([128, FGE, 1024], BF16, f"wd{i}") for i in range(2)]
    aT = [f.sb([128, FGE, 512], BF16, f"aTg{i}") for i in range(2)]
    gb = [f.sb([128, 512], F32, f"gb{i}") for i in range(NTT)]
    lg = f.sb([8, 512], F32, "lg")
    lt = f.sb([128, 4, 8], F32, "lt")
    gts = f.sb([128, 4, 8], F32, "gts")
    sm = {n: f.sb([128, 8], F32, "sm_" + n) for n in ("eq", "l2", "sel", "ex")}
    sc = {n: f.sb([128, 4], F32, "sc_" + n) for n in ("m1", "nm1", "m2", "sum")}
    wgi = 0
    ai = 0
    for half in range(2048 // NTE):
        for tt in range(NTT):
            ts = slice(half * NTE + 512 * tt, half * NTE + 512 * tt + 512)
            H, U = hT[tt], uT[tt]
            f.dma(H[:], hTd[:, ts].rearrange("(k p) t -> p k t", p=128), H, hTd)
            f.dma(zT[:], oTd[:, ts].rearrange("(k p) t -> p k t", p=128), zT, oTd)
            linear_res(f, c, wo, zT, H)
            rmsnorm_fm(f, c, H, pv, PVE["g3"], U, uF)
            P = nxt(c, "C")
            for k in range(8):
                f.op("tensor", lambda e: e.matmul(P[0:8, :], lhsT=wr[:, k, :], rhs=uF[:, k, :], start=(k == 0), stop=(k == 7)), [wr, uF.k(k)], [P])
            f.op("vector", lambda e: e.tensor_copy(out=lg[:], in_=P[0:8, :]), [P], [lg])
            P2 = nxt(c, "C")
            for j in range(4):
                f.op("tensor", lambda e: e.transpose(out=P2[:, j * 8:(j + 1) * 8], in_=lg[:, j * 128:(j + 1) * 128], identity=c.cs[0:8, 0:8]), [lg, c.cs], [P2])
            f.op("vector", lambda e: e.tensor_copy(out=lt[:].rearrange("p j e -> p (j e)"), in_=P2[:, 0:32]), [P2], [lt])
            f.op("vector", lambda e: e.tensor_reduce(out=sc["m1"][:], in_=lt[:], axis=AX.X, op=ALU.max), [lt], [sc["m1"]])
            f.op("vector", lambda e: e.tensor_scalar(out=sc["nm1"][:], in0=sc["m1"][:], scalar1=-1.0, scalar2=None, op0=ALU.mult), [sc["m1"]], [sc["nm1"]])
            for j in range(4):
                L = lt[:, j, :]
                f.op("vector", lambda e: e.tensor_scalar(out=sm["eq"][:], in0=L, scalar1=sc["m1"][:, j:j + 1], scalar2=None, op0=ALU.is_ge), [lt, sc["m1"]], [sm["eq"]])
                f.op("vector", lambda e: e.scalar_tensor_tensor(out=sm["l2"][:], in0=sm["eq"][:], scalar=-1e30, in1=L, op0=ALU.mult, op1=ALU.add), [sm["eq"], lt], [sm["l2"]])
                f.op("vector", lambda e: e.tensor_reduce(out=sc["m2"][:, j:j + 1], in_=sm["l2"][:], axis=AX.X, op=ALU.max), [sm["l2"]], [sc["m2"]])
                f.op("vector", lambda e: e.tensor_scalar(out=sm["sel"][:], in0=L, scalar1=sc["m2"][:, j:j + 1], scalar2=None, op0=ALU.is_ge), [lt, sc["m2"]], [sm["sel"]])
                f.op("scalar", lambda e: e.activation(out=sm["ex"][:], in_=L, func=AF.Exp, bias=sc["nm1"][:, j:j + 1]), [lt, sc["nm1"]], [sm["ex"]])
                f.op("vector", lambda e: e.tensor_tensor(out=sm["ex"][:], in0=sm["ex"][:], in1=sm["sel"][:], op=ALU.mult), [sm["ex"], sm["sel"]], [sm["ex"]])
                f.op("vector", lambda e: e.tensor_reduce(out=sc["sum"][:, j:j + 1], in_=sm["ex"][:], axis=AX.X, op=ALU.add), [sm["ex"]], [sc["sum"]])
                f.op("vector", lambda e: e.reciprocal(out=sc["sum"][:, j:j + 1], in_=sc["sum"][:, j:j + 1]), [sc["sum"]], [sc["sum"]])
                f.op("vector", lambda e: e.tensor_scalar(out=gts[:, j, :], in0=sm["ex"][:], scalar1=sc["sum"][:, j:j + 1], scalar2=None, op0=ALU.mult), [sm["ex"], sc["sum"]], [gts])
            P3 = nxt(c, "C")
            for j in range(4):
                f.op("tensor", lambda e: e.transpose(out=P3[0:8, j * 128:(j + 1) * 128], in_=gts[:, j, :], identity=c.cs[:, 0:128]), [gts, c.cs], [P3])
            f.op("vector", lambda e: e.tensor_copy(out=gTs[tt][:], in_=P3[0:8, :]), [P3], [gTs[tt]])
        for ex in range(NEXP):
            for tt in range(NTT):
                P4 = nxt(c, "C")
                f.op("tensor", lambda e: e.matmul(P4[:], lhsT=sele[:, ex, :], rhs=gTs[tt][:], start=True, stop=True), [sele, gTs[tt]], [P4])
                f.op("scalar", lambda e: e.activation(out=gb[tt][:], in_=P4[:], func=AF.Copy), [P4], [gb[tt]])
            ffn_ws(f, c, uT, hT, w_gu[ex], w_dn[ex], wgu_bufs, wd_bufs, aT, gbs=gb, FG=FGE)
        for tt in range(NTT):
            ts = slice(half * NTE + 512 * tt, half * NTE + 512 * tt + 512)
            rmsnorm_fm(f, c, hT[tt], pv, PVE["gf"], uT[tt], uF)
            f.dma(o_out[:, ts].rearrange("(k p) t -> p k t", p=128), uF[:], o_out, uF)
    f.final_wait([o_out])
    return f.build()


def inputs_E(inp, hT_list, oT):
    pvv = np.zeros((128, NPE), np.float32)
    pvv[:, 0:8] = fmcols(inp["norm_g"][1, 1]); pvv[:, 8:16] = fmcols(inp["final_g"])
    cst = consts_CE()
    sele = np.zeros((8, 8, 128), np.float32)
    for e in range(8):
        sele[e, e, :] = 1.0
    maps = []
    for c in range(8):
        maps.append({"hT": hT_list[c], "oT": np.ascontiguousarray(oT[:, 2048 * c:2048 * c + 2048]), "pv": pvv, "cst": cst,
                     "sele": sele.reshape(8, 1024), "w_o": inp["fox_w_o"][0], "w_r": inp["moe_w_router"][0],
                     "w_gu": inp["moe_w_gu"][0], "w_dn": inp["moe_w_down"][0]})
    return maps


T_ALL = 16384


def build_D(T=T_ALL):
    nc = bass.Bass("TRN2", target_bir_lowering=False)
    f = FW(nc)
    NQ = T // 512
    NKB = T // 128
    NSEG = T // 2048
    qT = f.dram("qT", [2, 64, T], BF16, "ExternalInput")
    kT = f.dram("kT", [2, 64, T], BF16, "ExternalInput")
    vt = f.dram("vt", [2, 128, NKB * 65], BF16, "ExternalInput")
    og = f.dram("og", [2, 64, T], BF16, "ExternalInput")
    lfd = f.dram("lf", [2, T], F32, "ExternalInput")
    mkd = f.dram("mk", [128, 4 * 512], F32, "ExternalInput")
    sld = f.dram("sel", [65, 64], F32, "ExternalInput")
    o_o = f.dram("o_o", [2, 64, T], BF16, "ExternalOutput")

    mkf = f.sb([128, 4, 512], F32, "mkf")
    f.dma(mkf[:].rearrange("p m t -> p (m t)"), mkd[:], mkf, mkd)
    mk = f.sb([128, 4, 512], BF16, "mk")
    f.op("vector", lambda e: e.tensor_copy(out=mk[:], in_=mkf[:]), [mkf], [mk])
    sel = f.sb([65, 64], F32, "sels")
    f.dma(sel[:], sld[:], sel, sld)
    ones = f.sb([1, 2048], F32, "ones1")
    f.op("gpsimd", lambda e: e.memset(ones[:], 1.0), [], [ones])

    Qa = f.sb([70, T], BF16, "Qa")
    Ka = f.sb([70, T], BF16, "Ka")
    Vt = f.sb([128, NKB, 65], BF16, "Vt")
    lfs = [f.sb([1, 2048], F32, f"lfs{i}") for i in range(2)]
    cseg = [f.sb([1, 2048], F32, f"cseg{i}") for i in range(2)]
    c_d = f.dram("c_scr", [2, T], F32, "Internal")
    p_d = f.dram("p_scr", [2, 3, T], BF16, "Internal")
    c2d = [f.sb([128, T // 128], F32, f"c2d{i}") for i in range(2)]
    r2d = [f.sb([128, T // 128], F32, f"r2d{i}") for i in range(2)]
    parts2 = [f.sb([128, 3, T // 128], BF16, f"parts2_{i}") for i in range(2)]
    for h in range(2):
        for sg in range(NSEG):
            b = (h * NSEG + sg) % 2
            ss = slice(sg * 2048, (sg + 1) * 2048)
            f.dma(lfs[b][:], lfd[h:h + 1, ss], lfs[b], lfd)
            init = 0.0 if sg == 0 else cseg[1 - b][:, 2047:2048]
            rd = [ones, lfs[b]] + ([] if sg == 0 else [cseg[1 - b]])
            f.op("vector", lambda e: e.tensor_tensor_scan(out=cseg[b][:], data0=ones[:], data1=lfs[b][:], initial=init, op0=ALU.mult, op1=ALU.add), rd, [cseg[b]])
            f.dma(c_d[h:h + 1, ss], cseg[b][:], c_d, cseg[b])
        C2, R2, P2 = c2d[h], r2d[h], parts2[h]
        f.dma(C2[:], c_d[h].rearrange("(p c) -> p c", p=128), C2, c_d)
        f.op("vector", lambda e: e.tensor_copy(out=P2[:, 0, :], in_=C2[:]), [C2], [P2])
        f.op("vector", lambda e: e.tensor_tensor(out=R2[:], in0=C2[:], in1=P2[:, 0, :], op=ALU.subtract), [C2, P2], [R2])
        f.op("vector", lambda e: e.tensor_copy(out=P2[:, 1, :], in_=R2[:]), [R2], [P2])
        f.op("vector", lambda e: e.tensor_tensor(out=R2[:], in0=R2[:], in1=P2[:, 1, :], op=ALU.subtract), [R2, P2], [R2])
        f.op("vector", lambda e: e.tensor_copy(out=P2[:, 2, :], in_=R2[:]), [R2], [P2])
        for r in range(3):
            f.dma(p_d[h, r].rearrange("(p c) -> p c", p=128), P2[:, r, :], p_d, P2)
    PSs = [f.ps([128, 512], F32, f"PSs{i}") for i in range(5)]
    PO = [f.ps([65, 512], F32, f"PO{i}") for i in range(2)]
    PD = f.ps([64, 512], F32, "PD")
    PT = [f.sb([128, 512], BF16, f"PT{i}") for i in range(6)]
    Osb = [f.sb([65, 512], F32, f"Osb{i}") for i in range(2)]
    rden = f.sb([64, 512], F32, "rden")
    o1 = f.sb([64, 512], F32, "o1")
    ogt = [f.sb([64, 512], BF16, f"ogt{i}") for i in range(2)]
    o2 = [f.sb([64, 512], BF16, f"o2{i}") for i in range(2)]
    scl = [f.sb([128, 512], F32, f"scl{i}") for i in range(2)]
    ti = 0
    for h in range(2):
        f.dma(Qa[0:64, :], qT[h], Qa.k("top"), qT)
        f.dma(Ka[0:64, :], kT[h], Ka.k("top"), kT)
        f.dma(Vt[:].rearrange("p k d -> p (k d)"), vt[h], Vt, vt)
        f.op("gpsimd", lambda e: e.memset(Vt[:, :, 64:65], 1.0), [], [Vt])
        f.op("gpsimd", lambda e: e.memset(Qa[64:70, :], -1.0), [], [Qa.k("aug")])
        f.op("gpsimd", lambda e: e.memset(Ka[64:70, :], 1.0), [], [Ka.k("aug")])
        for r in range(3):
            f.dma(Qa[64 + r:65 + r, :], p_d[h, r:r + 1, :], Qa.k("aug"), p_d)
            f.dma(Ka[67 + r:68 + r, :], p_d[h, r:r + 1, :], Ka.k("aug"), p_d)
        tiles = [(qi, kb) for qi in range(NQ) for kb in range(4 * qi + 4)]
        LA = 4
        base = ti

        def emit_qk(i):
            qi, kb = tiles[i]
            qs = slice(qi * 512, (qi + 1) * 512)
            ps = PSs[(base + i) % 5]
            f.op("tensor", lambda e: e.matmul(ps[:], lhsT=Ka[0:70, kb * 128:(kb + 1) * 128], rhs=Qa[0:70, qs], start=True, stop=True), [Ka, Qa], [ps])

        def emit_rest(i):
            qi, kb = tiles[i]
            qs = slice(qi * 512, (qi + 1) * 512)
            nkb = 4 * qi + 4
            ps = PSs[(base + i) % 5]; pt = PT[(base + i) % 6]
            po = PO[qi % 2]
            if kb == 0:
                f.dma(ogt[qi % 2][:], og[h, :, qs], ogt[qi % 2], og)
            if kb >= 4 * qi:
                sc = scl[kb % 2]
                f.op("vector", lambda e: e.tensor_scalar(out=sc[:], in0=ps[:], scalar1=30.0, scalar2=None, op0=ALU.min), [ps], [sc])
                f.op("scalar", lambda e: e.activation(out=pt[:], in_=sc[:], func=AF.Exp), [sc], [pt])
                m = kb - 4 * qi
                eng = "vector" if m % 2 == 0 else "gpsimd"
                f.op(eng, lambda e: e.tensor_tensor(out=pt[:], in0=pt[:], in1=mk[:, m, :], op=ALU.mult), [pt, mk], [pt])
            else:
                f.op("scalar", lambda e: e.activation(out=pt[:], in_=ps[:], func=AF.Exp), [ps], [pt])
            f.op("tensor", lambda e: e.matmul(po[:], lhsT=Vt[:, kb, :], rhs=pt[:], start=(kb == 0), stop=(kb == nkb - 1)), [Vt, pt], [po])
            if kb == nkb - 1:
                osb = Osb[qi % 2]
                f.op("vector", lambda e: e.tensor_copy(out=osb[:], in_=po[:]), [po], [osb])
                f.op("tensor", lambda e: e.matmul(PD[:], lhsT=sel[:], rhs=osb[:], start=True, stop=True), [sel, osb], [PD])
                f.op("vector", lambda e: e.reciprocal(out=rden[:], in_=PD[:]), [PD], [rden])
                f.op("gpsimd", lambda e: e.tensor_tensor(out=o1[:], in0=osb[0:64, :], in1=rden[:], op=ALU.mult), [osb, rden], [o1])
                f.op("gpsimd", lambda e: e.tensor_tensor(out=o2[qi % 2][:], in0=o1[:], in1=ogt[qi % 2][:], op=ALU.mult), [o1, ogt[qi % 2]], [o2[qi % 2]])
                f.dma(o_o[h, :, qs], o2[qi % 2][:], o_o, o2[qi % 2])
        n = len(tiles)
        for i in range(n + LA):
            if i < n:
                emit_qk(i)
            if i >= LA:
                emit_rest(i - LA)
        ti += n
    f.final_wait([o_o])
    return f.build()


def consts_D():
    m = np.zeros((128, 4, 512), np.float32)
    p = np.arange(128)[:, None]; j = np.arange(512)[None, :]
    for i in range(4):
        m[:, i, :] = ((i * 128 + p) <= j).astype(np.float32)
    sel = np.zeros((65, 64), np.float32); sel[64, :] = 1.0
    return m.reshape(128, 2048), sel


def inputs_D(oc_list, lf_list, T=T_ALL):
    oc = np.concatenate(oc_list, axis=2)[:, :, :T]
    lf = np.concatenate(lf_list, axis=1)[:, :T]
    mk, sel = consts_D()
    NKB = T // 128
    maps = []
    for c in range(8):
        cs = slice(128 * c, 128 * c + 128)
        q = oc[0][cs].reshape(2, 64, T); k = oc[1][cs].reshape(2, 64, T); og = oc[3][cs].reshape(2, 64, T)
        v = oc[2][cs].reshape(2, 64, NKB, 128)
        vp = np.zeros((2, 128, NKB, 65), dtype=oc.dtype)
        vp[:, :, :, 0:64] = v.transpose(0, 3, 2, 1)
        maps.append({"qT": np.ascontiguousarray(q), "kT": np.ascontiguousarray(k), "vt": vp.reshape(2, 128, NKB * 65),
                     "og": np.ascontiguousarray(og), "lf": np.ascontiguousarray(lf[2 * c:2 * c + 2]), "mk": mk, "sel": sel})
    return maps


def gather_D(results):
    return np.concatenate([np.asarray(r["o_o"]).reshape(128, -1) for r in results], axis=0)


def _run(nc, maps):
    return run_bass_kernel_spmd(nc, maps, core_ids=list(range(8)))


def kernel(**inputs):
    inp = {k: np.asarray(v) for k, v in inputs.items()}
    resA = _run(build_A(), inputs_A(inp))
    obfA = [np.asarray(r["o_bf"]) for r in resA.results]
    obf = np.concatenate(obfA, axis=2)
    ort = np.concatenate([np.asarray(r["o_rt"]) for r in resA.results], axis=1)
    owc = np.concatenate([np.asarray(r["o_wc"]) for r in resA.results], axis=1)
    resB = _run(build_B(), inputs_B(obf, ort, owc))
    y = gather_B(resB.results)
    del obf, ort, owc
    resC = _run(build_C(), inputs_C(inp, y, obfA))
    oc_list = [np.asarray(r["o_c"]) for r in resC.results]
    lf_list = [np.asarray(r["o_lf"]) for r in resC.results]
    hT_list = [np.asarray(r["o_h"]) for r in resC.results]
    resD = _run(build_D(), inputs_D(oc_list, lf_list))
    oT = gather_D(resD.results)
    resE = _run(build_E(), inputs_E(inp, hT_list, oT))
    out = np.concatenate([np.asarray(r["o_out"]) for r in resE.results], axis=1).T
    return np.ascontiguousarray(out, dtype=np.float32).reshape(1, 16384, 1024)
```

```python
import ml_dtypes
import numpy as np
import concourse.bass as bass
import concourse.mybir as mybir
from concourse.bass_utils import run_bass_kernel_spmd
from contextlib import ExitStack

F32 = mybir.dt.float32
BF16 = mybir.dt.bfloat16
I32 = mybir.dt.int32
ALU = mybir.AluOpType
AF = mybir.ActivationFunctionType
AX = mybir.AxisListType

SAME_ENGINE_SYNC = True


class _Rec:
    def __init__(self):
        self.call = None

    def __getattr__(self, name):
        def cap(*a, **k):
            self.call = (name, a, k)
            return self
        return cap


def _replay(call):
    name, a, k = call
    return lambda e: getattr(e, name)(*a, **k)


class _Trk:
    __slots__ = ("w", "r")

    def __init__(self):
        self.w = {}
        self.r = {}


class Buf:
    def __init__(self, fw, name, t, kind):
        self.fw = fw
        self.name = name
        self.t = t
        self.kind = kind
        self.whole = _Trk()
        self.subs = {}
        self.dsem = None

    def __getitem__(self, idx):
        return self.t[idx]

    def k(self, key):
        return (self, key)


def _split(b):
    if isinstance(b, tuple):
        return b
    return (b, None)


class FW:
    CE = ("tensor", "vector", "scalar", "gpsimd")

    def __init__(self, nc):
        self.nc = nc
        self.es = ExitStack()
        self.q = {e: [] for e in ("tensor", "vector", "scalar", "gpsimd", "sync")}
        self.cnt = {e: 0 for e in self.CE}
        self.waited = {}
        self.sems = {}
        self.dcnt = {}
        self.nbuf = 0

    def sb(self, shape, dt, name=None):
        self.nbuf += 1
        name = "S_" + (name or f"sb{self.nbuf}")
        t = self.es.enter_context(self.nc.sbuf_tensor(name, list(shape), dt))
        return Buf(self, name, t, "sb")

    def ps(self, shape, dt=F32, name=None):
        self.nbuf += 1
        name = "P_" + (name or f"ps{self.nbuf}")
        t = self.es.enter_context(self.nc.psum_tensor(name, list(shape), dt))
        return Buf(self, name, t, "ps")

    def dram(self, name, shape, dt, kind):
        t = self.nc.dram_tensor(name, list(shape), dt, kind=kind).ap()
        return Buf(self, name, t, "dram")

    def _sem(self, key):
        if key not in self.sems:
            self.sems[key] = self.es.enter_context(self.nc.semaphore("s_" + str(key)))
        return self.sems[key]

    def _collect(self, eng, reads, writes):
        waits = {}

        def need_w(wd):
            for kv in wd.items():
                need(kv)

        def need(tok):
            if tok is None:
                return
            k, v = tok
            if k == eng and (eng == "tensor" or not SAME_ENGINE_SYNC):
                return
            if waits.get(k, 0) < v:
                waits[k] = v

        reads = list(reads)
        writes = list(writes)
        for b in list(reads):
            bb, _k = _split(b)
            if bb.kind == "ps":
                writes.append(bb)
        writes = [(_split(b)[0] if _split(b)[0].kind == "ps" else b) for b in writes]
        for b in reads:
            b, key = _split(b)
            if b.kind == "ps":
                continue
            trks = [b.whole] + ([b.subs[key]] if (key is not None and key in b.subs) else
                                (list(b.subs.values()) if key is None else []))
            for t in trks:
                need_w(t.w)
        for b in writes:
            b, key = _split(b)
            trks = [b.whole] + ([b.subs[key]] if (key is not None and key in b.subs) else
                                (list(b.subs.values()) if key is None else []))
            for t in trks:
                need_w(t.w)
                for k, v in t.r.items():
                    need((k, v))
        out = []
        for k, v in waits.items():
            if self.waited.get((eng, k), 0) >= v:
                continue
            self.waited[(eng, k)] = v
            out.append((k, v))
        return out

    def _update(self, reads, writes, tok):
        reads = list(reads)
        writes = list(writes)
        for b in list(reads):
            bb, _k = _split(b)
            if bb.kind == "ps":
                writes.append(bb)
        reads = [b for b in reads if _split(b)[0].kind != "ps"]
        writes = [(_split(b)[0] if _split(b)[0].kind == "ps" else b) for b in writes]
        for b in reads:
            b, key = _split(b)
            t = b.whole if key is None else b.subs.setdefault(key, _Trk())
            k, v = tok
            if t.r.get(k, 0) < v:
                t.r[k] = v
        for b in writes:
            b, key = _split(b)
            t = b.whole if key is None else b.subs.setdefault(key, _Trk())
            if b.kind == "dram":
                if t.w.get(tok[0], 0) < tok[1]:
                    t.w[tok[0]] = tok[1]
            else:
                t.w = {tok[0]: tok[1]}
                t.r = {}
                if key is None:
                    b.subs = {}

    def op(self, eng, fn, reads=(), writes=()):
        waits = self._collect(eng, reads, writes)
        self.cnt[eng] += 1
        tok = (eng, self.cnt[eng])
        self._update(reads, writes, tok)
        rec = _Rec()
        fn(rec)
        self.q[eng].append((waits, _replay(rec.call), (eng, 1)))

    def dma(self, out, in_, outb, inb, q="sync", **kw):
        ob, ok_ = _split(outb)
        ib, ik_ = _split(inb)
        sb, sk = (ob, ok_) if ob.kind != "dram" else (ib, ik_)
        key = "d_" + sb.name + ("" if sk is None else "_" + "_".join(str(z) for z in (sk if isinstance(sk, tuple) else (sk,))))
        waits = self._collect(q, [inb], [outb])
        self.dcnt[key] = self.dcnt.get(key, 0) + 16
        tok = (key, self.dcnt[key])
        self._update([inb], [outb], tok)
        self.q[q].append((waits, lambda e: e.dma_start(out=out, in_=in_, **kw), (key, 16)))

    def final_wait(self, bufs, q="sync"):
        waits = self._collect(q, bufs, [])
        self.q[q].append((waits, None, None))

    def build(self):
        nc = self.nc
        for e in self.CE:
            self._sem(e)
        for q in self.q.values():
            for waits, fn, inc in q:
                for k, v in waits:
                    self._sem(k)
                if inc is not None:
                    self._sem(inc[0])
        fwself = self
        self.nsem = len(self.sems)
        with nc.Block() as block:
            def mk(ename):
                def body(eng):
                    for waits, fn, inc in fwself.q[ename]:
                        for k, v in waits:
                            eng.wait_ge(fwself.sems[k], v)
                        if fn is not None:
                            ins = fn(eng)
                            ins.then_inc(fwself.sems[inc[0]], inc[1])
                return body
            block.tensor(mk("tensor"))
            block.vector(mk("vector"))
            block.scalar(mk("scalar"))
            block.gpsimd(mk("gpsimd"))
            block.sync(mk("sync"))
        self.es.close()
        return nc


C0 = 0.6065306597126334
RMS_EPS = 1e-6
DEBUG = False

PVA = {"g0": 0, "mu": 8, "w0": 56, "a0": 64, "k_k": 72, "k_a": 80, "r_k": 88}
NPA = 96


def rmsnorm_to_fm(f, xsrc_ap, xbuf, uT, col0, ncols, src_col0, ident, gcol, pv, tmp, NTOK=128):
    pass


def build_A():
    nc = bass.Bass("TRN2", target_bir_lowering=False)
    f = FW(nc)
    xa = f.dram("xa", [17 * 128, 1024], F32, "ExternalInput")
    pvd = f.dram("pv", [128, NPA], F32, "ExternalInput")
    cst = f.dram("cst", [128, 256], F32, "ExternalInput")
    w_rkv = f.dram("w_rkv", [3, 1024, 1024], F32, "ExternalInput")
    w1d = f.dram("w1", [1024, 64], F32, "ExternalInput")
    a1d = f.dram("a1", [1024, 64], F32, "ExternalInput")
    g1d = f.dram("g1", [1024, 128], F32, "ExternalInput")
    w2d = f.dram("w2", [64, 1024], F32, "ExternalInput")
    a2d = f.dram("a2", [64, 1024], F32, "ExternalInput")
    g2d = f.dram("g2", [128, 1024], F32, "ExternalInput")
    o_bf = f.dram("o_bf", [8, 1024, 2048], BF16, "ExternalOutput")
    o_rt = f.dram("o_rt", [1024, 2048], F32, "ExternalOutput")
    o_wc = f.dram("o_wc", [1024, 32], F32, "ExternalOutput")

    pv = f.sb([128, NPA], F32, "pv")
    f.dma(pv[:], pvd[:], pv, pvd)
    cs = f.sb([128, 256], F32, "cs")
    f.dma(cs[:], cst[:], cs, cst)
    ident = f.sb([128, 128], BF16, "ident")
    bones = f.sb([128, 128], BF16, "bones")
    f.op("vector", lambda e: e.tensor_copy(out=ident[:], in_=cs[:, 0:128]), [cs], [ident])
    f.op("vector", lambda e: e.tensor_copy(out=bones[:], in_=cs[:, 128:256]), [cs], [bones])
    ones = f.sb([128, 64], F32, "ones")
    f.op("gpsimd", lambda e: e.memset(ones[:], 1.0), [], [ones])

    wr = f.sb([128, 3, 8, 1024], BF16, "wr")
    for n in range(3):
        for kc in range(0, 8, 4):
            f.dma(wr[:, n, kc:kc + 4, :], w_rkv[n, kc * 128:(kc + 4) * 128, :].rearrange("(k p) e -> p k e", p=128), wr.k((n, kc)), w_rkv, q="gpsimd")
    w1 = f.sb([128, 8, 64], BF16, "w1s"); a1 = f.sb([128, 8, 64], BF16, "a1s"); g1 = f.sb([128, 8, 128], BF16, "g1s")
    f.dma(w1[:], w1d[:].rearrange("(k p) e -> p k e", p=128), w1, w1d, q="gpsimd")
    f.dma(a1[:], a1d[:].rearrange("(k p) e -> p k e", p=128), a1, a1d, q="gpsimd")
    f.dma(g1[:], g1d[:].rearrange("(k p) e -> p k e", p=128), g1, g1d, q="gpsimd")
    w2 = f.sb([64, 1024], BF16, "w2s"); a2 = f.sb([64, 1024], BF16, "a2s"); g2 = f.sb([128, 1024], BF16, "g2s")
    f.dma(w2[:], w2d[:], w2, w2d, q="gpsimd")
    f.dma(a2[:], a2d[:], a2, a2d, q="gpsimd")
    f.dma(g2[:], g2d[:], g2, g2d, q="gpsimd")

    uT = f.sb([128, 8, 2049], BF16, "uT")
    xb = [f.sb([128, 1024], F32, f"xb{i}") for i in range(2)]
    xn = [f.sb([128, 1024], BF16, f"xn{i}") for i in range(2)]
    junk = f.sb([128, 1024], BF16, "junk")
    ssq = [f.sb([128, 1], F32, f"ssq{i}") for i in range(2)]
    ptr = [f.ps([128, 8, 128], BF16, "ptr0")] * 2
    for t in range(17):
        b = t % 2
        X, XN, SS, PT = xb[b], xn[b], ssq[b], ptr[b]
        f.dma(X[:], xa[t * 128:(t + 1) * 128, :], X, xa)
        f.op("scalar", lambda e, X=X, SS=SS: e.activation(out=junk[:], in_=X[:], func=AF.Square, accum_out=SS[:]), [X], [junk, SS])
        f.op("scalar", lambda e, SS=SS: e.activation(out=SS[:], in_=SS[:], func=AF.Sqrt, scale=1.0 / 1024, bias=RMS_EPS), [SS], [SS])
        f.op("vector", lambda e, SS=SS: e.reciprocal(out=SS[:], in_=SS[:]), [SS], [SS])
        f.op("vector", lambda e, X=X, XN=XN, SS=SS: e.tensor_scalar(out=XN[:], in0=X[:], scalar1=SS[:, 0:1], scalar2=None, op0=ALU.mult), [X, SS], [XN])
        for c in range(8):
            f.op("tensor", lambda e, c=c, XN=XN, PT=PT: e.transpose(out=PT[:, c, :], in_=XN[:, c * 128:(c + 1) * 128], identity=ident[:]), [XN, ident], [PT])
        for c in range(8):
            eng = "vector" if c % 2 == 0 else "gpsimd"
            eng = "vector"
            if t == 0:
                f.op(eng, lambda e, c=c, PT=PT: e.tensor_scalar(out=uT[:, c, 0:1], in0=PT[:, c, 127:128], scalar1=pv[:, PVA["g0"] + c:PVA["g0"] + c + 1], scalar2=None, op0=ALU.mult), [PT, pv], [uT.k(("t", t))])
            else:
                c0 = 1 + (t - 1) * 128
                f.op(eng, lambda e, c=c, PT=PT, c0=c0: e.tensor_scalar(out=uT[:, c, c0:c0 + 128], in0=PT[:, c, :], scalar1=pv[:, PVA["g0"] + c:PVA["g0"] + c + 1], scalar2=None, op0=ALU.mult), [PT, pv], [uT.k(("t", t))])

    xs = f.sb([128, 6, 8, 512], BF16, "xs")
    dd = [f.sb([128, 512], BF16, f"dd{i}") for i in range(2)]
    pr = f.ps([128, 512], F32, "pr"); pk = f.ps([128, 512], F32, "pk"); pvv = f.ps([128, 512], F32, "pvv")
    pw = f.ps([128, 512], F32, "pw"); pa = f.ps([128, 512], F32, "pa"); pg = f.ps([128, 512], F32, "pg")
    px1 = f.ps([128, 512], F32, "px1"); px2 = px1
    h1 = f.sb([64, 512], BF16, "h1"); ha = f.sb([64, 512], BF16, "ha"); hg = f.sb([128, 512], BF16, "hg")
    T = lambda n, dt=F32: f.sb([128, 512], dt, n)
    r_s, k_s, v_s, sg, a_s, cum, cprev = T("r_s"), T("k_s"), T("v_s"), T("sg"), T("a_s"), T("cum"), T("cprev")
    e_pos, e_neg, e_prev = T("e_pos"), T("e_neg"), T("e_prev")
    kkr, sq, ssm, kk, t1, kmod, bb, btf, ktf, rk = T("kkr"), T("sq", BF16), T("ssm"), T("kk"), T("t1"), T("kmod"), T("bb"), T("btf"), T("ktf"), T("rk", BF16)
    rt = T("rt")
    wc = f.sb([128, 8], F32, "wc")
    ob = f.sb([128, 8, 512], BF16, "ob")
    for s in range(4):
        cur = lambda c: uT[:, c, 1 + 512 * s:1 + 512 * s + 512]
        prv = lambda c: uT[:, c, 512 * s:512 * s + 512]
        ureads = [uT.k(("t", t)) for t in range(max(0, 4 * s), 4 * s + 5)]
        for c in range(8):
            D = dd[c % 2]
            f.op("gpsimd", lambda e, c=c, D=D: e.tensor_tensor(out=D[:], in0=prv(c), in1=cur(c), op=ALU.subtract), ureads, [D])
            for n in range(6):
                col = PVA["mu"] + n * 8 + c
                f.op("vector", lambda e, c=c, n=n, D=D, col=col: e.scalar_tensor_tensor(out=xs[:, n, c, :], in0=D[:], scalar=pv[:, col:col + 1], in1=cur(c), op0=ALU.mult, op1=ALU.add), [D, pv] + ureads, [xs.k(n)])
        for c in range(8):
            f.op("tensor", lambda e, c=c: e.matmul(pw[0:64, :], lhsT=w1[:, c, :], rhs=xs[:, 3, c, :], start=(c == 0), stop=(c == 7)), [w1, xs.k(3)], [pw])
        f.op("scalar", lambda e: e.activation(out=h1[:], in_=pw[0:64, :], func=AF.Tanh), [pw], [h1])
        for c in range(8):
            f.op("tensor", lambda e, c=c: e.matmul(pa[0:64, :], lhsT=a1[:, c, :], rhs=xs[:, 4, c, :], start=(c == 0), stop=(c == 7)), [a1, xs.k(4)], [pa])
        f.op("vector", lambda e: e.tensor_copy(out=ha[:], in_=pa[0:64, :]), [pa], [ha])
        for c in range(8):
            f.op("tensor", lambda e, c=c: e.matmul(pg[:], lhsT=g1[:, c, :], rhs=xs[:, 5, c, :], start=(c == 0), stop=(c == 7)), [g1, xs.k(5)], [pg])
        f.op("scalar", lambda e: e.activation(out=hg[:], in_=pg[:], func=AF.Sigmoid), [pg], [hg])
        for ec in range(8):
            es = slice(ec * 128, (ec + 1) * 128)
            for n, P in enumerate((pr, pk, pvv)):
                for c in range(8):
                    f.op("tensor", lambda e, c=c, n=n, P=P: e.matmul(P[:], lhsT=wr[:, n, c, es], rhs=xs[:, n, c, :], start=(c == 0), stop=(c == 7)), [wr.k((n, (c // 4) * 4)), xs.k(n)], [P])
            f.op("tensor", lambda e: e.matmul(pw[:], lhsT=w2[:, es], rhs=h1[:], start=True, stop=True), [w2, h1], [pw])
            f.op("tensor", lambda e: e.matmul(pa[:], lhsT=a2[:, es], rhs=ha[:], start=True, stop=True), [a2, ha], [pa])
            f.op("tensor", lambda e: e.matmul(pg[:], lhsT=g2[:, es], rhs=hg[:], start=True, stop=True), [g2, hg], [pg])
            pcol = lambda nm: pv[:, PVA[nm] + ec:PVA[nm] + ec + 1]
            f.op("scalar", lambda e: e.activation(out=r_s[:], in_=pr[:], func=AF.Copy), [pr], [r_s])
            f.op("scalar", lambda e: e.activation(out=k_s[:], in_=pk[:], func=AF.Copy), [pk], [k_s])
            f.op("vector", lambda e: e.tensor_copy(out=v_s[:], in_=pvv[:]), [pvv], [v_s])
            f.op("scalar", lambda e: e.activation(out=sg[:], in_=pw[:], func=AF.Sigmoid, bias=pcol("w0")), [pw, pv], [sg])
            f.op("scalar", lambda e: e.activation(out=a_s[:], in_=pa[:], func=AF.Sigmoid, bias=pcol("a0")), [pa, pv], [a_s])
            f.op("scalar", lambda e: e.activation(out=ob[:, 6, :], in_=pg[:], func=AF.Copy), [pg], [ob.k(6)])
            for q in range(8):
                f.op("vector", lambda e, q=q: e.tensor_tensor_scan(out=cum[:, q * 64:(q + 1) * 64], data0=ones[:], data1=sg[:, q * 64:(q + 1) * 64], initial=0.0, op0=ALU.mult, op1=ALU.add), [ones, sg], [cum])
            f.op("gpsimd", lambda e: e.tensor_tensor(out=cprev[:], in0=cum[:], in1=sg[:], op=ALU.subtract), [cum, sg], [cprev])
            f.op("scalar", lambda e: e.activation(out=e_pos[:], in_=cum[:], func=AF.Exp, scale=-C0), [cum], [e_pos])
            f.op("scalar", lambda e: e.activation(out=e_neg[:], in_=cum[:], func=AF.Exp, scale=C0), [cum], [e_neg])
            f.op("scalar", lambda e: e.activation(out=e_prev[:], in_=cprev[:], func=AF.Exp, scale=-C0), [cprev], [e_prev])
            f.op("scalar", lambda e: e.activation(out=wc[:], in_=cum[:].rearrange("p (q t) -> p q t", t=64)[:, :, 63], func=AF.Exp, scale=-C0), [cum], [wc])
            f.op("scalar", lambda e: e.activation(out=kkr[:], in_=k_s[:], func=AF.Identity, scale=pcol("k_k")), [k_s, pv], [kkr])
            f.op("scalar", lambda e: e.activation(out=sq[:], in_=k_s[:], func=AF.Square, scale=pcol("k_k")), [k_s, pv], [sq])
            f.op("tensor", lambda e: e.matmul(px1[:], lhsT=bones[:], rhs=sq[:], start=True, stop=True), [bones, sq], [px1])
            f.op("vector", lambda e: e.tensor_scalar(out=ssm[:], in0=px1[:], scalar1=1e-24, scalar2=None, op0=ALU.max), [px1], [ssm])
            f.op("scalar", lambda e: e.activation(out=ssm[:], in_=ssm[:], func=AF.Sqrt), [ssm], [ssm])
            f.op("vector", lambda e: e.reciprocal(out=ssm[:], in_=ssm[:]), [ssm], [ssm])
            f.op("gpsimd", lambda e: e.tensor_tensor(out=kk[:], in0=kkr[:], in1=ssm[:], op=ALU.mult), [kkr, ssm], [kk])
            f.op("vector", lambda e: e.tensor_scalar(out=t1[:], in0=a_s[:], scalar1=-1.0, scalar2=pcol("k_a"), op0=ALU.add, op1=ALU.mult), [a_s, pv], [t1])
            f.op("vector", lambda e: e.scalar_tensor_tensor(out=kmod[:], in0=t1[:], scalar=1.0, in1=k_s[:], op0=ALU.add, op1=ALU.mult), [t1, k_s], [kmod])
            f.op("gpsimd", lambda e: e.tensor_tensor(out=bb[:], in0=kk[:], in1=a_s[:], op=ALU.mult), [kk, a_s], [bb])
            f.op("vector", lambda e: e.scalar_tensor_tensor(out=ob[:, 0, :], in0=kk[:], scalar=-1.0, in1=e_prev[:], op0=ALU.mult, op1=ALU.mult), [kk, e_prev], [ob.k(0)])
            f.op("gpsimd", lambda e: e.tensor_tensor(out=rt[:], in0=r_s[:], in1=e_pos[:], op=ALU.mult), [r_s, e_pos], [rt])
            f.op("gpsimd", lambda e: e.tensor_tensor(out=btf[:], in0=bb[:], in1=e_neg[:], op=ALU.mult), [bb, e_neg], [btf])
            f.op("gpsimd", lambda e: e.tensor_tensor(out=ktf[:], in0=kmod[:], in1=e_neg[:], op=ALU.mult), [kmod, e_neg], [ktf])
            f.op("scalar", lambda e: e.activation(out=ob[:, 1, :], in_=btf[:], func=AF.Copy), [btf], [ob.k(1)])
            f.op("scalar", lambda e: e.activation(out=ob[:, 2, :], in_=ktf[:], func=AF.Copy), [ktf], [ob.k(2)])
            for q in range(8):
                qs = slice(q * 64, (q + 1) * 64)
                f.op("vector", lambda e, q=q, qs=qs: e.tensor_scalar(out=ob[:, 3, qs], in0=btf[:, qs], scalar1=wc[:, q:q + 1], scalar2=None, op0=ALU.mult), [btf, wc], [ob.k(3)])
                f.op("scalar", lambda e, q=q, qs=qs: e.activation(out=ob[:, 4, qs], in_=ktf[:, qs], func=AF.Identity, scale=wc[:, q:q + 1]), [ktf, wc], [ob.k(4)])
            f.op("vector", lambda e: e.scalar_tensor_tensor(out=rk[:], in0=r_s[:], scalar=pcol("r_k"), in1=kmod[:], op0=ALU.mult, op1=ALU.mult), [r_s, pv, kmod], [rk])
            f.op("tensor", lambda e: e.matmul(px2[:], lhsT=bones[:], rhs=rk[:], start=True, stop=True), [bones, rk], [px2])
            f.op("vector", lambda e: e.tensor_tensor(out=ob[:, 7, :], in0=px2[:], in1=v_s[:], op=ALU.mult), [px2, v_s], [ob.k(7)])
            f.op("scalar", lambda e: e.activation(out=ob[:, 5, :], in_=v_s[:], func=AF.Copy), [v_s], [ob.k(5)])
            f.dma(o_bf[:, es, 512 * s:512 * s + 512].rearrange("q p t -> p q t"), ob[:], o_bf, ob)
            f.dma(o_rt[es, 512 * s:512 * s + 512], rt[:], o_rt, rt)
            f.dma(o_wc[es, 8 * s:8 * s + 8], wc[:], o_wc, wc)
    if DEBUG:
        o_dbg = f.dram("o_dbg", [128, 8, 2049], BF16, "ExternalOutput")
        f.dma(o_dbg[:], uT[:], o_dbg, uT)
        o_dbg2 = f.dram("o_dbg2", [128, 8, 1024], BF16, "ExternalOutput")
        f.dma(o_dbg2[:], wr[:, 2, :, :], o_dbg2, wr)
        f.final_wait([o_dbg, o_dbg2])
    f.final_wait([o_bf, o_rt, o_wc])
    return f.build()


def consts_A():
    c = np.zeros((128, 256), np.float32)
    c[:, 0:128] = np.eye(128)
    blk = np.arange(128) // 64
    c[:, 128:256] = (blk[:, None] == blk[None, :]).astype(np.float32)
    return c


def fmcols(v):
    return np.ascontiguousarray(np.asarray(v, np.float32).reshape(8, 128).T)


def inputs_A(inp):
    x = inp["x"][0]
    pvv = np.zeros((128, NPA), np.float32)
    pvv[:, 0:8] = fmcols(inp["norm_g"][0, 0])
    for n in range(6):
        pvv[:, 8 + 8 * n:16 + 8 * n] = fmcols(inp["rwkv_mu"][0, n])
    pvv[:, 56:64] = fmcols(inp["rwkv_w0"][0]); pvv[:, 64:72] = fmcols(inp["rwkv_a0"][0])
    pvv[:, 72:80] = fmcols(inp["rwkv_k_k"][0]); pvv[:, 80:88] = fmcols(inp["rwkv_k_a"][0])
    pvv[:, 88:96] = fmcols(inp["rwkv_r_k"][0].reshape(-1))
    cst = consts_A()
    xpad = np.concatenate([np.zeros((128, 1024), np.float32), x], 0)
    maps = []
    for c in range(8):
        maps.append({"xa": np.ascontiguousarray(xpad[2048 * c:2048 * c + 2048 + 128]), "pv": pvv, "cst": cst,
                     "w_rkv": inp["rwkv_w_rkv"][0], "w1": inp["rwkv_w1"][0], "a1": inp["rwkv_a1"][0], "g1": inp["rwkv_g1"][0],
                     "w2": inp["rwkv_w2"][0], "a2": inp["rwkv_a2"][0], "g2": inp["rwkv_g2"][0]})
    return maps


GC = 8
NCH = 256
NG = NCH // GC
DEBUG = False


def build_B(nch=NCH):
    ng = nch // GC
    nc = bass.Bass("TRN2", target_bir_lowering=False)
    f = FW(nc)
    d_bk = f.dram("d_bk", [ng, 64, GC * 2 * 2 * 64], BF16, "ExternalInput")
    d_at = f.dram("d_at", [ng, 64, GC * 2 * 64], BF16, "ExternalInput")
    d_rf = f.dram("d_rf", [ng, 64, GC * 2 * 64], F32, "ExternalInput")
    d_tm = f.dram("d_tm", [ng, 64, GC * 2 * 4 * 64], BF16, "ExternalInput")
    d_wc = f.dram("d_wc", [64, nch * 2], F32, "ExternalInput")
    d_mask = f.dram("d_mask", [64, 2 * 320], F32, "ExternalInput")
    d_id = f.dram("d_id", [64, 128], F32, "ExternalInput")
    o_y = f.dram("o_y", [ng, 64, GC * 2 * 64], F32, "ExternalOutput")

    wcs = f.sb([64, nch, 2], F32, "wcs")
    f.dma(wcs[:].rearrange("p c h -> p (c h)"), d_wc[:], wcs, d_wc)
    mask = f.sb([64, 2, 320], F32, "mask")
    f.dma(mask[:].rearrange("p h c -> p (h c)"), d_mask[:], mask, d_mask)
    idf2 = f.sb([64, 2, 64], F32, "idf2")
    f.dma(idf2[:].rearrange("p h c -> p (h c)"), d_id[:], idf2, d_id)
    idb2 = f.sb([64, 2, 64], BF16, "idb2")
    f.op("vector", lambda e: e.tensor_copy(out=idb2[:], in_=idf2[:]), [idf2], [idb2])
    identb = idb2[:, 0, :]
    identf = idf2[:, 0, :]

    NB = 2
    BK = [f.sb([64, GC, 2, 2, 64], BF16, f"BK{i}") for i in range(NB)]
    AR = [f.sb([64, GC, 2, 128], BF16, f"AR{i}") for i in range(NB)]
    RF = [f.sb([64, GC, 2, 64], F32, f"RF{i}") for i in range(NB)]
    TM = [f.sb([64, GC, 2, 4, 64], BF16, f"TM{i}") for i in range(NB)]
    YB = [f.sb([64, GC, 2, 64], F32, f"YB{i}") for i in range(NB)]

    def load_group(g):
        b = g % NB
        f.dma(BK[b][:].rearrange("p c h q t -> p (c h q t)"), d_bk[g], BK[b], d_bk)
        f.dma(AR[b][:, :, :, 0:64], d_at[g].rearrange("p (c h t) -> p c h t", c=GC, h=2), AR[b].k("a"), d_at)
        f.dma(AR[b][:, :, :, 64:128], d_rf[g].rearrange("p (c h t) -> p c h t", c=GC, h=2), AR[b].k("r"), d_rf, q="gpsimd")
        f.dma(RF[b][:].rearrange("p c h t -> p (c h t)"), d_rf[g], RF[b], d_rf)
        f.dma(TM[b][:].rearrange("p c h q t -> p (c h q t)"), d_tm[g], TM[b], d_tm)

    class _V:
        def __init__(self, buf, ap):
            self.buf, self.ap = buf, ap

        def __getitem__(self, idx):
            return self.ap[idx]

        def k(self, key):
            return self.buf

    def bank(name):
        return f.ps([64, 512], F32, name)
    PM, PL, PXG = [], [], []
    for x in range(2):
        bm, bl, bx = bank(f"PM{x}"), bank(f"PL{x}"), bank(f"PXG{x}")
        PM.append(_V(bm, bm[:, 0:512].rearrange("p (h c) -> p h c", h=2)))
        PL.append(_V(bl, bl[:, 0:384].rearrange("p (h c) -> p h c", h=2)))
        PXG.append(_V(bx, bx[:, 0:512].rearrange("p (h c) -> p h c", h=2)))
    bus, by = bank("PUS"), bank("PYb")
    PU_ap = bus[:, 0:128].rearrange("p (h c) -> p h c", h=2)
    PS_ap = bus[:, 128:256].rearrange("p (h c) -> p h c", h=2)
    PY_ap = by[:, 0:128].rearrange("p (h c) -> p h c", h=2)
    PUk, PSk, PYk = bus, bus, by
    NP = 4
    Ms = [f.sb([64, 2, 320], BF16, f"Ms{i}") for i in range(NP)]
    XT0 = [f.sb([64, 2, 64], BF16, f"XT0{i}") for i in range(NP)]
    LV = [[f.sb([64, 2, 192], BF16, f"LV{i}_{k}") for k in range(7)] for i in range(NP)]
    GX = [f.sb([64, 2, 128], F32, f"GX{i}") for i in range(NP)]
    UT = [f.sb([64, 2, 64], BF16, f"UT{i}") for i in range(2)]
    S = [f.sb([64, 2, 64], F32, f"S{i}") for i in range(2)]
    f.op("vector", lambda e: e.memset(S[0][:], 0.0), [], [S[0]])

    def pre_stages(c):
        p = c % NP
        x = c % 2
        PMx, PLx, PXGx = PM[x], PL[x], PXG[x]
        g, cg = divmod(c, GC)
        b = g % NB
        bk, ar, tm = BK[b], AR[b], TM[b]
        st = []

        def p1():
            for h in range(2):
                f.op("tensor", lambda e: e.matmul(PMx[:, h, 0:128], lhsT=bk[:, cg, h, 0, :], rhs=ar[:, cg, h, :], start=True, stop=True), [bk, ar], [PMx.k(h)])
                f.op("tensor", lambda e: e.matmul(PMx[:, h, 128:256], lhsT=bk[:, cg, h, 1, :], rhs=ar[:, cg, h, :], start=True, stop=True), [bk, ar], [PMx.k(h)])
                f.op("tensor", lambda e: e.matmul(PXGx[:, h, 192:256], lhsT=ar[:, cg, h, 0:64], rhs=bk[:, cg, h, 0, :], start=True, stop=True), [bk, ar], [PXGx.k("l")])
        st.append(p1)

        def p2():
            f.op("vector", lambda e: e.tensor_tensor(out=Ms[p][:, :, 0:256], in0=PMx[:, :, 0:256], in1=mask[:, :, 0:256], op=ALU.mult), [PMx.buf, mask], [Ms[p].k("m")])
            f.op("vector", lambda e: e.tensor_tensor(out=Ms[p][:, :, 256:320], in0=PXGx[:, :, 192:256], in1=mask[:, :, 256:320], op=ALU.mult), [PXGx.k("l"), mask], [Ms[p].k("l")])
            f.op("gpsimd", lambda e: e.tensor_tensor(out=LV[p][1][:, :, 128:192], in0=Ms[p][:, :, 0:64], in1=idb2[:], op=ALU.add), [Ms[p].k("m"), idb2], [LV[p][1].k("T")])
        st.append(p2)

        def p3():
            for h in range(2):
                f.op("tensor", lambda e: e.matmul(PXGx[:, h, 0:64], lhsT=Ms[p][:, h, 128:192], rhs=tm[:, cg, h, 3, :], start=True, stop=True), [Ms[p].k("m"), tm], [PXGx.k("x")])
        st.append(p3)

        def p4():
            f.op("scalar", lambda e: e.activation(out=XT0[p][:], in_=PXGx[:, :, 0:64], func=AF.Copy), [PXGx.k("x")], [XT0[p]])
        st.append(p4)

        def level(k):
            def mm():
                for h in range(2):
                    if k == 1:
                        Np, Lp = Ms[p][:, h, 0:64], Ms[p][:, h, 256:320]
                        rd = [Ms[p].k("m"), Ms[p].k("l")]
                    else:
                        Np, Lp = LV[p][k - 1][:, h, 0:64], LV[p][k - 1][:, h, 64:128]
                        rd = [LV[p][k - 1].k("NL")]
                    if k <= 5:
                        f.op("tensor", lambda e: e.matmul(PLx[:, h, 0:64], lhsT=Lp, rhs=Np, start=True, stop=True), rd, [PLx.k(h)])
                        f.op("tensor", lambda e: e.matmul(PLx[:, h, 64:128], lhsT=Np, rhs=Lp, start=True, stop=True), rd, [PLx.k(h)])
                    if k >= 2:
                        Tp = LV[p][k - 1][:, h, 128:192]
                        rdt = rd + [LV[p][k - 1].k("T"), idb2]
                        f.op("tensor", lambda e: e.matmul(PLx[:, h, 128:192], lhsT=identb, rhs=Tp, start=True, stop=False), rdt, [PLx.k(h)])
                        f.op("tensor", lambda e: e.matmul(PLx[:, h, 128:192], lhsT=Lp, rhs=Tp, start=False, stop=True), rdt, [PLx.k(h)])

            def ev():
                eng = "scalar" if k % 2 == 0 else "vector"
                if k == 1:
                    sl, wk = slice(0, 128), [LV[p][k].k("NL")]
                elif k <= 5:
                    sl, wk = slice(0, 192), [LV[p][k].k("NL"), LV[p][k].k("T")]
                else:
                    sl, wk = slice(128, 192), [LV[p][k].k("T")]
                if eng == "scalar":
                    f.op("scalar", lambda e: e.activation(out=LV[p][k][:, :, sl], in_=PLx[:, :, sl], func=AF.Copy), [PLx.buf], wk)
                else:
                    f.op("vector", lambda e: e.tensor_copy(out=LV[p][k][:, :, sl], in_=PLx[:, :, sl]), [PLx.buf], wk)
            return [mm, ev]
        for k in range(1, 7):
            st.extend(level(k))

        def pf():
            for h in range(2):
                Tf = LV[p][6][:, h, 128:192]
                f.op("tensor", lambda e: e.matmul(PXGx[:, h, 64:128], lhsT=tm[:, cg, h, 0, :], rhs=Tf, start=True, stop=True), [tm, LV[p][6].k("T")], [PXGx.k("g")])
                f.op("tensor", lambda e: e.matmul(PXGx[:, h, 128:192], lhsT=Tf, rhs=XT0[p][:, h, :], start=True, stop=True), [XT0[p], LV[p][6].k("T")], [PXGx.k("g")])
        st.append(pf)

        def pe():
            f.op("scalar", lambda e: e.activation(out=GX[p][:], in_=PXGx[:, :, 64:192], func=AF.Copy), [PXGx.k("g")], [GX[p]])
        st.append(pe)
        return st

    def seq_stages(c):
        p = c % NP
        q2 = c % 2
        g, cg = divmod(c, GC)
        b = g % NB
        bk, ar, tm, rf, yb = BK[b], AR[b], TM[b], RF[b], YB[b]
        S0, S1 = S[q2], S[1 - q2]
        UTq = UT[q2]
        PU, PS, PY = PU_ap, PS_ap, PY_ap
        st = []

        def s1():
            for h in range(2):
                f.op("tensor", lambda e: e.matmul(PU[:, h, :], lhsT=GX[p][:, h, 0:64], rhs=S0[:, h, :], start=True, stop=False), [GX[p], S0], [PUk])
                f.op("tensor", lambda e: e.matmul(PU[:, h, :], lhsT=identf, rhs=GX[p][:, h, 64:128], start=False, stop=True), [GX[p], idf2], [PUk])
        st.append(s1)

        def s2():
            f.op("scalar", lambda e: e.activation(out=UTq[:], in_=PU, func=AF.Copy), [PUk], [UTq])
        st.append(s2)

        def s3():
            for h in range(2):
                f.op("tensor", lambda e: e.matmul(PS[:, h, :], lhsT=tm[:, cg, h, 1, :], rhs=UTq[:, h, :], start=True, stop=False), [tm, UTq], [PSk])
                f.op("tensor", lambda e: e.matmul(PS[:, h, :], lhsT=tm[:, cg, h, 2, :], rhs=tm[:, cg, h, 3, :], start=False, stop=True), [tm], [PSk])
            for h in range(2):
                f.op("tensor", lambda e: e.matmul(PY[:, h, :], lhsT=rf[:, cg, h, :], rhs=S0[:, h, :], start=True, stop=False), [rf, S0], [PYk])
                f.op("tensor", lambda e: e.matmul(PY[:, h, :], lhsT=Ms[p][:, h, 192:256], rhs=tm[:, cg, h, 3, :], start=False, stop=False), [Ms[p].k("m"), tm], [PYk])
                f.op("tensor", lambda e: e.matmul(PY[:, h, :], lhsT=Ms[p][:, h, 64:128], rhs=UTq[:, h, :], start=False, stop=True), [Ms[p].k("m"), UTq], [PYk])
        st.append(s3)

        def s4():
            for h in range(2):
                f.op("vector", lambda e: e.scalar_tensor_tensor(out=S1[:, h, :], in0=S0[:, h, :], scalar=wcs[:, c, h:h + 1], in1=PS[:, h, :], op0=ALU.mult, op1=ALU.add), [S0, wcs, PSk], [S1])
            f.op("scalar", lambda e: e.activation(out=yb[:, cg, :, :], in_=PY, func=AF.Copy), [PYk], [yb.k(cg)])
            if cg == GC - 1 or DEBUG:
                f.dma(o_y[g], yb[:].rearrange("p c h i -> p (c h i)"), o_y, yb)
        st.append(s4)
        return st

    load_group(0)
    if ng > 1:
        load_group(1)

    def lockstep(a, b):
        out = []
        for i in range(max(len(a), len(b))):
            if i < len(a):
                out.append(a[i])
            if i < len(b):
                out.append(b[i])
        return out

    for s_ in lockstep(pre_stages(0), pre_stages(1) if nch > 1 else []):
        s_()
    for c in range(0, nch, 2):
        g, cg = divmod(c, GC)
        if DEBUG and c >= 1:
            break
        a = []
        if not DEBUG:
            a = lockstep(pre_stages(c + 2) if c + 2 < nch else [], pre_stages(c + 3) if c + 3 < nch else [])
        bq = seq_stages(c) + (seq_stages(c + 1) if c + 1 < nch and not DEBUG else [])
        n = max(len(a), len(bq))
        ia = ib = 0
        for i in range(n):
            want_a = (i + 1) * len(a) // n
            while ia < want_a:
                a[ia](); ia += 1
            want_b = (i + 1) * len(bq) // n
            while ib < want_b:
                bq[ib](); ib += 1
        if (c + 1) % GC == GC - 1 and g + 2 < ng:
            load_group(g + 2)
    if DEBUG:
        dbg = f.dram("o_dbg", [64, 2 * (320 + 192 * 7 + 128 + 64)], F32, "ExternalOutput")
        db = f.sb([64, 2, 320 + 192 * 7 + 128 + 64], F32, "dbgs")
        f.op("vector", lambda e: e.tensor_copy(out=db[:, :, 0:320], in_=Ms[0][:]), [Ms[0]], [db])
        for k in range(7):
            f.op("vector", lambda e: e.tensor_copy(out=db[:, :, 320 + 192 * k:320 + 192 * (k + 1)], in_=LV[0][k][:]), [LV[0][k]], [db])
        o = 320 + 192 * 7
        f.op("vector", lambda e: e.tensor_copy(out=db[:, :, o:o + 128], in_=GX[0][:]), [GX[0]], [db])
        f.op("vector", lambda e: e.tensor_copy(out=db[:, :, o + 128:o + 192], in_=UT[0][:]), [UT[0]], [db])
        f.dma(dbg[:], db[:].rearrange("p h c -> p (h c)"), dbg, db)
        f.final_wait([dbg])
    f.final_wait([o_y])
    return f.build()


def consts_B():
    m = np.zeros((64, 2, 320), np.float32)
    s = np.arange(64)[:, None]; t = np.arange(64)[None, :]
    su = (s < t).astype(np.float32); iu = (s <= t).astype(np.float32)
    for h in range(2):
        m[:, h, 0:64] = su; m[:, h, 64:128] = iu; m[:, h, 128:192] = su; m[:, h, 192:256] = iu
        m[:, h, 256:320] = (t < s).astype(np.float32)
    idm = np.concatenate([np.eye(64, dtype=np.float32)] * 2, axis=1)
    return m.reshape(64, 640), idm


def inputs_B(obf, ort, owc, nch=NCH):
    ng = nch // GC
    T = nch * 64
    mask, idm = consts_B()
    maps = []
    for c in range(8):
        cs = slice(128 * c, 128 * c + 128)
        fm = lambda a: a[cs, :T].reshape(2, 64, ng, GC, 64)
        At, Bt, Kt, Bh, Kh, V = [fm(obf[q]) for q in range(6)]
        Rt = fm(ort)
        d_bk = np.stack([Bt, Kt], axis=0).transpose(3, 2, 4, 1, 0, 5)
        d_at = At.transpose(2, 1, 3, 0, 4)
        d_rf = Rt.transpose(2, 1, 3, 0, 4)
        tmq = np.stack([At, Bh, Kh, V], axis=0)
        d_tm = tmq.transpose(3, 5, 4, 1, 0, 2)
        d_wc = owc[cs, :nch].reshape(2, 64, nch).transpose(1, 2, 0)
        maps.append({"d_bk": np.ascontiguousarray(d_bk).reshape(ng, 64, -1), "d_at": np.ascontiguousarray(d_at).reshape(ng, 64, -1),
                     "d_rf": np.ascontiguousarray(d_rf).reshape(ng, 64, -1), "d_tm": np.ascontiguousarray(d_tm).reshape(ng, 64, -1),
                     "d_wc": np.ascontiguousarray(d_wc).reshape(64, -1), "d_mask": mask, "d_id": idm})
    return maps


def gather_B(results, nch=NCH):
    ng = nch // GC
    ys = []
    for r in results:
        y = np.asarray(r["o_y"]).reshape(ng, 64, GC, 2, 64)
        ys.append(y.transpose(0, 2, 1, 3, 4).reshape(nch * 64, 128))
    return np.concatenate(ys, axis=1)


RMS_EPS = 1e-6
GN_EPS = 64e-5
ST = 512


class Ctx:
    pass


def setup_common(f, c, cst_dram):
    cs = f.sb([128, 384], F32, "cs")
    f.dma(cs[:], cst_dram[:], cs, cst_dram)
    c.identf = cs
    c.bones = f.sb([128, 128], BF16, "bones")
    c.ones = f.sb([128, 128], BF16, "onesb")
    f.op("vector", lambda e: e.tensor_copy(out=c.bones[:], in_=cs[:, 128:256]), [cs], [c.bones])
    f.op("vector", lambda e: e.tensor_copy(out=c.ones[:], in_=cs[:, 256:384]), [cs], [c.ones])
    c.cs = cs
    c.PA = [f.ps([128, 512], F32, f"PA{i}") for i in range(4)]
    c.PB = [f.ps([128, 512], F32, f"PB{i}") for i in range(2)]
    c.PC = [f.ps([128, 512], F32, f"PC{i}") for i in range(2)]
    c.ia = c.ib = c.ic = 0
    c.tmpi = 0
    c.tmps = {}


def nxt(c, pool):
    if pool == "A":
        c.ia += 1; return c.PA[c.ia % 4]
    if pool == "B":
        c.ib += 1; return c.PB[c.ib % 2]
    c.ic += 1; return c.PC[c.ic % 2]


def tmp(f, c, name, dt=F32, n=2, shape=(128, 512)):
    key = (name, dt)
    if key not in c.tmps:
        c.tmps[key] = [[f.sb(list(shape), dt, f"t_{name}_{i}") for i in range(n)], 0]
    lst = c.tmps[key]
    lst[1] += 1
    return lst[0][lst[1] % n]


def rmsnorm_fm(f, c, hT, pv, gcol0, uT, uF=None, eps=RMS_EPS):
    P = nxt(c, "C")
    for k in range(8):
        sq = tmp(f, c, "sq", BF16, 3)
        f.op("scalar", lambda e: e.activation(out=sq[:], in_=hT[:, k, :], func=AF.Square), [hT.k(k)], [sq])
        f.op("tensor", lambda e: e.matmul(P[:], lhsT=c.ones[:], rhs=sq[:], start=(k == 0), stop=(k == 7)), [c.ones, sq], [P])
    rs = tmp(f, c, "rs")
    f.op("scalar", lambda e: e.activation(out=rs[:], in_=P[:], func=AF.Sqrt, scale=1.0 / 1024, bias=eps), [P], [rs])
    f.op("vector", lambda e: e.reciprocal(out=rs[:], in_=rs[:]), [rs], [rs])
    for k in range(8):
        if uT is not None:
            f.op("vector", lambda e: e.scalar_tensor_tensor(out=uT[:, k, :], in0=hT[:, k, :], scalar=pv[:, gcol0 + k:gcol0 + k + 1], in1=rs[:], op0=ALU.mult, op1=ALU.mult), [hT.k(k), pv, rs], [uT.k(k)])
        if uF is not None:
            f.op("vector", lambda e: e.scalar_tensor_tensor(out=uF[:, k, :], in0=hT[:, k, :], scalar=pv[:, gcol0 + k:gcol0 + k + 1], in1=rs[:], op0=ALU.mult, op1=ALU.mult), [hT.k(k), pv, rs], [uF.k(k)])


def linear_res(f, c, w_sb, zT, hT, nk=8):
    for ec in range(8):
        P = nxt(c, "B")
        for k in range(nk):
            f.op("tensor", lambda e: e.matmul(P[:], lhsT=w_sb[:, k, ec * 128:(ec + 1) * 128], rhs=zT[:, k, :], start=(k == 0), stop=(k == nk - 1)), [w_sb, zT.k(k)], [P])
        f.op("vector", lambda e: e.tensor_tensor(out=hT[:, ec, :], in0=hT[:, ec, :], in1=P[:], op=ALU.add), [hT.k(ec), P], [hT.k(ec)])


def ffn_pass(f, c, uT, aT, wgu_dram, wd_dram, wgu_bufs, wd_bufs, out_fn, gate_b=None, FG=2, EG=2):
    NF = 28
    for g in range(NF // FG):
        c.wgi = getattr(c, "wgi", 0) + 1
        W = wgu_bufs[c.wgi % len(wgu_bufs)]
        for half in range(2):
            col0 = half * 3584 + g * FG * 128
            f.dma(W[:, :, half, :], wgu_dram[:, col0:col0 + FG * 128].rearrange("(k p) e -> p k e", p=128), W.k(half), wgu_dram_buf(c, wgu_dram), q="gpsimd")
        for j in range(FG):
            fc = g * FG + j
            Pg = nxt(c, "A"); Pu = nxt(c, "A")
            for k in range(8):
                f.op("tensor", lambda e: e.matmul(Pg[:], lhsT=W[:, k, 0, j * 128:(j + 1) * 128], rhs=uT[:, k, :], start=(k == 0), stop=(k == 7)), [W.k(0), uT.k(k)], [Pg])
            for k in range(8):
                f.op("tensor", lambda e: e.matmul(Pu[:], lhsT=W[:, k, 1, j * 128:(j + 1) * 128], rhs=uT[:, k, :], start=(k == 0), stop=(k == 7)), [W.k(1), uT.k(k)], [Pu])
            sg = tmp(f, c, "sg", F32, 3)
            f.op("scalar", lambda e: e.activation(out=sg[:], in_=Pg[:], func=AF.Silu), [Pg], [sg])
            if gate_b is None:
                f.op("vector", lambda e: e.tensor_tensor(out=aT[:, fc, :], in0=sg[:], in1=Pu[:], op=ALU.mult), [sg, Pu], [aT.k(fc)])
            else:
                sg2 = tmp(f, c, "sg2", F32, 3)
                f.op("vector", lambda e: e.tensor_tensor(out=sg2[:], in0=sg[:], in1=Pu[:], op=ALU.mult), [sg, Pu], [sg2])
                f.op("gpsimd", lambda e: e.tensor_tensor(out=aT[:, fc, :], in0=sg2[:], in1=gate_b[:], op=ALU.mult), [sg2, gate_b], [aT.k(fc)])
    for g in range(8 // EG):
        c.wdi = getattr(c, "wdi", 0) + 1
        W = wd_bufs[c.wdi % len(wd_bufs)]
        for q4 in range(4):
            f.dma(W[:, q4 * 7:(q4 + 1) * 7, :], wd_dram[q4 * 896:(q4 + 1) * 896, g * EG * 128:(g + 1) * EG * 128].rearrange("(k p) e -> p k e", p=128), W.k(q4), wgu_dram_buf(c, wd_dram), q="gpsimd")
        for j in range(EG):
            ec = g * EG + j
            P = nxt(c, "B")
            for fc in range(NF):
                f.op("tensor", lambda e: e.matmul(P[:], lhsT=W[:, fc, j * 128:(j + 1) * 128], rhs=aT[:, fc, :], start=(fc == 0), stop=(fc == NF - 1)), [W.k(fc // 7), aT.k(fc)], [P])
            out_fn(ec, P)


_dram_bufs = {}


def wgu_dram_buf(c, ap):
    return c.wdram


def consts_CE():
    cst = np.zeros((128, 384), np.float32)
    cst[:, 0:128] = np.eye(128)
    blk = np.arange(128) // 64
    cst[:, 128:256] = (blk[:, None] == blk[None, :]).astype(np.float32)
    cst[:, 256:384] = 1.0
    return cst


def fmcols(v):
    return np.ascontiguousarray(np.asarray(v, np.float32).reshape(-1, 128).T)


def ffn_ws(f, c, uTs, hTs, w_gu_ap, w_dn_ap, wgu_bufs, wd_bufs, aT_bufs, gbs=None, FG=4):
    NTT = len(uTs)
    for g in range(28 // FG):
        c.wgi = getattr(c, "wgi", 0) + 1
        W = wgu_bufs[c.wgi % 2]; WD = wd_bufs[c.wgi % 2]
        for hf in range(2):
            col0 = hf * 3584 + g * FG * 128
            f.dma(W[:, :, hf, :], w_gu_ap[:, col0:col0 + FG * 128].rearrange("(k p) e -> p k e", p=128), W.k(hf), c.wdram, q="gpsimd")
        f.dma(WD[:], w_dn_ap[g * FG * 128:(g + 1) * FG * 128, :].rearrange("(k p) e -> p k e", p=128), WD, c.wdram, q="gpsimd")
        for tt in range(NTT):
            c.ai = getattr(c, "ai", 0) + 1
            A = aT_bufs[c.ai % 2]
            for j in range(FG):
                Pg = nxt(c, "A"); Pu = nxt(c, "A")
                for k in range(8):
                    f.op("tensor", lambda e: e.matmul(Pg[:], lhsT=W[:, k, 0, j * 128:(j + 1) * 128], rhs=uTs[tt][:, k, :], start=(k == 0), stop=(k == 7)), [W.k(0), uTs[tt].k(k)], [Pg])
                for k in range(8):
                    f.op("tensor", lambda e: e.matmul(Pu[:], lhsT=W[:, k, 1, j * 128:(j + 1) * 128], rhs=uTs[tt][:, k, :], start=(k == 0), stop=(k == 7)), [W.k(1), uTs[tt].k(k)], [Pu])
                sg = tmp(f, c, "sg", F32, 3)
                f.op("scalar", lambda e: e.activation(out=sg[:], in_=Pg[:], func=AF.Silu), [Pg], [sg])
                if gbs is None:
                    f.op("vector", lambda e: e.tensor_tensor(out=A[:, j, :], in0=sg[:], in1=Pu[:], op=ALU.mult), [sg, Pu], [A.k(j)])
                else:
                    sg2 = tmp(f, c, "sg2", F32, 3)
                    f.op("vector", lambda e: e.tensor_tensor(out=sg2[:], in0=sg[:], in1=Pu[:], op=ALU.mult), [sg, Pu], [sg2])
                    f.op("vector", lambda e: e.tensor_tensor(out=A[:, j, :], in0=sg2[:], in1=gbs[tt][:], op=ALU.mult), [sg2, gbs[tt]], [A.k(j)])
            for ec in range(8):
                P = nxt(c, "B")
                for j in range(FG):
                    f.op("tensor", lambda e: e.matmul(P[:], lhsT=WD[:, j, ec * 128:(ec + 1) * 128], rhs=A[:, j, :], start=(j == 0), stop=(j == FG - 1)), [WD, A.k(j)], [P])
                f.op("vector", lambda e: e.tensor_tensor(out=hTs[tt][:, ec, :], in0=hTs[tt][:, ec, :], in1=P[:], op=ALU.add), [hTs[tt].k(ec), P], [hTs[tt].k(ec)])


PVC = {"gn_g": 0, "gn_b": 8, "g1": 16, "g2": 24, "qg": 32, "kg": 33, "bf": 34}
NPC = 35


def build_C():
    nc = bass.Bass("TRN2", target_bir_lowering=False)
    f = FW(nc)
    c = Ctx()
    xT = f.dram("xT", [1024, 2048], F32, "ExternalInput")
    yT = f.dram("yT", [1024, 2048], F32, "ExternalInput")
    gb = f.dram("gb", [2, 1024, 2048], BF16, "ExternalInput")
    pvd = f.dram("pv", [128, NPC], F32, "ExternalInput")
    cst = f.dram("cst", [128, 384], F32, "ExternalInput")
    w_o = f.dram("w_o", [1024, 1024], F32, "ExternalInput")
    w_gu = f.dram("w_gu", [1024, 7168], F32, "ExternalInput")
    w_dn = f.dram("w_dn", [3584, 1024], F32, "ExternalInput")
    w_in = f.dram("w_in", [1024, 4112], F32, "ExternalInput")
    o_h = f.dram("o_h", [1024, 2048], F32, "ExternalOutput")
    o_c = f.dram("o_c", [4, 1024, 2048], BF16, "ExternalOutput")
    o_lf = f.dram("o_lf", [16, 2048], F32, "ExternalOutput")
    c.wdram = f.dram("wdummy", [1, 1], F32, "Internal")
    setup_common(f, c, cst)
    pv = f.sb([128, NPC], F32, "pvs")
    f.dma(pv[:], pvd[:], pv, pvd)
    wo = f.sb([128, 8, 1024], BF16, "wo")
    for k4 in range(0, 8, 4):
        f.dma(wo[:, k4:k4 + 4, :], w_o[k4 * 128:(k4 + 4) * 128, :].rearrange("(k p) e -> p k e", p=128), wo.k(k4), w_o, q="gpsimd")
    wfl = f.sb([128, 8, 16], BF16, "wfl")
    f.dma(wfl[:], w_in[:, 4096:4112].rearrange("(k p) e -> p k e", p=128), wfl, w_in, q="gpsimd")
    NTT = 2
    hTs = [f.sb([128, 8, 512], F32, f"hT{i}") for i in range(NTT)]
    uTs = [f.sb([128, 8, 512], BF16, f"uT{i}") for i in range(NTT)]
    zT = f.sb([128, 8, 512], BF16, "zT")
    aT = [f.sb([128, 4, 512], BF16, f"aTg{i}") for i in range(2)]
    wgu_bufs = [f.sb([128, 8, 2, 512], BF16, f"wgu{i}") for i in range(2)]
    wd_bufs = [f.sb([128, 4, 1024], BF16, f"wd{i}") for i in range(2)]
    oc = [f.sb([128, 512], BF16, f"oc{i}") for i in range(3)]
    oci = [0]
    lf = f.sb([16, 512], F32, "lf")
    for half in range(2):
        for tt in range(NTT):
            hT = hTs[tt]
            ts = slice(1024 * half + 512 * tt, 1024 * half + 512 * tt + 512)
            f.dma(hT[:], xT[:, ts].rearrange("(k p) t -> p k t", p=128), hT, xT)
            for ec in range(8):
                es = slice(ec * 128, (ec + 1) * 128)
                y = tmp(f, c, "y"); g2 = tmp(f, c, "gbt", BF16, 2, (128, 2, 512))
                f.dma(y[:], yT[es, ts], y, yT)
                f.dma(g2[:], gb[:, es, ts].rearrange("q p t -> p q t"), g2, gb)
                yb = tmp(f, c, "yb", BF16)
                f.op("scalar", lambda e: e.activation(out=yb[:], in_=y[:], func=AF.Copy), [y], [yb])
                P = nxt(c, "C")
                f.op("tensor", lambda e: e.matmul(P[:], lhsT=c.bones[:], rhs=yb[:], start=True, stop=True), [c.bones, yb], [P])
                yc = tmp(f, c, "yc")
                f.op("vector", lambda e: e.scalar_tensor_tensor(out=yc[:], in0=P[:], scalar=-1.0 / 64, in1=y[:], op0=ALU.mult, op1=ALU.add), [P, y], [yc])
                sq = tmp(f, c, "sq", BF16, 3)
                f.op("scalar", lambda e: e.activation(out=sq[:], in_=yc[:], func=AF.Square), [yc], [sq])
                P2 = nxt(c, "C")
                f.op("tensor", lambda e: e.matmul(P2[:], lhsT=c.bones[:], rhs=sq[:], start=True, stop=True), [c.bones, sq], [P2])
                rs = tmp(f, c, "rs")
                f.op("scalar", lambda e: e.activation(out=rs[:], in_=P2[:], func=AF.Sqrt, scale=1.0 / 64, bias=GN_EPS), [P2], [rs])
                f.op("vector", lambda e: e.reciprocal(out=rs[:], in_=rs[:]), [rs], [rs])
                f.op("vector", lambda e: e.tensor_tensor(out=yc[:], in0=yc[:], in1=rs[:], op=ALU.mult), [yc, rs], [yc])
                f.op("vector", lambda e: e.tensor_scalar(out=yc[:], in0=yc[:], scalar1=pv[:, PVC["gn_g"] + ec:PVC["gn_g"] + ec + 1], scalar2=pv[:, PVC["gn_b"] + ec:PVC["gn_b"] + ec + 1], op0=ALU.mult, op1=ALU.add), [yc, pv], [yc])
                f.op("vector", lambda e: e.tensor_tensor(out=yc[:], in0=yc[:], in1=g2[:, 1, :], op=ALU.add), [yc, g2], [yc])
                f.op("vector", lambda e: e.tensor_tensor(out=zT[:, ec, :], in0=yc[:], in1=g2[:, 0, :], op=ALU.mult), [yc, g2], [zT.k(ec)])
            linear_res(f, c, wo, zT, hT)
            rmsnorm_fm(f, c, hT, pv, PVC["g1"], uTs[tt])
        ffn_ws(f, c, uTs, hTs, w_gu[:], w_dn[:], wgu_bufs, wd_bufs, aT)
        for tt in range(NTT):
            ts = slice(1024 * half + 512 * tt, 1024 * half + 512 * tt + 512)
            f.dma(o_h[:, ts].rearrange("(k p) t -> p k t", p=128), hTs[tt][:], o_h, hTs[tt])
            rmsnorm_fm(f, c, hTs[tt], pv, PVC["g2"], uTs[tt])
        for g in range(8):
            c.wgi += 1
            W = wgu_bufs[c.wgi % 2]
            Wv = W[:].rearrange("p k h e -> p k (h e)")
            f.dma(Wv[:, :, 0:512], w_in[:, g * 512:(g + 1) * 512].rearrange("(k p) e -> p k e", p=128), W, c.wdram, q="gpsimd")
            for tt in range(NTT):
                ts = slice(1024 * half + 512 * tt, 1024 * half + 512 * tt + 512)
                uT = uTs[tt]
                for j in range(4):
                    e32 = g * 4 + j
                    kind, ec = divmod(e32, 8)
                    P = nxt(c, "A")
                    for k in range(8):
                        f.op("tensor", lambda e: e.matmul(P[:], lhsT=Wv[:, k, j * 128:(j + 1) * 128], rhs=uT[:, k, :], start=(k == 0), stop=(k == 7)), [W, uT.k(k)], [P])
                    oci[0] += 1
                    O = oc[oci[0] % 3]
                    if kind in (0, 1):
                        qs = tmp(f, c, "qs")
                        f.op("scalar", lambda e: e.activation(out=qs[:], in_=P[:], func=AF.Copy), [P], [qs])
                        sq = tmp(f, c, "sq", BF16, 3)
                        f.op("scalar", lambda e: e.activation(out=sq[:], in_=qs[:], func=AF.Square), [qs], [sq])
                        P2 = nxt(c, "C")
                        f.op("tensor", lambda e: e.matmul(P2[:], lhsT=c.bones[:], rhs=sq[:], start=True, stop=True), [c.bones, sq], [P2])
                        rs = tmp(f, c, "rs")
                        if kind == 0:
                            f.op("scalar", lambda e: e.activation(out=rs[:], in_=P2[:], func=AF.Sqrt, scale=1.0, bias=64 * RMS_EPS), [P2], [rs])
                        else:
                            f.op("scalar", lambda e: e.activation(out=rs[:], in_=P2[:], func=AF.Sqrt, scale=1.0 / 64, bias=RMS_EPS), [P2], [rs])
                        f.op("vector", lambda e: e.reciprocal(out=rs[:], in_=rs[:]), [rs], [rs])
                        gc = PVC["qg"] if kind == 0 else PVC["kg"]
                        f.op("vector", lambda e: e.scalar_tensor_tensor(out=O[:], in0=qs[:], scalar=pv[:, gc:gc + 1], in1=rs[:], op0=ALU.mult, op1=ALU.mult), [qs, pv, rs], [O])
                    elif kind == 2:
                        f.op("scalar", lambda e: e.activation(out=O[:], in_=P[:], func=AF.Copy), [P], [O])
                    else:
                        f.op("scalar", lambda e: e.activation(out=O[:], in_=P[:], func=AF.Sigmoid), [P], [O])
                    f.dma(o_c[kind, ec * 128:(ec + 1) * 128, ts], O[:], o_c, O)
        for tt in range(NTT):
            ts = slice(1024 * half + 512 * tt, 1024 * half + 512 * tt + 512)
            uT = uTs[tt]
            P = nxt(c, "C")
            for k in range(8):
                f.op("tensor", lambda e: e.matmul(P[0:16, :], lhsT=wfl[:, k, :], rhs=uT[:, k, :], start=(k == 0), stop=(k == 7)), [wfl, uT.k(k)], [P])
            f.op("vector", lambda e: e.tensor_scalar(out=lf[:], in0=P[0:16, :], scalar1=pv[0:16, PVC["bf"]:PVC["bf"] + 1], scalar2=None, op0=ALU.add), [P, pv], [lf])
            f.op("scalar", lambda e: e.activation(out=lf[:], in_=lf[:], func=AF.Exp, scale=-1.0), [lf], [lf])
            f.op("scalar", lambda e: e.activation(out=lf[:], in_=lf[:], func=AF.Ln, bias=1.0), [lf], [lf])
            f.op("vector", lambda e: e.tensor_scalar(out=lf[:], in0=lf[:], scalar1=-1.0, scalar2=None, op0=ALU.mult), [lf], [lf])
            f.dma(o_lf[:, ts], lf[:], o_lf, lf)
    f.final_wait([o_h, o_c, o_lf])
    return f.build()


def inputs_C(inp, yfull, obfA):
    x = inp["x"][0]
    pvv = np.zeros((128, NPC), np.float32)
    pvv[:, 0:8] = fmcols(inp["rwkv_gn_g"][0]); pvv[:, 8:16] = fmcols(inp["rwkv_gn_b"][0])
    pvv[:, 16:24] = fmcols(inp["norm_g"][0, 1]); pvv[:, 24:32] = fmcols(inp["norm_g"][1, 0])
    pvv[:, 32] = np.tile(inp["fox_q_gain"][0], 2); pvv[:, 33] = np.tile(inp["fox_k_gain"][0], 2)
    pvv[0:16, 34] = inp["fox_b_f"][0]
    cst = consts_CE()
    maps = []
    for c in range(8):
        ts = slice(2048 * c, 2048 * c + 2048)
        maps.append({"xT": np.ascontiguousarray(x[ts].T), "yT": np.ascontiguousarray(yfull[ts].T),
                     "gb": np.ascontiguousarray(obfA[c][6:8]), "pv": pvv, "cst": cst,
                     "w_o": inp["rwkv_w_o"][0], "w_gu": inp["ffn_w_gu"][0], "w_dn": inp["ffn_w_down"][0], "w_in": inp["fox_w_in"][0]})
    return maps


PVE = {"g3": 0, "gf": 8}
NPE = 16
NEXP = 8
NTE = 1024
FGE = 4


def build_E():
    nc = bass.Bass("TRN2", target_bir_lowering=False)
    f = FW(nc)
    c = Ctx()
    hTd = f.dram("hT", [1024, 2048], F32, "ExternalInput")
    oTd = f.dram("oT", [1024, 2048], BF16, "ExternalInput")
    pvd = f.dram("pv", [128, NPE], F32, "ExternalInput")
    cst = f.dram("cst", [128, 384], F32, "ExternalInput")
    seld = f.dram("sele", [8, 8 * 128], F32, "ExternalInput")
    w_o = f.dram("w_o", [1024, 1024], F32, "ExternalInput")
    w_r = f.dram("w_r", [1024, 8], F32, "ExternalInput")
    w_gu = f.dram("w_gu", [8, 1024, 7168], F32, "ExternalInput")
    w_dn = f.dram("w_dn", [8, 3584, 1024], F32, "ExternalInput")
    o_out = f.dram("o_out", [1024, 2048], F32, "ExternalOutput")
    c.wdram = f.dram("wdummy", [1, 1], F32, "Internal")
    setup_common(f, c, cst)
    pv = f.sb([128, NPE], F32, "pvs")
    f.dma(pv[:], pvd[:], pv, pvd)
    sele = f.sb([8, 8, 128], F32, "sele")
    f.dma(sele[:].rearrange("k e m -> k (e m)"), seld[:], sele, seld)
    wo = f.sb([128, 8, 1024], BF16, "wo")
    for k4 in range(0, 8, 4):
        f.dma(wo[:, k4:k4 + 4, :], w_o[k4 * 128:(k4 + 4) * 128, :].rearrange("(k p) e -> p k e", p=128), wo.k(k4), w_o, q="gpsimd")
    wr = f.sb([128, 8, 8], F32, "wr")
    f.dma(wr[:], w_r[:].rearrange("(k p) e -> p k e", p=128), wr, w_r)
    NTT = NTE // 512
    hT = [f.sb([128, 8, 512], F32, f"hT{i}") for i in range(NTT)]
    uT = [f.sb([128, 8, 512], BF16, f"uT{i}") for i in range(NTT)]
    gTs = [f.sb([8, 512], F32, f"gTs{i}") for i in range(NTT)]
    zT = f.sb([128, 8, 512], BF16, "zT")
    uF = f.sb([128, 8, 512], F32, "uF")
    wgu_bufs = [f.sb([128, 8, 2, FGE * 128], BF16, f"wgu{i}") for i in range(2)]
    wd_bufs = [f.sb([128, FGE, 1024], BF16, f"wd{i}") for i in range(2)]
    aT = [f.sb([128, FGE, 512], BF16, f"aTg{i}") for i in range(2)]
    gb = [f.sb([128, 512], F32, f"gb{i}") for i in range(NTT)]
    lg = f.sb([8, 512], F32, "lg")
    lt = f.sb([128, 4, 8], F32, "lt")
    gts = f.sb([128, 4, 8], F32, "gts")
    sm = {n: f.sb([128, 8], F32, "sm_" + n) for n in ("eq", "l2", "sel", "ex")}
    sc = {n: f.sb([128, 4], F32, "sc_" + n) for n in ("m1", "nm1", "m2", "sum")}
    wgi = 0
    ai = 0
    for half in range(2048 // NTE):
        for tt in range(NTT):
            ts = slice(half * NTE + 512 * tt, half * NTE + 512 * tt + 512)
            H, U = hT[tt], uT[tt]
            f.dma(H[:], hTd[:, ts].rearrange("(k p) t -> p k t", p=128), H, hTd)
            f.dma(zT[:], oTd[:, ts].rearrange("(k p) t -> p k t", p=128), zT, oTd)
            linear_res(f, c, wo, zT, H)
            rmsnorm_fm(f, c, H, pv, PVE["g3"], U, uF)
            P = nxt(c, "C")
            for k in range(8):
                f.op("tensor", lambda e: e.matmul(P[0:8, :], lhsT=wr[:, k, :], rhs=uF[:, k, :], start=(k == 0), stop=(k == 7)), [wr, uF.k(k)], [P])
            f.op("vector", lambda e: e.tensor_copy(out=lg[:], in_=P[0:8, :]), [P], [lg])
            P2 = nxt(c, "C")
            for j in range(4):
                f.op("tensor", lambda e: e.transpose(out=P2[:, j * 8:(j + 1) * 8], in_=lg[:, j * 128:(j + 1) * 128], identity=c.cs[0:8, 0:8]), [lg, c.cs], [P2])
            f.op("vector", lambda e: e.tensor_copy(out=lt[:].rearrange("p j e -> p (j e)"), in_=P2[:, 0:32]), [P2], [lt])
            f.op("vector", lambda e: e.tensor_reduce(out=sc["m1"][:], in_=lt[:], axis=AX.X, op=ALU.max), [lt], [sc["m1"]])
            f.op("vector", lambda e: e.tensor_scalar(out=sc["nm1"][:], in0=sc["m1"][:], scalar1=-1.0, scalar2=None, op0=ALU.mult), [sc["m1"]], [sc["nm1"]])
            for j in range(4):
                L = lt[:, j, :]
                f.op("vector", lambda e: e.tensor_scalar(out=sm["eq"][:], in0=L, scalar1=sc["m1"][:, j:j + 1], scalar2=None, op0=ALU.is_ge), [lt, sc["m1"]], [sm["eq"]])
                f.op("vector", lambda e: e.scalar_tensor_tensor(out=sm["l2"][:], in0=sm["eq"][:], scalar=-1e30, in1=L, op0=ALU.mult, op1=ALU.add), [sm["eq"], lt], [sm["l2"]])
                f.op("vector", lambda e: e.tensor_reduce(out=sc["m2"][:, j:j + 1], in_=sm["l2"][:], axis=AX.X, op=ALU.max), [sm["l2"]], [sc["m2"]])
                f.op("vector", lambda e: e.tensor_scalar(out=sm["sel"][:], in0=L, scalar1=sc["m2"][:, j:j + 1], scalar2=None, op0=ALU.is_ge), [lt, sc["m2"]], [sm["sel"]])
                f.op("scalar", lambda e: e.activation(out=sm["ex"][:], in_=L, func=AF.Exp, bias=sc["nm1"][:, j:j + 1]), [lt, sc["nm1"]], [sm["ex"]])
                f.op("vector", lambda e: e.tensor_tensor(out=sm["ex"][:], in0=sm["ex"][:], in1=sm["sel"][:], op=ALU.mult), [sm["ex"], sm["sel"]], [sm["ex"]])
                f.op("vector", lambda e: e.tensor_reduce(out=sc["sum"][:, j:j + 1], in_=sm["ex"][:], axis=AX.X, op=ALU.add), [sm["ex"]], [sc["sum"]])
                f.op("vector", lambda e: e.reciprocal(out=sc["sum"][:, j:j + 1], in_=sc["sum"][:, j:j + 1]), [sc["sum"]], [sc["sum"]])
                f.op("vector", lambda e: e.tensor_scalar(out=gts[:, j, :], in0=sm["ex"][:], scalar1=sc["sum"][:, j:j + 1], scalar2=None, op0=ALU.mult), [sm["ex"], sc["sum"]], [gts])
            P3 = nxt(c, "C")
            for j in range(4):
                f.op("tensor", lambda e: e.transpose(out=P3[0:8, j * 128:(j + 1) * 128], in_=gts[:, j, :], identity=c.cs[:, 0:128]), [gts, c.cs], [P3])
            f.op("vector", lambda e: e.tensor_copy(out=gTs[tt][:], in_=P3[0:8, :]), [P3], [gTs[tt]])
        for ex in range(NEXP):
            for tt in range(NTT):
                P4 = nxt(c, "C")
                f.op("tensor", lambda e: e.matmul(P4[:], lhsT=sele[:, ex, :], rhs=gTs[tt][:], start=True, stop=True), [sele, gTs[tt]], [P4])
                f.op("scalar", lambda e: e.activation(out=gb[tt][:], in_=P4[:], func=AF.Copy), [P4], [gb[tt]])
            ffn_ws(f, c, uT, hT, w_gu[ex], w_dn[ex], wgu_bufs, wd_bufs, aT, gbs=gb, FG=FGE)
        for tt in range(NTT):
            ts = slice(half * NTE + 512 * tt, half * NTE + 512 * tt + 512)
            rmsnorm_fm(f, c, hT[tt], pv, PVE["gf"], None, uF)
            f.dma(o_out[:, ts].rearrange("(k p) t -> p k t", p=128), uF[:], o_out, uF)
    f.final_wait([o_out])
    return f.build()


def inputs_E(inp, hT_list, oT):
    pvv = np.zeros((128, NPE), np.float32)
    pvv[:, 0:8] = fmcols(inp["norm_g"][1, 1]); pvv[:, 8:16] = fmcols(inp["final_g"])
    cst = consts_CE()
    sele = np.zeros((8, 8, 128), np.float32)
    for e in range(8):
        sele[e, e, :] = 1.0
    maps = []
    for c in range(8):
        maps.append({"hT": hT_list[c], "oT": np.ascontiguousarray(oT[:, 2048 * c:2048 * c + 2048]), "pv": pvv, "cst": cst,
                     "sele": sele.reshape(8, 1024), "w_o": inp["fox_w_o"][0], "w_r": inp["moe_w_router"][0],
                     "w_gu": inp["moe_w_gu"][0], "w_dn": inp["moe_w_down"][0]})
    return maps


T_ALL = 16384


def build_D(T=T_ALL):
    nc = bass.Bass("TRN2", target_bir_lowering=False)
    f = FW(nc)
    NQ = T // 512
    NKB = T // 128
    NSEG = T // 2048
    qT = f.dram("qT", [2, 64, T], BF16, "ExternalInput")
    kT = f.dram("kT", [2, 64, T], BF16, "ExternalInput")
    vt = f.dram("vt", [2, 128, NKB * 65], BF16, "ExternalInput")
    og = f.dram("og", [2, 64, T], BF16, "ExternalInput")
    lfd = f.dram("lf", [2, T], F32, "ExternalInput")
    mkd = f.dram("mk", [128, 4 * 512], F32, "ExternalInput")
    sld = f.dram("sel", [65, 64], F32, "ExternalInput")
    o_o = f.dram("o_o", [2, 64, T], BF16, "ExternalOutput")

    mkf = f.sb([128, 4, 512], F32, "mkf")
    f.dma(mkf[:].rearrange("p m t -> p (m t)"), mkd[:], mkf, mkd)
    mk = f.sb([128, 4, 512], BF16, "mk")
    f.op("vector", lambda e: e.tensor_copy(out=mk[:], in_=mkf[:]), [mkf], [mk])
    sel = f.sb([65, 64], F32, "sels")
    f.dma(sel[:], sld[:], sel, sld)
    ones = f.sb([1, 2048], F32, "ones1")
    f.op("gpsimd", lambda e: e.memset(ones[:], 1.0), [], [ones])

    Qa = f.sb([70, T], BF16, "Qa")
    Ka = f.sb([70, T], BF16, "Ka")
    Vt = f.sb([128, NKB, 65], BF16, "Vt")
    lfs = [f.sb([1, 2048], F32, f"lfs{i}") for i in range(2)]
    cseg = [f.sb([1, 2048], F32, f"cseg{i}") for i in range(2)]
    c_d = f.dram("c_scr", [2, T], F32, "Internal")
    p_d = f.dram("p_scr", [2, 3, T], BF16, "Internal")
    c2d = [f.sb([128, T // 128], F32, f"c2d{i}") for i in range(2)]
    r2d = [f.sb([128, T // 128], F32, f"r2d{i}") for i in range(2)]
    parts2 = [f.sb([128, 3, T // 128], BF16, f"parts2_{i}") for i in range(2)]
    for h in range(2):
        for sg in range(NSEG):
            b = (h * NSEG + sg) % 2
            ss = slice(sg * 2048, (sg + 1) * 2048)
            f.dma(lfs[b][:], lfd[h:h + 1, ss], lfs[b], lfd)
            init = 0.0 if sg == 0 else cseg[1 - b][:, 2047:2048]
            rd = [ones, lfs[b]] + ([] if sg == 0 else [cseg[1 - b]])
            f.op("vector", lambda e: e.tensor_tensor_scan(out=cseg[b][:], data0=ones[:], data1=lfs[b][:], initial=init, op0=ALU.mult, op1=ALU.add), rd, [cseg[b]])
            f.dma(c_d[h:h + 1, ss], cseg[b][:], c_d, cseg[b])
        C2, R2, P2 = c2d[h], r2d[h], parts2[h]
        f.dma(C2[:], c_d[h].rearrange("(p c) -> p c", p=128), C2, c_d)
        f.op("vector", lambda e: e.tensor_copy(out=P2[:, 0, :], in_=C2[:]), [C2], [P2])
        f.op("vector", lambda e: e.tensor_tensor(out=R2[:], in0=C2[:], in1=P2[:, 0, :], op=ALU.subtract), [C2, P2], [R2])
        f.op("vector", lambda e: e.tensor_copy(out=P2[:, 1, :], in_=R2[:]), [R2], [P2])
        f.op("vector", lambda e: e.tensor_tensor(out=R2[:], in0=R2[:], in1=P2[:, 1, :], op=ALU.subtract), [R2, P2], [R2])
        f.op("vector", lambda e: e.tensor_copy(out=P2[:, 2, :], in_=R2[:]), [R2], [P2])
        for r in range(3):
            f.dma(p_d[h, r].rearrange("(p c) -> p c", p=128), P2[:, r, :], p_d, P2)
    PSs = [f.ps([128, 512], F32, f"PSs{i}") for i in range(5)]
    PO = [f.ps([65, 512], F32, f"PO{i}") for i in range(2)]
    PD = f.ps([64, 512], F32, "PD")
    PT = [f.sb([128, 512], BF16, f"PT{i}") for i in range(6)]
    Osb = [f.sb([65, 512], F32, f"Osb{i}") for i in range(2)]
    rden = f.sb([64, 512], F32, "rden")
    o1 = f.sb([64, 512], F32, "o1")
    ogt = [f.sb([64, 512], BF16, f"ogt{i}") for i in range(2)]
    o2 = [f.sb([64, 512], BF16, f"o2{i}") for i in range(2)]
    scl = [f.sb([128, 512], F32, f"scl{i}") for i in range(2)]
    ti = 0
    for h in range(2):
        f.dma(Qa[0:64, :], qT[h], Qa.k("top"), qT)
        f.dma(Ka[0:64, :], kT[h], Ka.k("top"), kT)
        f.dma(Vt[:].rearrange("p k d -> p (k d)"), vt[h], Vt, vt)
        f.op("gpsimd", lambda e: e.memset(Vt[:, :, 64:65], 1.0), [], [Vt])
        f.op("gpsimd", lambda e: e.memset(Qa[64:70, :], -1.0), [], [Qa.k("aug")])
        f.op("gpsimd", lambda e: e.memset(Ka[64:70, :], 1.0), [], [Ka.k("aug")])
        for r in range(3):
            f.dma(Qa[64 + r:65 + r, :], p_d[h, r:r + 1, :], Qa.k("aug"), p_d)
            f.dma(Ka[67 + r:68 + r, :], p_d[h, r:r + 1, :], Ka.k("aug"), p_d)
        tiles = [(qi, kb) for qi in range(NQ) for kb in range(4 * qi + 4)]
        LA = 4
        base = ti

        def emit_qk(i):
            qi, kb = tiles[i]
            qs = slice(qi * 512, (qi + 1) * 512)
            ps = PSs[(base + i) % 5]
            f.op("tensor", lambda e: e.matmul(ps[:], lhsT=Ka[0:70, kb * 128:(kb + 1) * 128], rhs=Qa[0:70, qs], start=True, stop=True), [Ka, Qa], [ps])

        def emit_rest(i):
            qi, kb = tiles[i]
            qs = slice(qi * 512, (qi + 1) * 512)
            nkb = 4 * qi + 4
            ps = PSs[(base + i) % 5]; pt = PT[(base + i) % 6]
            po = PO[qi % 2]
            if kb == 0:
                f.dma(ogt[qi % 2][:], og[h, :, qs], ogt[qi % 2], og)
            if kb >= 4 * qi:
                sc = scl[kb % 2]
                f.op("vector", lambda e: e.tensor_scalar(out=sc[:], in0=ps[:], scalar1=30.0, scalar2=None, op0=ALU.min), [ps], [sc])
                f.op("scalar", lambda e: e.activation(out=pt[:], in_=sc[:], func=AF.Exp), [sc], [pt])
                m = kb - 4 * qi
                eng = "vector" if m % 2 == 0 else "gpsimd"
                f.op(eng, lambda e: e.tensor_tensor(out=pt[:], in0=pt[:], in1=mk[:, m, :], op=ALU.mult), [pt, mk], [pt])
            else:
                f.op("scalar", lambda e: e.activation(out=pt[:], in_=ps[:], func=AF.Exp), [ps], [pt])
            f.op("tensor", lambda e: e.matmul(po[:], lhsT=Vt[:, kb, :], rhs=pt[:], start=(kb == 0), stop=(kb == nkb - 1)), [Vt, pt], [po])
            if kb == nkb - 1:
                osb = Osb[qi % 2]
                f.op("vector", lambda e: e.tensor_copy(out=osb[:], in_=po[:]), [po], [osb])
                f.op("tensor", lambda e: e.matmul(PD[:], lhsT=sel[:], rhs=osb[:], start=True, stop=True), [sel, osb], [PD])
                f.op("vector", lambda e: e.reciprocal(out=rden[:], in_=PD[:]), [PD], [rden])
                f.op("gpsimd", lambda e: e.tensor_tensor(out=o1[:], in0=osb[0:64, :], in1=rden[:], op=ALU.mult), [osb, rden], [o1])
                f.op("gpsimd", lambda e: e.tensor_tensor(out=o2[qi % 2][:], in0=o1[:], in1=ogt[qi % 2][:], op=ALU.mult), [o1, ogt[qi % 2]], [o2[qi % 2]])
                f.dma(o_o[h, :, qs], o2[qi % 2][:], o_o, o2[qi % 2])
        n = len(tiles)
        for i in range(n + LA):
            if i < n:
                emit_qk(i)
            if i >= LA:
                emit_rest(i - LA)
        ti += n
    f.final_wait([o_o])
    return f.build()


def consts_D():
    m = np.zeros((128, 4, 512), np.float32)
    p = np.arange(128)[:, None]; j = np.arange(512)[None, :]
    for i in range(4):
        m[:, i, :] = ((i * 128 + p) <= j).astype(np.float32)
    sel = np.zeros((65, 64), np.float32); sel[64, :] = 1.0
    return m.reshape(128, 2048), sel


def inputs_D(oc_list, lf_list, T=T_ALL):
    oc = np.concatenate(oc_list, axis=2)[:, :, :T]
    lf = np.concatenate(lf_list, axis=1)[:, :T]
    mk, sel = consts_D()
    NKB = T // 128
    maps = []
    for c in range(8):
        cs = slice(128 * c, 128 * c + 128)
        q = oc[0][cs].reshape(2, 64, T); k = oc[1][cs].reshape(2, 64, T); og = oc[3][cs].reshape(2, 64, T)
        v = oc[2][cs].reshape(2, 64, NKB, 128)
        vp = np.zeros((2, 128, NKB, 65), dtype=oc.dtype)
        vp[:, :, :, 0:64] = v.transpose(0, 3, 2, 1)
        maps.append({"qT": np.ascontiguousarray(q), "kT": np.ascontiguousarray(k), "vt": vp.reshape(2, 128, NKB * 65),
                     "og": np.ascontiguousarray(og), "lf": np.ascontiguousarray(lf[2 * c:2 * c + 2]), "mk": mk, "sel": sel})
    return maps


def gather_D(results):
    return np.concatenate([np.asarray(r["o_o"]).reshape(128, -1) for r in results], axis=0)


def _run(nc, maps):
    return run_bass_kernel_spmd(nc, maps, core_ids=list(range(8)))


def kernel(**inputs):
    inp = {k: np.asarray(v) for k, v in inputs.items()}
    resA = _run(build_A(), inputs_A(inp))
    obfA = [np.asarray(r["o_bf"]) for r in resA.results]
    obf = np.concatenate(obfA, axis=2)
    ort = np.concatenate([np.asarray(r["o_rt"]) for r in resA.results], axis=1)
    owc = np.concatenate([np.asarray(r["o_wc"]) for r in resA.results], axis=1)
    resB = _run(build_B(), inputs_B(obf, ort, owc))
    y = gather_B(resB.results)
    del obf, ort, owc
    resC = _run(build_C(), inputs_C(inp, y, obfA))
    oc_list = [np.asarray(r["o_c"]) for r in resC.results]
    lf_list = [np.asarray(r["o_lf"]) for r in resC.results]
    hT_list = [np.asarray(r["o_h"]) for r in resC.results]
    resD = _run(build_D(), inputs_D(oc_list, lf_list))
    oT = gather_D(resD.results)
    resE = _run(build_E(), inputs_E(inp, hT_list, oT))
    out = np.concatenate([np.asarray(r["o_out"]) for r in resE.results], axis=1).T
    return np.ascontiguousarray(out, dtype=np.float32).reshape(1, 16384, 1024)
```

```python
import ml_dtypes
import numpy as np
import concourse.bass as bass
import concourse.mybir as mybir
from concourse.bass_utils import run_bass_kernel_spmd
from contextlib import ExitStack

F32 = mybir.dt.float32
BF16 = mybir.dt.bfloat16
I32 = mybir.dt.int32
ALU = mybir.AluOpType
AF = mybir.ActivationFunctionType
AX = mybir.AxisListType

SAME_ENGINE_SYNC = True


class _Rec:
    def __init__(self):
        self.call = None

    def __getattr__(self, name):
        def cap(*a, **k):
            self.call = (name, a, k)
            return self
        return cap


def _replay(call):
    name, a, k = call
    return lambda e: getattr(e, name)(*a, **k)


class _Trk:
    __slots__ = ("w", "r")

    def __init__(self):
        self.w = {}
        self.r = {}


class Buf:
    def __init__(self, fw, name, t, kind):
        self.fw = fw
        self.name = name
        self.t = t
        self.kind = kind
        self.whole = _Trk()
        self.subs = {}
        self.dsem = None

    def __getitem__(self, idx):
        return self.t[idx]

    def k(self, key):
        return (self, key)


def _split(b):
    if isinstance(b, tuple):
        return b
    return (b, None)


class FW:
    CE = ("tensor", "vector", "scalar", "gpsimd")

    def __init__(self, nc):
        self.nc = nc
        self.es = ExitStack()
        self.q = {e: [] for e in ("tensor", "vector", "scalar", "gpsimd", "sync")}
        self.cnt = {e: 0 for e in self.CE}
        self.waited = {}
        self.sems = {}
        self.dcnt = {}
        self.nbuf = 0

    def sb(self, shape, dt, name=None):
        self.nbuf += 1
        name = "S_" + (name or f"sb{self.nbuf}")
        t = self.es.enter_context(self.nc.sbuf_tensor(name, list(shape), dt))
        return Buf(self, name, t, "sb")

    def ps(self, shape, dt=F32, name=None):
        self.nbuf += 1
        name = "P_" + (name or f"ps{self.nbuf}")
        t = self.es.enter_context(self.nc.psum_tensor(name, list(shape), dt))
        return Buf(self, name, t, "ps")

    def dram(self, name, shape, dt, kind):
        t = self.nc.dram_tensor(name, list(shape), dt, kind=kind).ap()
        return Buf(self, name, t, "dram")

    def _sem(self, key):
        if key not in self.sems:
            self.sems[key] = self.es.enter_context(self.nc.semaphore("s_" + str(key)))
        return self.sems[key]

    def _collect(self, eng, reads, writes):
        waits = {}

        def need_w(wd):
            for kv in wd.items():
                need(kv)

        def need(tok):
            if tok is None:
                return
            k, v = tok
            if k == eng and (eng == "tensor" or not SAME_ENGINE_SYNC):
                return
            if waits.get(k, 0) < v:
                waits[k] = v

        reads = list(reads)
        writes = list(writes)
        for b in list(reads):
            bb, _k = _split(b)
            if bb.kind == "ps":
                writes.append(bb)
        writes = [(_split(b)[0] if _split(b)[0].kind == "ps" else b) for b in writes]
        for b in reads:
            b, key = _split(b)
            if b.kind == "ps":
                continue
            trks = [b.whole] + ([b.subs[key]] if (key is not None and key in b.subs) else
                                (list(b.subs.values()) if key is None else []))
            for t in trks:
                need_w(t.w)
        for b in writes:
            b, key = _split(b)
            trks = [b.whole] + ([b.subs[key]] if (key is not None and key in b.subs) else
                                (list(b.subs.values()) if key is None else []))
            for t in trks:
                need_w(t.w)
                for k, v in t.r.items():
                    need((k, v))
        out = []
        for k, v in waits.items():
            if self.waited.get((eng, k), 0) >= v:
                continue
            self.waited[(eng, k)] = v
            out.append((k, v))
        return out

    def _update(self, reads, writes, tok):
        reads = list(reads)
        writes = list(writes)
        for b in list(reads):
            bb, _k = _split(b)
            if bb.kind == "ps":
                writes.append(bb)
        reads = [b for b in reads if _split(b)[0].kind != "ps"]
        writes = [(_split(b)[0] if _split(b)[0].kind == "ps" else b) for b in writes]
        for b in reads:
            b, key = _split(b)
            t = b.whole if key is None else b.subs.setdefault(key, _Trk())
            k, v = tok
            if t.r.get(k, 0) < v:
                t.r[k] = v
        for b in writes:
            b, key = _split(b)
            t = b.whole if key is None else b.subs.setdefault(key, _Trk())
            if b.kind == "dram":
                if t.w.get(tok[0], 0) < tok[1]:
                    t.w[tok[0]] = tok[1]
            else:
                t.w = {tok[0]: tok[1]}
                t.r = {}
                if key is None:
                    b.subs = {}

    def op(self, eng, fn, reads=(), writes=()):
        waits = self._collect(eng, reads, writes)
        self.cnt[eng] += 1
        tok = (eng, self.cnt[eng])
        self._update(reads, writes, tok)
        rec = _Rec()
        fn(rec)
        self.q[eng].append((waits, _replay(rec.call), (eng, 1)))

    def dma(self, out, in_, outb, inb, q="sync", **kw):
        ob, ok_ = _split(outb)
        ib, ik_ = _split(inb)
        sb, sk = (ob, ok_) if ob.kind != "dram" else (ib, ik_)
        key = "d_" + sb.name + ("" if sk is None else "_" + "_".join(str(z) for z in (sk if isinstance(sk, tuple) else (sk,))))
        waits = self._collect(q, [inb], [outb])
        self.dcnt[key] = self.dcnt.get(key, 0) + 16
        tok = (key, self.dcnt[key])
        self._update([inb], [outb], tok)
        self.q[q].append((waits, lambda e: e.dma_start(out=out, in_=in_, **kw), (key, 16)))

    def final_wait(self, bufs, q="sync"):
        waits = self._collect(q, bufs, [])
        self.q[q].append((waits, None, None))

    def build(self):
        nc = self.nc
        for e in self.CE:
            self._sem(e)
        for q in self.q.values():
            for waits, fn, inc in q:
                for k, v in waits:
                    self._sem(k)
                if inc is not None:
                    self._sem(inc[0])
        fwself = self
        self.nsem = len(self.sems)
        with nc.Block() as block:
            def mk(ename):
                def body(eng):
                    for waits, fn, inc in fwself.q[ename]:
                        for k, v in waits:
                            eng.wait_ge(fwself.sems[k], v)
                        if fn is not None:
                            ins = fn(eng)
                            ins.then_inc(fwself.sems[inc[0]], inc[1])
                return body
            block.tensor(mk("tensor"))
            block.vector(mk("vector"))
            block.scalar(mk("scalar"))
            block.gpsimd(mk("gpsimd"))
            block.sync(mk("sync"))
        self.es.close()
        return nc


C0 = 0.6065306597126334
RMS_EPS = 1e-6
DEBUG = False

PVA = {"g0": 0, "mu": 8, "w0": 56, "a0": 64, "k_k": 72, "k_a": 80, "r_k": 88}
NPA = 96


def rmsnorm_to_fm(f, xsrc_ap, xbuf, uT, col0, ncols, src_col0, ident, gcol, pv, tmp, NTOK=128):
    pass


def build_A():
    nc = bass.Bass("TRN2", target_bir_lowering=False)
    f = FW(nc)
    xa = f.dram("xa", [17 * 128, 1024], F32, "ExternalInput")
    pvd = f.dram("pv", [128, NPA], F32, "ExternalInput")
    cst = f.dram("cst", [128, 256], F32, "ExternalInput")
    w_rkv = f.dram("w_rkv", [3, 1024, 1024], F32, "ExternalInput")
    w1d = f.dram("w1", [1024, 64], F32, "ExternalInput")
    a1d = f.dram("a1", [1024, 64], F32, "ExternalInput")
    g1d = f.dram("g1", [1024, 128], F32, "ExternalInput")
    w2d = f.dram("w2", [64, 1024], F32, "ExternalInput")
    a2d = f.dram("a2", [64, 1024], F32, "ExternalInput")
    g2d = f.dram("g2", [128, 1024], F32, "ExternalInput")
    o_bf = f.dram("o_bf", [8, 1024, 2048], BF16, "ExternalOutput")
    o_rt = f.dram("o_rt", [1024, 2048], F32, "ExternalOutput")
    o_wc = f.dram("o_wc", [1024, 32], F32, "ExternalOutput")

    pv = f.sb([128, NPA], F32, "pv")
    f.dma(pv[:], pvd[:], pv, pvd)
    cs = f.sb([128, 256], F32, "cs")
    f.dma(cs[:], cst[:], cs, cst)
    ident = f.sb([128, 128], BF16, "ident")
    bones = f.sb([128, 128], BF16, "bones")
    f.op("vector", lambda e: e.tensor_copy(out=ident[:], in_=cs[:, 0:128]), [cs], [ident])
    f.op("vector", lambda e: e.tensor_copy(out=bones[:], in_=cs[:, 128:256]), [cs], [bones])
    ones = f.sb([128, 64], F32, "ones")
    f.op("gpsimd", lambda e: e.memset(ones[:], 1.0), [], [ones])

    wr = f.sb([128, 3, 8, 1024], BF16, "wr")
    for n in range(3):
        for kc in range(0, 8, 4):
            f.dma(wr[:, n, kc:kc + 4, :], w_rkv[n, kc * 128:(kc + 4) * 128, :].rearrange("(k p) e -> p k e", p=128), wr.k((n, kc)), w_rkv, q="gpsimd")
    w1 = f.sb([128, 8, 64], BF16, "w1s"); a1 = f.sb([128, 8, 64], BF16, "a1s"); g1 = f.sb([128, 8, 128], BF16, "g1s")
    f.dma(w1[:], w1d[:].rearrange("(k p) e -> p k e", p=128), w1, w1d, q="gpsimd")
    f.dma(a1[:], a1d[:].rearrange("(k p) e -> p k e", p=128), a1, a1d, q="gpsimd")
    f.dma(g1[:], g1d[:].rearrange("(k p) e -> p k e", p=128), g1, g1d, q="gpsimd")
    w2 = f.sb([64, 1024], BF16, "w2s"); a2 = f.sb([64, 1024], BF16, "a2s"); g2 = f.sb([128, 1024], BF16, "g2s")
    f.dma(w2[:], w2d[:], w2, w2d, q="gpsimd")
    f.dma(a2[:], a2d[:], a2, a2d, q="gpsimd")
    f.dma(g2[:], g2d[:], g2, g2d, q="gpsimd")

    uT = f.sb([128, 8, 2049], BF16, "uT")
    xb = [f.sb([128, 1024], F32, f"xb{i}") for i in range(2)]
    xn = [f.sb([128, 1024], BF16, f"xn{i}") for i in range(2)]
    junk = f.sb([128, 1024], BF16, "junk")
    ssq = [f.sb([128, 1], F32, f"ssq{i}") for i in range(2)]
    ptr = [f.ps([128, 8, 128], BF16, "ptr0")] * 2
    for t in range(17):
        b = t % 2
        X, XN, SS, PT = xb[b], xn[b], ssq[b], ptr[b]
        f.dma(X[:], xa[t * 128:(t + 1) * 128, :], X, xa)
        f.op("scalar", lambda e, X=X, SS=SS: e.activation(out=junk[:], in_=X[:], func=AF.Square, accum_out=SS[:]), [X], [junk, SS])
        f.op("scalar", lambda e, SS=SS: e.activation(out=SS[:], in_=SS[:], func=AF.Sqrt, scale=1.0 / 1024, bias=RMS_EPS), [SS], [SS])
        f.op("vector", lambda e, SS=SS: e.reciprocal(out=SS[:], in_=SS[:]), [SS], [SS])
        f.op("vector", lambda e, X=X, XN=XN, SS=SS: e.tensor_scalar(out=XN[:], in0=X[:], scalar1=SS[:, 0:1], scalar2=None, op0=ALU.mult), [X, SS], [XN])
        for c in range(8):
            f.op("tensor", lambda e, c=c, XN=XN, PT=PT: e.transpose(out=PT[:, c, :], in_=XN[:, c * 128:(c + 1) * 128], identity=ident[:]), [XN, ident], [PT])
        for c in range(8):
            eng = "vector" if c % 2 == 0 else "gpsimd"
            eng = "vector"
            if t == 0:
                f.op(eng, lambda e, c=c, PT=PT: e.tensor_scalar(out=uT[:, c, 0:1], in0=PT[:, c, 127:128], scalar1=pv[:, PVA["g0"] + c:PVA["g0"] + c + 1], scalar2=None, op0=ALU.mult), [PT, pv], [uT.k(("t", t))])
            else:
                c0 = 1 + (t - 1) * 128
                f.op(eng, lambda e, c=c, PT=PT, c0=c0: e.tensor_scalar(out=uT[:, c, c0:c0 + 128], in0=PT[:, c, :], scalar1=pv[:, PVA["g0"] + c:PVA["g0"] + c + 1], scalar2=None, op0=ALU.mult), [PT, pv], [uT.k(("t", t))])

    xs = f.sb([128, 6, 8, 512], BF16, "xs")
    dd = [f.sb([128, 512], BF16, f"dd{i}") for i in range(2)]
    pr = f.ps([128, 512], F32, "pr"); pk = f.ps([128, 512], F32, "pk"); pvv = f.ps([128, 512], F32, "pvv")
    pw = f.ps([128, 512], F32, "pw"); pa = f.ps([128, 512], F32, "pa"); pg = f.ps([128, 512], F32, "pg")
    px1 = f.ps([128, 512], F32, "px1"); px2 = px1
    h1 = f.sb([64, 512], BF16, "h1"); ha = f.sb([64, 512], BF16, "ha"); hg = f.sb([128, 512], BF16, "hg")
    T = lambda n, dt=F32: f.sb([128, 512], dt, n)
    r_s, k_s, v_s, sg, a_s, cum, cprev = T("r_s"), T("k_s"), T("v_s"), T("sg"), T("a_s"), T("cum"), T("cprev")
    e_pos, e_neg, e_prev = T("e_pos"), T("e_neg"), T("e_prev")
    kkr, sq, ssm, kk, t1, kmod, bb, btf, ktf, rk = T("kkr"), T("sq", BF16), T("ssm"), T("kk"), T("t1"), T("kmod"), T("bb"), T("btf"), T("ktf"), T("rk", BF16)
    rt = T("rt")
    wc = f.sb([128, 8], F32, "wc")
    ob = f.sb([128, 8, 512], BF16, "ob")
    for s in range(4):
        cur = lambda c: uT[:, c, 1 + 512 * s:1 + 512 * s + 512]
        prv = lambda c: uT[:, c, 512 * s:512 * s + 512]
        ureads = [uT.k(("t", t)) for t in range(max(0, 4 * s), 4 * s + 5)]
        for c in range(8):
            D = dd[c % 2]
            f.op("gpsimd", lambda e, c=c, D=D: e.tensor_tensor(out=D[:], in0=prv(c), in1=cur(c), op=ALU.subtract), ureads, [D])
            for n in range(6):
                col = PVA["mu"] + n * 8 + c
                f.op("vector", lambda e, c=c, n=n, D=D, col=col: e.scalar_tensor_tensor(out=xs[:, n, c, :], in0=D[:], scalar=pv[:, col:col + 1], in1=cur(c), op0=ALU.mult, op1=ALU.add), [D, pv] + ureads, [xs.k(n)])
        for c in range(8):
            f.op("tensor", lambda e, c=c: e.matmul(pw[0:64, :], lhsT=w1[:, c, :], rhs=xs[:, 3, c, :], start=(c == 0), stop=(c == 7)), [w1, xs.k(3)], [pw])
        f.op("scalar", lambda e: e.activation(out=h1[:], in_=pw[0:64, :], func=AF.Tanh), [pw], [h1])
        for c in range(8):
            f.op("tensor", lambda e, c=c: e.matmul(pa[0:64, :], lhsT=a1[:, c, :], rhs=xs[:, 4, c, :], start=(c == 0), stop=(c == 7)), [a1, xs.k(4)], [pa])
        f.op("vector", lambda e: e.tensor_copy(out=ha[:], in_=pa[0:64, :]), [pa], [ha])
        for c in range(8):
            f.op("tensor", lambda e, c=c: e.matmul(pg[:], lhsT=g1[:, c, :], rhs=xs[:, 5, c, :], start=(c == 0), stop=(c == 7)), [g1, xs.k(5)], [pg])
        f.op("scalar", lambda e: e.activation(out=hg[:], in_=pg[:], func=AF.Sigmoid), [pg], [hg])
        for ec in range(8):
            es = slice(ec * 128, (ec + 1) * 128)
            for n, P in enumerate((pr, pk, pvv)):
                for c in range(8):
                    f.op("tensor", lambda e, c=c, n=n, P=P: e.matmul(P[:], lhsT=wr[:, n, c, es], rhs=xs[:, n, c, :], start=(c == 0), stop=(c == 7)), [wr.k((n, (c // 4) * 4)), xs.k(n)], [P])
            f.op("tensor", lambda e: e.matmul(pw[:], lhsT=w2[:, es], rhs=h1[:], start=True, stop=True), [w2, h1], [pw])
            f.op("tensor", lambda e: e.matmul(pa[:], lhsT=a2[:, es], rhs=ha[:], start=True, stop=True), [a2, ha], [pa])
            f.op("tensor", lambda e: e.matmul(pg[:], lhsT=g2[:, es], rhs=hg[:], start=True, stop=True), [g2, hg], [pg])
            pcol = lambda nm: pv[:, PVA[nm] + ec:PVA[nm] + ec + 1]
            f.op("scalar", lambda e: e.activation(out=r_s[:], in_=pr[:], func=AF.Copy), [pr], [r_s])
            f.op("scalar", lambda e: e.activation(out=k_s[:], in_=pk[:], func=AF.Copy), [pk], [k_s])
            f.op("vector", lambda e: e.tensor_copy(out=v_s[:], in_=pvv[:]), [pvv], [v_s])
            f.op("scalar", lambda e: e.activation(out=sg[:], in_=pw[:], func=AF.Sigmoid, bias=pcol("w0")), [pw, pv], [sg])
            f.op("scalar", lambda e: e.activation(out=a_s[:], in_=pa[:], func=AF.Sigmoid, bias=pcol("a0")), [pa, pv], [a_s])
            f.op("scalar", lambda e: e.activation(out=ob[:, 6, :], in_=pg[:], func=AF.Copy), [pg], [ob.k(6)])
            for q in range(8):
                f.op("vector", lambda e, q=q: e.tensor_tensor_scan(out=cum[:, q * 64:(q + 1) * 64], data0=ones[:], data1=sg[:, q * 64:(q + 1) * 64], initial=0.0, op0=ALU.mult, op1=ALU.add), [ones, sg], [cum])
            f.op("gpsimd", lambda e: e.tensor_tensor(out=cprev[:], in0=cum[:], in1=sg[:], op=ALU.subtract), [cum, sg], [cprev])
            f.op("scalar", lambda e: e.activation(out=e_pos[:], in_=cum[:], func=AF.Exp, scale=-C0), [cum], [e_pos])
            f.op("scalar", lambda e: e.activation(out=e_neg[:], in_=cum[:], func=AF.Exp, scale=C0), [cum], [e_neg])
            f.op("scalar", lambda e: e.activation(out=e_prev[:], in_=cprev[:], func=AF.Exp, scale=-C0), [cprev], [e_prev])
            f.op("scalar", lambda e: e.activation(out=wc[:], in_=cum[:].rearrange("p (q t) -> p q t", t=64)[:, :, 63], func=AF.Exp, scale=-C0), [cum], [wc])
            f.op("scalar", lambda e: e.activation(out=kkr[:], in_=k_s[:], func=AF.Identity, scale=pcol("k_k")), [k_s, pv], [kkr])
            f.op("scalar", lambda e: e.activation(out=sq[:], in_=k_s[:], func=AF.Square, scale=pcol("k_k")), [k_s, pv], [sq])
            f.op("tensor", lambda e: e.matmul(px1[:], lhsT=bones[:], rhs=sq[:], start=True, stop=True), [bones, sq], [px1])
            f.op("vector", lambda e: e.tensor_scalar(out=ssm[:], in0=px1[:], scalar1=1e-24, scalar2=None, op0=ALU.max), [px1], [ssm])
            f.op("scalar", lambda e: e.activation(out=ssm[:], in_=ssm[:], func=AF.Sqrt), [ssm], [ssm])
            f.op("vector", lambda e: e.reciprocal(out=ssm[:], in_=ssm[:]), [ssm], [ssm])
            f.op("vector", lambda e: e.tensor_tensor(out=kk[:], in0=kkr[:], in1=ssm[:], op=ALU.mult), [kkr, ssm], [kk])
            f.op("vector", lambda e: e.tensor_scalar(out=t1[:], in0=a_s[:], scalar1=-1.0, scalar2=pcol("k_a"), op0=ALU.add, op1=ALU.mult), [a_s, pv], [t1])
            f.op("vector", lambda e: e.scalar_tensor_tensor(out=kmod[:], in0=t1[:], scalar=1.0, in1=k_s[:], op0=ALU.add, op1=ALU.mult), [t1, k_s], [kmod])
            f.op("vector", lambda e: e.tensor_tensor(out=bb[:], in0=kk[:], in1=a_s[:], op=ALU.mult), [kk, a_s], [bb])
            f.op("vector", lambda e: e.scalar_tensor_tensor(out=ob[:, 0, :], in0=kk[:], scalar=-1.0, in1=e_prev[:], op0=ALU.mult, op1=ALU.mult), [kk, e_prev], [ob.k(0)])
            f.op("gpsimd", lambda e: e.tensor_tensor(out=rt[:], in0=r_s[:], in1=e_pos[:], op=ALU.mult), [r_s, e_pos], [rt])
            f.op("vector", lambda e: e.tensor_tensor(out=btf[:], in0=bb[:], in1=e_neg[:], op=ALU.mult), [bb, e_neg], [btf])
            f.op("gpsimd", lambda e: e.tensor_tensor(out=ktf[:], in0=kmod[:], in1=e_neg[:], op=ALU.mult), [kmod, e_neg], [ktf])
            f.op("scalar", lambda e: e.activation(out=ob[:, 1, :], in_=btf[:], func=AF.Copy), [btf], [ob.k(1)])
            f.op("scalar", lambda e: e.activation(out=ob[:, 2, :], in_=ktf[:], func=AF.Copy), [ktf], [ob.k(2)])
            for q in range(8):
                qs = slice(q * 64, (q + 1) * 64)
                f.op("vector", lambda e, q=q, qs=qs: e.tensor_scalar(out=ob[:, 3, qs], in0=btf[:, qs], scalar1=wc[:, q:q + 1], scalar2=None, op0=ALU.mult), [btf, wc], [ob.k(3)])
                f.op("scalar", lambda e, q=q, qs=qs: e.activation(out=ob[:, 4, qs], in_=ktf[:, qs], func=AF.Identity, scale=wc[:, q:q + 1]), [ktf, wc], [ob.k(4)])
            f.op("vector", lambda e: e.scalar_tensor_tensor(out=rk[:], in0=r_s[:], scalar=pcol("r_k"), in1=kmod[:], op0=ALU.mult, op1=ALU.mult), [r_s, pv, kmod], [rk])
            f.op("tensor", lambda e: e.matmul(px2[:], lhsT=bones[:], rhs=rk[:], start=True, stop=True), [bones, rk], [px2])
            f.op("vector", lambda e: e.tensor_tensor(out=ob[:, 7, :], in0=px2[:], in1=v_s[:], op=ALU.mult), [px2, v_s], [ob.k(7)])
            f.op("scalar", lambda e: e.activation(out=ob[:, 5, :], in_=v_s[:], func=AF.Copy), [v_s], [ob.k(5)])
            f.dma(o_bf[:, es, 512 * s:512 * s + 512].rearrange("q p t -> p q t"), ob[:], o_bf, ob)
            f.dma(o_rt[es, 512 * s:512 * s + 512], rt[:], o_rt, rt)
            f.dma(o_wc[es, 8 * s:8 * s + 8], wc[:], o_wc, wc)
    if DEBUG:
        o_dbg = f.dram("o_dbg", [128, 8, 2049], BF16, "ExternalOutput")
        f.dma(o_dbg[:], uT[:], o_dbg, uT)
        o_dbg2 = f.dram("o_dbg2", [128, 8, 1024], BF16, "ExternalOutput")
        f.dma(o_dbg2[:], wr[:, 2, :, :], o_dbg2, wr)
        f.final_wait([o_dbg, o_dbg2])
    f.final_wait([o_bf, o_rt, o_wc])
    return f.build()


def consts_A():
    c = np.zeros((128, 256), np.float32)
    c[:, 0:128] = np.eye(128)
    blk = np.arange(128) // 64
    c[:, 128:256] = (blk[:, None] == blk[None, :]).astype(np.float32)
    return c


def fmcols(v):
    return np.ascontiguousarray(np.asarray(v, np.float32).reshape(8, 128).T)


def inputs_A(inp):
    x = inp["x"][0]
    pvv = np.zeros((128, NPA), np.float32)
    pvv[:, 0:8] = fmcols(inp["norm_g"][0, 0])
    for n in range(6):
        pvv[:, 8 + 8 * n:16 + 8 * n] = fmcols(inp["rwkv_mu"][0, n])
    pvv[:, 56:64] = fmcols(inp["rwkv_w0"][0]); pvv[:, 64:72] = fmcols(inp["rwkv_a0"][0])
    pvv[:, 72:80] = fmcols(inp["rwkv_k_k"][0]); pvv[:, 80:88] = fmcols(inp["rwkv_k_a"][0])
    pvv[:, 88:96] = fmcols(inp["rwkv_r_k"][0].reshape(-1))
    cst = consts_A()
    xpad = np.concatenate([np.zeros((128, 1024), np.float32), x], 0)
    maps = []
    for c in range(8):
        maps.append({"xa": np.ascontiguousarray(xpad[2048 * c:2048 * c + 2048 + 128]), "pv": pvv, "cst": cst,
                     "w_rkv": inp["rwkv_w_rkv"][0], "w1": inp["rwkv_w1"][0], "a1": inp["rwkv_a1"][0], "g1": inp["rwkv_g1"][0],
                     "w2": inp["rwkv_w2"][0], "a2": inp["rwkv_a2"][0], "g2": inp["rwkv_g2"][0]})
    return maps


GC = 8
NCH = 256
NG = NCH // GC
DEBUG = False


def build_B(nch=NCH):
    ng = nch // GC
    nc = bass.Bass("TRN2", target_bir_lowering=False)
    f = FW(nc)
    d_bk = f.dram("d_bk", [ng, 64, GC * 2 * 2 * 64], BF16, "ExternalInput")
    d_at = f.dram("d_at", [ng, 64, GC * 2 * 64], BF16, "ExternalInput")
    d_rf = f.dram("d_rf", [ng, 64, GC * 2 * 64], F32, "ExternalInput")
    d_tm = f.dram("d_tm", [ng, 64, GC * 2 * 4 * 64], BF16, "ExternalInput")
    d_wc = f.dram("d_wc", [64, nch * 2], F32, "ExternalInput")
    d_mask = f.dram("d_mask", [64, 2 * 320], F32, "ExternalInput")
    d_id = f.dram("d_id", [64, 128], F32, "ExternalInput")
    o_y = f.dram("o_y", [ng, 64, GC * 2 * 64], F32, "ExternalOutput")

    wcs = f.sb([64, nch, 2], F32, "wcs")
    f.dma(wcs[:].rearrange("p c h -> p (c h)"), d_wc[:], wcs, d_wc)
    mask = f.sb([64, 2, 320], F32, "mask")
    f.dma(mask[:].rearrange("p h c -> p (h c)"), d_mask[:], mask, d_mask)
    idf2 = f.sb([64, 2, 64], F32, "idf2")
    f.dma(idf2[:].rearrange("p h c -> p (h c)"), d_id[:], idf2, d_id)
    idb2 = f.sb([64, 2, 64], BF16, "idb2")
    f.op("vector", lambda e: e.tensor_copy(out=idb2[:], in_=idf2[:]), [idf2], [idb2])
    identb = idb2[:, 0, :]
    identf = idf2[:, 0, :]

    NB = 2
    BK = [f.sb([64, GC, 2, 2, 64], BF16, f"BK{i}") for i in range(NB)]
    AR = [f.sb([64, GC, 2, 128], BF16, f"AR{i}") for i in range(NB)]
    RF = [f.sb([64, GC, 2, 64], F32, f"RF{i}") for i in range(NB)]
    TM = [f.sb([64, GC, 2, 4, 64], BF16, f"TM{i}") for i in range(NB)]
    YB = [f.sb([64, GC, 2, 64], F32, f"YB{i}") for i in range(NB)]

    def load_group(g):
        b = g % NB
        f.dma(BK[b][:].rearrange("p c h q t -> p (c h q t)"), d_bk[g], BK[b], d_bk)
        f.dma(AR[b][:, :, :, 0:64], d_at[g].rearrange("p (c h t) -> p c h t", c=GC, h=2), AR[b].k("a"), d_at)
        f.dma(AR[b][:, :, :, 64:128], d_rf[g].rearrange("p (c h t) -> p c h t", c=GC, h=2), AR[b].k("r"), d_rf, q="gpsimd")
        f.dma(RF[b][:].rearrange("p c h t -> p (c h t)"), d_rf[g], RF[b], d_rf)
        f.dma(TM[b][:].rearrange("p c h q t -> p (c h q t)"), d_tm[g], TM[b], d_tm)

    class _V:
        def __init__(self, buf, ap):
            self.buf, self.ap = buf, ap

        def __getitem__(self, idx):
            return self.ap[idx]

        def k(self, key):
            return self.buf

    def bank(name):
        return f.ps([64, 512], F32, name)
    PM, PL, PXG = [], [], []
    for x in range(2):
        bm, bl, bx = bank(f"PM{x}"), bank(f"PL{x}"), bank(f"PXG{x}")
        PM.append(_V(bm, bm[:, 0:512].rearrange("p (h c) -> p h c", h=2)))
        PL.append(_V(bl, bl[:, 0:384].rearrange("p (h c) -> p h c", h=2)))
        PXG.append(_V(bx, bx[:, 0:512].rearrange("p (h c) -> p h c", h=2)))
    bus, by = bank("PUS"), bank("PYb")
    PU_ap = bus[:, 0:128].rearrange("p (h c) -> p h c", h=2)
    PS_ap = bus[:, 128:256].rearrange("p (h c) -> p h c", h=2)
    PY_ap = by[:, 0:128].rearrange("p (h c) -> p h c", h=2)
    PUk, PSk, PYk = bus, bus, by
    NP = 4
    Ms = [f.sb([64, 2, 320], BF16, f"Ms{i}") for i in range(NP)]
    XT0 = [f.sb([64, 2, 64], BF16, f"XT0{i}") for i in range(NP)]
    LV = [[f.sb([64, 2, 192], BF16, f"LV{i}_{k}") for k in range(7)] for i in range(NP)]
    GX = [f.sb([64, 2, 128], F32, f"GX{i}") for i in range(NP)]
    UT = [f.sb([64, 2, 64], BF16, f"UT{i}") for i in range(2)]
    S = [f.sb([64, 2, 64], F32, f"S{i}") for i in range(2)]
    f.op("vector", lambda e: e.memset(S[0][:], 0.0), [], [S[0]])

    def pre_stages(c):
        p = c % NP
        x = c % 2
        PMx, PLx, PXGx = PM[x], PL[x], PXG[x]
        g, cg = divmod(c, GC)
        b = g % NB
        bk, ar, tm = BK[b], AR[b], TM[b]
        st = []

        def p1():
            for h in range(2):
                f.op("tensor", lambda e: e.matmul(PMx[:, h, 0:128], lhsT=bk[:, cg, h, 0, :], rhs=ar[:, cg, h, :], start=True, stop=True), [bk, ar], [PMx.k(h)])
                f.op("tensor", lambda e: e.matmul(PMx[:, h, 128:256], lhsT=bk[:, cg, h, 1, :], rhs=ar[:, cg, h, :], start=True, stop=True), [bk, ar], [PMx.k(h)])
                f.op("tensor", lambda e: e.matmul(PXGx[:, h, 192:256], lhsT=ar[:, cg, h, 0:64], rhs=bk[:, cg, h, 0, :], start=True, stop=True), [bk, ar], [PXGx.k("l")])
        st.append(p1)

        def p2():
            f.op("vector", lambda e: e.tensor_tensor(out=Ms[p][:, :, 0:256], in0=PMx[:, :, 0:256], in1=mask[:, :, 0:256], op=ALU.mult), [PMx.buf, mask], [Ms[p].k("m")])
            f.op("vector", lambda e: e.tensor_tensor(out=Ms[p][:, :, 256:320], in0=PXGx[:, :, 192:256], in1=mask[:, :, 256:320], op=ALU.mult), [PXGx.k("l"), mask], [Ms[p].k("l")])
            f.op("gpsimd", lambda e: e.tensor_tensor(out=LV[p][1][:, :, 128:192], in0=Ms[p][:, :, 0:64], in1=idb2[:], op=ALU.add), [Ms[p].k("m"), idb2], [LV[p][1].k("T")])
        st.append(p2)

        def p3():
            for h in range(2):
                f.op("tensor", lambda e: e.matmul(PXGx[:, h, 0:64], lhsT=Ms[p][:, h, 128:192], rhs=tm[:, cg, h, 3, :], start=True, stop=True), [Ms[p].k("m"), tm], [PXGx.k("x")])
        st.append(p3)

        def p4():
            f.op("scalar", lambda e: e.activation(out=XT0[p][:], in_=PXGx[:, :, 0:64], func=AF.Copy), [PXGx.k("x")], [XT0[p]])
        st.append(p4)

        def level(k):
            def mm():
                for h in range(2):
                    if k == 1:
                        Np, Lp = Ms[p][:, h, 0:64], Ms[p][:, h, 256:320]
                        rd = [Ms[p].k("m"), Ms[p].k("l")]
                    else:
                        Np, Lp = LV[p][k - 1][:, h, 0:64], LV[p][k - 1][:, h, 64:128]
                        rd = [LV[p][k - 1].k("NL")]
                    if k <= 5:
                        f.op("tensor", lambda e: e.matmul(PLx[:, h, 0:64], lhsT=Lp, rhs=Np, start=True, stop=True), rd, [PLx.k(h)])
                        f.op("tensor", lambda e: e.matmul(PLx[:, h, 64:128], lhsT=Np, rhs=Lp, start=True, stop=True), rd, [PLx.k(h)])
                    if k >= 2:
                        Tp = LV[p][k - 1][:, h, 128:192]
                        rdt = rd + [LV[p][k - 1].k("T"), idb2]
                        f.op("tensor", lambda e: e.matmul(PLx[:, h, 128:192], lhsT=identb, rhs=Tp, start=True, stop=False), rdt, [PLx.k(h)])
                        f.op("tensor", lambda e: e.matmul(PLx[:, h, 128:192], lhsT=Lp, rhs=Tp, start=False, stop=True), rdt, [PLx.k(h)])

            def ev():
                eng = "scalar" if k % 2 == 0 else "vector"
                if k == 1:
                    sl, wk = slice(0, 128), [LV[p][k].k("NL")]
                elif k <= 5:
                    sl, wk = slice(0, 192), [LV[p][k].k("NL"), LV[p][k].k("T")]
                else:
                    sl, wk = slice(128, 192), [LV[p][k].k("T")]
                if eng == "scalar":
                    f.op("scalar", lambda e: e.activation(out=LV[p][k][:, :, sl], in_=PLx[:, :, sl], func=AF.Copy), [PLx.buf], wk)
                else:
                    f.op("vector", lambda e: e.tensor_copy(out=LV[p][k][:, :, sl], in_=PLx[:, :, sl]), [PLx.buf], wk)
            return [mm, ev]
        for k in range(1, 7):
            st.extend(level(k))

        def pf():
            for h in range(2):
                Tf = LV[p][6][:, h, 128:192]
                f.op("tensor", lambda e: e.matmul(PXGx[:, h, 64:128], lhsT=tm[:, cg, h, 0, :], rhs=Tf, start=True, stop=True), [tm, LV[p][6].k("T")], [PXGx.k("g")])
                f.op("tensor", lambda e: e.matmul(PXGx[:, h, 128:192], lhsT=Tf, rhs=XT0[p][:, h, :], start=True, stop=True), [XT0[p], LV[p][6].k("T")], [PXGx.k("g")])
        st.append(pf)

        def pe():
            f.op("scalar", lambda e: e.activation(out=GX[p][:], in_=PXGx[:, :, 64:192], func=AF.Copy), [PXGx.k("g")], [GX[p]])
        st.append(pe)
        return st

    def seq_stages(c):
        p = c % NP
        q2 = c % 2
        g, cg = divmod(c, GC)
        b = g % NB
        bk, ar, tm, rf, yb = BK[b], AR[b], TM[b], RF[b], YB[b]
        S0, S1 = S[q2], S[1 - q2]
        UTq = UT[q2]
        PU, PS, PY = PU_ap, PS_ap, PY_ap
        st = []

        def s1():
            for h in range(2):
                f.op("tensor", lambda e: e.matmul(PU[:, h, :], lhsT=GX[p][:, h, 0:64], rhs=S0[:, h, :], start=True, stop=False), [GX[p], S0], [PUk])
                f.op("tensor", lambda e: e.matmul(PU[:, h, :], lhsT=identf, rhs=GX[p][:, h, 64:128], start=False, stop=True), [GX[p], idf2], [PUk])
        st.append(s1)

        def s2():
            f.op("scalar", lambda e: e.activation(out=UTq[:], in_=PU, func=AF.Copy), [PUk], [UTq])
        st.append(s2)

        def s3():
            for h in range(2):
                f.op("tensor", lambda e: e.matmul(PS[:, h, :], lhsT=tm[:, cg, h, 1, :], rhs=UTq[:, h, :], start=True, stop=False), [tm, UTq], [PSk])
                f.op("tensor", lambda e: e.matmul(PS[:, h, :], lhsT=tm[:, cg, h, 2, :], rhs=tm[:, cg, h, 3, :], start=False, stop=True), [tm], [PSk])
            for h in range(2):
                f.op("tensor", lambda e: e.matmul(PY[:, h, :], lhsT=rf[:, cg, h, :], rhs=S0[:, h, :], start=True, stop=False), [rf, S0], [PYk])
                f.op("tensor", lambda e: e.matmul(PY[:, h, :], lhsT=Ms[p][:, h, 192:256], rhs=tm[:, cg, h, 3, :], start=False, stop=False), [Ms[p].k("m"), tm], [PYk])
                f.op("tensor", lambda e: e.matmul(PY[:, h, :], lhsT=Ms[p][:, h, 64:128], rhs=UTq[:, h, :], start=False, stop=True), [Ms[p].k("m"), UTq], [PYk])
        st.append(s3)

        def s4():
            for h in range(2):
                f.op("vector", lambda e: e.scalar_tensor_tensor(out=S1[:, h, :], in0=S0[:, h, :], scalar=wcs[:, c, h:h + 1], in1=PS[:, h, :], op0=ALU.mult, op1=ALU.add), [S0, wcs, PSk], [S1])
            f.op("scalar", lambda e: e.activation(out=yb[:, cg, :, :], in_=PY, func=AF.Copy), [PYk], [yb.k(cg)])
            if cg == GC - 1 or DEBUG:
                f.dma(o_y[g], yb[:].rearrange("p c h i -> p (c h i)"), o_y, yb)
        st.append(s4)
        return st

    load_group(0)
    if ng > 1:
        load_group(1)

    def lockstep(a, b):
        out = []
        for i in range(max(len(a), len(b))):
            if i < len(a):
                out.append(a[i])
            if i < len(b):
                out.append(b[i])
        return out

    for s_ in lockstep(pre_stages(0), pre_stages(1) if nch > 1 else []):
        s_()
    for c in range(0, nch, 2):
        g, cg = divmod(c, GC)
        if DEBUG and c >= 1:
            break
        a = []
        if not DEBUG:
            a = lockstep(pre_stages(c + 2) if c + 2 < nch else [], pre_stages(c + 3) if c + 3 < nch else [])
        bq = seq_stages(c) + (seq_stages(c + 1) if c + 1 < nch and not DEBUG else [])
        n = max(len(a), len(bq))
        ia = ib = 0
        for i in range(n):
            want_a = (i + 1) * len(a) // n
            while ia < want_a:
                a[ia](); ia += 1
            want_b = (i + 1) * len(bq) // n
            while ib < want_b:
                bq[ib](); ib += 1
        if (c + 1) % GC == GC - 1 and g + 2 < ng:
            load_group(g + 2)
    if DEBUG:
        dbg = f.dram("o_dbg", [64, 2 * (320 + 192 * 7 + 128 + 64)], F32, "ExternalOutput")
        db = f.sb([64, 2, 320 + 192 * 7 + 128 + 64], F32, "dbgs")
        f.op("vector", lambda e: e.tensor_copy(out=db[:, :, 0:320], in_=Ms[0][:]), [Ms[0]], [db])
        for k in range(7):
            f.op("vector", lambda e: e.tensor_copy(out=db[:, :, 320 + 192 * k:320 + 192 * (k + 1)], in_=LV[0][k][:]), [LV[0][k]], [db])
        o = 320 + 192 * 7
        f.op("vector", lambda e: e.tensor_copy(out=db[:, :, o:o + 128], in_=GX[0][:]), [GX[0]], [db])
        f.op("vector", lambda e: e.tensor_copy(out=db[:, :, o + 128:o + 192], in_=UT[0][:]), [UT[0]], [db])
        f.dma(dbg[:], db[:].rearrange("p h c -> p (h c)"), dbg, db)
        f.final_wait([dbg])
    f.final_wait([o_y])
    return f.build()


def consts_B():
    m = np.zeros((64, 2, 320), np.float32)
    s = np.arange(64)[:, None]; t = np.arange(64)[None, :]
    su = (s < t).astype(np.float32); iu = (s <= t).astype(np.float32)
    for h in range(2):
        m[:, h, 0:64] = su; m[:, h, 64:128] = iu; m[:, h, 128:192] = su; m[:, h, 192:256] = iu
        m[:, h, 256:320] = (t < s).astype(np.float32)
    idm = np.concatenate([np.eye(64, dtype=np.float32)] * 2, axis=1)
    return m.reshape(64, 640), idm


def inputs_B(obf, ort, owc, nch=NCH):
    ng = nch // GC
    T = nch * 64
    mask, idm = consts_B()
    maps = []
    for c in range(8):
        cs = slice(128 * c, 128 * c + 128)
        fm = lambda a: a[cs, :T].reshape(2, 64, ng, GC, 64)
        At, Bt, Kt, Bh, Kh, V = [fm(obf[q]) for q in range(6)]
        Rt = fm(ort)
        d_bk = np.stack([Bt, Kt], axis=0).transpose(3, 2, 4, 1, 0, 5)
        d_at = At.transpose(2, 1, 3, 0, 4)
        d_rf = Rt.transpose(2, 1, 3, 0, 4)
        tmq = np.stack([At, Bh, Kh, V], axis=0)
        d_tm = tmq.transpose(3, 5, 4, 1, 0, 2)
        d_wc = owc[cs, :nch].reshape(2, 64, nch).transpose(1, 2, 0)
        maps.append({"d_bk": np.ascontiguousarray(d_bk).reshape(ng, 64, -1), "d_at": np.ascontiguousarray(d_at).reshape(ng, 64, -1),
                     "d_rf": np.ascontiguousarray(d_rf).reshape(ng, 64, -1), "d_tm": np.ascontiguousarray(d_tm).reshape(ng, 64, -1),
                     "d_wc": np.ascontiguousarray(d_wc).reshape(64, -1), "d_mask": mask, "d_id": idm})
    return maps


def gather_B(results, nch=NCH):
    ng = nch // GC
    ys = []
    for r in results:
        y = np.asarray(r["o_y"]).reshape(ng, 64, GC, 2, 64)
        ys.append(y.transpose(0, 2, 1, 3, 4).reshape(nch * 64, 128))
    return np.concatenate(ys, axis=1)


RMS_EPS = 1e-6
GN_EPS = 64e-5
ST = 512


class Ctx:
    pass


def setup_common(f, c, cst_dram):
    cs = f.sb([128, 384], F32, "cs")
    f.dma(cs[:], cst_dram[:], cs, cst_dram)
    c.identf = cs
    c.bones = f.sb([128, 128], BF16, "bones")
    c.ones = f.sb([128, 128], BF16, "onesb")
    f.op("vector", lambda e: e.tensor_copy(out=c.bones[:], in_=cs[:, 128:256]), [cs], [c.bones])
    f.op("vector", lambda e: e.tensor_copy(out=c.ones[:], in_=cs[:, 256:384]), [cs], [c.ones])
    c.cs = cs
    c.PA = [f.ps([128, 512], F32, f"PA{i}") for i in range(4)]
    c.PB = [f.ps([128, 512], F32, f"PB{i}") for i in range(2)]
    c.PC = [f.ps([128, 512], F32, f"PC{i}") for i in range(2)]
    c.ia = c.ib = c.ic = 0
    c.tmpi = 0
    c.tmps = {}


def nxt(c, pool):
    if pool == "A":
        c.ia += 1; return c.PA[c.ia % 4]
    if pool == "B":
        c.ib += 1; return c.PB[c.ib % 2]
    c.ic += 1; return c.PC[c.ic % 2]


def tmp(f, c, name, dt=F32, n=2, shape=(128, 512)):
    key = (name, dt)
    if key not in c.tmps:
        c.tmps[key] = [[f.sb(list(shape), dt, f"t_{name}_{i}") for i in range(n)], 0]
    lst = c.tmps[key]
    lst[1] += 1
    return lst[0][lst[1] % n]


def rmsnorm_fm(f, c, hT, pv, gcol0, uT, uF=None, eps=RMS_EPS):
    P = nxt(c, "C")
    for k in range(8):
        sq = tmp(f, c, "sq", BF16, 3)
        f.op("scalar", lambda e: e.activation(out=sq[:], in_=hT[:, k, :], func=AF.Square), [hT.k(k)], [sq])
        f.op("tensor", lambda e: e.matmul(P[:], lhsT=c.ones[:], rhs=sq[:], start=(k == 0), stop=(k == 7)), [c.ones, sq], [P])
    rs = tmp(f, c, "rs")
    f.op("scalar", lambda e: e.activation(out=rs[:], in_=P[:], func=AF.Sqrt, scale=1.0 / 1024, bias=eps), [P], [rs])
    f.op("vector", lambda e: e.reciprocal(out=rs[:], in_=rs[:]), [rs], [rs])
    for k in range(8):
        if uT is not None:
            f.op("vector", lambda e: e.scalar_tensor_tensor(out=uT[:, k, :], in0=hT[:, k, :], scalar=pv[:, gcol0 + k:gcol0 + k + 1], in1=rs[:], op0=ALU.mult, op1=ALU.mult), [hT.k(k), pv, rs], [uT.k(k)])
        if uF is not None:
            f.op("vector", lambda e: e.scalar_tensor_tensor(out=uF[:, k, :], in0=hT[:, k, :], scalar=pv[:, gcol0 + k:gcol0 + k + 1], in1=rs[:], op0=ALU.mult, op1=ALU.mult), [hT.k(k), pv, rs], [uF.k(k)])


def linear_res(f, c, w_sb, zT, hT, nk=8):
    for ec in range(8):
        P = nxt(c, "B")
        for k in range(nk):
            f.op("tensor", lambda e: e.matmul(P[:], lhsT=w_sb[:, k, ec * 128:(ec + 1) * 128], rhs=zT[:, k, :], start=(k == 0), stop=(k == nk - 1)), [w_sb, zT.k(k)], [P])
        f.op("vector", lambda e: e.tensor_tensor(out=hT[:, ec, :], in0=hT[:, ec, :], in1=P[:], op=ALU.add), [hT.k(ec), P], [hT.k(ec)])


def ffn_pass(f, c, uT, aT, wgu_dram, wd_dram, wgu_bufs, wd_bufs, out_fn, gate_b=None, FG=2, EG=2):
    NF = 28
    for g in range(NF // FG):
        c.wgi = getattr(c, "wgi", 0) + 1
        W = wgu_bufs[c.wgi % len(wgu_bufs)]
        for half in range(2):
            col0 = half * 3584 + g * FG * 128
            f.dma(W[:, :, half, :], wgu_dram[:, col0:col0 + FG * 128].rearrange("(k p) e -> p k e", p=128), W.k(half), wgu_dram_buf(c, wgu_dram), q="gpsimd")
        for j in range(FG):
            fc = g * FG + j
            Pg = nxt(c, "A"); Pu = nxt(c, "A")
            for k in range(8):
                f.op("tensor", lambda e: e.matmul(Pg[:], lhsT=W[:, k, 0, j * 128:(j + 1) * 128], rhs=uT[:, k, :], start=(k == 0), stop=(k == 7)), [W.k(0), uT.k(k)], [Pg])
            for k in range(8):
                f.op("tensor", lambda e: e.matmul(Pu[:], lhsT=W[:, k, 1, j * 128:(j + 1) * 128], rhs=uT[:, k, :], start=(k == 0), stop=(k == 7)), [W.k(1), uT.k(k)], [Pu])
            sg = tmp(f, c, "sg", F32, 3)
            f.op("scalar", lambda e: e.activation(out=sg[:], in_=Pg[:], func=AF.Silu), [Pg], [sg])
            if gate_b is None:
                f.op("vector", lambda e: e.tensor_tensor(out=aT[:, fc, :], in0=sg[:], in1=Pu[:], op=ALU.mult), [sg, Pu], [aT.k(fc)])
            else:
                sg2 = tmp(f, c, "sg2", F32, 3)
                f.op("vector", lambda e: e.tensor_tensor(out=sg2[:], in0=sg[:], in1=Pu[:], op=ALU.mult), [sg, Pu], [sg2])
                f.op("gpsimd", lambda e: e.tensor_tensor(out=aT[:, fc, :], in0=sg2[:], in1=gate_b[:], op=ALU.mult), [sg2, gate_b], [aT.k(fc)])
    for g in range(8 // EG):
        c.wdi = getattr(c, "wdi", 0) + 1
        W = wd_bufs[c.wdi % len(wd_bufs)]
        for q4 in range(4):
            f.dma(W[:, q4 * 7:(q4 + 1) * 7, :], wd_dram[q4 * 896:(q4 + 1) * 896, g * EG * 128:(g + 1) * EG * 128].rearrange("(k p) e -> p k e", p=128), W.k(q4), wgu_dram_buf(c, wd_dram), q="gpsimd")
        for j in range(EG):
            ec = g * EG + j
            P = nxt(c, "B")
            for fc in range(NF):
                f.op("tensor", lambda e: e.matmul(P[:], lhsT=W[:, fc, j * 128:(j + 1) * 128], rhs=aT[:, fc, :], start=(fc == 0), stop=(fc == NF - 1)), [W.k(fc // 7), aT.k(fc)], [P])
            out_fn(ec, P)


_dram_bufs = {}


def wgu_dram_buf(c, ap):
    return c.wdram


def consts_CE():
    cst = np.zeros((128, 384), np.float32)
    cst[:, 0:128] = np.eye(128)
    blk = np.arange(128) // 64
    cst[:, 128:256] = (blk[:, None] == blk[None, :]).astype(np.float32)
    cst[:, 256:384] = 1.0
    return cst


def fmcols(v):
    return np.ascontiguousarray(np.asarray(v, np.float32).reshape(-1, 128).T)


def ffn_ws(f, c, uTs, hTs, w_gu_ap, w_dn_ap, wgu_bufs, wd_bufs, aT_bufs, gbs=None, FG=4):
    NTT = len(uTs)
    for g in range(28 // FG):
        c.wgi = getattr(c, "wgi", 0) + 1
        W = wgu_bufs[c.wgi % 2]; WD = wd_bufs[c.wgi % 2]
        for hf in range(2):
            col0 = hf * 3584 + g * FG * 128
            f.dma(W[:, :, hf, :], w_gu_ap[:, col0:col0 + FG * 128].rearrange("(k p) e -> p k e", p=128), W.k(hf), c.wdram, q="gpsimd")
        f.dma(WD[:], w_dn_ap[g * FG * 128:(g + 1) * FG * 128, :].rearrange("(k p) e -> p k e", p=128), WD, c.wdram, q="gpsimd")
        for tt in range(NTT):
            c.ai = getattr(c, "ai", 0) + 1
            A = aT_bufs[c.ai % 2]
            for j in range(FG):
                Pg = nxt(c, "A"); Pu = nxt(c, "A")
                for k in range(8):
                    f.op("tensor", lambda e: e.matmul(Pg[:], lhsT=W[:, k, 0, j * 128:(j + 1) * 128], rhs=uTs[tt][:, k, :], start=(k == 0), stop=(k == 7)), [W.k(0), uTs[tt].k(k)], [Pg])
                for k in range(8):
                    f.op("tensor", lambda e: e.matmul(Pu[:], lhsT=W[:, k, 1, j * 128:(j + 1) * 128], rhs=uTs[tt][:, k, :], start=(k == 0), stop=(k == 7)), [W.k(1), uTs[tt].k(k)], [Pu])
                sg = tmp(f, c, "sg", F32, 3)
                f.op("scalar", lambda e: e.activation(out=sg[:], in_=Pg[:], func=AF.Silu), [Pg], [sg])
                if gbs is None:
                    f.op("vector", lambda e: e.tensor_tensor(out=A[:, j, :], in0=sg[:], in1=Pu[:], op=ALU.mult), [sg, Pu], [A.k(j)])
                else:
                    sg2 = tmp(f, c, "sg2", F32, 3)
                    f.op("vector", lambda e: e.tensor_tensor(out=sg2[:], in0=sg[:], in1=Pu[:], op=ALU.mult), [sg, Pu], [sg2])
                    f.op("vector", lambda e: e.tensor_tensor(out=A[:, j, :], in0=sg2[:], in1=gbs[tt][:], op=ALU.mult), [sg2, gbs[tt]], [A.k(j)])
            for ec in range(8):
                P = nxt(c, "B")
                for j in range(FG):
                    f.op("tensor", lambda e: e.matmul(P[:], lhsT=WD[:, j, ec * 128:(ec + 1) * 128], rhs=A[:, j, :], start=(j == 0), stop=(j == FG - 1)), [WD, A.k(j)], [P])
                f.op("vector", lambda e: e.tensor_tensor(out=hTs[tt][:, ec, :], in0=hTs[tt][:, ec, :], in1=P[:], op=ALU.add), [hTs[tt].k(ec), P], [hTs[tt].k(ec)])


PVC = {"gn_g": 0, "gn_b": 8, "g1": 16, "g2": 24, "qg": 32, "kg": 33, "bf": 34}
NPC = 35


def build_C():
    nc = bass.Bass("TRN2", target_bir_lowering=False)
    f = FW(nc)
    c = Ctx()
    xT = f.dram("xT", [1024, 2048], F32, "ExternalInput")
    yT = f.dram("yT", [1024, 2048], F32, "ExternalInput")
    gb = f.dram("gb", [2, 1024, 2048], BF16, "ExternalInput")
    pvd = f.dram("pv", [128, NPC], F32, "ExternalInput")
    cst = f.dram("cst", [128, 384], F32, "ExternalInput")
    w_o = f.dram("w_o", [1024, 1024], F32, "ExternalInput")
    w_gu = f.dram("w_gu", [1024, 7168], F32, "ExternalInput")
    w_dn = f.dram("w_dn", [3584, 1024], F32, "ExternalInput")
    w_in = f.dram("w_in", [1024, 4112], F32, "ExternalInput")
    o_h = f.dram("o_h", [1024, 2048], F32, "ExternalOutput")
    o_c = f.dram("o_c", [4, 1024, 2048], BF16, "ExternalOutput")
    o_lf = f.dram("o_lf", [16, 2048], F32, "ExternalOutput")
    c.wdram = f.dram("wdummy", [1, 1], F32, "Internal")
    setup_common(f, c, cst)
    pv = f.sb([128, NPC], F32, "pvs")
    f.dma(pv[:], pvd[:], pv, pvd)
    wo = f.sb([128, 8, 1024], BF16, "wo")
    for k4 in range(0, 8, 4):
        f.dma(wo[:, k4:k4 + 4, :], w_o[k4 * 128:(k4 + 4) * 128, :].rearrange("(k p) e -> p k e", p=128), wo.k(k4), w_o, q="gpsimd")
    wfl = f.sb([128, 8, 16], BF16, "wfl")
    f.dma(wfl[:], w_in[:, 4096:4112].rearrange("(k p) e -> p k e", p=128), wfl, w_in, q="gpsimd")
    NTT = 2
    hTs = [f.sb([128, 8, 512], F32, f"hT{i}") for i in range(NTT)]
    uTs = [f.sb([128, 8, 512], BF16, f"uT{i}") for i in range(NTT)]
    zT = f.sb([128, 8, 512], BF16, "zT")
    aT = [f.sb([128, 4, 512], BF16, f"aTg{i}") for i in range(2)]
    wgu_bufs = [f.sb([128, 8, 2, 512], BF16, f"wgu{i}") for i in range(2)]
    wd_bufs = [f.sb([128, 4, 1024], BF16, f"wd{i}") for i in range(2)]
    oc = [f.sb([128, 512], BF16, f"oc{i}") for i in range(3)]
    oci = [0]
    lf = f.sb([16, 512], F32, "lf")
    for half in range(2):
        for tt in range(NTT):
            hT = hTs[tt]
            ts = slice(1024 * half + 512 * tt, 1024 * half + 512 * tt + 512)
            f.dma(hT[:], xT[:, ts].rearrange("(k p) t -> p k t", p=128), hT, xT)
            for ec in range(8):
                es = slice(ec * 128, (ec + 1) * 128)
                y = tmp(f, c, "y"); g2 = tmp(f, c, "gbt", BF16, 2, (128, 2, 512))
                f.dma(y[:], yT[es, ts], y, yT)
                f.dma(g2[:], gb[:, es, ts].rearrange("q p t -> p q t"), g2, gb)
                yb = tmp(f, c, "yb", BF16)
                f.op("scalar", lambda e: e.activation(out=yb[:], in_=y[:], func=AF.Copy), [y], [yb])
                P = nxt(c, "C")
                f.op("tensor", lambda e: e.matmul(P[:], lhsT=c.bones[:], rhs=yb[:], start=True, stop=True), [c.bones, yb], [P])
                yc = tmp(f, c, "yc")
                f.op("vector", lambda e: e.scalar_tensor_tensor(out=yc[:], in0=P[:], scalar=-1.0 / 64, in1=y[:], op0=ALU.mult, op1=ALU.add), [P, y], [yc])
                sq = tmp(f, c, "sq", BF16, 3)
                f.op("scalar", lambda e: e.activation(out=sq[:], in_=yc[:], func=AF.Square), [yc], [sq])
                P2 = nxt(c, "C")
                f.op("tensor", lambda e: e.matmul(P2[:], lhsT=c.bones[:], rhs=sq[:], start=True, stop=True), [c.bones, sq], [P2])
                rs = tmp(f, c, "rs")
                f.op("scalar", lambda e: e.activation(out=rs[:], in_=P2[:], func=AF.Sqrt, scale=1.0 / 64, bias=GN_EPS), [P2], [rs])
                f.op("vector", lambda e: e.reciprocal(out=rs[:], in_=rs[:]), [rs], [rs])
                f.op("vector", lambda e: e.tensor_tensor(out=yc[:], in0=yc[:], in1=rs[:], op=ALU.mult), [yc, rs], [yc])
                f.op("vector", lambda e: e.tensor_scalar(out=yc[:], in0=yc[:], scalar1=pv[:, PVC["gn_g"] + ec:PVC["gn_g"] + ec + 1], scalar2=pv[:, PVC["gn_b"] + ec:PVC["gn_b"] + ec + 1], op0=ALU.mult, op1=ALU.add), [yc, pv], [yc])
                f.op("vector", lambda e: e.tensor_tensor(out=yc[:], in0=yc[:], in1=g2[:, 1, :], op=ALU.add), [yc, g2], [yc])
                f.op("vector", lambda e: e.tensor_tensor(out=zT[:, ec, :], in0=yc[:], in1=g2[:, 0, :], op=ALU.mult), [yc, g2], [zT.k(ec)])
            linear_res(f, c, wo, zT, hT)
            rmsnorm_fm(f, c, hT, pv, PVC["g1"], uTs[tt])
        ffn_ws(f, c, uTs, hTs, w_gu[:], w_dn[:], wgu_bufs, wd_bufs, aT)
        for tt in range(NTT):
            ts = slice(1024 * half + 512 * tt, 1024 * half + 512 * tt + 512)
            f.dma(o_h[:, ts].rearrange("(k p) t -> p k t", p=128), hTs[tt][:], o_h, hTs[tt])
            rmsnorm_fm(f, c, hTs[tt], pv, PVC["g2"], uTs[tt])
        for g in range(8):
            c.wgi += 1
            W = wgu_bufs[c.wgi % 2]
            Wv = W[:].rearrange("p k h e -> p k (h e)")
            f.dma(Wv[:, :, 0:512], w_in[:, g * 512:(g + 1) * 512].rearrange("(k p) e -> p k e", p=128), W, c.wdram, q="gpsimd")
            for tt in range(NTT):
                ts = slice(1024 * half + 512 * tt, 1024 * half + 512 * tt + 512)
                uT = uTs[tt]
                for j in range(4):
                    e32 = g * 4 + j
                    kind, ec = divmod(e32, 8)
                    P = nxt(c, "A")
                    for k in range(8):
                        f.op("tensor", lambda e: e.matmul(P[:], lhsT=Wv[:, k, j * 128:(j + 1) * 128], rhs=uT[:, k, :], start=(k == 0), stop=(k == 7)), [W, uT.k(k)], [P])
                    oci[0] += 1
                    O = oc[oci[0] % 3]
                    if kind in (0, 1):
                        qs = tmp(f, c, "qs")
                        f.op("scalar", lambda e: e.activation(out=qs[:], in_=P[:], func=AF.Copy), [P], [qs])
                        sq = tmp(f, c, "sq", BF16, 3)
                        f.op("scalar", lambda e: e.activation(out=sq[:], in_=qs[:], func=AF.Square), [qs], [sq])
                        P2 = nxt(c, "C")
                        f.op("tensor", lambda e: e.matmul(P2[:], lhsT=c.bones[:], rhs=sq[:], start=True, stop=True), [c.bones, sq], [P2])
                        rs = tmp(f, c, "rs")
                        if kind == 0:
                            f.op("scalar", lambda e: e.activation(out=rs[:], in_=P2[:], func=AF.Sqrt, scale=1.0, bias=64 * RMS_EPS), [P2], [rs])
                        else:
                            f.op("scalar", lambda e: e.activation(out=rs[:], in_=P2[:], func=AF.Sqrt, scale=1.0 / 64, bias=RMS_EPS), [P2], [rs])
                        f.op("vector", lambda e: e.reciprocal(out=rs[:], in_=rs[:]), [rs], [rs])
                        gc = PVC["qg"] if kind == 0 else PVC["kg"]
                        f.op("vector", lambda e: e.scalar_tensor_tensor(out=O[:], in0=qs[:], scalar=pv[:, gc:gc + 1], in1=rs[:], op0=ALU.mult, op1=ALU.mult), [qs, pv, rs], [O])
                    elif kind == 2:
                        f.op("scalar", lambda e: e.activation(out=O[:], in_=P[:], func=AF.Copy), [P], [O])
                    else:
                        f.op("scalar", lambda e: e.activation(out=O[:], in_=P[:], func=AF.Sigmoid), [P], [O])
                    f.dma(o_c[kind, ec * 128:(ec + 1) * 128, ts], O[:], o_c, O)
        for tt in range(NTT):
            ts = slice(1024 * half + 512 * tt, 1024 * half + 512 * tt + 512)
            uT = uTs[tt]
            P = nxt(c, "C")
            for k in range(8):
                f.op("tensor", lambda e: e.matmul(P[0:16, :], lhsT=wfl[:, k, :], rhs=uT[:, k, :], start=(k == 0), stop=(k == 7)), [wfl, uT.k(k)], [P])
            f.op("vector", lambda e: e.tensor_scalar(out=lf[:], in0=P[0:16, :], scalar1=pv[0:16, PVC["bf"]:PVC["bf"] + 1], scalar2=None, op0=ALU.add), [P, pv], [lf])
            f.op("scalar", lambda e: e.activation(out=lf[:], in_=lf[:], func=AF.Exp, scale=-1.0), [lf], [lf])
            f.op("scalar", lambda e: e.activation(out=lf[:], in_=lf[:], func=AF.Ln, bias=1.0), [lf], [lf])
            f.op("vector", lambda e: e.tensor_scalar(out=lf[:], in0=lf[:], scalar1=-1.0, scalar2=None, op0=ALU.mult), [lf], [lf])
            f.dma(o_lf[:, ts], lf[:], o_lf, lf)
    f.final_wait([o_h, o_c, o_lf])
    return f.build()


def inputs_C(inp, yfull, obfA):
    x = inp["x"][0]
    pvv = np.zeros((128, NPC), np.float32)
    pvv[:, 0:8] = fmcols(inp["rwkv_gn_g"][0]); pvv[:, 8:16] = fmcols(inp["rwkv_gn_b"][0])
    pvv[:, 16:24] = fmcols(inp["norm_g"][0, 1]); pvv[:, 24:32] = fmcols(inp["norm_g"][1, 0])
    pvv[:, 32] = np.tile(inp["fox_q_gain"][0], 2); pvv[:, 33] = np.tile(inp["fox_k_gain"][0], 2)
    pvv[0:16, 34] = inp["fox_b_f"][0]
    cst = consts_CE()
    maps = []
    for c in range(8):
        ts = slice(2048 * c, 2048 * c + 2048)
        maps.append({"xT": np.ascontiguousarray(x[ts].T), "yT": np.ascontiguousarray(yfull[ts].T),
                     "gb": np.ascontiguousarray(obfA[c][6:8]), "pv": pvv, "cst": cst,
                     "w_o": inp["rwkv_w_o"][0], "w_gu": inp["ffn_w_gu"][0], "w_dn": inp["ffn_w_down"][0], "w_in": inp["fox_w_in"][0]})
    return maps


PVE = {"g3": 0, "gf": 8}
NPE = 16
NEXP = 8
NTE = 1024
FGE = 4


def build_E():
    nc = bass.Bass("TRN2", target_bir_lowering=False)
    f = FW(nc)
    c = Ctx()
    hTd = f.dram("hT", [1024, 2048], F32, "ExternalInput")
    oTd = f.dram("oT", [1024, 2048], BF16, "ExternalInput")
    pvd = f.dram("pv", [128, NPE], F32, "ExternalInput")
    cst = f.dram("cst", [128, 384], F32, "ExternalInput")
    seld = f.dram("sele", [8, 8 * 128], F32, "ExternalInput")
    w_o = f.dram("w_o", [1024, 1024], F32, "ExternalInput")
    w_r = f.dram("w_r", [1024, 8], F32, "ExternalInput")
    w_gu = f.dram("w_gu", [8, 1024, 7168], F32, "ExternalInput")
    w_dn = f.dram("w_dn", [8, 3584, 1024], F32, "ExternalInput")
    o_out = f.dram("o_out", [1024, 2048], F32, "ExternalOutput")
    c.wdram = f.dram("wdummy", [1, 1], F32, "Internal")
    setup_common(f, c, cst)
    pv = f.sb([128, NPE], F32, "pvs")
    f.dma(pv[:], pvd[:], pv, pvd)
    sele = f.sb([8, 8, 128], F32, "sele")
    f.dma(sele[:].rearrange("k e m -> k (e m)"), seld[:], sele, seld)
    wo = f.sb([128, 8, 1024], BF16, "wo")
    for k4 in range(0, 8, 4):
        f.dma(wo[:, k4:k4 + 4, :], w_o[k4 * 128:(k4 + 4) * 128, :].rearrange("(k p) e -> p k e", p=128), wo.k(k4), w_o, q="gpsimd")
    wr = f.sb([128, 8, 8], F32, "wr")
    f.dma(wr[:], w_r[:].rearrange("(k p) e -> p k e", p=128), wr, w_r)
    NTT = NTE // 512
    hT = [f.sb([128, 8, 512], F32, f"hT{i}") for i in range(NTT)]
    uT = [f.sb([128, 8, 512], BF16, f"uT{i}") for i in range(NTT)]
    gTs = [f.sb([8, 512], F32, f"gTs{i}") for i in range(NTT)]
    zT = f.sb([128, 8, 512], BF16, "zT")
    uF = f.sb([128, 8, 512], F32, "uF")
    wgu_bufs = [f.sb([128, 8, 2, FGE * 128], BF16, f"wgu{i}") for i in range(2)]
    wd_bufs = [f.sb([128, FGE, 1024], BF16, f"wd{i}") for i in range(2)]
    aT = [f.sb([128, FGE, 512], BF16, f"aTg{i}") for i in range(2)]
    gb = [f.sb([128, 512], F32, f"gb{i}") for i in range(NTT)]
    lg = f.sb([8, 512], F32, "lg")
    lt = f.sb([128, 4, 8], F32, "lt")
    gts = f.sb([128, 4, 8], F32, "gts")
    sm = {n: f.sb([128, 8], F32, "sm_" + n) for n in ("eq", "l2", "sel", "ex")}
    sc = {n: f.sb([128, 4], F32, "sc_" + n) for n in ("m1", "nm1", "m2", "sum")}
    wgi = 0
    ai = 0
    for half in range(2048 // NTE):
        for tt in range(NTT):
            ts = slice(half * NTE + 512 * tt, half * NTE + 512 * tt + 512)
            H, U = hT[tt], uT[tt]
            f.dma(H[:], hTd[:, ts].rearrange("(k p) t -> p k t", p=128), H, hTd)
            f.dma(zT[:], oTd[:, ts].rearrange("(k p) t -> p k t", p=128), zT, oTd)
            linear_res(f, c, wo, zT, H)
            rmsnorm_fm(f, c, H, pv, PVE["g3"], U, uF)
            P = nxt(c, "C")
            for k in range(8):
                f.op("tensor", lambda e: e.matmul(P[0:8, :], lhsT=wr[:, k, :], rhs=uF[:, k, :], start=(k == 0), stop=(k == 7)), [wr, uF.k(k)], [P])
            f.op("vector", lambda e: e.tensor_copy(out=lg[:], in_=P[0:8, :]), [P], [lg])
            P2 = nxt(c, "C")
            for j in range(4):
                f.op("tensor", lambda e: e.transpose(out=P2[:, j * 8:(j + 1) * 8], in_=lg[:, j * 128:(j + 1) * 128], identity=c.cs[0:8, 0:8]), [lg, c.cs], [P2])
            f.op("vector", lambda e: e.tensor_copy(out=lt[:].rearrange("p j e -> p (j e)"), in_=P2[:, 0:32]), [P2], [lt])
            f.op("vector", lambda e: e.tensor_reduce(out=sc["m1"][:], in_=lt[:], axis=AX.X, op=ALU.max), [lt], [sc["m1"]])
            f.op("vector", lambda e: e.tensor_scalar(out=sc["nm1"][:], in0=sc["m1"][:], scalar1=-1.0, scalar2=None, op0=ALU.mult), [sc["m1"]], [sc["nm1"]])
            for j in range(4):
                L = lt[:, j, :]
                f.op("vector", lambda e: e.tensor_scalar(out=sm["eq"][:], in0=L, scalar1=sc["m1"][:, j:j + 1], scalar2=None, op0=ALU.is_ge), [lt, sc["m1"]], [sm["eq"]])
                f.op("vector", lambda e: e.scalar_tensor_tensor(out=sm["l2"][:], in0=sm["eq"][:], scalar=-1e30, in1=L, op0=ALU.mult, op1=ALU.add), [sm["eq"], lt], [sm["l2"]])
                f.op("vector", lambda e: e.tensor_reduce(out=sc["m2"][:, j:j + 1], in_=sm["l2"][:], axis=AX.X, op=ALU.max), [sm["l2"]], [sc["m2"]])
                f.op("vector", lambda e: e.tensor_scalar(out=sm["sel"][:], in0=L, scalar1=sc["m2"][:, j:j + 1], scalar2=None, op0=ALU.is_ge), [lt, sc["m2"]], [sm["sel"]])
                f.op("scalar", lambda e: e.activation(out=sm["ex"][:], in_=L, func=AF.Exp, bias=sc["nm1"][:, j:j + 1]), [lt, sc["nm1"]], [sm["ex"]])
                f.op("vector", lambda e: e.tensor_tensor(out=sm["ex"][:], in0=sm["ex"][:], in1=sm["sel"][:], op=ALU.mult), [sm["ex"], sm["sel"]], [sm["ex"]])
                f.op("vector", lambda e: e.tensor_reduce(out=sc["sum"][:, j:j + 1], in_=sm["ex"][:], axis=AX.X, op=ALU.add), [sm["ex"]], [sc["sum"]])
                f.op("vector", lambda e: e.reciprocal(out=sc["sum"][:, j:j + 1], in_=sc["sum"][:, j:j + 1]), [sc["sum"]], [sc["sum"]])
                f.op("vector", lambda e: e.tensor_scalar(out=gts[:, j, :], in0=sm["ex"][:], scalar1=sc["sum"][:, j:j + 1], scalar2=None, op0=ALU.mult), [sm["ex"], sc["sum"]], [gts])
            P3 = nxt(c, "C")
            for j in range(4):
                f.op("tensor", lambda e: e.transpose(out=P3[0:8, j * 128:(j + 1) * 128], in_=gts[:, j, :], identity=c.cs[:, 0:128]), [gts, c.cs], [P3])
            f.op("vector", lambda e: e.tensor_copy(out=gTs[tt][:], in_=P3[0:8, :]), [P3], [gTs[tt]])
        for ex in range(NEXP):
            for tt in range(NTT):
                P4 = nxt(c, "C")
                f.op("tensor", lambda e: e.matmul(P4[:], lhsT=sele[:, ex, :], rhs=gTs[tt][:], start=True, stop=True), [sele, gTs[tt]], [P4])
                f.op("scalar", lambda e: e.activation(out=gb[tt][:], in_=P4[:], func=AF.Copy), [P4], [gb[tt]])
            ffn_ws(f, c, uT, hT, w_gu[ex], w_dn[ex], wgu_bufs, wd_bufs, aT, gbs=gb, FG=FGE)
        for tt in range(NTT):
            ts = slice(half * NTE + 512 * tt, half * NTE + 512 * tt + 512)
            rmsnorm_fm(f, c, hT[tt], pv, PVE["gf"], None, uF)
            f.dma(o_out[:, ts].rearrange("(k p) t -> p k t", p=128), uF[:], o_out, uF)
    f.final_wait([o_out])
    return f.build()


def inputs_E(inp, hT_list, oT):
    pvv = np.zeros((128, NPE), np.float32)
    pvv[:, 0:8] = fmcols(inp["norm_g"][1, 1]); pvv[:, 8:16] = fmcols(inp["final_g"])
    cst = consts_CE()
    sele = np.zeros((8, 8, 128), np.float32)
    for e in range(8):
        sele[e, e, :] = 1.0
    maps = []
    for c in range(8):
        maps.append({"hT": hT_list[c], "oT": np.ascontiguousarray(oT[:, 2048 * c:2048 * c + 2048]), "pv": pvv, "cst": cst,
                     "sele": sele.reshape(8, 1024), "w_o": inp["fox_w_o"][0], "w_r": inp["moe_w_router"][0],
                     "w_gu": inp["moe_w_gu"][0], "w_dn": inp["moe_w_down"][0]})
    return maps


T_ALL = 16384


def build_D(T=T_ALL):
    nc = bass.Bass("TRN2", target_bir_lowering=False)
    f = FW(nc)
    NQ = T // 512
    NKB = T // 128
    NSEG = T // 2048
    qT = f.dram("qT", [2, 64, T], BF16, "ExternalInput")
    kT = f.dram("kT", [2, 64, T], BF16, "ExternalInput")
    vt = f.dram("vt", [2, 128, NKB * 65], BF16, "ExternalInput")
    og = f.dram("og", [2, 64, T], BF16, "ExternalInput")
    lfd = f.dram("lf", [2, T], F32, "ExternalInput")
    mkd = f.dram("mk", [128, 4 * 512], F32, "ExternalInput")
    sld = f.dram("sel", [65, 64], F32, "ExternalInput")
    o_o = f.dram("o_o", [2, 64, T], BF16, "ExternalOutput")

    mkf = f.sb([128, 4, 512], F32, "mkf")
    f.dma(mkf[:].rearrange("p m t -> p (m t)"), mkd[:], mkf, mkd)
    mk = f.sb([128, 4, 512], BF16, "mk")
    f.op("vector", lambda e: e.tensor_copy(out=mk[:], in_=mkf[:]), [mkf], [mk])
    sel = f.sb([65, 64], F32, "sels")
    f.dma(sel[:], sld[:], sel, sld)
    ones = f.sb([1, 2048], F32, "ones1")
    f.op("gpsimd", lambda e: e.memset(ones[:], 1.0), [], [ones])

    Qa = f.sb([70, T], BF16, "Qa")
    Ka = f.sb([70, T], BF16, "Ka")
    Vt = f.sb([128, NKB, 65], BF16, "Vt")
    lfs = [f.sb([1, 2048], F32, f"lfs{i}") for i in range(2)]
    cseg = [f.sb([1, 2048], F32, f"cseg{i}") for i in range(2)]
    c_d = f.dram("c_scr", [2, T], F32, "Internal")
    p_d = f.dram("p_scr", [2, 3, T], BF16, "Internal")
    c2d = [f.sb([128, T // 128], F32, f"c2d{i}") for i in range(2)]
    r2d = [f.sb([128, T // 128], F32, f"r2d{i}") for i in range(2)]
    parts2 = [f.sb([128, 3, T // 128], BF16, f"parts2_{i}") for i in range(2)]
    for h in range(2):
        for sg in range(NSEG):
            b = (h * NSEG + sg) % 2
            ss = slice(sg * 2048, (sg + 1) * 2048)
            f.dma(lfs[b][:], lfd[h:h + 1, ss], lfs[b], lfd)
            init = 0.0 if sg == 0 else cseg[1 - b][:, 2047:2048]
            rd = [ones, lfs[b]] + ([] if sg == 0 else [cseg[1 - b]])
            f.op("vector", lambda e: e.tensor_tensor_scan(out=cseg[b][:], data0=ones[:], data1=lfs[b][:], initial=init, op0=ALU.mult, op1=ALU.add), rd, [cseg[b]])
            f.dma(c_d[h:h + 1, ss], cseg[b][:], c_d, cseg[b])
        C2, R2, P2 = c2d[h], r2d[h], parts2[h]
        f.dma(C2[:], c_d[h].rearrange("(p c) -> p c", p=128), C2, c_d)
        f.op("vector", lambda e: e.tensor_copy(out=P2[:, 0, :], in_=C2[:]), [C2], [P2])
        f.op("vector", lambda e: e.tensor_tensor(out=R2[:], in0=C2[:], in1=P2[:, 0, :], op=ALU.subtract), [C2, P2], [R2])
        f.op("vector", lambda e: e.tensor_copy(out=P2[:, 1, :], in_=R2[:]), [R2], [P2])
        f.op("vector", lambda e: e.tensor_tensor(out=R2[:], in0=R2[:], in1=P2[:, 1, :], op=ALU.subtract), [R2, P2], [R2])
        f.op("vector", lambda e: e.tensor_copy(out=P2[:, 2, :], in_=R2[:]), [R2], [P2])
        for r in range(3):
            f.dma(p_d[h, r].rearrange("(p c) -> p c", p=128), P2[:, r, :], p_d, P2)
    PSs = [f.ps([128, 512], F32, f"PSs{i}") for i in range(5)]
    PO = [f.ps([65, 512], F32, f"PO{i}") for i in range(2)]
    PD = f.ps([64, 512], F32, "PD")
    PT = [f.sb([128, 512], BF16, f"PT{i}") for i in range(6)]
    Osb = [f.sb([65, 512], F32, f"Osb{i}") for i in range(2)]
    rden = f.sb([64, 512], F32, "rden")
    o1 = f.sb([64, 512], F32, "o1")
    ogt = [f.sb([64, 512], BF16, f"ogt{i}") for i in range(2)]
    o2 = [f.sb([64, 512], BF16, f"o2{i}") for i in range(2)]
    scl = [f.sb([128, 512], F32, f"scl{i}") for i in range(2)]
    ti = 0
    for h in range(2):
        f.dma(Qa[0:64, :], qT[h], Qa.k("top"), qT)
        f.dma(Ka[0:64, :], kT[h], Ka.k("top"), kT)
        f.dma(Vt[:].rearrange("p k d -> p (k d)"), vt[h], Vt, vt)
        f.op("gpsimd", lambda e: e.memset(Vt[:, :, 64:65], 1.0), [], [Vt])
        f.op("gpsimd", lambda e: e.memset(Qa[64:70, :], -1.0), [], [Qa.k("aug")])
        f.op("gpsimd", lambda e: e.memset(Ka[64:70, :], 1.0), [], [Ka.k("aug")])
        for r in range(3):
            f.dma(Qa[64 + r:65 + r, :], p_d[h, r:r + 1, :], Qa.k("aug"), p_d)
            f.dma(Ka[67 + r:68 + r, :], p_d[h, r:r + 1, :], Ka.k("aug"), p_d)
        tiles = [(qi, kb) for qi in range(NQ) for kb in range(4 * qi + 4)]
        LA = 4
        base = ti

        def emit_qk(i):
            qi, kb = tiles[i]
            qs = slice(qi * 512, (qi + 1) * 512)
            ps = PSs[(base + i) % 5]
            f.op("tensor", lambda e: e.matmul(ps[:], lhsT=Ka[0:70, kb * 128:(kb + 1) * 128], rhs=Qa[0:70, qs], start=True, stop=True), [Ka, Qa], [ps])

        def emit_rest(i):
            qi, kb = tiles[i]
            qs = slice(qi * 512, (qi + 1) * 512)
            nkb = 4 * qi + 4
            ps = PSs[(base + i) % 5]; pt = PT[(base + i) % 6]
            po = PO[qi % 2]
            if kb == 0:
                f.dma(ogt[qi % 2][:], og[h, :, qs], ogt[qi % 2], og)
            if kb >= 4 * qi:
                sc = scl[kb % 2]
                f.op("vector", lambda e: e.tensor_scalar(out=sc[:], in0=ps[:], scalar1=30.0, scalar2=None, op0=ALU.min), [ps], [sc])
                f.op("scalar", lambda e: e.activation(out=pt[:], in_=sc[:], func=AF.Exp), [sc], [pt])
                m = kb - 4 * qi
                eng = "vector" if m % 2 == 0 else "gpsimd"
                f.op(eng, lambda e: e.tensor_tensor(out=pt[:], in0=pt[:], in1=mk[:, m, :], op=ALU.mult), [pt, mk], [pt])
            else:
                f.op("scalar", lambda e: e.activation(out=pt[:], in_=ps[:], func=AF.Exp), [ps], [pt])
            f.op("tensor", lambda e: e.matmul(po[:], lhsT=Vt[:, kb, :], rhs=pt[:], start=(kb == 0), stop=(kb == nkb - 1)), [Vt, pt], [po])
            if kb == nkb - 1:
                osb = Osb[qi % 2]
                f.op("vector", lambda e: e.tensor_copy(out=osb[:], in_=po[:]), [po], [osb])
                f.op("tensor", lambda e: e.matmul(PD[:], lhsT=sel[:], rhs=osb[:], start=True, stop=True), [sel, osb], [PD])
                f.op("vector", lambda e: e.reciprocal(out=rden[:], in_=PD[:]), [PD], [rden])
                f.op("gpsimd", lambda e: e.tensor_tensor(out=o1[:], in0=osb[0:64, :], in1=rden[:], op=ALU.mult), [osb, rden], [o1])
                f.op("gpsimd", lambda e: e.tensor_tensor(out=o2[qi % 2][:], in0=o1[:], in1=ogt[qi % 2][:], op=ALU.mult), [o1, ogt[qi % 2]], [o2[qi % 2]])
                f.dma(o_o[h, :, qs], o2[qi % 2][:], o_o, o2[qi % 2])
        n = len(tiles)
        for i in range(n + LA):
            if i < n:
                emit_qk(i)
            if i >= LA:
                emit_rest(i - LA)
        ti += n
    f.final_wait([o_o])
    return f.build()


def consts_D():
    m = np.zeros((128, 4, 512), np.float32)
    p = np.arange(128)[:, None]; j = np.arange(512)[None, :]
    for i in range(4):
        m[:, i, :] = ((i * 128 + p) <= j).astype(np.float32)
    sel = np.zeros((65, 64), np.float32); sel[64, :] = 1.0
    return m.reshape(128, 2048), sel


def inputs_D(oc_list, lf_list, T=T_ALL):
    oc = np.concatenate(oc_list, axis=2)[:, :, :T]
    lf = np.concatenate(lf_list, axis=1)[:, :T]
    mk, sel = consts_D()
    NKB = T // 128
    maps = []
    for c in range(8):
        cs = slice(128 * c, 128 * c + 128)
        q = oc[0][cs].reshape(2, 64, T); k = oc[1][cs].reshape(2, 64, T); og = oc[3][cs].reshape(2, 64, T)
        v = oc[2][cs].reshape(2, 64, NKB, 128)
        vp = np.zeros((2, 128, NKB, 65), dtype=oc.dtype)
        vp[:, :, :, 0:64] = v.transpose(0, 3, 2, 1)
        maps.append({"qT": np.ascontiguousarray(q), "kT": np.ascontiguousarray(k), "vt": vp.reshape(2, 128, NKB * 65),
                     "og": np.ascontiguousarray(og), "lf": np.ascontiguousarray(lf[2 * c:2 * c + 2]), "mk": mk, "sel": sel})
    return maps


def gather_D(results):
    return np.concatenate([np.asarray(r["o_o"]).reshape(128, -1) for r in results], axis=0)


def _run(nc, maps):
    return run_bass_kernel_spmd(nc, maps, core_ids=list(range(8)))


def kernel(**inputs):
    inp = {k: np.asarray(v) for k, v in inputs.items()}
    resA = _run(build_A(), inputs_A(inp))
    obfA = [np.asarray(r["o_bf"]) for r in resA.results]
    obf = np.concatenate(obfA, axis=2)
    ort = np.concatenate([np.asarray(r["o_rt"]) for r in resA.results], axis=1)
    owc = np.concatenate([np.asarray(r["o_wc"]) for r in resA.results], axis=1)
    resB = _run(build_B(), inputs_B(obf, ort, owc))
    y = gather_B(resB.results)
    del obf, ort, owc
    resC = _run(build_C(), inputs_C(inp, y, obfA))
    oc_list = [np.asarray(r["o_c"]) for r in resC.results]
    lf_list = [np.asarray(r["o_lf"]) for r in resC.results]
    hT_list = [np.asarray(r["o_h"]) for r in resC.results]
    resD = _run(build_D(), inputs_D(oc_list, lf_list))
    oT = gather_D(resD.results)
    resE = _run(build_E(), inputs_E(inp, hT_list, oT))
    out = np.concatenate([np.asarray(r["o_out"]) for r in resE.results], axis=1).T
    return np.ascontiguousarray(out, dtype=np.float32).reshape(1, 16384, 1024)
```
